# Optimizing a Trainium2 kernel written in Bass

```python
import math
import jax
import jax.numpy as jnp
from jax import lax
import numpy as np

D_MODEL = 1024
BATCH = 16
SEQ = 2048
DEPTH = 2

CTX_LEN = 256
GRID_W = 64
EPS = 1e-6

HEAD_DIM = 64
N_Q_HEADS = 8
N_KV_HEADS = 2
Q_PER_KV = N_Q_HEADS // N_KV_HEADS
AXIS_DIM = HEAD_DIM // 2
ROPE_THETA = 10000.0
Q_BLOCK = 128
ATTN_WIDTH = N_Q_HEADS * HEAD_DIM
KV_WIDTH = N_KV_HEADS * HEAD_DIM

GMLP_GROUPS = 8
GMLP_GROUP_DIM = 64
GMLP_WIDTH = GMLP_GROUPS * GMLP_GROUP_DIM
GMLP_CHUNK = 128

MIX_IN_WIDTH = ATTN_WIDTH + 2 * KV_WIDTH + 2 * GMLP_WIDTH
MIX_SPLITS = (ATTN_WIDTH, ATTN_WIDTH + KV_WIDTH, ATTN_WIDTH + 2 * KV_WIDTH,
              ATTN_WIDTH + 2 * KV_WIDTH + GMLP_WIDTH)
MIX_OUT_WIDTH = ATTN_WIDTH + GMLP_WIDTH

S5_GROUPS = 32
S5_GROUP_DIM = 16
S5_STATE = 64
S5_WIDTH = S5_GROUPS * S5_GROUP_DIM

N_EXPERT_GROUPS = 4
EXPERTS_PER_GROUP = 8
N_EXPERTS = N_EXPERT_GROUPS * EXPERTS_PER_GROUP
TOP_K_INNER = 2
EXPERT_HIDDEN = 512
MOE_BLOCK = 256

kernel_name = 'hybrid_flow_gqa_gmlp_s5_hmoe'


def rms_norm(x, g):
    x32 = x.astype(jnp.float32)
    y = x32 * lax.rsqrt(jnp.mean(x32 * x32, axis=-1, keepdims=True) + EPS)
    return (y * g.astype(jnp.float32)).astype(x.dtype)


def layer_norm(x, g):
    x32 = x.astype(jnp.float32)
    mu = jnp.mean(x32, axis=-1, keepdims=True)
    xc = x32 - mu
    y = xc * lax.rsqrt(jnp.mean(xc * xc, axis=-1, keepdims=True) + EPS)
    return (y * g.astype(jnp.float32)).astype(x.dtype)


def modulate(x, g, shift, scale):
    return rms_norm(x, g) * (1 + scale) + shift


def adaln(cond, w_mod, b_mod):
    m = jax.nn.silu(cond) @ w_mod + b_mod
    return jnp.split(m, 6, axis=-1)


def axial_rope_tables(n_tokens):
    rows = n_tokens // GRID_W
    row = jnp.repeat(jnp.arange(rows, dtype=jnp.int32), GRID_W).astype(jnp.float32)
    col = jnp.tile(jnp.arange(GRID_W, dtype=jnp.int32), rows).astype(jnp.float32)
    inv_freq = jnp.power(ROPE_THETA, -jnp.arange(0, AXIS_DIM, 2, dtype=jnp.float32) / AXIS_DIM)
    ang_r = row[:, None] * inv_freq[None, :]
    ang_c = col[:, None] * inv_freq[None, :]
    return (jnp.cos(ang_r), jnp.sin(ang_r), jnp.cos(ang_c), jnp.sin(ang_c))


def _rotate_half(x, cos, sin):
    x1, x2 = jnp.split(x, 2, axis=-1)
    return jnp.concatenate([x1 * cos - x2 * sin, x2 * cos + x1 * sin], axis=-1)


def apply_axial_rope(x, tables):
    shape = (x.shape[1],) + (1,) * (x.ndim - 3) + (AXIS_DIM // 2,)
    cr, sr, cc, sc = [t.reshape(shape) for t in tables]
    x32 = x.astype(jnp.float32)
    xr, xcl = jnp.split(x32, 2, axis=-1)
    out = jnp.concatenate([_rotate_half(xr, cr, sr), _rotate_half(xcl, cc, sc)], axis=-1)
    return out.astype(x.dtype)


def gqa_attend(q, k, v):
    s = jnp.einsum('bqhgd,bkhd->bhgqk', q, k).astype(jnp.float32) * (HEAD_DIM ** -0.5)
    p = jax.nn.softmax(s, axis=-1).astype(v.dtype)
    return jnp.einsum('bhgqk,bkhd->bqhgd', p, v)


def attn_gmlp_mixer(h_lat, h_ctx, rope, w_in, w_out, q_g, k_g, ln_g, w_sp, b_sp, with_ctx):
    B, S, _ = h_lat.shape
    C = h_ctx.shape[1]

    def project(h):
        L = h.shape[1]
        q, k, v, u, vg = jnp.split(h @ w_in, MIX_SPLITS, axis=-1)
        q = rms_norm(q.reshape(B, L, N_KV_HEADS, Q_PER_KV, HEAD_DIM), q_g)
        k = rms_norm(k.reshape(B, L, N_KV_HEADS, HEAD_DIM), k_g)
        v = v.reshape(B, L, N_KV_HEADS, HEAD_DIM)
        return q, k, v, u, vg

    def spatial_gate(u, vg):
        L = u.shape[1]
        u = jax.nn.gelu(u)
        vg = layer_norm(jax.nn.gelu(vg), ln_g)
        vc = vg.reshape(B, L // GMLP_CHUNK, GMLP_CHUNK, GMLP_GROUPS, GMLP_GROUP_DIM)
        mixed = jnp.einsum('gpq,bcqgd->bcpgd', w_sp, vc) + b_sp.T[:, :, None]
        return u * mixed.reshape(B, L, GMLP_WIDTH)

    q_l, k_l, v_l, u_l, g_l = project(h_lat)
    q_c, k_c, v_c, u_c, g_c = project(h_ctx)
    q_l = apply_axial_rope(q_l, rope)
    k_l = apply_axial_rope(k_l, rope)
    k_all = jnp.concatenate([k_c, k_l], axis=1)
    v_all = jnp.concatenate([v_c, v_l], axis=1)
    n_blk = S // Q_BLOCK
    q_blocks = q_l.reshape(B, n_blk, Q_BLOCK, N_KV_HEADS, Q_PER_KV, HEAD_DIM).swapaxes(0, 1)
    o_l = lax.map(lambda qb: gqa_attend(qb, k_all, v_all), q_blocks)
    o_l = o_l.swapaxes(0, 1).reshape(B, S, ATTN_WIDTH)
    y_lat = jnp.concatenate([o_l, spatial_gate(u_l, g_l)], axis=-1) @ w_out
    y_ctx = None
    if with_ctx:
        o_c = gqa_attend(q_c, k_c, v_c).reshape(B, C, ATTN_WIDTH)
        y_ctx = jnp.concatenate([o_c, spatial_gate(u_c, g_c)], axis=-1) @ w_out
    return y_lat, y_ctx


def s5_zoh(a_re, a_im, log_dt, b_re, b_im):
    lam = lax.complex(a_re.astype(jnp.float32), a_im.astype(jnp.float32))
    dt = jnp.exp(log_dt.astype(jnp.float32))[:, None]
    lam_bar = jnp.exp(lam * dt)
    b = lax.complex(b_re.astype(jnp.float32), b_im.astype(jnp.float32))
    b_bar = ((lam_bar - 1.0) / lam)[..., None] * b
    return lam_bar, b_bar


def s5_drive(b_bar, u):
    return jnp.einsum('gnp,blgp->blgn', b_bar, u.astype(jnp.float32).astype(jnp.complex64))


def s5_readout(c_mat, h):
    return jnp.real(jnp.einsum('gpn,blgn->blgp', c_mat, h))


def diag_scan(lam_bar, bu, reverse):
    a = jnp.broadcast_to(lam_bar, bu.shape)

    def combine(e1, e2):
        a1, b1 = e1
        a2, b2 = e2
        return a1 * a2, a2 * b1 + b2

    _, h = lax.associative_scan(combine, (a, bu), reverse=reverse, axis=1)
    return h


def s5_mixer(h_lat, h_ctx, w_in, a_re, a_im, log_dt, b_re, b_im, c_re, c_im, d_skip,
             w_glu, b_glu, w_out, with_ctx):
    B, S, _ = h_lat.shape
    C = h_ctx.shape[1]
    u_l = (h_lat @ w_in).reshape(B, S, S5_GROUPS, S5_GROUP_DIM)
    u_c = (h_ctx @ w_in).reshape(B, C, S5_GROUPS, S5_GROUP_DIM)
    d = d_skip.astype(jnp.float32).reshape(S5_GROUPS, S5_GROUP_DIM)
    y_l = d * u_l.astype(jnp.float32)
    y_c = d * u_c.astype(jnp.float32) if with_ctx else None
    for direction in range(2):
        reverse = direction == 1
        lam_bar, b_bar = s5_zoh(a_re[direction], a_im[direction], log_dt[direction],
                                b_re[direction], b_im[direction])
        c_mat = lax.complex(c_re[direction].astype(jnp.float32), c_im[direction].astype(jnp.float32))
        h_c = diag_scan(lam_bar, s5_drive(b_bar, u_c), reverse)
        h0 = h_c[:, 0] if reverse else h_c[:, -1]
        edge = -1 if reverse else 0
        bu_l = s5_drive(b_bar, u_l).at[:, edge].add(lam_bar * h0)
        h_l = diag_scan(lam_bar, bu_l, reverse)
        y_l = y_l + s5_readout(c_mat, h_l)
        if with_ctx:
            y_c = y_c + s5_readout(c_mat, h_c)

    def gated_out(y):
        y = y.reshape(y.shape[0], y.shape[1], S5_WIDTH).astype(h_lat.dtype)
        g = jax.nn.gelu(y)
        return (g * jax.nn.sigmoid(g @ w_glu + b_glu)) @ w_out

    y_ctx = gated_out(y_c) if with_ctx else None
    return gated_out(y_l), y_ctx


def hier_moe(h, w_grp, b_grp, w_rt, b_rt, w1, w3, w2):
    T, D = h.shape
    grp_prob = jax.nn.softmax((h @ w_grp + b_grp).astype(jnp.float32), axis=-1)
    g_w, g_idx = lax.top_k(grp_prob, 1)
    exp_logits = (h @ w_rt + b_rt).astype(jnp.float32).reshape(T, N_EXPERT_GROUPS, EXPERTS_PER_GROUP)
    sel = jnp.take_along_axis(exp_logits, g_idx[:, :, None], axis=1)[:, 0]
    top_v, top_i = lax.top_k(sel, TOP_K_INNER)
    wts = jax.nn.softmax(top_v, axis=-1) * g_w
    eid = (g_idx * EXPERTS_PER_GROUP + top_i).reshape(-1).astype(jnp.int32)
    wt = wts.reshape(-1)
    tok = jnp.repeat(jnp.arange(T, dtype=jnp.int32), TOP_K_INNER)
    n_assign = T * TOP_K_INNER
    order = jnp.argsort(eid)
    eid_s, tok_s, wt_s = eid[order], tok[order], wt[order]
    counts = jnp.bincount(eid, length=N_EXPERTS).astype(jnp.int32)
    start = jnp.cumsum(counts) - counts
    padded = (counts + MOE_BLOCK - 1) // MOE_BLOCK * MOE_BLOCK
    pstart = jnp.cumsum(padded) - padded
    pend = pstart + padded
    dest = pstart[eid_s] + jnp.arange(n_assign, dtype=jnp.int32) - start[eid_s]
    n_slots = (n_assign + MOE_BLOCK - 1) // MOE_BLOCK * MOE_BLOCK + N_EXPERTS * MOE_BLOCK
    n_blocks = n_slots // MOE_BLOCK
    buf_tok = jnp.zeros((n_slots,), jnp.int32).at[dest].set(tok_s)
    buf_w = jnp.zeros((n_slots,), jnp.float32).at[dest].set(wt_s)
    block_start = jnp.arange(n_blocks, dtype=jnp.int32) * MOE_BLOCK
    block_e = jnp.minimum(jnp.sum(block_start[:, None] >= pend[None, :], axis=1), N_EXPERTS - 1)
    xb = h[buf_tok].reshape(n_blocks, MOE_BLOCK, D)

    def expert_block(args):
        xblk, e = args
        return (jax.nn.silu(xblk @ w1[e]) * (xblk @ w3[e])) @ w2[e]

    yb = lax.map(expert_block, (xb, block_e)).reshape(n_slots, D)
    return jnp.zeros((T, D), h.dtype).at[buf_tok].add(yb * buf_w[:, None].astype(h.dtype))


def setup_inputs(seed: int = 0) -> dict:
    key = jax.random.key(seed)
    ks = iter(jax.random.split(key, 48))
    n_even = (DEPTH + 1) // 2
    n_odd = DEPTH // 2
    D = D_MODEL

    def nrm(shape, scale):
        return jax.random.normal(next(ks), shape, jnp.float32) * scale

    def gain(shape):
        return 1.0 + nrm(shape, 0.02)

    inp = {}
    inp['x'] = nrm((BATCH, SEQ, D), 1.0)
    inp['c'] = nrm((BATCH, D), 1.0)
    inp['ctx'] = nrm((BATCH, CTX_LEN, D), 1.0)
    inp['c_ctx'] = nrm((D,), 1.0)
    inp['w_mod'] = nrm((DEPTH, D, 6 * D), 0.5 * D ** -0.5)
    inp['b_mod'] = nrm((DEPTH, 6 * D), 0.02)
    inp['norm_mix_g'] = gain((DEPTH, D))
    inp['norm_ffn_g'] = gain((DEPTH, D))
    inp['mix_w_in'] = nrm((n_even, D, MIX_IN_WIDTH), D ** -0.5)
    inp['mix_w_out'] = nrm((n_even, MIX_OUT_WIDTH, D), MIX_OUT_WIDTH ** -0.5)
    inp['q_norm_g'] = gain((n_even, HEAD_DIM))
    inp['k_norm_g'] = gain((n_even, HEAD_DIM))
    inp['gmlp_norm_g'] = gain((n_even, GMLP_WIDTH))
    inp['gmlp_w_spatial'] = nrm((n_even, GMLP_GROUPS, GMLP_CHUNK, GMLP_CHUNK), GMLP_CHUNK ** -0.5)
    inp['gmlp_b_spatial'] = 1.0 + nrm((n_even, GMLP_GROUPS, GMLP_CHUNK), 0.02)
    inp['s5_w_in'] = nrm((n_odd, D, S5_WIDTH), D ** -0.5)
    inp['s5_a_re'] = -0.5 + nrm((n_odd, 2, S5_GROUPS, S5_STATE), 0.01)
    inp['s5_a_im'] = jnp.pi * jnp.arange(S5_STATE, dtype=jnp.float32) + nrm((n_odd, 2, S5_GROUPS, S5_STATE), 0.01)
    inp['s5_log_dt'] = jax.random.uniform(next(ks), (n_odd, 2, S5_GROUPS), jnp.float32,
                                          math.log(1e-3), math.log(1e-1))
    inp['s5_b_re'] = nrm((n_odd, 2, S5_GROUPS, S5_STATE, S5_GROUP_DIM), (2 * S5_GROUP_DIM) ** -0.5)
    inp['s5_b_im'] = nrm((n_odd, 2, S5_GROUPS, S5_STATE, S5_GROUP_DIM), (2 * S5_GROUP_DIM) ** -0.5)
    inp['s5_c_re'] = nrm((n_odd, 2, S5_GROUPS, S5_GROUP_DIM, S5_STATE), S5_STATE ** -0.5)
    inp['s5_c_im'] = nrm((n_odd, 2, S5_GROUPS, S5_GROUP_DIM, S5_STATE), S5_STATE ** -0.5)
    inp['s5_d'] = nrm((n_odd, S5_WIDTH), 1.0)
    inp['s5_w_glu'] = nrm((n_odd, S5_WIDTH, S5_WIDTH), S5_WIDTH ** -0.5)
    inp['s5_b_glu'] = nrm((n_odd, S5_WIDTH), 0.02)
    inp['s5_w_out'] = nrm((n_odd, S5_WIDTH, D), S5_WIDTH ** -0.5)
    inp['moe_w_group'] = nrm((DEPTH, D, N_EXPERT_GROUPS), D ** -0.5)
    inp['moe_b_group'] = nrm((DEPTH, N_EXPERT_GROUPS), 0.01)
    inp['moe_w_router'] = nrm((DEPTH, D, N_EXPERTS), D ** -0.5)
    inp['moe_b_router'] = nrm((DEPTH, N_EXPERTS), 0.01)
    inp['moe_w1'] = nrm((DEPTH, N_EXPERTS, D, EXPERT_HIDDEN), D ** -0.5)
    inp['moe_w3'] = nrm((DEPTH, N_EXPERTS, D, EXPERT_HIDDEN), D ** -0.5)
    inp['moe_w2'] = nrm((DEPTH, N_EXPERTS, EXPERT_HIDDEN, D), EXPERT_HIDDEN ** -0.5)
    inp['final_norm_g'] = gain((D,))
    return inp


def reference(x, c, ctx, c_ctx, w_mod, b_mod, norm_mix_g, norm_ffn_g, mix_w_in, mix_w_out,
              q_norm_g, k_norm_g, gmlp_norm_g, gmlp_w_spatial, gmlp_b_spatial,
              s5_w_in, s5_a_re, s5_a_im, s5_log_dt, s5_b_re, s5_b_im, s5_c_re, s5_c_im, s5_d,
              s5_w_glu, s5_b_glu, s5_w_out,
              moe_w_group, moe_b_group, moe_w_router, moe_b_router, moe_w1, moe_w3, moe_w2,
              final_norm_g):
    B, S, D = x.shape
    C = ctx.shape[1]
    rope = axial_rope_tables(S)
    for layer in range(DEPTH):
        last = layer == DEPTH - 1
        i = layer // 2
        m_lat = [m[:, None, :] for m in adaln(c, w_mod[layer], b_mod[layer])]
        m_ctx = adaln(c_ctx, w_mod[layer], b_mod[layer])
        h_lat = modulate(x, norm_mix_g[layer], m_lat[0], m_lat[1])
        h_ctx = modulate(ctx, norm_mix_g[layer], m_ctx[0], m_ctx[1])
        if layer % 2 == 0:
            y_lat, y_ctx = attn_gmlp_mixer(h_lat, h_ctx, rope, mix_w_in[i], mix_w_out[i],
                                           q_norm_g[i], k_norm_g[i], gmlp_norm_g[i],
                                           gmlp_w_spatial[i], gmlp_b_spatial[i], not last)
        else:
            y_lat, y_ctx = s5_mixer(h_lat, h_ctx, s5_w_in[i], s5_a_re[i], s5_a_im[i], s5_log_dt[i],
                                    s5_b_re[i], s5_b_im[i], s5_c_re[i], s5_c_im[i], s5_d[i],
                                    s5_w_glu[i], s5_b_glu[i], s5_w_out[i], not last)
        x = x + m_lat[2] * y_lat
        f_lat = modulate(x, norm_ffn_g[layer], m_lat[3], m_lat[4]).reshape(B * S, D)
        moe_args = (moe_w_group[layer], moe_b_group[layer], moe_w_router[layer], moe_b_router[layer],
                    moe_w1[layer], moe_w3[layer], moe_w2[layer])
        if last:
            x = x + m_lat[5] * hier_moe(f_lat, *moe_args).reshape(B, S, D)
        else:
            ctx = ctx + m_ctx[2] * y_ctx
            f_ctx = modulate(ctx, norm_ffn_g[layer], m_ctx[3], m_ctx[4]).reshape(B * C, D)
            f = hier_moe(jnp.concatenate([f_lat, f_ctx], axis=0), *moe_args)
            x = x + m_lat[5] * f[:B * S].reshape(B, S, D)
            ctx = ctx + m_ctx[5] * f[B * S:].reshape(B, C, D)
    return rms_norm(x, final_norm_g)
```

```python
import numpy as np
from contextlib import ExitStack
import concourse.bass as bass
import concourse.mybir as mybir
from concourse.alu_op_type import AluOpType as ALU
from concourse.bass_utils import run_bass_kernel_spmd

F32 = mybir.dt.float32
BF16 = mybir.dt.bfloat16
I32 = mybir.dt.int32
AF = mybir.ActivationFunctionType
AX = mybir.AxisListType

D = 1024
S_LAT = 2048
C_CTX = 256
NB = 2
T_LAT = NB * S_LAT
T_CTX = NB * C_CTX
SEQ = C_CTX + S_LAT
EPS = 1e-6
CAP = 768
NSLOT = 32 * CAP
TRASH = NSLOT
NCONST = 128 * 4 + 32
S5_DEBUG_STAGE = 9


class Buf:
    __slots__ = ("w", "r")

    def __init__(self):
        self.w = None
        self.r = []


class Sched:
    COMPUTE = ("pe", "act", "dve", "pool")
    NDMA = 8

    def __init__(self, nc, es):
        self.nc = nc
        self.streams = {e: [] for e in ("pe", "act", "dve", "pool", "sp")}
        self.sem = {}
        self.cnt = {}
        for e in self.COMPUTE:
            self.sem[e] = es.enter_context(nc.semaphore("s_" + e))
            self.cnt[e] = 0
        self.drr = {}
        for q in ("sp", "act", "pool"):
            for k in range(self.NDMA):
                key = "d_%s%d" % (q, k)
                self.sem[key] = es.enter_context(nc.semaphore(key))
                self.cnt[key] = 0
            self.drr[q] = 0
        self.waited = {e: {} for e in self.streams}
        self.nops = 0

    def _deps(self, reads, writes):
        deps = []
        for b in reads:
            if b.w is not None:
                deps.append(b.w)
        for b in writes:
            if b.w is not None:
                deps.append(b.w)
            deps.extend(b.r)
        return deps

    def _emit_waits(self, eng, deps, skip_self=None):
        best = {}
        for (k, v) in deps:
            if k == skip_self:
                continue
            if v > best.get(k, 0):
                best[k] = v
        w = self.waited[eng]
        for k, v in best.items():
            if w.get(k, 0) < v:
                w[k] = v
                self.streams[eng].append(("wait", k, v))

    def _mark(self, tok, reads, writes):
        for b in writes:
            b.w = tok
            b.r = []
        for b in reads:
            if b.w is tok:
                continue
            b.r.append(tok)
            if len(b.r) > 16:
                best = {}
                for (k, v) in b.r:
                    if v > best.get(k, 0):
                        best[k] = v
                b.r = list(best.items())

    def op(self, eng, fn, reads=(), writes=()):
        deps = self._deps(reads, writes)
        self._emit_waits(eng, deps, skip_self=("pe" if eng == "pe" else None))
        self.cnt[eng] += 1
        tok = (eng, self.cnt[eng])
        self.streams[eng].append(("op", fn, eng, 1))
        self._mark(tok, reads, writes)
        self.nops += 1
        return tok

    def dma(self, q, fn, reads=(), writes=()):
        k = self.drr[q]
        self.drr[q] = (k + 1) % self.NDMA
        key = "d_%s%d" % (q, k)
        deps = self._deps(reads, writes)
        if self.cnt[key] > 0:
            deps.append((key, self.cnt[key]))
        self._emit_waits(q, deps)
        self.cnt[key] += 16
        tok = (key, self.cnt[key])
        self.streams[q].append(("op", fn, key, 16))
        self._mark(tok, reads, writes)
        self.nops += 1
        return tok

    def barrier(self):
        toks = [(k, v) for k, v in self.cnt.items() if v > 0]
        for e in self.streams:
            self._emit_waits(e, toks)

    def emit(self, block):
        sem = self.sem
        streams = self.streams

        def run(e, lst):
            for it in lst:
                if it[0] == "wait":
                    e.wait_ge(sem[it[1]], it[2])
                else:
                    getattr(e, it[1][0])(**it[1][1]).then_inc(sem[it[2]], it[3])

        @block.sync
        def _(e):
            run(e, streams["sp"])

        @block.scalar
        def _(e):
            run(e, streams["act"])

        @block.vector
        def _(e):
            run(e, streams["dve"])

        @block.gpsimd
        def _(e):
            run(e, streams["pool"])

        @block.tensor
        def _(e):
            run(e, streams["pe"])


class Arena:
    def __init__(self, t, size):
        self.t = t
        self.size = size
        self.off = 0

    def f32(self, n):
        a = self.t[:, self.off:self.off + n]
        self.off += n
        assert self.off <= self.size, ("arena overflow", self.off, self.size)
        return a

    def bf16(self, n):
        return self.f32((n + 1) // 2).bitcast(BF16)[:, 0:n]

    def i32(self, n):
        return self.f32(n).bitcast(I32)


def r3(ap, a):
    return ap.rearrange("p (a b) -> p a b", a=a)


def build_program(stop_after=99, dbg=()):
    nc = bass.Bass("TRN2", target_bir_lowering=False)
    dt = nc.dram_tensor

    def din(name, shape, dtype=F32):
        return dt(name, list(shape), dtype, kind="ExternalInput").ap()

    x_d = din("x", [T_LAT, D])
    ctx_d = din("ctx", [T_CTX, D])
    cT_d = din("cT", [128, 24])
    consts_d = din("consts", [128, NCONST])
    rope_d = din("rope", [128, 2 * SEQ])
    qkg_d = din("qkg", [128, 4])
    w_mod_d = din("w_mod", [2, D, 6 * D])
    b_mod_d = din("b_mod", [2, 1, 6 * D])
    gmix_d = din("norm_mix_g", [2, 1, D])
    gffn_d = din("norm_ffn_g", [2, 1, D])
    w_in_d = din("mix_w_in", [D, 1792])
    w_out_d = din("mix_w_out", [D, D])
    lng_d = din("gmlp_norm_g", [1, 512])
    wsp_d = din("gmlp_w_spatial", [8, 128, 128])
    bsp_d = din("gmlp_b_spatial", [1, 1024])
    wgrp_d = din("moe_w_group", [2, D, 4])
    bgrp_d = din("moe_b_group", [2, 1, 4])
    wrt_d = din("moe_w_router", [2, D, 32])
    brt_d = din("moe_b_router", [2, 1, 32])
    w1_d = din("moe_w1", [2, 32, D, 512])
    w3_d = din("moe_w3", [2, 32, D, 512])
    w2_d = din("moe_w2", [2, 32, 512, D])
    fng_d = din("final_norm_g", [1, D])
    s5win_d = din("s5_w_in", [D, 512])
    s5par_d = din("s5_par", [128, 2 * 16 * 3])
    s5iota_d = din("s5_iota", [128, SEQ])
    s5bT_d = din("s5_bT", [2, 2, 16, 128, 128])
    s5c_d = din("s5_c", [2, 2, 16, 128, 128])
    s5d_d = din("s5_d", [128, 4])
    s5glu_d = din("s5_w_glu", [512, 512])
    s5bglu_d = din("s5_b_glu", [128, 4])
    s5wout_d = din("s5_w_out", [512, D])

    out_d = dt("out", [T_LAT, D], F32, kind="ExternalOutput").ap()
    modrows_d = dt("modrows", [2, 6, 3, D], F32, kind="Internal").ap()
    xr_d = dt("xr", [T_LAT + T_CTX, D], F32, kind="Internal").ap()
    xr2_d = dt("xr2", [T_LAT + T_CTX, D], F32, kind="Internal").ap()
    xall_d = dt("xall", [NSLOT + 128, D], BF16, kind="Internal").ap()
    yall_d = dt("yall", [NSLOT + 128, D], F32, kind="Internal").ap()
    dbg_d = {}
    for name, shape in dbg:
        dbg_d[name] = dt("dbg_" + name, list(shape), F32, kind="ExternalOutput").ap()

    with ExitStack() as es:
        S = Sched(nc, es)
        ARENA_WORDS = 52500
        arena_t = es.enter_context(nc.sbuf_tensor("arena", [128, ARENA_WORDS], F32))
        psum_t = es.enter_context(nc.psum_tensor("psum", [128, 4096], F32))
        A = Arena(arena_t, ARENA_WORDS)

        def bank(i, n=512, off=0):
            return psum_t[:, i * 512 + off:i * 512 + off + n]

        def bank_bf(i):
            return psum_t[:, i * 512:(i + 1) * 512].bitcast(BF16)

        PB = [Buf() for _ in range(8)]

        cst = A.f32(NCONST)
        Bc = Buf()
        S.dma("sp", ("dma_start", dict(out=cst, in_=consts_d)), writes=[Bc])
        ident_f = cst[:, 0:128]
        utri_f = cst[:, 128:256]
        ones_f = cst[:, 256:384]
        blk_f = cst[:, 384:512]
        iotaC = cst[:, 512:544]
        cbf = A.bf16(512)
        S.op("dve", ("tensor_copy", dict(out=cbf, in_=cst[:, 0:512])), reads=[Bc], writes=[Bc])
        ident_b = cbf[:, 0:128]
        utri_b = cbf[:, 128:256]
        ones_b = cbf[:, 256:384]
        blk_b = cbf[:, 384:512]
        modT = A.f32(2 * 2 * 8 * 3)
        BmodT = Buf()
        persist_off = A.off

        cT = A.f32(24)
        sc = A.f32(24)
        Bsc = Buf()
        S.dma("sp", ("dma_start", dict(out=cT, in_=cT_d)), writes=[Bsc])
        S.op("act", ("activation", dict(out=sc, in_=cT, func=AF.Silu)), reads=[Bsc], writes=[Bsc])
        sc3 = r3(sc, 8)
        wblk = [A.f32(8 * 512) for _ in range(2)]
        Bwblk = [Buf(), Buf()]
        mrow = A.f32(6 * D)
        gb = A.f32(2 * D)
        bb = A.f32(6 * D)
        Bmrow, Bgb, Bbb = Buf(), Buf(), Buf()
        Bmodrows = Buf()
        for l in range(2):
            S.dma("sp", ("dma_start", dict(out=bb[0:3, :], in_=b_mod_d[l].broadcast_to([3, 6 * D]))), writes=[Bbb])
            S.dma("sp", ("dma_start", dict(out=gb[0:3, 0:D], in_=gmix_d[l].broadcast_to([3, D]))), writes=[Bgb])
            S.dma("sp", ("dma_start", dict(out=gb[0:3, D:2 * D], in_=gffn_d[l].broadcast_to([3, D]))), writes=[Bgb])
            for nb in range(12):
                wb = wblk[nb % 2]
                Bw = Bwblk[nb % 2]
                S.dma("sp", ("dma_start", dict(
                    out=r3(wb, 8), in_=w_mod_d[l][:, nb * 512:(nb + 1) * 512].rearrange("(k p) n -> p k n", p=128))), writes=[Bw])
                pb = nb % 2
                for k in range(8):
                    S.op("pe", ("matmul", dict(out=bank(pb)[0:3, :], lhsT=sc3[:, k, :], rhs=r3(wb, 8)[:, k, :],
                                                                       start=(k == 0), stop=(k == 7))), reads=[Bsc, Bw], writes=[PB[pb]])
                S.op("dve", ("tensor_tensor", dict(out=mrow[0:3, nb * 512:(nb + 1) * 512], in0=bank(pb)[0:3, :],
                                                                      in1=bb[0:3, nb * 512:(nb + 1) * 512], op=ALU.add)),
                     reads=[PB[pb], Bbb], writes=[Bmrow])
            for (kind, goff) in ((1, 0), (4, D)):
                S.op("dve", ("scalar_tensor_tensor", dict(
                    out=mrow[0:3, kind * D:(kind + 1) * D], in0=mrow[0:3, kind * D:(kind + 1) * D], scalar=1.0,
                    in1=gb[0:3, goff:goff + D], op0=ALU.add, op1=ALU.mult)), reads=[Bmrow, Bgb], writes=[Bmrow])
            S.dma("sp", ("dma_start", dict(out=modrows_d[l].rearrange("k c d -> c k d"), in_=r3(mrow[0:3, :], 6))),
                  reads=[Bmrow], writes=[Bmodrows])
        modT5 = modT.rearrange("p (l m k c) -> p l m k c", l=2, m=2, k=8)
        for l in range(2):
            for m in range(2):
                for c in range(3):
                    S.dma("sp", ("dma_start", dict(
                        out=modT5[:, l, m, :, c], in_=modrows_d[l, m, c].rearrange("(k p) -> p k", p=128),
                        allow_slow_non_contiguous=True)), reads=[Bmodrows], writes=[BmodT])
        S.barrier()
        A.off = persist_off
        if "modrows" in dbg_d:
            S.dma("sp", ("dma_start", dict(out=dbg_d["modrows"], in_=modrows_d.rearrange("l k c d -> (l k c) d"))), reads=[Bmodrows])

        def tile_src(layer, ti):
            if layer == 0:
                if ti < 32:
                    return x_d[ti * 128:(ti + 1) * 128, :]
                return ctx_d[(ti - 32) * 128:(ti - 31) * 128, :]
            return xr2_d[ti * 128:(ti + 1) * 128, :]

        def tile_col(ti):
            if ti < 32:
                return ti // 16
            return 2

        class NormT:
            def __init__(self):
                self.xt = [A.f32(D) for _ in range(2)]
                self.Bxt = [Buf(), Buf()]
                self.junk = A.bf16(D)
                self.Bjunk = Buf()
                self.xn = [A.bf16(D) for _ in range(2)]
                self.Bxn = [Buf(), Buf()]
                self.ss = [A.f32(1) for _ in range(2)]
                self.Bss = [Buf(), Buf()]
                self.i = 0

            def run(self, layer, ti, hT3, BhT, c0, psb):
                i = self.i
                self.i += 1
                xt, Bxt = self.xt[i % 2], self.Bxt[i % 2]
                xn, Bxn = self.xn[i % 2], self.Bxn[i % 2]
                ss, Bss = self.ss[i % 2], self.Bss[i % 2]
                junk, Bjunk = self.junk, self.Bjunk
                src = tile_src(layer, ti)
                col = tile_col(ti)
                S.dma("sp", ("dma_start", dict(out=xt, in_=src)), writes=[Bxt])
                S.op("act", ("activation", dict(out=junk, in_=xt, func=AF.Square, accum_out=ss)), reads=[Bxt], writes=[Bjunk, Bss])
                S.op("act", ("activation", dict(out=ss, in_=ss, func=AF.Sqrt, scale=1.0 / D, bias=EPS)), reads=[Bss], writes=[Bss])
                S.op("dve", ("reciprocal", dict(out=ss, in_=ss)), reads=[Bss], writes=[Bss])
                S.op("dve", ("tensor_scalar", dict(out=xn, in0=xt, scalar1=ss, scalar2=None, op0=ALU.mult)), reads=[Bxt, Bss], writes=[Bxn])
                pT = r3(bank_bf(psb), 8)
                for k in range(8):
                    S.op("pe", ("transpose", dict(out=pT[:, k, :], in_=xn[:, k * 128:(k + 1) * 128], identity=ident_b)),
                         reads=[Bxn, Bc], writes=[PB[psb]])
                for k in range(8):
                    eng = "act" if k % 2 == 0 else "dve"
                    if eng == "act":
                        S.op("act", ("activation", dict(out=hT3[:, k, c0:c0 + 128], in_=pT[:, k, :], func=AF.Identity,
                                                                  scale=modT5[:, layer, 1, k, col:col + 1], bias=modT5[:, layer, 0, k, col:col + 1])),
                             reads=[PB[psb], BmodT], writes=[BhT])
                    else:
                        S.op("dve", ("tensor_scalar", dict(out=hT3[:, k, c0:c0 + 128], in0=pT[:, k, :],
                                                                     scalar1=modT5[:, layer, 1, k, col:col + 1], scalar2=modT5[:, layer, 0, k, col:col + 1],
                                                                     op0=ALU.mult, op1=ALU.add)),
                             reads=[PB[psb], BmodT], writes=[BhT])

        def phase1():
            L = 0
            GELU = AF.Gelu_apprx_tanh
            WC = 1280
            w_in = A.bf16(8 * WC)
            w_in3 = r3(w_in, 8)
            Bwin = Buf()
            for k in range(8):
                S.dma("pool", ("dma_start", dict(out=w_in3[:, k, :], in_=w_in_d[k * 128:(k + 1) * 128, 512:1792])), writes=[Bwin])
            wst = A.bf16(8 * 512)
            wst5 = wst.rearrange("p (k j two d) -> p k j two d", k=8, j=4, two=2)
            for k in range(8):
                for two in range(2):
                    S.dma("pool", ("dma_start", dict(out=wst5[:, k, :, two, :],
                                                     in_=w_in_d[k * 128:(k + 1) * 128, two * 256:(two + 1) * 256].rearrange("p (j d) -> p j d", j=4))), writes=[Bwin])
            wst3 = r3(wst, 8)
            wpst = A.bf16(8 * 512)
            wpst3 = r3(wpst, 8)
            wkp = A.bf16(8 * 128)
            wkp3 = r3(wkp, 8)
            Bwperm = Buf()
            sv = wst.rearrange("p (k x b i) -> p k x b i", k=8, b=2, i=16)
            dv = wpst.rearrange("p (k x b i) -> p k x b i", k=8, b=2, i=16)
            svk = w_in3[:, :, 0:128].rearrange("p k (x b i) -> p k x b i", b=2, i=16)
            dvk = wkp3.rearrange("p k (x b i) -> p k x b i", b=2, i=16)
            for b_ in range(2):
                S.op("dve", ("tensor_copy", dict(out=dv[:, :, :, b_, :], in_=sv[:, :, :, 1 - b_, :])), reads=[Bwin], writes=[Bwperm])
                S.op("dve", ("tensor_copy", dict(out=dvk[:, :, :, b_, :], in_=svk[:, :, :, 1 - b_, :])), reads=[Bwin], writes=[Bwperm])
            wout = A.bf16(16 * 1024)
            wout3 = r3(wout, 16)
            Bwout = Buf()
            for c4 in range(4):
                S.dma("pool", ("dma_start", dict(out=wout3[0:64, c4 * 4:(c4 + 1) * 4, :],
                                                            in_=w_out_d[c4 * 256:(c4 + 1) * 256, :].rearrange("(c r) n -> r c n", r=64))), writes=[Bwout])
            yt = A.f32(D)
            wspn = yt
            Bwspn = Buf()
            S.dma("sp", ("dma_start", dict(out=r3(wspn, 8), in_=wsp_d.rearrange("g p q -> p g q"))), writes=[Bwspn])
            wspT = A.bf16(1024)
            wspT3 = r3(wspT, 8)
            BwspT = Buf()
            pw = r3(psum_t[:, 0:1024], 8)
            for g in range(8):
                S.op("pe", ("transpose", dict(out=pw[:, g, :], in_=r3(wspn, 8)[:, g, :], identity=ident_f)),
                     reads=[Bwspn, Bc], writes=[PB[0], PB[1]])
            S.op("act", ("copy", dict(out=wspT, in_=psum_t[:, 0:1024])), reads=[PB[0], PB[1]], writes=[BwspT])
            lng_bc = A.f32(512)
            bsp_bc = A.f32(1024)
            qkg = A.f32(4)
            cosT = A.f32(SEQ)
            sinT = A.f32(SEQ)
            gate_bc = [A.f32(D) for _ in range(3)]
            Bsm = Buf()
            S.dma("sp", ("dma_start", dict(out=lng_bc, in_=lng_d.broadcast_to([128, 512]))), writes=[Bsm])
            S.dma("sp", ("dma_start", dict(out=bsp_bc[0:64, :], in_=bsp_d.broadcast_to([64, 1024]))), writes=[Bsm])
            S.dma("sp", ("dma_start", dict(out=qkg, in_=qkg_d)), writes=[Bsm])
            S.dma("sp", ("dma_start", dict(out=cosT, in_=rope_d[:, 0:SEQ])), writes=[Bsm])
            S.dma("sp", ("dma_start", dict(out=sinT, in_=rope_d[:, SEQ:2 * SEQ])), writes=[Bsm])
            for c in range(3):
                S.dma("sp", ("dma_start", dict(out=gate_bc[c], in_=modrows_d[L, 2, c:c + 1, :].broadcast_to([128, D]))),
                      reads=[Bmodrows], writes=[Bsm])
            bsp3 = r3(bsp_bc[0:64, :], 8)

            QT = A.bf16(4 * SEQ)
            QT3 = r3(QT, 4)
            KT = A.bf16(SEQ)
            Vt = A.bf16(18 * 128)
            V3 = r3(Vt, 18)
            BQ, BK, BV = Buf(), Buf(), Buf()
            hT1 = A.bf16(8 * 512)
            hT = [hT1, hT1]
            BhT1 = Buf()
            BhT = [BhT1, BhT1]
            NT = NormT()
            sqb = A.bf16(512)
            rs = A.f32(512)
            t1 = A.f32(512)
            t2 = A.f32(512)
            Bsq, Brs, Bt1, Bt2 = Buf(), Buf(), Buf(), Buf()
            UG = A.bf16(8 * 512)
            UG3 = r3(UG, 8)
            GT3 = UG3
            OT = A.bf16(8 * 512)
            OT3 = r3(OT, 8)
            BUG = Buf()
            BGT = BUG
            BOT = Buf()
            gv = t1
            vnf = t2
            vnb = A.bf16(512)
            st6 = A.f32(6)
            mv = A.f32(2)
            mx = A.f32(1024)
            Bgv, Bvnf = Bt1, Bt2
            Bvnb, Bst, Bmv, Bmx = Buf(), Buf(), Buf(), Buf()
            PT = [A.bf16(512) for _ in range(3)]
            BPT = [Buf(), Buf(), Buf()]
            rec = A.f32(512)
            Brec = Buf()
            xt2 = A.f32(D)
            Byt, Bxt2 = Buf(), Buf()
            Bxr = Buf()
            hcount = [0]

            def make_hT(blk):
                tiles, c0seq, N = blk
                i = hcount[0]
                hcount[0] += 1
                h3 = r3(hT[i % 2], 8)
                for t, ti in enumerate(tiles):
                    NT.run(L, ti, h3, BhT[i % 2], t * 128, t % 2)
                return h3, BhT[i % 2]

            for b in range(NB):
                blocks = [([32 + 2 * b, 33 + 2 * b], 0, 256)]
                for i in range(4):
                    blocks.append(([16 * b + 4 * i + t for t in range(4)], 256 + 512 * i, 512))
                for blk in blocks:
                    tiles, c0, N = blk
                    h3, Bh = make_hT(blk)
                    for j in range(5):
                        for (pb, wq, wk) in ((2, wst3, w_in3), (3, wpst3, wkp3)):
                            for k in range(8):
                                lw = wq[:, k, j * 128:(j + 1) * 128] if j < 4 else wk[:, k, 0:128]
                                S.op("pe", ("matmul", dict(out=bank(pb)[:, 0:N], lhsT=lw, rhs=h3[:, k, 0:N], start=(k == 0), stop=(k == 7))),
                                     reads=[Bwin, Bwperm, Bh], writes=[PB[pb]])
                        gi = 0 if j < 4 else 2
                        S.op("act", ("activation", dict(out=sqb[:, 0:N], in_=bank(2)[:, 0:N], func=AF.Square)), reads=[PB[2]], writes=[Bsq])
                        S.op("pe", ("matmul", dict(out=bank(4)[:, 0:N], lhsT=blk_b, rhs=sqb[:, 0:N], start=True, stop=True)), reads=[Bsq, Bc], writes=[PB[4]])
                        S.op("act", ("activation", dict(out=rs[:, 0:N], in_=bank(4)[:, 0:N], func=AF.Sqrt, bias=EPS, scale=1.0)), reads=[PB[4]], writes=[Brs])
                        S.op("dve", ("reciprocal", dict(out=rs[:, 0:N], in_=rs[:, 0:N])), reads=[Brs], writes=[Brs])
                        S.op("dve", ("scalar_tensor_tensor", dict(out=t1[:, 0:N], in0=bank(2)[:, 0:N], scalar=qkg[:, gi:gi + 1], in1=cosT[:, c0:c0 + N],
                                                                             op0=ALU.mult, op1=ALU.mult)), reads=[PB[2], Bsm], writes=[Bt1])
                        S.op("dve", ("scalar_tensor_tensor", dict(out=t2[:, 0:N], in0=bank(3)[:, 0:N], scalar=qkg[:, gi + 1:gi + 2], in1=sinT[:, c0:c0 + N],
                                                                             op0=ALU.mult, op1=ALU.mult)), reads=[PB[3], Bsm], writes=[Bt2])
                        S.op("dve", ("tensor_tensor", dict(out=t1[:, 0:N], in0=t1[:, 0:N], in1=t2[:, 0:N], op=ALU.add)), reads=[Bt1, Bt2], writes=[Bt1])
                        if j < 4:
                            dst, Bd = QT3[:, j, c0:c0 + N], BQ
                        else:
                            dst, Bd = KT[:, c0:c0 + N], BK
                        S.op("dve", ("tensor_tensor", dict(out=dst, in0=t1[:, 0:N], in1=rs[:, 0:N], op=ALU.mult)), reads=[Bt1, Brs], writes=[Bd])
                    for t in range(len(tiles)):
                        kt = c0 // 128 + t
                        for k in range(8):
                            S.op("pe", ("matmul", dict(out=bank(5)[:, 0:128], lhsT=h3[:, k, t * 128:(t + 1) * 128], rhs=w_in3[:, k, 128:256],
                                                                      start=(k == 0), stop=(k == 7))), reads=[Bwin, Bh], writes=[PB[5]])
                        S.op("act", ("copy", dict(out=V3[:, kt, :], in_=bank(5)[:, 0:128])), reads=[PB[5]], writes=[BV])
                for bi, blk in enumerate(blocks):
                    tiles, c0, N = blk
                    h3, Bh = make_hT(blk)
                    for g in range(8):
                        pb = 2 + g % 2
                        for k in range(8):
                            S.op("pe", ("matmul", dict(out=bank(pb)[0:64, 0:N], lhsT=w_in3[:, k, 256 + g * 64:320 + g * 64], rhs=h3[:, k, 0:N],
                                                                             start=(k == 0), stop=(k == 7))), reads=[Bwin, Bh], writes=[PB[pb]])
                        S.op("act", ("activation", dict(out=UG3[0:64, g, 0:N], in_=bank(pb)[0:64, 0:N], func=GELU)), reads=[PB[pb]], writes=[BUG])
                    for t in range(len(tiles)):
                        tc0 = t * 128
                        for k in range(8):
                            S.op("pe", ("matmul", dict(out=bank(4), lhsT=h3[:, k, tc0:tc0 + 128], rhs=w_in3[:, k, 768:1280],
                                                                          start=(k == 0), stop=(k == 7))), reads=[Bwin, Bh], writes=[PB[4]])
                        S.op("act", ("activation", dict(out=gv, in_=bank(4), func=GELU)), reads=[PB[4]], writes=[Bgv])
                        S.op("dve", ("bn_stats", dict(out=st6, in_=gv)), reads=[Bgv], writes=[Bst])
                        S.op("dve", ("bn_aggr", dict(out=mv, in_=st6)), reads=[Bst], writes=[Bmv])
                        S.op("act", ("activation", dict(out=mv[:, 1:2], in_=mv[:, 1:2], func=AF.Sqrt, bias=EPS, scale=1.0)), reads=[Bmv], writes=[Bmv])
                        S.op("dve", ("reciprocal", dict(out=mv[:, 1:2], in_=mv[:, 1:2])), reads=[Bmv], writes=[Bmv])
                        S.op("dve", ("tensor_scalar", dict(out=vnf, in0=gv, scalar1=mv[:, 0:1], scalar2=mv[:, 1:2], op0=ALU.subtract, op1=ALU.mult)),
                             reads=[Bgv, Bmv], writes=[Bvnf])
                        S.op("dve", ("tensor_tensor", dict(out=vnb, in0=vnf, in1=lng_bc, op=ALU.mult)), reads=[Bvnf, Bsm], writes=[Bvnb])
                        pm = r3(psum_t[0:64, 6 * 512:8 * 512], 8)
                        for g in range(8):
                            S.op("pe", ("matmul", dict(out=pm[:, g, :], lhsT=vnb[:, g * 64:(g + 1) * 64], rhs=wspT3[:, g, :], start=True, stop=True)),
                                 reads=[Bvnb, BwspT], writes=[PB[6], PB[7]])
                        S.op("dve", ("tensor_tensor", dict(out=r3(mx[0:64, :], 8), in0=pm, in1=bsp3, op=ALU.add)), reads=[PB[6], PB[7], Bsm], writes=[Bmx])
                        S.op("dve", ("tensor_tensor", dict(out=GT3[0:64, :, tc0:tc0 + 128], in0=r3(mx[0:64, :], 8), in1=UG3[0:64, :, tc0:tc0 + 128], op=ALU.mult)),
                             reads=[Bmx, BUG], writes=[BGT])
                    kts = list(range(2)) if bi == 0 else list(range(18))
                    pti = 0
                    for h in range(8):
                        half, j = h // 4, h % 4
                        p0 = half * 64
                        bo, bd = 2 + (h % 2) * 2, 3 + (h % 2) * 2
                        for ki, kt in enumerate(kts):
                            sb_ = ki % 2
                            S.op("pe", ("matmul", dict(out=bank(sb_)[:, 0:N], lhsT=KT[p0:p0 + 64, kt * 128:(kt + 1) * 128],
                                                                                  rhs=QT3[p0:p0 + 64, j, c0:c0 + N], start=True, stop=True)),
                                 reads=[BK, BQ], writes=[PB[sb_]])
                            pt, Bpt = PT[pti % 3], BPT[pti % 3]
                            pti += 1
                            S.op("act", ("activation", dict(out=pt[:, 0:N], in_=bank(sb_)[:, 0:N], func=AF.Exp, scale=0.125)), reads=[PB[sb_]], writes=[Bpt])
                            S.op("pe", ("matmul", dict(out=bank(bo)[0:64, 0:N], lhsT=V3[:, kt, p0:p0 + 64], rhs=pt[:, 0:N],
                                                                                         start=(ki == 0), stop=(ki == len(kts) - 1))), reads=[BV, Bpt], writes=[PB[bo]])
                            S.op("pe", ("matmul", dict(out=bank(bd)[0:64, 0:N], lhsT=ones_b[:, 0:64], rhs=pt[:, 0:N],
                                                                                start=(ki == 0), stop=(ki == len(kts) - 1))), reads=[Bc, Bpt], writes=[PB[bd]])
                        S.op("dve", ("reciprocal", dict(out=rec[0:64, 0:N], in_=bank(bd)[0:64, 0:N])), reads=[PB[bd]], writes=[Brec])
                        S.op("dve", ("tensor_tensor", dict(out=OT3[0:64, h, 0:N], in0=bank(bo)[0:64, 0:N], in1=rec[0:64, 0:N], op=ALU.mult)),
                             reads=[PB[bo], Brec], writes=[BOT])
                    for t, ti in enumerate(tiles):
                        tc0 = t * 128
                        col = tile_col(ti)
                        for hf in range(2):
                            for c in range(16):
                                lw = OT3[0:64, c, tc0:tc0 + 128] if c < 8 else GT3[0:64, c - 8, tc0:tc0 + 128]
                                S.op("pe", ("matmul", dict(out=bank(6 + hf), lhsT=lw, rhs=wout3[0:64, c, hf * 512:(hf + 1) * 512],
                                                                                 start=(c == 0), stop=(c == 15))), reads=[BOT, BGT, Bwout], writes=[PB[6 + hf]])
                        S.dma("sp", ("dma_start", dict(out=xt2, in_=tile_src(L, ti))), writes=[Bxt2])
                        S.op("dve", ("tensor_tensor", dict(out=yt, in0=psum_t[:, 6 * 512:8 * 512], in1=gate_bc[col], op=ALU.mult)),
                             reads=[PB[6], PB[7], Bsm], writes=[Byt])
                        S.op("dve", ("tensor_tensor", dict(out=yt, in0=yt, in1=xt2, op=ALU.add)), reads=[Byt, Bxt2], writes=[Byt])
                        S.dma("sp", ("dma_start", dict(out=xr_d[ti * 128:(ti + 1) * 128, :], in_=yt)), reads=[Byt], writes=[Bxr])
            return Bxr

        if stop_after >= 1:
            Bxr = phase1()
            S.barrier()
            A.off = persist_off
            if "xr" in dbg_d:
                S.dma("sp", ("dma_start", dict(out=dbg_d["xr"], in_=xr_d)), reads=[Bxr])
        def moe(L, ntiles, Bsrc):
            last = (L == 1)
            ncol = 2 if last else 3
            Abc = [A.f32(D) for _ in range(ncol)]
            Sbc = [A.f32(D) for _ in range(ncol)]
            Gbc = [A.f32(D) for _ in range(ncol)]
            Bbc = Buf()
            for c in range(ncol):
                for (kind, dst) in ((4, Abc), (3, Sbc), (5, Gbc)):
                    S.dma("sp", ("dma_start", dict(out=dst[c], in_=modrows_d[L, kind, c:c + 1, :].broadcast_to([128, D]))), reads=[Bmodrows], writes=[Bbc])
            fng = A.f32(D)
            if last:
                S.dma("sp", ("dma_start", dict(out=fng, in_=fng_d.broadcast_to([128, D]))), writes=[Bbc])
            w36 = A.f32(8 * 36)
            w36_3 = r3(w36, 8)
            b36 = A.f32(36)
            S.dma("sp", ("dma_start", dict(out=w36_3[:, :, 0:4], in_=wgrp_d[L].rearrange("(k p) n -> p k n", p=128), allow_slow_non_contiguous=True)), writes=[Bbc])
            S.dma("sp", ("dma_start", dict(out=w36_3[:, :, 4:36], in_=wrt_d[L].rearrange("(k p) n -> p k n", p=128), allow_slow_non_contiguous=True)), writes=[Bbc])
            S.dma("sp", ("dma_start", dict(out=b36[:, 0:4], in_=bgrp_d[L].broadcast_to([128, 4]))), writes=[Bbc])
            S.dma("sp", ("dma_start", dict(out=b36[:, 4:36], in_=brt_d[L].broadcast_to([128, 32]))), writes=[Bbc])
            slot_i = A.i32(ntiles * 2)
            slot_i3 = r3(slot_i, ntiles)
            wts = A.f32(ntiles * 2)
            wts3 = r3(wts, ntiles)
            Bslot = Buf()
            run = A.f32(32)
            Brun = Buf()
            S.op("dve", ("memset", dict(ap=run, constant=0.0)), writes=[Brun])
            ztile = A.f32(D)
            Bz = Buf()
            Byall = Buf()
            Bxall = Buf()
            S.op("dve", ("memset", dict(ap=ztile, constant=0.0)), writes=[Bz])
            S.dma("sp", ("dma_start", dict(out=yall_d[NSLOT:NSLOT + 128, :], in_=ztile)), reads=[Bz], writes=[Byall])
            mark = A.off
            xt = [A.f32(D) for _ in range(2)]
            Bxt = [Buf(), Buf()]
            ff = [A.f32(D) for _ in range(2)]
            Bff = [Buf(), Buf()]
            fb = [A.bf16(D) for _ in range(2)]
            Bfb = [Buf(), Buf()]
            fTs = A.f32(D)
            BfTs = Buf()
            junk = A.bf16(D)
            Bjunk = Buf()
            sm = A.f32(256)
            Bsmall = Buf()
            ss = sm[:, 0:1]
            lg = sm[:, 4:40]
            gmax = sm[:, 40:41]
            ngmax = sm[:, 41:42]
            gsum = sm[:, 42:43]
            eg = sm[:, 44:48]
            gmask = sm[:, 48:52]
            pen = sm[:, 52:56]
            masked = sm[:, 56:88]
            m8 = sm[:, 88:96]
            ntop1 = sm[:, 96:97]
            e2 = sm[:, 97:98]
            wa = sm[:, 98:99]
            wb = sm[:, 99:100]
            slotf = sm[:, 100:102]
            vab = sm[:, 102:104]
            sel1 = sm[:, 104:136]
            sel = sm[:, 136:168]
            pos = sm[:, 168:200]
            valid = sm[:, 200:232]
            tmp32 = A.f32(32)
            selb = A.bf16(32)
            for ti in range(ntiles):
                col = tile_col(ti)
                x_, Bx_ = xt[ti % 2], Bxt[ti % 2]
                f_, Bf_ = ff[ti % 2], Bff[ti % 2]
                fb_, Bfb_ = fb[ti % 2], Bfb[ti % 2]
                S.dma("sp", ("dma_start", dict(out=x_, in_=xr_d[ti * 128:(ti + 1) * 128, :])), reads=[Bsrc], writes=[Bx_])
                S.op("act", ("activation", dict(out=junk, in_=x_, func=AF.Square, accum_out=ss)), reads=[Bx_], writes=[Bjunk, Bsmall])
                S.op("act", ("activation", dict(out=ss, in_=ss, func=AF.Sqrt, scale=1.0 / D, bias=EPS)), reads=[Bsmall], writes=[Bsmall])
                S.op("dve", ("reciprocal", dict(out=ss, in_=ss)), reads=[Bsmall], writes=[Bsmall])
                S.op("dve", ("scalar_tensor_tensor", dict(out=f_, in0=x_, scalar=ss, in1=Abc[col], op0=ALU.mult, op1=ALU.mult)), reads=[Bx_, Bsmall, Bbc], writes=[Bf_])
                S.op("dve", ("tensor_tensor", dict(out=f_, in0=f_, in1=Sbc[col], op=ALU.add)), reads=[Bf_, Bbc], writes=[Bf_])
                S.op("act", ("copy", dict(out=fb_, in_=f_)), reads=[Bf_], writes=[Bfb_])
                pT = r3(psum_t[:, 0:1024], 8)
                for k in range(8):
                    S.op("pe", ("transpose", dict(out=pT[:, k, :], in_=f_[:, k * 128:(k + 1) * 128], identity=ident_f)), reads=[Bf_, Bc], writes=[PB[0], PB[1]])
                S.op("act", ("copy", dict(out=fTs, in_=psum_t[:, 0:1024])), reads=[PB[0], PB[1]], writes=[BfTs])
                for k in range(8):
                    S.op("pe", ("matmul", dict(out=bank(2)[:, 0:36], lhsT=fTs[:, k * 128:(k + 1) * 128], rhs=w36_3[:, k, :], start=(k == 0), stop=(k == 7))),
                         reads=[BfTs, Bbc], writes=[PB[2]])
                dv = lambda name, **kw: S.op("dve", (name, kw), reads=[Bsmall, Brun], writes=[Bsmall])
                S.op("dve", ("tensor_tensor", dict(out=lg, in0=bank(2)[:, 0:36], in1=b36, op=ALU.add)), reads=[PB[2], Bbc], writes=[Bsmall])
                dv("tensor_reduce", out=gmax, in_=lg[:, 0:4], axis=AX.X, op=ALU.max)
                dv("tensor_scalar", out=ngmax, in0=gmax, scalar1=-1.0, scalar2=None, op0=ALU.mult)
                S.op("act", ("activation", dict(out=eg, in_=lg[:, 0:4], func=AF.Exp, bias=ngmax, scale=1.0, accum_out=gsum)), reads=[Bsmall], writes=[Bsmall])
                dv("tensor_scalar", out=gmask, in0=lg[:, 0:4], scalar1=gmax, scalar2=None, op0=ALU.is_ge)
                dv("tensor_scalar", out=pen, in0=gmask, scalar1=1e30, scalar2=-1e30, op0=ALU.mult, op1=ALU.add)
                dv("tensor_tensor", out=r3(masked, 4), in0=r3(lg[:, 4:36], 4), in1=pen.unsqueeze(2).broadcast_to([128, 4, 8]), op=ALU.add)
                dv("max", out=m8, in_=masked)
                dv("tensor_scalar", out=sel1, in0=masked, scalar1=m8[:, 0:1], scalar2=None, op0=ALU.is_ge)
                dv("tensor_scalar", out=sel, in0=masked, scalar1=m8[:, 1:2], scalar2=None, op0=ALU.is_ge)
                dv("tensor_scalar", out=ntop1, in0=m8[:, 0:1], scalar1=-1.0, scalar2=None, op0=ALU.mult)
                S.op("act", ("activation", dict(out=e2, in_=m8[:, 1:2], func=AF.Exp, bias=ntop1, scale=1.0)), reads=[Bsmall], writes=[Bsmall])
                dv("tensor_scalar", out=wa, in0=e2, scalar1=1.0, scalar2=gsum, op0=ALU.add, op1=ALU.mult)
                dv("reciprocal", out=wa, in_=wa)
                dv("tensor_tensor", out=wb, in0=wa, in1=e2, op=ALU.mult)
                dv("tensor_copy", out=selb, in_=sel)
                S.op("pe", ("matmul", dict(out=bank(3)[:, 0:32], lhsT=utri_b, rhs=selb, start=True, stop=True)), reads=[Bsmall, Bc], writes=[PB[3]])
                S.op("pe", ("matmul", dict(out=bank(3)[:, 32:64], lhsT=ones_b, rhs=selb, start=True, stop=True)), reads=[Bsmall, Bc], writes=[PB[3]])
                S.op("dve", ("tensor_tensor", dict(out=pos, in0=bank(3)[:, 0:32], in1=run, op=ALU.add)), reads=[PB[3], Brun, Bsmall], writes=[Bsmall])
                S.op("dve", ("tensor_tensor", dict(out=run, in0=bank(3)[:, 32:64], in1=run, op=ALU.add)), reads=[PB[3], Brun, Bsmall], writes=[Brun])
                dv("tensor_scalar", out=valid, in0=pos, scalar1=float(CAP), scalar2=None, op0=ALU.is_lt)
                dv("tensor_tensor", out=pos, in0=pos, in1=iotaC, op=ALU.add)
                dv("tensor_scalar", out=pos, in0=pos, scalar1=-float(TRASH), scalar2=None, op0=ALU.add)
                dv("tensor_tensor", out=pos, in0=pos, in1=valid, op=ALU.mult)
                dv("tensor_scalar", out=pos, in0=pos, scalar1=float(TRASH), scalar2=None, op0=ALU.add)
                dv("tensor_tensor", out=tmp32, in0=sel1, in1=pos, op=ALU.mult)
                dv("tensor_reduce", out=slotf[:, 0:1], in_=tmp32, axis=AX.X, op=ALU.add)
                dv("tensor_tensor", out=tmp32, in0=sel1, in1=valid, op=ALU.mult)
                dv("tensor_reduce", out=vab[:, 0:1], in_=tmp32, axis=AX.X, op=ALU.add)
                dv("tensor_tensor", out=sel, in0=sel, in1=sel1, op=ALU.subtract)
                dv("tensor_tensor", out=tmp32, in0=sel, in1=pos, op=ALU.mult)
                dv("tensor_reduce", out=slotf[:, 1:2], in_=tmp32, axis=AX.X, op=ALU.add)
                dv("tensor_tensor", out=tmp32, in0=sel, in1=valid, op=ALU.mult)
                dv("tensor_reduce", out=vab[:, 1:2], in_=tmp32, axis=AX.X, op=ALU.add)
                S.op("dve", ("tensor_tensor", dict(out=wts3[:, ti, :], in0=sm[:, 98:100], in1=vab, op=ALU.mult)), reads=[Bsmall], writes=[Bslot])
                S.op("dve", ("tensor_copy", dict(out=slot_i3[:, ti, :], in_=slotf)), reads=[Bsmall], writes=[Bslot])
                for a_ in range(2):
                    S.dma("pool", ("indirect_dma_start", dict(out=xall_d, out_offset=bass.IndirectOffsetOnAxis(ap=slot_i3[:, ti, a_:a_ + 1], axis=0),
                                                             in_=fb_, in_offset=None)), reads=[Bfb_, Bslot], writes=[Bxall])
            S.barrier()
            A.off = mark
            if last is False and "slots" in dbg_d:
                pass
            NJ = CAP // 128
            wbuf = [(A.bf16(8 * 512), A.bf16(8 * 512), A.bf16(4 * 1024)) for _ in range(2)]
            Bwb = [Buf(), Buf()]
            xrows = [A.bf16(NJ * D) for _ in range(2)]
            Bxrows = [Buf(), Buf()]
            XT = A.bf16(8 * CAP)
            XT3 = r3(XT, 8)
            BXT = Buf()
            sl = [A.f32(512) for _ in range(2)]
            Bsl = [Buf(), Buf()]
            hs = A.bf16(4 * CAP)
            hs3 = r3(hs, 4)
            Bhs = Buf()
            ysb = [A.f32(D) for _ in range(2)]
            Bysb = [Buf(), Buf()]
            yi = 0
            stg = (A.f32(8 * 512), A.f32(8 * 512), A.f32(4 * 1024))
            Bstg = [Buf(), Buf(), Buf()]

            def load_w(e_):
                S.dma("sp", ("dma_start", dict(out=r3(stg[0], 8), in_=w1_d[L, e_].rearrange("(k p) n -> p k n", p=128))), writes=[Bstg[0]])
                S.dma("sp", ("dma_start", dict(out=r3(stg[1], 8), in_=w3_d[L, e_].rearrange("(k p) n -> p k n", p=128))), writes=[Bstg[1]])
                S.dma("sp", ("dma_start", dict(out=r3(stg[2], 4), in_=w2_d[L, e_].rearrange("(k p) n -> p k n", p=128))), writes=[Bstg[2]])

            def cast_w(e_, which):
                dst = wbuf[e_ % 2][which]
                Bw_ = Bwb[e_ % 2]
                if which == 0:
                    S.op("act", ("copy", dict(out=dst, in_=stg[0])), reads=[Bstg[0]], writes=[Bw_])
                elif which == 1:
                    S.op("dve", ("tensor_copy", dict(out=dst, in_=stg[1])), reads=[Bstg[1]], writes=[Bw_])
                else:
                    S.op("act", ("copy", dict(out=dst[:, 0:2048], in_=stg[2][:, 0:2048])), reads=[Bstg[2]], writes=[Bw_])
                    S.op("dve", ("tensor_copy", dict(out=dst[:, 2048:4096], in_=stg[2][:, 2048:4096])), reads=[Bstg[2]], writes=[Bw_])

            def load_x(e_):
                S.dma("sp", ("dma_start", dict(out=r3(xrows[e_ % 2], NJ), in_=xall_d[e_ * CAP:(e_ + 1) * CAP, :].rearrange("(j p) d -> p j d", p=128))),
                      reads=[Bxall], writes=[Bxrows[e_ % 2]])

            load_w(0)
            load_x(0)
            for w_ in range(3):
                cast_w(0, w_)
            for e_ in range(32):
                w1b, w3b, w2b = wbuf[e_ % 2]
                Bw = Bwb[e_ % 2]
                xr_, Bxr_ = xrows[e_ % 2], Bxrows[e_ % 2]
                if e_ + 1 < 32:
                    load_w(e_ + 1)
                    load_x(e_ + 1)
                for j in range(NJ):
                    pb = j % 2
                    pT = r3(bank_bf(pb), 8)
                    for k in range(8):
                        S.op("pe", ("transpose", dict(out=pT[:, k, :], in_=r3(xr_, NJ)[:, j, k * 128:(k + 1) * 128], identity=ident_b)),
                             reads=[Bxr_, Bc], writes=[PB[pb]])
                    S.op("act" if j % 2 == 0 else "dve", ("tensor_copy" if j % 2 else "copy", dict(out=XT3[:, :, j * 128:(j + 1) * 128], in_=pT)),
                         reads=[PB[pb]], writes=[BXT])
                w1v, w3v, w2v = r3(w1b, 8), r3(w3b, 8), r3(w2b, 4)
                for bi_, (c0, n) in enumerate(((0, 512), (512, CAP - 512))):
                    for m in range(4):
                        for (pb, wv) in ((2 + (m % 2) * 2, w1v), (3 + (m % 2) * 2, w3v)):
                            for k in range(8):
                                S.op("pe", ("matmul", dict(out=bank(pb)[:, 0:n], lhsT=wv[:, k, m * 128:(m + 1) * 128], rhs=XT3[:, k, c0:c0 + n], start=(k == 0), stop=(k == 7))),
                                     reads=[Bw, BXT], writes=[PB[pb]])
                        p1, p3 = 2 + (m % 2) * 2, 3 + (m % 2) * 2
                        s_, Bs_ = sl[m % 2], Bsl[m % 2]
                        S.op("act", ("activation", dict(out=s_[:, 0:n], in_=bank(p1)[:, 0:n], func=AF.Silu)), reads=[PB[p1]], writes=[Bs_])
                        S.op("dve", ("tensor_tensor", dict(out=hs3[:, m, c0:c0 + n], in0=bank(p3)[:, 0:n], in1=s_[:, 0:n], op=ALU.mult)), reads=[PB[p3], Bs_], writes=[Bhs])
                    if e_ + 1 < 32:
                        cast_w(e_ + 1, bi_)
                for j in range(NJ):
                    for hf in range(2):
                        for m in range(4):
                            S.op("pe", ("matmul", dict(out=bank(6 + hf), lhsT=hs3[:, m, j * 128:(j + 1) * 128], rhs=w2v[:, m, hf * 512:(hf + 1) * 512], start=(m == 0), stop=(m == 3))),
                                 reads=[Bhs, Bw], writes=[PB[6 + hf]])
                    y_, By_ = ysb[yi % 2], Bysb[yi % 2]
                    yi += 1
                    S.op("act", ("copy", dict(out=y_[:, 0:512], in_=bank(6))), reads=[PB[6]], writes=[By_])
                    S.op("dve", ("tensor_copy", dict(out=y_[:, 512:1024], in_=bank(7))), reads=[PB[7]], writes=[By_])
                    r0 = e_ * CAP + j * 128
                    S.dma("sp", ("dma_start", dict(out=yall_d[r0:r0 + 128, :], in_=y_)), reads=[By_], writes=[Byall])
                    if j == 1 and e_ + 1 < 32:
                        cast_w(e_ + 1, 2)
            S.barrier()
            A.off = mark
            ya = [A.f32(D) for _ in range(2)]
            yb = [A.f32(D) for _ in range(2)]
            x1 = [A.f32(D) for _ in range(2)]
            Bya, Byb, Bx1 = [Buf(), Buf()], [Buf(), Buf()], [Buf(), Buf()]
            junk2 = A.bf16(D)
            Bj2 = Buf()
            ss2 = [A.f32(1) for _ in range(2)]
            Bss2 = [Buf(), Buf()]
            Bdst = Buf()
            for ti in range(ntiles):
                col = tile_col(ti)
                i2 = ti % 2
                S.dma("pool", ("indirect_dma_start", dict(out=ya[i2], out_offset=None, in_=yall_d, in_offset=bass.IndirectOffsetOnAxis(ap=slot_i3[:, ti, 0:1], axis=0))),
                      reads=[Byall, Bslot], writes=[Bya[i2]])
                S.dma("pool", ("indirect_dma_start", dict(out=yb[i2], out_offset=None, in_=yall_d, in_offset=bass.IndirectOffsetOnAxis(ap=slot_i3[:, ti, 1:2], axis=0))),
                      reads=[Byall, Bslot], writes=[Byb[i2]])
                S.dma("sp", ("dma_start", dict(out=x1[i2], in_=xr_d[ti * 128:(ti + 1) * 128, :])), reads=[Bsrc], writes=[Bx1[i2]])
                S.op("act", ("activation", dict(out=ya[i2], in_=ya[i2], func=AF.Copy, scale=wts3[:, ti, 0:1])), reads=[Bya[i2], Bslot], writes=[Bya[i2]])
                S.op("dve", ("scalar_tensor_tensor", dict(out=yb[i2], in0=yb[i2], scalar=wts3[:, ti, 1:2], in1=ya[i2], op0=ALU.mult, op1=ALU.add)),
                     reads=[Byb[i2], Bya[i2], Bslot], writes=[Byb[i2]])
                S.op("dve", ("tensor_tensor", dict(out=yb[i2], in0=yb[i2], in1=Gbc[col], op=ALU.mult)), reads=[Byb[i2], Bbc], writes=[Byb[i2]])
                S.op("dve", ("tensor_tensor", dict(out=x1[i2], in0=x1[i2], in1=yb[i2], op=ALU.add)), reads=[Bx1[i2], Byb[i2]], writes=[Bx1[i2]])
                if not last:
                    S.dma("sp", ("dma_start", dict(out=xr2_d[ti * 128:(ti + 1) * 128, :], in_=x1[i2])), reads=[Bx1[i2]], writes=[Bdst])
                else:
                    S.op("act", ("activation", dict(out=junk2, in_=x1[i2], func=AF.Square, accum_out=ss2[i2])), reads=[Bx1[i2]], writes=[Bj2, Bss2[i2]])
                    S.op("act", ("activation", dict(out=ss2[i2], in_=ss2[i2], func=AF.Sqrt, scale=1.0 / D, bias=EPS)), reads=[Bss2[i2]], writes=[Bss2[i2]])
                    S.op("dve", ("reciprocal", dict(out=ss2[i2], in_=ss2[i2])), reads=[Bss2[i2]], writes=[Bss2[i2]])
                    S.op("dve", ("scalar_tensor_tensor", dict(out=x1[i2], in0=x1[i2], scalar=ss2[i2], in1=fng, op0=ALU.mult, op1=ALU.mult)),
                         reads=[Bx1[i2], Bss2[i2], Bbc], writes=[Bx1[i2]])
                    S.dma("sp", ("dma_start", dict(out=out_d[ti * 128:(ti + 1) * 128, :], in_=x1[i2])), reads=[Bx1[i2]], writes=[Bdst])
            return Bdst

        if stop_after >= 2:
            Bxr2 = moe(0, 36, Bxr)
            S.barrier()
            A.off = persist_off
            if "xr2" in dbg_d:
                S.dma("sp", ("dma_start", dict(out=dbg_d["xr2"], in_=xr2_d)), reads=[Bxr2])
        def s5_mixer(Bsrc):
            L = 1
            TWO_PI = 6.283185307179586
            MAGIC = 12582912.0
            yacc = [A.f32(4 * S_LAT) for _ in range(NB)]
            yacc3 = [r3(y, 4) for y in yacc]
            Byacc = [Buf(), Buf()]
            uT = [A.bf16(4 * SEQ) for _ in range(NB)]
            uT3 = [r3(u, 4) for u in uT]
            BuT = [Buf(), Buf()]
            sd = A.f32(8)
            Bsd = Buf()
            S.dma("sp", ("dma_start", dict(out=sd[:, 0:4], in_=s5d_d)), writes=[Bsd])
            S.dma("sp", ("dma_start", dict(out=sd[:, 4:8], in_=s5bglu_d)), writes=[Bsd])
            mark = A.off
            if S5_DEBUG_STAGE < 1:
                return Byacc[0]
            w5 = A.bf16(8 * 512)
            w5_3 = r3(w5, 8)
            Bw5 = Buf()
            S.dma("pool", ("dma_start", dict(out=w5_3, in_=s5win_d.rearrange("(k p) n -> p k n", p=128))), writes=[Bw5])
            hT1 = A.bf16(8 * 512)
            h3 = r3(hT1, 8)
            Bh = Buf()
            NT = NormT()
            for b in range(NB):
                blocks = [([32 + 2 * b, 33 + 2 * b], 0, 256)]
                for i in range(4):
                    blocks.append(([16 * b + 4 * i + t for t in range(4)], 256 + 512 * i, 512))
                for (tiles, c0, N) in blocks:
                    for t, ti in enumerate(tiles):
                        NT.run(L, ti, h3, Bh, t * 128, t % 2)
                    for r in range(4 if S5_DEBUG_STAGE >= 1.5 else 0):
                        pb = 2 + r % 2
                        for k in range(8):
                            S.op("pe", ("matmul", dict(out=bank(pb)[:, 0:N], lhsT=w5_3[:, k, r * 128:(r + 1) * 128], rhs=h3[:, k, 0:N], start=(k == 0), stop=(k == 7))),
                                 reads=[Bw5, Bh], writes=[PB[pb]])
                        S.op("act", ("copy", dict(out=uT3[b][:, r, c0:c0 + N], in_=bank(pb)[:, 0:N])), reads=[PB[pb]], writes=[BuT[b]])
                        if c0 >= 256:
                            S.op("dve", ("tensor_scalar", dict(out=yacc3[b][:, r, c0 - 256:c0 - 256 + N], in0=uT3[b][:, r, c0:c0 + N], scalar1=sd[:, r:r + 1], scalar2=None, op0=ALU.mult)),
                                 reads=[BuT[b], Bsd], writes=[Byacc[b]])
            S.barrier()
            A.off = mark
            if S5_DEBUG_STAGE < 2:
                return Byacc[0]
            par = A.f32(96)
            par3 = par.rearrange("p (c t) -> p c t", t=3)
            Bpar = Buf()
            S.dma("sp", ("dma_start", dict(out=par, in_=s5par_d)), writes=[Bpar])
            iot = A.f32(SEQ)
            S.dma("sp", ("dma_start", dict(out=iot, in_=s5iota_d)), writes=[Bpar])
            NCB = 32
            pr_ = A.f32(NCB * 16)
            P3 = r3(pr_, 16)
            dtv, rho, tht, frv, sn, cs, nr, ni, inv, cfr, cfi, ncfr, ncfi, tmpa, tmpb, tmpc = [P3[:, i, :] for i in range(16)]
            are, aim, ldt = par3[:, :, 0], par3[:, :, 1], par3[:, :, 2]
            pv = lambda name, **kw: S.op("dve", (name, kw), reads=[Bpar], writes=[Bpar])
            pa = lambda **kw: S.op("act", ("activation", kw), reads=[Bpar], writes=[Bpar])
            pa(out=dtv, in_=ldt, func=AF.Exp)
            pv("tensor_tensor", out=tmpa, in0=are, in1=dtv, op=ALU.mult)
            pa(out=rho, in_=tmpa, func=AF.Exp)
            pv("tensor_tensor", out=tht, in0=aim, in1=dtv, op=ALU.mult)
            pv("tensor_scalar", out=tht, in0=tht, scalar1=1.0 / TWO_PI, scalar2=None, op0=ALU.mult)
            pv("tensor_scalar", out=tmpa, in0=tht, scalar1=MAGIC, scalar2=None, op0=ALU.add)
            pv("tensor_scalar", out=tmpa, in0=tmpa, scalar1=MAGIC, scalar2=None, op0=ALU.subtract)
            pv("tensor_tensor", out=frv, in0=tht, in1=tmpa, op=ALU.subtract)
            SC = TWO_PI * (1.0 - 1e-6)
            pa(out=sn, in_=frv, func=AF.Sin, scale=SC)
            pa(out=tmpb, in_=frv, func=AF.Sin, scale=SC / 2)
            pv("tensor_tensor", out=tmpb, in0=tmpb, in1=tmpb, op=ALU.mult)
            pv("tensor_scalar", out=cs, in0=tmpb, scalar1=-2.0, scalar2=1.0, op0=ALU.mult, op1=ALU.add)
            pv("tensor_tensor", out=nr, in0=rho, in1=cs, op=ALU.mult)
            pv("tensor_scalar", out=nr, in0=nr, scalar1=-1.0, scalar2=None, op0=ALU.add)
            pv("tensor_tensor", out=ni, in0=rho, in1=sn, op=ALU.mult)
            pv("tensor_tensor", out=tmpa, in0=are, in1=are, op=ALU.mult)
            pv("tensor_tensor", out=tmpb, in0=aim, in1=aim, op=ALU.mult)
            pv("tensor_tensor", out=inv, in0=tmpa, in1=tmpb, op=ALU.add)
            pv("reciprocal", out=inv, in_=inv)
            pv("tensor_tensor", out=tmpa, in0=nr, in1=are, op=ALU.mult)
            pv("tensor_tensor", out=tmpb, in0=ni, in1=aim, op=ALU.mult)
            pv("tensor_tensor", out=tmpa, in0=tmpa, in1=tmpb, op=ALU.add)
            pv("tensor_tensor", out=cfr, in0=tmpa, in1=inv, op=ALU.mult)
            pv("tensor_tensor", out=tmpa, in0=ni, in1=are, op=ALU.mult)
            pv("tensor_tensor", out=tmpb, in0=nr, in1=aim, op=ALU.mult)
            pv("tensor_tensor", out=tmpa, in0=tmpa, in1=tmpb, op=ALU.subtract)
            pv("tensor_tensor", out=cfi, in0=tmpa, in1=inv, op=ALU.mult)
            pv("tensor_scalar", out=ncfr, in0=cfr, scalar1=-1.0, scalar2=None, op0=ALU.mult)
            pv("tensor_scalar", out=ncfi, in0=cfi, scalar1=-1.0, scalar2=None, op0=ALU.mult)

            if S5_DEBUG_STAGE < 3:
                return Bpar
            cosT = A.f32(SEQ)
            sinT = A.f32(SEQ)
            tA = A.f32(SEQ)
            tB = A.f32(SEQ)
            Btab, BtA, BtB = Buf(), Buf(), Buf()
            dr = A.f32(SEQ)
            di = A.f32(SEQ)
            qr = A.f32(SEQ)
            qi = A.f32(SEQ)
            Bdr, Bdi, Bqr, Bqi = Buf(), Buf(), Buf(), Buf()
            m1 = [A.f32(512) for _ in range(2)]
            m2 = [A.f32(512) for _ in range(2)]
            Bm1, Bm2 = [Buf(), Buf()], [Buf(), Buf()]
            hr = [A.bf16(512) for _ in range(2)]
            hi = [A.bf16(512) for _ in range(2)]
            Bhr, Bhi = [Buf(), Buf()], [Buf(), Buf()]
            bt = [(A.bf16(128), A.bf16(128)) for _ in range(2)]
            Bbt = [Buf(), Buf()]
            cst_ = [(A.f32(128), A.f32(128)) for _ in range(2)]
            Bcst = [Buf(), Buf()]
            cw = [(A.bf16(128), A.bf16(128)) for _ in range(2)]
            Bcw = [Buf(), Buf()]
            ctmp = A.f32(128)
            Bctmp = Buf()
            mi = 0
            for d_ in range(2):
                for pr in range(16):
                    ci_ = d_ * 16 + pr
                    r = pr // 4
                    k2 = ci_ % 2
                    btr, bti = bt[k2]
                    S.dma("pool", ("dma_start", dict(out=btr, in_=s5bT_d[d_, 0, pr])), writes=[Bbt[k2]])
                    S.dma("pool", ("dma_start", dict(out=bti, in_=s5bT_d[d_, 1, pr])), writes=[Bbt[k2]])
                    c_r, c_i = cst_[k2]
                    S.dma("sp", ("dma_start", dict(out=c_r, in_=s5c_d[d_, 0, pr])), writes=[Bcst[k2]])
                    S.dma("sp", ("dma_start", dict(out=c_i, in_=s5c_d[d_, 1, pr])), writes=[Bcst[k2]])
                    cwr, cwi = cw[k2]
                    col1 = lambda v, ci_=ci_: v[:, ci_:ci_ + 1]
                    S.op("dve", ("tensor_scalar", dict(out=ctmp, in0=c_r, scalar1=col1(cfr), scalar2=None, op0=ALU.mult)), reads=[Bcst[k2], Bpar], writes=[Bctmp])
                    S.op("dve", ("scalar_tensor_tensor", dict(out=cwr, in0=c_i, scalar=col1(ncfi), in1=ctmp, op0=ALU.mult, op1=ALU.add)), reads=[Bcst[k2], Bpar, Bctmp], writes=[Bcw[k2]])
                    S.op("dve", ("tensor_scalar", dict(out=ctmp, in0=c_r, scalar1=col1(ncfi), scalar2=None, op0=ALU.mult)), reads=[Bcst[k2], Bpar, Bcw[k2]], writes=[Bctmp])
                    S.op("dve", ("scalar_tensor_tensor", dict(out=cwi, in0=c_i, scalar=col1(ncfr), in1=ctmp, op0=ALU.mult, op1=ALU.add)), reads=[Bcst[k2], Bpar, Bctmp], writes=[Bcw[k2]])
                    S.op("dve", ("tensor_scalar", dict(out=tA, in0=iot, scalar1=col1(tht), scalar2=None, op0=ALU.mult)), reads=[Bpar], writes=[BtA])
                    S.op("dve", ("tensor_scalar", dict(out=tB, in0=tA, scalar1=MAGIC, scalar2=None, op0=ALU.add)), reads=[BtA], writes=[BtB])
                    S.op("dve", ("tensor_scalar", dict(out=tB, in0=tB, scalar1=MAGIC, scalar2=None, op0=ALU.subtract)), reads=[BtB], writes=[BtB])
                    S.op("dve", ("tensor_tensor", dict(out=tA, in0=tA, in1=tB, op=ALU.subtract)), reads=[BtA, BtB], writes=[BtA])
                    S.op("act", ("activation", dict(out=sinT, in_=tA, func=AF.Sin, scale=SC)), reads=[BtA], writes=[Btab])
                    S.op("act", ("activation", dict(out=tB, in_=tA, func=AF.Sin, scale=SC / 2)), reads=[BtA], writes=[BtB])
                    S.op("act", ("activation", dict(out=tB, in_=tB, func=AF.Square, scale=1.4142135623730951)), reads=[BtB], writes=[BtB])
                    S.op("act", ("activation", dict(out=cosT, in_=tB, func=AF.Identity, scale=-1.0, bias=1.0)), reads=[BtB], writes=[Btab])
                    rho_c = col1(rho)
                    for b in range(NB):
                        blocks = [(0, 256)] + [(256 + 512 * i, 512) for i in range(4)]
                        for bi, (s0, N) in enumerate(blocks):
                            if d_ == 0:
                                ucols = uT3[b][:, r, s0:s0 + N]
                            else:
                                if bi == 0:
                                    ucols = uT3[b][:, r, 255::-1]
                                else:
                                    hi_c = SEQ - 1 - (bi - 1) * 512
                                    ucols = uT3[b][:, r, hi_c:hi_c - 512:-1]
                            pbr, pbi = (bi % 2) * 2, (bi % 2) * 2 + 1
                            S.op("pe", ("matmul", dict(out=bank(pbr)[:, 0:N], lhsT=btr, rhs=ucols, start=True, stop=True)), reads=[Bbt[k2], BuT[b]], writes=[PB[pbr]])
                            S.op("pe", ("matmul", dict(out=bank(pbi)[:, 0:N], lhsT=bti, rhs=ucols, start=True, stop=True)), reads=[Bbt[k2], BuT[b]], writes=[PB[pbi]])
                            a1, a2 = m1[mi % 2], m2[mi % 2]
                            Ba1, Ba2 = Bm1[mi % 2], Bm2[mi % 2]
                            mi += 1
                            cS, sS = cosT[:, s0:s0 + N], sinT[:, s0:s0 + N]
                            S.op("dve", ("tensor_tensor", dict(out=a1[:, 0:N], in0=bank(pbr)[:, 0:N], in1=cS, op=ALU.mult)), reads=[PB[pbr], Btab], writes=[Ba1])
                            S.op("dve", ("tensor_tensor", dict(out=a2[:, 0:N], in0=bank(pbi)[:, 0:N], in1=sS, op=ALU.mult)), reads=[PB[pbi], Btab], writes=[Ba2])
                            S.op("dve", ("tensor_tensor", dict(out=dr[:, s0:s0 + N], in0=a1[:, 0:N], in1=a2[:, 0:N], op=ALU.add)), reads=[Ba1, Ba2], writes=[Bdr])
                            a1, a2 = m1[mi % 2], m2[mi % 2]
                            Ba1, Ba2 = Bm1[mi % 2], Bm2[mi % 2]
                            mi += 1
                            S.op("dve", ("tensor_tensor", dict(out=a1[:, 0:N], in0=bank(pbi)[:, 0:N], in1=cS, op=ALU.mult)), reads=[PB[pbi], Btab], writes=[Ba1])
                            S.op("dve", ("tensor_tensor", dict(out=a2[:, 0:N], in0=bank(pbr)[:, 0:N], in1=sS, op=ALU.mult)), reads=[PB[pbr], Btab], writes=[Ba2])
                            S.op("dve", ("tensor_tensor", dict(out=di[:, s0:s0 + N], in0=a1[:, 0:N], in1=a2[:, 0:N], op=ALU.subtract)), reads=[Ba1, Ba2], writes=[Bdi])
                        rb = rho_c.broadcast_to([128, SEQ])
                        S.op("dve", ("tensor_tensor_scan", dict(out=qr, data0=rb, data1=dr, initial=0.0, op0=ALU.mult, op1=ALU.add)), reads=[Bdr, Bpar], writes=[Bqr])
                        S.op("dve", ("tensor_tensor_scan", dict(out=qi, data0=rb, data1=di, initial=0.0, op0=ALU.mult, op1=ALU.add)), reads=[Bdi, Bpar], writes=[Bqi])
                        for bi in range(1, 5):
                            s0, N = blocks[bi]
                            cS, sS = cosT[:, s0:s0 + N], sinT[:, s0:s0 + N]
                            a1, a2 = m1[mi % 2], m2[mi % 2]
                            Ba1, Ba2 = Bm1[mi % 2], Bm2[mi % 2]
                            mi += 1
                            h_r, h_i = hr[bi % 2], hi[bi % 2]
                            S.op("dve", ("tensor_tensor", dict(out=a1, in0=qr[:, s0:s0 + N], in1=cS, op=ALU.mult)), reads=[Bqr, Btab], writes=[Ba1])
                            S.op("dve", ("tensor_tensor", dict(out=a2, in0=qi[:, s0:s0 + N], in1=sS, op=ALU.mult)), reads=[Bqi, Btab], writes=[Ba2])
                            S.op("dve", ("tensor_tensor", dict(out=h_r, in0=a1, in1=a2, op=ALU.subtract)), reads=[Ba1, Ba2], writes=[Bhr[bi % 2]])
                            a1, a2 = m1[mi % 2], m2[mi % 2]
                            Ba1, Ba2 = Bm1[mi % 2], Bm2[mi % 2]
                            mi += 1
                            S.op("dve", ("tensor_tensor", dict(out=a1, in0=qr[:, s0:s0 + N], in1=sS, op=ALU.mult)), reads=[Bqr, Btab], writes=[Ba1])
                            S.op("dve", ("tensor_tensor", dict(out=a2, in0=qi[:, s0:s0 + N], in1=cS, op=ALU.mult)), reads=[Bqi, Btab], writes=[Ba2])
                            S.op("dve", ("tensor_tensor", dict(out=h_i, in0=a1, in1=a2, op=ALU.add)), reads=[Ba1, Ba2], writes=[Bhi[bi % 2]])
                            pby = 4 + bi % 2
                            S.op("pe", ("matmul", dict(out=bank(pby), lhsT=cwr, rhs=h_r, start=True, stop=False)), reads=[Bcw[k2], Bhr[bi % 2]], writes=[PB[pby]])
                            S.op("pe", ("matmul", dict(out=bank(pby), lhsT=cwi, rhs=h_i, start=False, stop=True)), reads=[Bcw[k2], Bhi[bi % 2]], writes=[PB[pby]])
                            if d_ == 0:
                                j0 = s0 - 256
                                ycols = yacc3[b][:, r, j0:j0 + 512]
                            else:
                                hj = S_LAT - 1 - (bi - 1) * 512
                                stop = hj - 512
                                ycols = yacc3[b][:, r, hj::-1] if stop < 0 else yacc3[b][:, r, hj:stop:-1]
                            S.op("dve", ("tensor_tensor", dict(out=ycols, in0=bank(pby), in1=ycols, op=ALU.add)), reads=[PB[pby], Byacc[b]], writes=[Byacc[b]])
            S.barrier()
            A.off = mark
            if "yacc" in dbg_d:
                for b in range(NB):
                    S.dma("sp", ("dma_start", dict(out=dbg_d["yacc"][b * 128:(b + 1) * 128, :], in_=yacc[b])), reads=[Byacc[b]])
            wg = A.bf16(4 * 512)
            wg3 = r3(wg, 4)
            wo = A.bf16(4 * 1024)
            wo3 = r3(wo, 4)
            BwC = Buf()
            S.dma("pool", ("dma_start", dict(out=wg3, in_=s5glu_d.rearrange("(k p) n -> p k n", p=128))), writes=[BwC])
            S.dma("pool", ("dma_start", dict(out=wo3, in_=s5wout_d.rearrange("(k p) n -> p k n", p=128))), writes=[BwC])
            gate_bc = [A.f32(D) for _ in range(2)]
            for c in range(2):
                S.dma("sp", ("dma_start", dict(out=gate_bc[c], in_=modrows_d[L, 2, c:c + 1, :].broadcast_to([128, D]))), reads=[Bmodrows], writes=[BwC])
            gT = A.bf16(4 * 512)
            gT3 = r3(gT, 4)
            vT = A.bf16(4 * 512)
            vT3 = r3(vT, 4)
            BgT, BvT = Buf(), Buf()
            sg = [A.f32(512) for _ in range(2)]
            Bsg = [Buf(), Buf()]
            yt = [A.f32(D) for _ in range(2)]
            xt2 = [A.f32(D) for _ in range(2)]
            Byt, Bxt2 = [Buf(), Buf()], [Buf(), Buf()]
            Bxr = Buf()
            oi = 0
            for b in range(NB):
                for i in range(4):
                    j0 = i * 512
                    S.op("act", ("activation", dict(out=gT3, in_=yacc3[b][:, :, j0:j0 + 512], func=AF.Gelu_apprx_tanh)), reads=[Byacc[b]], writes=[BgT])
                    for m in range(4):
                        pb = 2 + m % 2
                        for k in range(4):
                            S.op("pe", ("matmul", dict(out=bank(pb), lhsT=wg3[:, k, m * 128:(m + 1) * 128], rhs=gT3[:, k, :], start=(k == 0), stop=(k == 3))),
                                 reads=[BwC, BgT], writes=[PB[pb]])
                        S.op("act", ("activation", dict(out=sg[m % 2], in_=bank(pb), func=AF.Sigmoid, bias=sd[:, 4 + m:5 + m], scale=1.0)), reads=[PB[pb], Bsd], writes=[Bsg[m % 2]])
                        S.op("dve", ("tensor_tensor", dict(out=vT3[:, m, :], in0=gT3[:, m, :], in1=sg[m % 2], op=ALU.mult)), reads=[BgT, Bsg[m % 2]], writes=[BvT])
                    for t in range(4):
                        ti = 16 * b + 4 * i + t
                        for hf in range(2):
                            for k in range(4):
                                S.op("pe", ("matmul", dict(out=bank(6 + hf), lhsT=vT3[:, k, t * 128:(t + 1) * 128], rhs=wo3[:, k, hf * 512:(hf + 1) * 512], start=(k == 0), stop=(k == 3))),
                                     reads=[BvT, BwC], writes=[PB[6 + hf]])
                        o2 = oi % 2
                        oi += 1
                        S.dma("sp", ("dma_start", dict(out=xt2[o2], in_=xr2_d[ti * 128:(ti + 1) * 128, :])), reads=[Bsrc], writes=[Bxt2[o2]])
                        S.op("dve", ("tensor_tensor", dict(out=yt[o2], in0=psum_t[:, 6 * 512:8 * 512], in1=gate_bc[b], op=ALU.mult)), reads=[PB[6], PB[7], BwC], writes=[Byt[o2]])
                        S.op("dve", ("tensor_tensor", dict(out=yt[o2], in0=yt[o2], in1=xt2[o2], op=ALU.add)), reads=[Byt[o2], Bxt2[o2]], writes=[Byt[o2]])
                        S.dma("sp", ("dma_start", dict(out=xr_d[ti * 128:(ti + 1) * 128, :], in_=yt[o2])), reads=[Byt[o2]], writes=[Bxr])
            return Bxr

        if stop_after >= 3:
            Bxr_b = s5_mixer(Bxr2)
            S.barrier()
            A.off = persist_off
            if "xr3" in dbg_d:
                S.dma("sp", ("dma_start", dict(out=dbg_d["xr3"], in_=xr_d[0:T_LAT, :])), reads=[Bxr_b])
        if stop_after >= 4:
            Bout = moe(1, 32, Bxr_b)

        S.barrier()
        with nc.Block() as block:
            S.emit(block)
    return nc


def _rope_tables():
    inv = np.power(10000.0, -np.arange(0, 32, 2, dtype=np.float32) / 32).astype(np.float32)
    t = np.arange(S_LAT)
    row = (t // 64).astype(np.float32)
    colp = (t % 64).astype(np.float32)
    ang_r = row[:, None] * inv[None, :]
    ang_c = colp[:, None] * inv[None, :]
    cos64 = np.ones((64, SEQ), np.float32)
    sin64 = np.zeros((64, SEQ), np.float32)
    for d in range(64):
        ang = ang_r if d < 32 else ang_c
        i = d % 16
        sgn = -1.0 if (d % 32) < 16 else 1.0
        cos64[d, C_CTX:] = np.cos(ang[:, i])
        sin64[d, C_CTX:] = sgn * np.sin(ang[:, i])
    return np.concatenate([np.concatenate([cos64, cos64], 0), np.concatenate([sin64, sin64], 0)], 1).astype(np.float32)


_PERM64 = np.array([(d // 32) * 32 + ((d % 32) + 16) % 32 for d in range(64)])


def _consts():
    c = np.zeros((128, NCONST), np.float32)
    c[:, 0:128] = np.eye(128)
    c[:, 128:256] = np.triu(np.ones((128, 128)), 1)
    c[:, 256:384] = 1.0
    c[0:64, 384:448] = 1.0 / 64
    c[64:128, 448:512] = 1.0 / 64
    c[:, 512:544] = (np.arange(32) * CAP)[None, :]
    return c


def make_in_maps(inp, cores):
    f = lambda a: np.ascontiguousarray(np.asarray(a, dtype=np.float32))
    shared = {
        "consts": _consts(), "rope": _rope_tables(),
        "w_mod": f(inp["w_mod"]), "b_mod": f(inp["b_mod"]).reshape(2, 1, 6 * D),
        "norm_mix_g": f(inp["norm_mix_g"]).reshape(2, 1, D), "norm_ffn_g": f(inp["norm_ffn_g"]).reshape(2, 1, D),
        "mix_w_in": f(inp["mix_w_in"][0]), "mix_w_out": f(inp["mix_w_out"][0]),
        "gmlp_norm_g": f(inp["gmlp_norm_g"]).reshape(1, 512), "gmlp_w_spatial": f(inp["gmlp_w_spatial"][0]),
        "gmlp_b_spatial": f(inp["gmlp_b_spatial"][0]).reshape(1, 1024),
        "moe_w_group": f(inp["moe_w_group"]), "moe_b_group": f(inp["moe_b_group"]).reshape(2, 1, 4),
        "moe_w_router": f(inp["moe_w_router"]), "moe_b_router": f(inp["moe_b_router"]).reshape(2, 1, 32),
        "moe_w1": f(inp["moe_w1"]), "moe_w3": f(inp["moe_w3"]), "moe_w2": f(inp["moe_w2"]),
        "final_norm_g": f(inp["final_norm_g"]).reshape(1, D),
        "s5_w_in": f(inp["s5_w_in"][0]), "s5_w_glu": f(inp["s5_w_glu"][0]), "s5_w_out": f(inp["s5_w_out"][0]),
    }
    qg = f(inp["q_norm_g"][0]); kg = f(inp["k_norm_g"][0])
    idx = np.arange(128) % 64
    shared["qkg"] = np.stack([qg[idx], qg[_PERM64[idx]], kg[idx], kg[_PERM64[idx]]], 1).astype(np.float32)
    a_re = f(inp["s5_a_re"][0]); a_im = f(inp["s5_a_im"][0]); ldt = f(inp["s5_log_dt"][0])
    par = np.zeros((128, 2, 16, 3), np.float32)
    for d in range(2):
        for pr in range(16):
            for gl in range(2):
                g = 2 * pr + gl
                par[gl * 64:(gl + 1) * 64, d, pr, 0] = a_re[d, g]
                par[gl * 64:(gl + 1) * 64, d, pr, 1] = a_im[d, g]
                par[gl * 64:(gl + 1) * 64, d, pr, 2] = ldt[d, g]
    shared["s5_par"] = par.reshape(128, 96)
    b_re = f(inp["s5_b_re"][0]); b_im = f(inp["s5_b_im"][0]); c_re = f(inp["s5_c_re"][0]); c_im = f(inp["s5_c_im"][0])
    bT = np.zeros((2, 2, 16, 128, 128), np.float32)
    cc = np.zeros((2, 2, 16, 128, 128), np.float32)
    for d in range(2):
        for pr in range(16):
            for gl in range(2):
                g = 2 * pr + gl
                gic = g % 8
                for ri, (bsrc, csrc) in enumerate(((b_re, c_re), (b_im, c_im))):
                    bT[d, ri, pr, gic * 16:(gic + 1) * 16, gl * 64:(gl + 1) * 64] = bsrc[d, g].T
                    cc[d, ri, pr, gl * 64:(gl + 1) * 64, gic * 16:(gic + 1) * 16] = csrc[d, g].T
    shared["s5_bT"] = bT
    shared["s5_iota"] = np.tile(np.arange(SEQ, dtype=np.float32)[None, :], (128, 1))
    shared["s5_c"] = cc
    shared["s5_d"] = f(inp["s5_d"][0]).reshape(4, 128).T.copy()
    shared["s5_b_glu"] = f(inp["s5_b_glu"][0]).reshape(4, 128).T.copy()
    maps = []
    x = np.asarray(inp["x"]); ctx = np.asarray(inp["ctx"]); c = np.asarray(inp["c"]); cc_ = np.asarray(inp["c_ctx"])
    for core in cores:
        m = dict(shared)
        m["x"] = f(x[2 * core:2 * core + 2]).reshape(T_LAT, D)
        m["ctx"] = f(ctx[2 * core:2 * core + 2]).reshape(T_CTX, D)
        cvec = np.stack([c[2 * core], c[2 * core + 1], cc_], 0).astype(np.float32)
        m["cT"] = np.ascontiguousarray(cvec.reshape(3, 8, 128).transpose(2, 1, 0).reshape(128, 24))
        maps.append(m)
    return maps


_NC_CACHE = {}


def kernel(**inputs):
    if "nc" not in _NC_CACHE:
        _NC_CACHE["nc"] = build_program()
    nc = _NC_CACHE["nc"]
    cores = list(range(8))
    maps = make_in_maps(inputs, cores)
    res = run_bass_kernel_spmd(nc, maps, core_ids=cores)
    outs = [np.asarray(r["out"]).reshape(NB, S_LAT, D) for r in res.results]
    return np.concatenate(outs, 0).astype(np.float32)
```

```python
import numpy as np
from contextlib import ExitStack
import concourse.bass as bass
import concourse.mybir as mybir
from concourse.alu_op_type import AluOpType as ALU
from concourse.bass_utils import run_bass_kernel_spmd

F32 = mybir.dt.float32
BF16 = mybir.dt.bfloat16
I32 = mybir.dt.int32
AF = mybir.ActivationFunctionType
AX = mybir.AxisListType

D = 1024
S_LAT = 2048
C_CTX = 256
NB = 2
T_LAT = NB * S_LAT
T_CTX = NB * C_CTX
SEQ = C_CTX + S_LAT
EPS = 1e-6
CAP = 640
NSLOT = 32 * CAP
TRASH = NSLOT
NCONST = 128 * 4 + 32
S5_DEBUG_STAGE = 9


class Buf:
    __slots__ = ("w", "r")

    def __init__(self):
        self.w = None
        self.r = []


class Sched:
    COMPUTE = ("pe", "act", "dve", "pool")
    NDMA = 8

    def __init__(self, nc, es):
        self.nc = nc
        self.streams = {e: [] for e in ("pe", "act", "dve", "pool", "sp")}
        self.sem = {}
        self.cnt = {}
        for e in self.COMPUTE:
            self.sem[e] = es.enter_context(nc.semaphore("s_" + e))
            self.cnt[e] = 0
        self.drr = {}
        for q in ("sp", "act", "pool"):
            for k in range(self.NDMA):
                key = "d_%s%d" % (q, k)
                self.sem[key] = es.enter_context(nc.semaphore(key))
                self.cnt[key] = 0
            self.drr[q] = 0
        self.waited = {e: {} for e in self.streams}
        self.nops = 0

    def _deps(self, reads, writes):
        deps = []
        for b in reads:
            if b.w is not None:
                deps.append(b.w)
        for b in writes:
            if b.w is not None:
                deps.append(b.w)
            deps.extend(b.r)
        return deps

    def _emit_waits(self, eng, deps, skip_self=None):
        best = {}
        for (k, v) in deps:
            if k == skip_self:
                continue
            if v > best.get(k, 0):
                best[k] = v
        w = self.waited[eng]
        for k, v in best.items():
            if w.get(k, 0) < v:
                w[k] = v
                self.streams[eng].append(("wait", k, v))

    def _mark(self, tok, reads, writes):
        for b in writes:
            b.w = tok
            b.r = []
        for b in reads:
            if b.w is tok:
                continue
            b.r.append(tok)
            if len(b.r) > 16:
                best = {}
                for (k, v) in b.r:
                    if v > best.get(k, 0):
                        best[k] = v
                b.r = list(best.items())

    def op(self, eng, fn, reads=(), writes=()):
        deps = self._deps(reads, writes)
        self._emit_waits(eng, deps, skip_self=("pe" if eng == "pe" else None))
        self.cnt[eng] += 1
        tok = (eng, self.cnt[eng])
        self.streams[eng].append(("op", fn, eng, 1))
        self._mark(tok, reads, writes)
        self.nops += 1
        return tok

    def dma(self, q, fn, reads=(), writes=()):
        k = self.drr[q]
        self.drr[q] = (k + 1) % self.NDMA
        key = "d_%s%d" % (q, k)
        deps = self._deps(reads, writes)
        if self.cnt[key] > 0:
            deps.append((key, self.cnt[key]))
        self._emit_waits(q, deps)
        self.cnt[key] += 16
        tok = (key, self.cnt[key])
        self.streams[q].append(("op", fn, key, 16))
        self._mark(tok, reads, writes)
        self.nops += 1
        return tok

    def barrier(self):
        toks = [(k, v) for k, v in self.cnt.items() if v > 0]
        for e in self.streams:
            self._emit_waits(e, toks)

    def emit(self, block):
        sem = self.sem
        streams = self.streams

        def run(e, lst):
            for it in lst:
                if it[0] == "wait":
                    e.wait_ge(sem[it[1]], it[2])
                else:
                    getattr(e, it[1][0])(**it[1][1]).then_inc(sem[it[2]], it[3])

        @block.sync
        def _(e):
            run(e, streams["sp"])

        @block.scalar
        def _(e):
            run(e, streams["act"])

        @block.vector
        def _(e):
            run(e, streams["dve"])

        @block.gpsimd
        def _(e):
            run(e, streams["pool"])

        @block.tensor
        def _(e):
            run(e, streams["pe"])


class Arena:
    def __init__(self, t, size):
        self.t = t
        self.size = size
        self.off = 0

    def f32(self, n):
        a = self.t[:, self.off:self.off + n]
        self.off += n
        assert self.off <= self.size, ("arena overflow", self.off, self.size)
        return a

    def bf16(self, n):
        return self.f32((n + 1) // 2).bitcast(BF16)[:, 0:n]

    def i32(self, n):
        return self.f32(n).bitcast(I32)


def r3(ap, a):
    return ap.rearrange("p (a b) -> p a b", a=a)


def build_program(stop_after=99, dbg=()):
    nc = bass.Bass("TRN2", target_bir_lowering=False)
    dt = nc.dram_tensor

    def din(name, shape, dtype=F32):
        return dt(name, list(shape), dtype, kind="ExternalInput").ap()

    x_d = din("x", [T_LAT, D])
    ctx_d = din("ctx", [T_CTX, D])
    cT_d = din("cT", [128, 24])
    consts_d = din("consts", [128, NCONST])
    rope_d = din("rope", [128, 2 * SEQ])
    qkg_d = din("qkg", [128, 4])
    w_mod_d = din("w_mod", [2, D, 6 * D])
    b_mod_d = din("b_mod", [2, 1, 6 * D])
    gmix_d = din("norm_mix_g", [2, 1, D])
    gffn_d = din("norm_ffn_g", [2, 1, D])
    w_in_d = din("mix_w_in", [D, 1792])
    w_out_d = din("mix_w_out", [D, D])
    lng_d = din("gmlp_norm_g", [1, 512])
    wsp_d = din("gmlp_w_spatial", [8, 128, 128])
    bsp_d = din("gmlp_b_spatial", [1, 1024])
    wgrp_d = din("moe_w_group", [2, D, 4])
    bgrp_d = din("moe_b_group", [2, 1, 4])
    wrt_d = din("moe_w_router", [2, D, 32])
    brt_d = din("moe_b_router", [2, 1, 32])
    w1_d = din("moe_w1", [2, 32, D, 512])
    w3_d = din("moe_w3", [2, 32, D, 512])
    w2_d = din("moe_w2", [2, 32, 512, D])
    fng_d = din("final_norm_g", [1, D])
    s5win_d = din("s5_w_in", [D, 512])
    s5par_d = din("s5_par", [128, 2 * 16 * 3])
    s5iota_d = din("s5_iota", [128, SEQ])
    s5bT_d = din("s5_bT", [2, 2, 16, 128, 128])
    s5c_d = din("s5_c", [2, 2, 16, 128, 128])
    s5d_d = din("s5_d", [128, 4])
    s5glu_d = din("s5_w_glu", [512, 512])
    s5bglu_d = din("s5_b_glu", [128, 4])
    s5wout_d = din("s5_w_out", [512, D])

    out_d = dt("out", [T_LAT, D], F32, kind="ExternalOutput").ap()
    modrows_d = dt("modrows", [2, 6, 3, D], F32, kind="Internal").ap()
    xr_d = dt("xr", [T_LAT + T_CTX, D], F32, kind="Internal").ap()
    xr2_d = dt("xr2", [T_LAT + T_CTX, D], F32, kind="Internal").ap()
    xall_d = dt("xall", [NSLOT + 128, D], BF16, kind="Internal").ap()
    yall_d = dt("yall", [NSLOT + 128, D], F32, kind="Internal").ap()
    dbg_d = {}
    for name, shape in dbg:
        dbg_d[name] = dt("dbg_" + name, list(shape), F32, kind="ExternalOutput").ap()

    with ExitStack() as es:
        S = Sched(nc, es)
        ARENA_WORDS = 53100
        arena_t = es.enter_context(nc.sbuf_tensor("arena", [128, ARENA_WORDS], F32))
        psum_t = es.enter_context(nc.psum_tensor("psum", [128, 4096], F32))
        A = Arena(arena_t, ARENA_WORDS)

        def bank(i, n=512, off=0):
            return psum_t[:, i * 512 + off:i * 512 + off + n]

        def bank_bf(i):
            return psum_t[:, i * 512:(i + 1) * 512].bitcast(BF16)

        PB = [Buf() for _ in range(8)]

        cst = A.f32(NCONST)
        Bc = Buf()
        S.dma("sp", ("dma_start", dict(out=cst, in_=consts_d)), writes=[Bc])
        ident_f = cst[:, 0:128]
        utri_f = cst[:, 128:256]
        ones_f = cst[:, 256:384]
        blk_f = cst[:, 384:512]
        iotaC = cst[:, 512:544]
        cbf = A.bf16(512)
        S.op("dve", ("tensor_copy", dict(out=cbf, in_=cst[:, 0:512])), reads=[Bc], writes=[Bc])
        ident_b = cbf[:, 0:128]
        utri_b = cbf[:, 128:256]
        ones_b = cbf[:, 256:384]
        blk_b = cbf[:, 384:512]
        modT = A.f32(2 * 2 * 8 * 3)
        BmodT = Buf()
        persist_off = A.off

        cT = A.f32(24)
        sc = A.f32(24)
        Bsc = Buf()
        S.dma("sp", ("dma_start", dict(out=cT, in_=cT_d)), writes=[Bsc])
        S.op("act", ("activation", dict(out=sc, in_=cT, func=AF.Silu)), reads=[Bsc], writes=[Bsc])
        sc3 = r3(sc, 8)
        wblk = [A.f32(8 * 512) for _ in range(2)]
        Bwblk = [Buf(), Buf()]
        mrow = A.f32(6 * D)
        gb = A.f32(2 * D)
        bb = A.f32(6 * D)
        Bmrow, Bgb, Bbb = Buf(), Buf(), Buf()
        Bmodrows = Buf()
        for l in range(2):
            S.dma("sp", ("dma_start", dict(out=bb[0:3, :], in_=b_mod_d[l].broadcast_to([3, 6 * D]))), writes=[Bbb])
            S.dma("sp", ("dma_start", dict(out=gb[0:3, 0:D], in_=gmix_d[l].broadcast_to([3, D]))), writes=[Bgb])
            S.dma("sp", ("dma_start", dict(out=gb[0:3, D:2 * D], in_=gffn_d[l].broadcast_to([3, D]))), writes=[Bgb])
            for nb in range(12):
                wb = wblk[nb % 2]
                Bw = Bwblk[nb % 2]
                S.dma("sp", ("dma_start", dict(
                    out=r3(wb, 8), in_=w_mod_d[l][:, nb * 512:(nb + 1) * 512].rearrange("(k p) n -> p k n", p=128))), writes=[Bw])
                pb = nb % 2
                for k in range(8):
                    S.op("pe", ("matmul", dict(out=bank(pb)[0:3, :], lhsT=sc3[:, k, :], rhs=r3(wb, 8)[:, k, :],
                                                                       start=(k == 0), stop=(k == 7))), reads=[Bsc, Bw], writes=[PB[pb]])
                S.op("dve", ("tensor_tensor", dict(out=mrow[0:3, nb * 512:(nb + 1) * 512], in0=bank(pb)[0:3, :],
                                                                      in1=bb[0:3, nb * 512:(nb + 1) * 512], op=ALU.add)),
                     reads=[PB[pb], Bbb], writes=[Bmrow])
            for (kind, goff) in ((1, 0), (4, D)):
                S.op("dve", ("scalar_tensor_tensor", dict(
                    out=mrow[0:3, kind * D:(kind + 1) * D], in0=mrow[0:3, kind * D:(kind + 1) * D], scalar=1.0,
                    in1=gb[0:3, goff:goff + D], op0=ALU.add, op1=ALU.mult)), reads=[Bmrow, Bgb], writes=[Bmrow])
            S.dma("sp", ("dma_start", dict(out=modrows_d[l].rearrange("k c d -> c k d"), in_=r3(mrow[0:3, :], 6))),
                  reads=[Bmrow], writes=[Bmodrows])
        modT5 = modT.rearrange("p (l m k c) -> p l m k c", l=2, m=2, k=8)
        for l in range(2):
            for m in range(2):
                for c in range(3):
                    S.dma("sp", ("dma_start", dict(
                        out=modT5[:, l, m, :, c], in_=modrows_d[l, m, c].rearrange("(k p) -> p k", p=128),
                        allow_slow_non_contiguous=True)), reads=[Bmodrows], writes=[BmodT])
        S.barrier()
        A.off = persist_off
        if "modrows" in dbg_d:
            S.dma("sp", ("dma_start", dict(out=dbg_d["modrows"], in_=modrows_d.rearrange("l k c d -> (l k c) d"))), reads=[Bmodrows])

        def tile_src(layer, ti):
            if layer == 0:
                if ti < 32:
                    return x_d[ti * 128:(ti + 1) * 128, :]
                return ctx_d[(ti - 32) * 128:(ti - 31) * 128, :]
            return xr2_d[ti * 128:(ti + 1) * 128, :]

        def tile_col(ti):
            if ti < 32:
                return ti // 16
            return 2

        class NormT:
            def __init__(self):
                self.xt = [A.f32(D) for _ in range(2)]
                self.Bxt = [Buf(), Buf()]
                self.junk = A.bf16(D)
                self.Bjunk = Buf()
                self.xn = [A.bf16(D) for _ in range(2)]
                self.Bxn = [Buf(), Buf()]
                self.ss = [A.f32(1) for _ in range(2)]
                self.Bss = [Buf(), Buf()]
                self.i = 0

            def run(self, layer, ti, hT3, BhT, c0, psb):
                i = self.i
                self.i += 1
                xt, Bxt = self.xt[i % 2], self.Bxt[i % 2]
                xn, Bxn = self.xn[i % 2], self.Bxn[i % 2]
                ss, Bss = self.ss[i % 2], self.Bss[i % 2]
                junk, Bjunk = self.junk, self.Bjunk
                src = tile_src(layer, ti)
                col = tile_col(ti)
                S.dma("sp", ("dma_start", dict(out=xt, in_=src)), writes=[Bxt])
                S.op("act", ("activation", dict(out=junk, in_=xt, func=AF.Square, accum_out=ss)), reads=[Bxt], writes=[Bjunk, Bss])
                S.op("act", ("activation", dict(out=ss, in_=ss, func=AF.Sqrt, scale=1.0 / D, bias=EPS)), reads=[Bss], writes=[Bss])
                S.op("dve", ("reciprocal", dict(out=ss, in_=ss)), reads=[Bss], writes=[Bss])
                S.op("dve", ("tensor_scalar", dict(out=xn, in0=xt, scalar1=ss, scalar2=None, op0=ALU.mult)), reads=[Bxt, Bss], writes=[Bxn])
                pT = r3(bank_bf(psb), 8)
                for k in range(8):
                    S.op("pe", ("transpose", dict(out=pT[:, k, :], in_=xn[:, k * 128:(k + 1) * 128], identity=ident_b)),
                         reads=[Bxn, Bc], writes=[PB[psb]])
                for k in range(8):
                    eng = "act" if k % 2 == 0 else "dve"
                    if eng == "act":
                        S.op("act", ("activation", dict(out=hT3[:, k, c0:c0 + 128], in_=pT[:, k, :], func=AF.Identity,
                                                                  scale=modT5[:, layer, 1, k, col:col + 1], bias=modT5[:, layer, 0, k, col:col + 1])),
                             reads=[PB[psb], BmodT], writes=[BhT])
                    else:
                        S.op("dve", ("tensor_scalar", dict(out=hT3[:, k, c0:c0 + 128], in0=pT[:, k, :],
                                                                     scalar1=modT5[:, layer, 1, k, col:col + 1], scalar2=modT5[:, layer, 0, k, col:col + 1],
                                                                     op0=ALU.mult, op1=ALU.add)),
                             reads=[PB[psb], BmodT], writes=[BhT])

        def phase1():
            L = 0
            GELU = AF.Gelu_apprx_tanh
            WC = 1280
            w_in = A.bf16(8 * WC)
            w_in3 = r3(w_in, 8)
            Bwin = Buf()
            for k in range(8):
                S.dma("pool", ("dma_start", dict(out=w_in3[:, k, :], in_=w_in_d[k * 128:(k + 1) * 128, 512:1792])), writes=[Bwin])
            wst = A.bf16(8 * 512)
            wst5 = wst.rearrange("p (k j two d) -> p k j two d", k=8, j=4, two=2)
            for k in range(8):
                for two in range(2):
                    S.dma("pool", ("dma_start", dict(out=wst5[:, k, :, two, :],
                                                     in_=w_in_d[k * 128:(k + 1) * 128, two * 256:(two + 1) * 256].rearrange("p (j d) -> p j d", j=4))), writes=[Bwin])
            wst3 = r3(wst, 8)
            wpst = A.bf16(8 * 512)
            wpst3 = r3(wpst, 8)
            wkp = A.bf16(8 * 128)
            wkp3 = r3(wkp, 8)
            Bwperm = Buf()
            sv = wst.rearrange("p (k x b i) -> p k x b i", k=8, b=2, i=16)
            dv = wpst.rearrange("p (k x b i) -> p k x b i", k=8, b=2, i=16)
            svk = w_in3[:, :, 0:128].rearrange("p k (x b i) -> p k x b i", b=2, i=16)
            dvk = wkp3.rearrange("p k (x b i) -> p k x b i", b=2, i=16)
            for b_ in range(2):
                S.op("dve", ("tensor_copy", dict(out=dv[:, :, :, b_, :], in_=sv[:, :, :, 1 - b_, :])), reads=[Bwin], writes=[Bwperm])
                S.op("dve", ("tensor_copy", dict(out=dvk[:, :, :, b_, :], in_=svk[:, :, :, 1 - b_, :])), reads=[Bwin], writes=[Bwperm])
            wout = A.bf16(16 * 1024)
            wout3 = r3(wout, 16)
            Bwout = Buf()
            for c4 in range(4):
                S.dma("pool", ("dma_start", dict(out=wout3[0:64, c4 * 4:(c4 + 1) * 4, :],
                                                            in_=w_out_d[c4 * 256:(c4 + 1) * 256, :].rearrange("(c r) n -> r c n", r=64))), writes=[Bwout])
            yt = A.f32(D)
            wspn = yt
            Bwspn = Buf()
            S.dma("sp", ("dma_start", dict(out=r3(wspn, 8), in_=wsp_d.rearrange("g p q -> p g q"))), writes=[Bwspn])
            wspT = A.bf16(1024)
            wspT3 = r3(wspT, 8)
            BwspT = Buf()
            pw = r3(psum_t[:, 0:1024], 8)
            for g in range(8):
                S.op("pe", ("transpose", dict(out=pw[:, g, :], in_=r3(wspn, 8)[:, g, :], identity=ident_f)),
                     reads=[Bwspn, Bc], writes=[PB[0], PB[1]])
            S.op("act", ("copy", dict(out=wspT, in_=psum_t[:, 0:1024])), reads=[PB[0], PB[1]], writes=[BwspT])
            lng_bc = A.f32(512)
            bsp_bc = A.f32(1024)
            qkg = A.f32(4)
            cosT = A.f32(SEQ)
            sinT = A.f32(SEQ)
            gate_bc = [A.f32(D) for _ in range(3)]
            Bsm = Buf()
            S.dma("sp", ("dma_start", dict(out=lng_bc, in_=lng_d.broadcast_to([128, 512]))), writes=[Bsm])
            S.dma("sp", ("dma_start", dict(out=bsp_bc[0:64, :], in_=bsp_d.broadcast_to([64, 1024]))), writes=[Bsm])
            S.dma("sp", ("dma_start", dict(out=qkg, in_=qkg_d)), writes=[Bsm])
            S.dma("sp", ("dma_start", dict(out=cosT, in_=rope_d[:, 0:SEQ])), writes=[Bsm])
            S.dma("sp", ("dma_start", dict(out=sinT, in_=rope_d[:, SEQ:2 * SEQ])), writes=[Bsm])
            for c in range(3):
                S.dma("sp", ("dma_start", dict(out=gate_bc[c], in_=modrows_d[L, 2, c:c + 1, :].broadcast_to([128, D]))),
                      reads=[Bmodrows], writes=[Bsm])
            bsp3 = r3(bsp_bc[0:64, :], 8)

            QT = A.bf16(4 * SEQ)
            QT3 = r3(QT, 4)
            KT = A.bf16(SEQ)
            Vt = A.bf16(18 * 128)
            V3 = r3(Vt, 18)
            BQ, BK, BV = Buf(), Buf(), Buf()
            hT1 = A.bf16(8 * 512)
            hT = [hT1, hT1]
            BhT1 = Buf()
            BhT = [BhT1, BhT1]
            NT = NormT()
            sqb = A.bf16(512)
            rs = A.f32(512)
            t1 = A.f32(512)
            t2 = A.f32(512)
            Bsq, Brs, Bt1, Bt2 = Buf(), Buf(), Buf(), Buf()
            mx = A.f32(1024)
            Bmx = Buf()
            rec = A.f32(512)
            Brec = Buf()
            sqb2 = [sqb, A.bf16(512)]
            rs2 = [rs, rec]
            t12 = [t1, mx[:, 0:512]]
            t22 = [t2, mx[:, 512:1024]]
            Bsq2, Brs2, Bt12, Bt22 = [Bsq, Buf()], [Brs, Brec], [Bt1, Bmx], [Bt2, Bmx]
            UG = A.bf16(8 * 512)
            UG3 = r3(UG, 8)
            GT3 = UG3
            OT = A.bf16(8 * 512)
            OT3 = r3(OT, 8)
            BUG = Buf()
            BGT = BUG
            BOT = Buf()
            gv = t1
            vnf = t2
            vnb = A.bf16(512)
            st6 = A.f32(6)
            mv = A.f32(2)
            Bgv, Bvnf = Bt1, Bt2
            Bvnb, Bst, Bmv = Buf(), Buf(), Buf()
            PT = [A.bf16(512) for _ in range(3)]
            BPT = [Buf(), Buf(), Buf()]
            rec = A.f32(512)
            Brec = Buf()
            xt2 = A.f32(D)
            Byt, Bxt2 = Buf(), Buf()
            Bxr = Buf()
            print('phase1 arena words', A.off)
            hcount = [0]

            def make_hT(blk):
                tiles, c0seq, N = blk
                i = hcount[0]
                hcount[0] += 1
                h3 = r3(hT[i % 2], 8)
                for t, ti in enumerate(tiles):
                    NT.run(L, ti, h3, BhT[i % 2], t * 128, t % 2)
                return h3, BhT[i % 2]

            for b in range(NB):
                blocks = [([32 + 2 * b, 33 + 2 * b], 0, 256)]
                for i in range(4):
                    blocks.append(([16 * b + 4 * i + t for t in range(4)], 256 + 512 * i, 512))
                for blk in blocks:
                    tiles, c0, N = blk
                    h3, Bh = make_hT(blk)
                    for j in range(5):
                        bq, bqp, bms = (2, 3, 4) if j % 2 == 0 else (5, 6, 7)
                        sqb_, rs_, t1_, t2_ = sqb2[j % 2], rs2[j % 2], t12[j % 2], t22[j % 2]
                        Bsq_, Brs_, Bt1_, Bt2_ = Bsq2[j % 2], Brs2[j % 2], Bt12[j % 2], Bt22[j % 2]
                        for (pb, wq, wk) in ((bq, wst3, w_in3), (bqp, wpst3, wkp3)):
                            for k in range(8):
                                lw = wq[:, k, j * 128:(j + 1) * 128] if j < 4 else wk[:, k, 0:128]
                                S.op("pe", ("matmul", dict(out=bank(pb)[:, 0:N], lhsT=lw, rhs=h3[:, k, 0:N], start=(k == 0), stop=(k == 7))),
                                     reads=[Bwin, Bwperm, Bh], writes=[PB[pb]])
                        gi = 0 if j < 4 else 2
                        S.op("act", ("activation", dict(out=sqb_[:, 0:N], in_=bank(bq)[:, 0:N], func=AF.Square)), reads=[PB[bq]], writes=[Bsq_])
                        S.op("pe", ("matmul", dict(out=bank(bms)[:, 0:N], lhsT=blk_b, rhs=sqb_[:, 0:N], start=True, stop=True)), reads=[Bsq_, Bc], writes=[PB[bms]])
                        S.op("act", ("activation", dict(out=rs_[:, 0:N], in_=bank(bms)[:, 0:N], func=AF.Sqrt, bias=EPS, scale=1.0)), reads=[PB[bms]], writes=[Brs_])
                        S.op("dve", ("reciprocal", dict(out=rs_[:, 0:N], in_=rs_[:, 0:N])), reads=[Brs_], writes=[Brs_])
                        S.op("dve", ("scalar_tensor_tensor", dict(out=t1_[:, 0:N], in0=bank(bq)[:, 0:N], scalar=qkg[:, gi:gi + 1], in1=cosT[:, c0:c0 + N],
                                                                             op0=ALU.mult, op1=ALU.mult)), reads=[PB[bq], Bsm], writes=[Bt1_])
                        S.op("dve", ("scalar_tensor_tensor", dict(out=t2_[:, 0:N], in0=bank(bqp)[:, 0:N], scalar=qkg[:, gi + 1:gi + 2], in1=sinT[:, c0:c0 + N],
                                                                             op0=ALU.mult, op1=ALU.mult)), reads=[PB[bqp], Bsm], writes=[Bt2_])
                        S.op("dve", ("tensor_tensor", dict(out=t1_[:, 0:N], in0=t1_[:, 0:N], in1=t2_[:, 0:N], op=ALU.add)), reads=[Bt1_, Bt2_], writes=[Bt1_])
                        if j < 4:
                            dst, Bd = QT3[:, j, c0:c0 + N], BQ
                        else:
                            dst, Bd = KT[:, c0:c0 + N], BK
                        S.op("dve", ("tensor_tensor", dict(out=dst, in0=t1_[:, 0:N], in1=rs_[:, 0:N], op=ALU.mult)), reads=[Bt1_, Brs_], writes=[Bd])
                    for t in range(len(tiles)):
                        kt = c0 // 128 + t
                        for k in range(8):
                            S.op("pe", ("matmul", dict(out=bank(5)[:, 0:128], lhsT=h3[:, k, t * 128:(t + 1) * 128], rhs=w_in3[:, k, 128:256],
                                                                      start=(k == 0), stop=(k == 7))), reads=[Bwin, Bh], writes=[PB[5]])
                        S.op("act", ("copy", dict(out=V3[:, kt, :], in_=bank(5)[:, 0:128])), reads=[PB[5]], writes=[BV])
                for bi, blk in enumerate(blocks):
                    tiles, c0, N = blk
                    h3, Bh = make_hT(blk)
                    for g in range(8):
                        pb = 2 + g % 2
                        for k in range(8):
                            S.op("pe", ("matmul", dict(out=bank(pb)[0:64, 0:N], lhsT=w_in3[:, k, 256 + g * 64:320 + g * 64], rhs=h3[:, k, 0:N],
                                                                             start=(k == 0), stop=(k == 7))), reads=[Bwin, Bh], writes=[PB[pb]])
                        S.op("act", ("activation", dict(out=UG3[0:64, g, 0:N], in_=bank(pb)[0:64, 0:N], func=GELU)), reads=[PB[pb]], writes=[BUG])
                    for t in range(len(tiles)):
                        tc0 = t * 128
                        for k in range(8):
                            S.op("pe", ("matmul", dict(out=bank(4), lhsT=h3[:, k, tc0:tc0 + 128], rhs=w_in3[:, k, 768:1280],
                                                                          start=(k == 0), stop=(k == 7))), reads=[Bwin, Bh], writes=[PB[4]])
                        S.op("act", ("activation", dict(out=gv, in_=bank(4), func=GELU)), reads=[PB[4]], writes=[Bgv])
                        S.op("dve", ("bn_stats", dict(out=st6, in_=gv)), reads=[Bgv], writes=[Bst])
                        S.op("dve", ("bn_aggr", dict(out=mv, in_=st6)), reads=[Bst], writes=[Bmv])
                        S.op("act", ("activation", dict(out=mv[:, 1:2], in_=mv[:, 1:2], func=AF.Sqrt, bias=EPS, scale=1.0)), reads=[Bmv], writes=[Bmv])
                        S.op("dve", ("reciprocal", dict(out=mv[:, 1:2], in_=mv[:, 1:2])), reads=[Bmv], writes=[Bmv])
                        S.op("dve", ("tensor_scalar", dict(out=vnf, in0=gv, scalar1=mv[:, 0:1], scalar2=mv[:, 1:2], op0=ALU.subtract, op1=ALU.mult)),
                             reads=[Bgv, Bmv], writes=[Bvnf])
                        S.op("dve", ("tensor_tensor", dict(out=vnb, in0=vnf, in1=lng_bc, op=ALU.mult)), reads=[Bvnf, Bsm], writes=[Bvnb])
                        pm = r3(psum_t[0:64, 6 * 512:8 * 512], 8)
                        for g in range(8):
                            S.op("pe", ("matmul", dict(out=pm[:, g, :], lhsT=vnb[:, g * 64:(g + 1) * 64], rhs=wspT3[:, g, :], start=True, stop=True)),
                                 reads=[Bvnb, BwspT], writes=[PB[6], PB[7]])
                        S.op("dve", ("tensor_tensor", dict(out=r3(mx[0:64, :], 8), in0=pm, in1=bsp3, op=ALU.add)), reads=[PB[6], PB[7], Bsm], writes=[Bmx])
                        S.op("dve", ("tensor_tensor", dict(out=GT3[0:64, :, tc0:tc0 + 128], in0=r3(mx[0:64, :], 8), in1=UG3[0:64, :, tc0:tc0 + 128], op=ALU.mult)),
                             reads=[Bmx, BUG], writes=[BGT])
                    kts = list(range(2)) if bi == 0 else list(range(18))
                    steps = [(h, ki, kt) for h in range(8) for ki, kt in enumerate(kts)]
                    nk = len(kts)

                    def issue_S(idx):
                        h, ki, kt = steps[idx]
                        half, j = h // 4, h % 4
                        p0 = half * 64
                        sb_ = idx % 2
                        S.op("pe", ("matmul", dict(out=bank(sb_)[:, 0:N], lhsT=KT[p0:p0 + 64, kt * 128:(kt + 1) * 128],
                                                   rhs=QT3[p0:p0 + 64, j, c0:c0 + N], start=True, stop=True)), reads=[BK, BQ], writes=[PB[sb_]])
                        pt, Bpt = PT[idx % 3], BPT[idx % 3]
                        S.op("act", ("activation", dict(out=pt[:, 0:N], in_=bank(sb_)[:, 0:N], func=AF.Exp, scale=0.125)), reads=[PB[sb_]], writes=[Bpt])

                    def issue_PV(idx):
                        h, ki, kt = steps[idx]
                        half = h // 4
                        p0 = half * 64
                        bo, bd = 2 + (h % 2) * 2, 3 + (h % 2) * 2
                        pt, Bpt = PT[idx % 3], BPT[idx % 3]
                        S.op("pe", ("matmul", dict(out=bank(bo)[0:64, 0:N], lhsT=V3[:, kt, p0:p0 + 64], rhs=pt[:, 0:N],
                                                   start=(ki == 0), stop=(ki == nk - 1))), reads=[BV, Bpt], writes=[PB[bo]])
                        S.op("pe", ("matmul", dict(out=bank(bd)[0:64, 0:N], lhsT=ones_b[:, 0:64], rhs=pt[:, 0:N],
                                                   start=(ki == 0), stop=(ki == nk - 1))), reads=[Bc, Bpt], writes=[PB[bd]])
                        if ki == nk - 1:
                            S.op("dve", ("reciprocal", dict(out=rec[0:64, 0:N], in_=bank(bd)[0:64, 0:N])), reads=[PB[bd]], writes=[Brec])
                            S.op("dve", ("tensor_tensor", dict(out=OT3[0:64, h, 0:N], in0=bank(bo)[0:64, 0:N], in1=rec[0:64, 0:N], op=ALU.mult)),
                                 reads=[PB[bo], Brec], writes=[BOT])

                    for idx in range(len(steps) + 1):
                        if idx < len(steps):
                            issue_S(idx)
                        if idx >= 1:
                            issue_PV(idx - 1)
                    for t, ti in enumerate(tiles):
                        tc0 = t * 128
                        col = tile_col(ti)
                        for hf in range(2):
                            for c in range(16):
                                lw = OT3[0:64, c, tc0:tc0 + 128] if c < 8 else GT3[0:64, c - 8, tc0:tc0 + 128]
                                S.op("pe", ("matmul", dict(out=bank(6 + hf), lhsT=lw, rhs=wout3[0:64, c, hf * 512:(hf + 1) * 512],
                                                                                 start=(c == 0), stop=(c == 15))), reads=[BOT, BGT, Bwout], writes=[PB[6 + hf]])
                        S.dma("sp", ("dma_start", dict(out=xt2, in_=tile_src(L, ti))), writes=[Bxt2])
                        S.op("dve", ("tensor_tensor", dict(out=yt, in0=psum_t[:, 6 * 512:8 * 512], in1=gate_bc[col], op=ALU.mult)),
                             reads=[PB[6], PB[7], Bsm], writes=[Byt])
                        S.op("dve", ("tensor_tensor", dict(out=yt, in0=yt, in1=xt2, op=ALU.add)), reads=[Byt, Bxt2], writes=[Byt])
                        S.dma("sp", ("dma_start", dict(out=xr_d[ti * 128:(ti + 1) * 128, :], in_=yt)), reads=[Byt], writes=[Bxr])
            return Bxr

        if stop_after >= 1:
            Bxr = phase1()
            S.barrier()
            A.off = persist_off
            if "xr" in dbg_d:
                S.dma("sp", ("dma_start", dict(out=dbg_d["xr"], in_=xr_d)), reads=[Bxr])
        def moe(L, ntiles, Bsrc):
            last = (L == 1)
            ncol = 2 if last else 3
            Abc = [A.f32(D) for _ in range(ncol)]
            Sbc = [A.f32(D) for _ in range(ncol)]
            Gbc = [A.f32(D) for _ in range(ncol)]
            Bbc = Buf()
            for c in range(ncol):
                for (kind, dst) in ((4, Abc), (3, Sbc), (5, Gbc)):
                    S.dma("sp", ("dma_start", dict(out=dst[c], in_=modrows_d[L, kind, c:c + 1, :].broadcast_to([128, D]))), reads=[Bmodrows], writes=[Bbc])
            fng = A.f32(D)
            if last:
                S.dma("sp", ("dma_start", dict(out=fng, in_=fng_d.broadcast_to([128, D]))), writes=[Bbc])
            w36 = A.f32(8 * 36)
            w36_3 = r3(w36, 8)
            b36 = A.f32(36)
            S.dma("sp", ("dma_start", dict(out=w36_3[:, :, 0:4], in_=wgrp_d[L].rearrange("(k p) n -> p k n", p=128), allow_slow_non_contiguous=True)), writes=[Bbc])
            S.dma("sp", ("dma_start", dict(out=w36_3[:, :, 4:36], in_=wrt_d[L].rearrange("(k p) n -> p k n", p=128), allow_slow_non_contiguous=True)), writes=[Bbc])
            S.dma("sp", ("dma_start", dict(out=b36[:, 0:4], in_=bgrp_d[L].broadcast_to([128, 4]))), writes=[Bbc])
            S.dma("sp", ("dma_start", dict(out=b36[:, 4:36], in_=brt_d[L].broadcast_to([128, 32]))), writes=[Bbc])
            slot_i = A.i32(ntiles * 2)
            slot_i3 = r3(slot_i, ntiles)
            wts = A.f32(ntiles * 2)
            wts3 = r3(wts, ntiles)
            Bslot = Buf()
            run = A.f32(32)
            Brun = Buf()
            S.op("dve", ("memset", dict(ap=run, constant=0.0)), writes=[Brun])
            ztile = A.f32(D)
            Bz = Buf()
            Byall = Buf()
            Bxall = Buf()
            S.op("dve", ("memset", dict(ap=ztile, constant=0.0)), writes=[Bz])
            S.dma("sp", ("dma_start", dict(out=yall_d[NSLOT:NSLOT + 128, :], in_=ztile)), reads=[Bz], writes=[Byall])
            mark = A.off
            xt = [A.f32(D) for _ in range(2)]
            Bxt = [Buf(), Buf()]
            ff = [A.f32(D) for _ in range(2)]
            Bff = [Buf(), Buf()]
            fb = [A.bf16(D) for _ in range(2)]
            Bfb = [Buf(), Buf()]
            fTs = A.f32(D)
            BfTs = Buf()
            junk = A.bf16(D)
            Bjunk = Buf()
            sm = A.f32(256)
            Bsmall = Buf()
            ss = sm[:, 0:1]
            lg = sm[:, 4:40]
            gmax = sm[:, 40:41]
            ngmax = sm[:, 41:42]
            gsum = sm[:, 42:43]
            eg = sm[:, 44:48]
            gmask = sm[:, 48:52]
            pen = sm[:, 52:56]
            masked = sm[:, 56:88]
            m8 = sm[:, 88:96]
            ntop1 = sm[:, 96:97]
            e2 = sm[:, 97:98]
            wa = sm[:, 98:99]
            wb = sm[:, 99:100]
            slotf = sm[:, 100:102]
            vab = sm[:, 102:104]
            sel1 = sm[:, 104:136]
            sel = sm[:, 136:168]
            pos = sm[:, 168:200]
            valid = sm[:, 200:232]
            tmp32 = A.f32(32)
            selb = A.bf16(32)
            fTs2 = [fTs, A.f32(D)]
            BfTs2 = [BfTs, Buf()]
            ssA = [A.f32(1), A.f32(1)]
            BssA = [Buf(), Buf()]
            lgb = [A.f32(36), A.f32(36)]
            Blg = [Buf(), Buf()]

            def stageA(ti):
                col = tile_col(ti)
                x_, Bx_ = xt[ti % 2], Bxt[ti % 2]
                f_, Bf_ = ff[ti % 2], Bff[ti % 2]
                fb_, Bfb_ = fb[ti % 2], Bfb[ti % 2]
                ss_, Bss_ = ssA[ti % 2], BssA[ti % 2]
                fT_, BfT_ = fTs2[ti % 2], BfTs2[ti % 2]
                S.dma("sp", ("dma_start", dict(out=x_, in_=xr_d[ti * 128:(ti + 1) * 128, :])), reads=[Bsrc], writes=[Bx_])
                S.op("act", ("activation", dict(out=junk, in_=x_, func=AF.Square, accum_out=ss_)), reads=[Bx_], writes=[Bjunk, Bss_])
                S.op("act", ("activation", dict(out=ss_, in_=ss_, func=AF.Sqrt, scale=1.0 / D, bias=EPS)), reads=[Bss_], writes=[Bss_])
                S.op("dve", ("reciprocal", dict(out=ss_, in_=ss_)), reads=[Bss_], writes=[Bss_])
                S.op("dve", ("scalar_tensor_tensor", dict(out=f_, in0=x_, scalar=ss_, in1=Abc[col], op0=ALU.mult, op1=ALU.mult)), reads=[Bx_, Bss_, Bbc], writes=[Bf_])
                S.op("dve", ("tensor_tensor", dict(out=f_, in0=f_, in1=Sbc[col], op=ALU.add)), reads=[Bf_, Bbc], writes=[Bf_])
                S.op("act", ("copy", dict(out=fb_, in_=f_)), reads=[Bf_], writes=[Bfb_])
                pT = r3(psum_t[:, 0:1024], 8)
                for k in range(8):
                    S.op("pe", ("transpose", dict(out=pT[:, k, :], in_=f_[:, k * 128:(k + 1) * 128], identity=ident_f)), reads=[Bf_, Bc], writes=[PB[0], PB[1]])
                S.op("act", ("copy", dict(out=fT_, in_=psum_t[:, 0:1024])), reads=[PB[0], PB[1]], writes=[BfT_])
                pl = 2 + ti % 2
                for k in range(8):
                    S.op("pe", ("matmul", dict(out=bank(pl)[:, 0:36], lhsT=fT_[:, k * 128:(k + 1) * 128], rhs=w36_3[:, k, :], start=(k == 0), stop=(k == 7))),
                         reads=[BfT_, Bbc], writes=[PB[pl]])
                S.op("dve", ("tensor_tensor", dict(out=lgb[ti % 2], in0=bank(pl)[:, 0:36], in1=b36, op=ALU.add)), reads=[PB[pl], Bbc], writes=[Blg[ti % 2]])

            def stageB(ti):
                lg = lgb[ti % 2]
                fb_, Bfb_ = fb[ti % 2], Bfb[ti % 2]
                dv = lambda name, **kw: S.op("dve", (name, kw), reads=[Bsmall, Brun, Blg[ti % 2]], writes=[Bsmall])
                dv("tensor_reduce", out=gmax, in_=lg[:, 0:4], axis=AX.X, op=ALU.max)
                dv("tensor_scalar", out=ngmax, in0=gmax, scalar1=-1.0, scalar2=None, op0=ALU.mult)
                dv("tensor_scalar", out=gmask, in0=lg[:, 0:4], scalar1=gmax, scalar2=None, op0=ALU.is_ge)
                dv("tensor_scalar", out=pen, in0=gmask, scalar1=1e30, scalar2=-1e30, op0=ALU.mult, op1=ALU.add)
                dv("tensor_tensor", out=r3(masked, 4), in0=r3(lg[:, 4:36], 4), in1=pen.unsqueeze(2).broadcast_to([128, 4, 8]), op=ALU.add)
                dv("max", out=m8, in_=masked)
                dv("tensor_scalar", out=ntop1, in0=m8[:, 0:1], scalar1=-1.0, scalar2=None, op0=ALU.mult)
                S.op("act", ("activation", dict(out=eg, in_=lg[:, 0:4], func=AF.Exp, bias=ngmax, scale=1.0, accum_out=gsum)), reads=[Bsmall, Blg[ti % 2]], writes=[Bact])
                S.op("act", ("activation", dict(out=e2, in_=m8[:, 1:2], func=AF.Exp, bias=ntop1, scale=1.0)), reads=[Bsmall], writes=[Bact])
                dv("tensor_scalar", out=sel1, in0=masked, scalar1=m8[:, 0:1], scalar2=None, op0=ALU.is_ge)
                dv("tensor_scalar", out=sel, in0=masked, scalar1=m8[:, 1:2], scalar2=None, op0=ALU.is_ge)
                dv("tensor_copy", out=selb, in_=sel)
                S.op("pe", ("matmul", dict(out=bank(4)[:, 0:32], lhsT=utri_b, rhs=selb, start=True, stop=True)), reads=[Bsmall, Bc], writes=[PB[4]])
                S.op("pe", ("matmul", dict(out=bank(4)[:, 32:64], lhsT=ones_b, rhs=selb, start=True, stop=True)), reads=[Bsmall, Bc], writes=[PB[4]])
                dv("tensor_tensor", out=sel, in0=sel, in1=sel1, op=ALU.subtract)
                S.op("dve", ("tensor_tensor", dict(out=pos, in0=bank(4)[:, 0:32], in1=run, op=ALU.add)), reads=[PB[4], Brun, Bsmall], writes=[Bsmall])
                S.op("dve", ("tensor_tensor", dict(out=run, in0=bank(4)[:, 32:64], in1=run, op=ALU.add)), reads=[PB[4], Brun, Bsmall], writes=[Brun])
                dv("tensor_scalar", out=valid, in0=pos, scalar1=float(CAP), scalar2=None, op0=ALU.is_lt)
                dv("tensor_tensor", out=pos, in0=pos, in1=iotaC, op=ALU.add)
                dv("tensor_scalar", out=pos, in0=pos, scalar1=-float(TRASH), scalar2=None, op0=ALU.add)
                dv("tensor_tensor", out=pos, in0=pos, in1=valid, op=ALU.mult)
                dv("tensor_scalar", out=pos, in0=pos, scalar1=float(TRASH), scalar2=None, op0=ALU.add)
                dv("tensor_tensor", out=tmp32, in0=sel1, in1=pos, op=ALU.mult)
                dv("tensor_reduce", out=slotf[:, 0:1], in_=tmp32, axis=AX.X, op=ALU.add)
                dv("tensor_tensor", out=tmp32, in0=sel1, in1=valid, op=ALU.mult)
                dv("tensor_reduce", out=vab[:, 0:1], in_=tmp32, axis=AX.X, op=ALU.add)
                dv("tensor_tensor", out=tmp32, in0=sel, in1=pos, op=ALU.mult)
                dv("tensor_reduce", out=slotf[:, 1:2], in_=tmp32, axis=AX.X, op=ALU.add)
                dv("tensor_tensor", out=tmp32, in0=sel, in1=valid, op=ALU.mult)
                dv("tensor_reduce", out=vab[:, 1:2], in_=tmp32, axis=AX.X, op=ALU.add)
                S.op("dve", ("tensor_copy", dict(out=slot_i3[:, ti, :], in_=slotf)), reads=[Bsmall], writes=[Bslot])
                for a_ in range(2):
                    S.dma("pool", ("indirect_dma_start", dict(out=xall_d, out_offset=bass.IndirectOffsetOnAxis(ap=slot_i3[:, ti, a_:a_ + 1], axis=0),
                                                             in_=fb_, in_offset=None)), reads=[Bfb_, Bslot], writes=[Bxall])
                S.op("dve", ("tensor_scalar", dict(out=wa, in0=e2, scalar1=1.0, scalar2=gsum, op0=ALU.add, op1=ALU.mult)), reads=[Bact, Bsmall], writes=[Bsmall])
                dv("reciprocal", out=wa, in_=wa)
                S.op("dve", ("tensor_tensor", dict(out=wb, in0=wa, in1=e2, op=ALU.mult)), reads=[Bact, Bsmall], writes=[Bsmall])
                S.op("dve", ("tensor_tensor", dict(out=wts3[:, ti, :], in0=sm[:, 98:100], in1=vab, op=ALU.mult)), reads=[Bsmall], writes=[Bslot])

            Bact = Buf()
            sma = A.f32(8)
            eg = sma[:, 0:4]
            gsum = sma[:, 4:5]
            e2 = sma[:, 5:6]
            for step in range(ntiles + 1):
                if step < ntiles:
                    stageA(step)
                if step >= 1:
                    stageB(step - 1)
            S.barrier()
            A.off = mark
            if last is False and "slots" in dbg_d:
                pass
            NJ = CAP // 128
            wbuf = [(A.bf16(8 * 512), A.bf16(8 * 512), A.bf16(4 * 1024)) for _ in range(2)]
            Bwb = [Buf(), Buf()]
            xrows = [A.bf16(NJ * D) for _ in range(2)]
            Bxrows = [Buf(), Buf()]
            XT = A.bf16(8 * CAP)
            XT3 = r3(XT, 8)
            BXT = Buf()
            sl = [A.f32(512) for _ in range(2)]
            Bsl = [Buf(), Buf()]
            hs = A.bf16(4 * CAP)
            hs3 = r3(hs, 4)
            Bhs = Buf()
            ysb = [A.f32(D) for _ in range(2)]
            Bysb = [Buf(), Buf()]
            yi = 0
            stg = (A.f32(8 * 512), A.f32(8 * 512), A.f32(4 * 1024))
            Bstg = [Buf(), Buf(), Buf()]

            def load_w(e_):
                S.dma("sp", ("dma_start", dict(out=r3(stg[0], 8), in_=w1_d[L, e_].rearrange("(k p) n -> p k n", p=128))), writes=[Bstg[0]])
                S.dma("sp", ("dma_start", dict(out=r3(stg[1], 8), in_=w3_d[L, e_].rearrange("(k p) n -> p k n", p=128))), writes=[Bstg[1]])
                S.dma("sp", ("dma_start", dict(out=r3(stg[2], 4), in_=w2_d[L, e_].rearrange("(k p) n -> p k n", p=128))), writes=[Bstg[2]])

            def cast_w(e_, which):
                dst = wbuf[e_ % 2][which]
                Bw_ = Bwb[e_ % 2]
                if which == 0:
                    S.op("act", ("copy", dict(out=dst, in_=stg[0])), reads=[Bstg[0]], writes=[Bw_])
                elif which == 1:
                    S.op("dve", ("tensor_copy", dict(out=dst, in_=stg[1])), reads=[Bstg[1]], writes=[Bw_])
                else:
                    S.op("act", ("copy", dict(out=dst[:, 0:2048], in_=stg[2][:, 0:2048])), reads=[Bstg[2]], writes=[Bw_])
                    S.op("dve", ("tensor_copy", dict(out=dst[:, 2048:4096], in_=stg[2][:, 2048:4096])), reads=[Bstg[2]], writes=[Bw_])

            def load_x(e_):
                S.dma("sp", ("dma_start", dict(out=r3(xrows[e_ % 2], NJ), in_=xall_d[e_ * CAP:(e_ + 1) * CAP, :].rearrange("(j p) d -> p j d", p=128))),
                      reads=[Bxall], writes=[Bxrows[e_ % 2]])

            load_w(0)
            load_x(0)
            for w_ in range(3):
                cast_w(0, w_)
            for e_ in range(32):
                w1b, w3b, w2b = wbuf[e_ % 2]
                Bw = Bwb[e_ % 2]
                xr_, Bxr_ = xrows[e_ % 2], Bxrows[e_ % 2]
                if e_ + 1 < 32:
                    load_w(e_ + 1)
                    load_x(e_ + 1)
                for j in range(NJ):
                    pb = j % 2
                    pT = r3(bank_bf(pb), 8)
                    for k in range(8):
                        S.op("pe", ("transpose", dict(out=pT[:, k, :], in_=r3(xr_, NJ)[:, j, k * 128:(k + 1) * 128], identity=ident_b)),
                             reads=[Bxr_, Bc], writes=[PB[pb]])
                    S.op("act" if j % 2 == 0 else "dve", ("tensor_copy" if j % 2 else "copy", dict(out=XT3[:, :, j * 128:(j + 1) * 128], in_=pT)),
                         reads=[PB[pb]], writes=[BXT])
                w1v, w3v, w2v = r3(w1b, 8), r3(w3b, 8), r3(w2b, 4)
                for bi_, (c0, n) in enumerate(((0, 512), (512, CAP - 512))):
                    for m in range(4):
                        for (pb, wv) in ((2 + (m % 2) * 2, w1v), (3 + (m % 2) * 2, w3v)):
                            for k in range(8):
                                S.op("pe", ("matmul", dict(out=bank(pb)[:, 0:n], lhsT=wv[:, k, m * 128:(m + 1) * 128], rhs=XT3[:, k, c0:c0 + n], start=(k == 0), stop=(k == 7))),
                                     reads=[Bw, BXT], writes=[PB[pb]])
                        p1, p3 = 2 + (m % 2) * 2, 3 + (m % 2) * 2
                        s_, Bs_ = sl[m % 2], Bsl[m % 2]
                        S.op("act", ("activation", dict(out=s_[:, 0:n], in_=bank(p1)[:, 0:n], func=AF.Silu)), reads=[PB[p1]], writes=[Bs_])
                        S.op("dve", ("tensor_tensor", dict(out=hs3[:, m, c0:c0 + n], in0=bank(p3)[:, 0:n], in1=s_[:, 0:n], op=ALU.mult)), reads=[PB[p3], Bs_], writes=[Bhs])
                    if e_ + 1 < 32:
                        cast_w(e_ + 1, bi_)
                for j in range(NJ):
                    for hf in range(2):
                        for m in range(4):
                            S.op("pe", ("matmul", dict(out=bank(6 + hf), lhsT=hs3[:, m, j * 128:(j + 1) * 128], rhs=w2v[:, m, hf * 512:(hf + 1) * 512], start=(m == 0), stop=(m == 3))),
                                 reads=[Bhs, Bw], writes=[PB[6 + hf]])
                    y_, By_ = ysb[yi % 2], Bysb[yi % 2]
                    yi += 1
                    S.op("act", ("copy", dict(out=y_[:, 0:512], in_=bank(6))), reads=[PB[6]], writes=[By_])
                    S.op("dve", ("tensor_copy", dict(out=y_[:, 512:1024], in_=bank(7))), reads=[PB[7]], writes=[By_])
                    r0 = e_ * CAP + j * 128
                    S.dma("sp", ("dma_start", dict(out=yall_d[r0:r0 + 128, :], in_=y_)), reads=[By_], writes=[Byall])
                    if j == 1 and e_ + 1 < 32:
                        cast_w(e_ + 1, 2)
            S.barrier()
            A.off = mark
            ya = [A.f32(D) for _ in range(2)]
            yb = [A.f32(D) for _ in range(2)]
            x1 = [A.f32(D) for _ in range(2)]
            Bya, Byb, Bx1 = [Buf(), Buf()], [Buf(), Buf()], [Buf(), Buf()]
            junk2 = A.bf16(D)
            Bj2 = Buf()
            ss2 = [A.f32(1) for _ in range(2)]
            Bss2 = [Buf(), Buf()]
            Bdst = Buf()
            for ti in range(ntiles):
                col = tile_col(ti)
                i2 = ti % 2
                S.dma("pool", ("indirect_dma_start", dict(out=ya[i2], out_offset=None, in_=yall_d, in_offset=bass.IndirectOffsetOnAxis(ap=slot_i3[:, ti, 0:1], axis=0))),
                      reads=[Byall, Bslot], writes=[Bya[i2]])
                S.dma("pool", ("indirect_dma_start", dict(out=yb[i2], out_offset=None, in_=yall_d, in_offset=bass.IndirectOffsetOnAxis(ap=slot_i3[:, ti, 1:2], axis=0))),
                      reads=[Byall, Bslot], writes=[Byb[i2]])
                S.dma("sp", ("dma_start", dict(out=x1[i2], in_=xr_d[ti * 128:(ti + 1) * 128, :])), reads=[Bsrc], writes=[Bx1[i2]])
                S.op("act", ("activation", dict(out=ya[i2], in_=ya[i2], func=AF.Copy, scale=wts3[:, ti, 0:1])), reads=[Bya[i2], Bslot], writes=[Bya[i2]])
                S.op("dve", ("scalar_tensor_tensor", dict(out=yb[i2], in0=yb[i2], scalar=wts3[:, ti, 1:2], in1=ya[i2], op0=ALU.mult, op1=ALU.add)),
                     reads=[Byb[i2], Bya[i2], Bslot], writes=[Byb[i2]])
                S.op("dve", ("tensor_tensor", dict(out=yb[i2], in0=yb[i2], in1=Gbc[col], op=ALU.mult)), reads=[Byb[i2], Bbc], writes=[Byb[i2]])
                S.op("dve", ("tensor_tensor", dict(out=x1[i2], in0=x1[i2], in1=yb[i2], op=ALU.add)), reads=[Bx1[i2], Byb[i2]], writes=[Bx1[i2]])
                if not last:
                    S.dma("sp", ("dma_start", dict(out=xr2_d[ti * 128:(ti + 1) * 128, :], in_=x1[i2])), reads=[Bx1[i2]], writes=[Bdst])
                else:
                    S.op("act", ("activation", dict(out=junk2, in_=x1[i2], func=AF.Square, accum_out=ss2[i2])), reads=[Bx1[i2]], writes=[Bj2, Bss2[i2]])
                    S.op("act", ("activation", dict(out=ss2[i2], in_=ss2[i2], func=AF.Sqrt, scale=1.0 / D, bias=EPS)), reads=[Bss2[i2]], writes=[Bss2[i2]])
                    S.op("dve", ("reciprocal", dict(out=ss2[i2], in_=ss2[i2])), reads=[Bss2[i2]], writes=[Bss2[i2]])
                    S.op("dve", ("scalar_tensor_tensor", dict(out=x1[i2], in0=x1[i2], scalar=ss2[i2], in1=fng, op0=ALU.mult, op1=ALU.mult)),
                         reads=[Bx1[i2], Bss2[i2], Bbc], writes=[Bx1[i2]])
                    S.dma("sp", ("dma_start", dict(out=out_d[ti * 128:(ti + 1) * 128, :], in_=x1[i2])), reads=[Bx1[i2]], writes=[Bdst])
            return Bdst

        if stop_after >= 2:
            Bxr2 = moe(0, 36, Bxr)
            S.barrier()
            A.off = persist_off
            if "xr2" in dbg_d:
                S.dma("sp", ("dma_start", dict(out=dbg_d["xr2"], in_=xr2_d)), reads=[Bxr2])
        def s5_mixer(Bsrc):
            L = 1
            TWO_PI = 6.283185307179586
            MAGIC = 12582912.0
            yacc = [A.f32(4 * S_LAT) for _ in range(NB)]
            yacc3 = [r3(y, 4) for y in yacc]
            Byacc = [Buf(), Buf()]
            uT = [A.bf16(4 * SEQ) for _ in range(NB)]
            uT3 = [r3(u, 4) for u in uT]
            BuT = [Buf(), Buf()]
            sd = A.f32(8)
            Bsd = Buf()
            S.dma("sp", ("dma_start", dict(out=sd[:, 0:4], in_=s5d_d)), writes=[Bsd])
            S.dma("sp", ("dma_start", dict(out=sd[:, 4:8], in_=s5bglu_d)), writes=[Bsd])
            mark = A.off
            if S5_DEBUG_STAGE < 1:
                return Byacc[0]
            w5 = A.bf16(8 * 512)
            w5_3 = r3(w5, 8)
            Bw5 = Buf()
            S.dma("pool", ("dma_start", dict(out=w5_3, in_=s5win_d.rearrange("(k p) n -> p k n", p=128))), writes=[Bw5])
            hT1 = A.bf16(8 * 512)
            h3 = r3(hT1, 8)
            Bh = Buf()
            NT = NormT()
            for b in range(NB):
                blocks = [([32 + 2 * b, 33 + 2 * b], 0, 256)]
                for i in range(4):
                    blocks.append(([16 * b + 4 * i + t for t in range(4)], 256 + 512 * i, 512))
                for (tiles, c0, N) in blocks:
                    for t, ti in enumerate(tiles):
                        NT.run(L, ti, h3, Bh, t * 128, t % 2)
                    for r in range(4 if S5_DEBUG_STAGE >= 1.5 else 0):
                        pb = 2 + r % 2
                        for k in range(8):
                            S.op("pe", ("matmul", dict(out=bank(pb)[:, 0:N], lhsT=w5_3[:, k, r * 128:(r + 1) * 128], rhs=h3[:, k, 0:N], start=(k == 0), stop=(k == 7))),
                                 reads=[Bw5, Bh], writes=[PB[pb]])
                        S.op("act", ("copy", dict(out=uT3[b][:, r, c0:c0 + N], in_=bank(pb)[:, 0:N])), reads=[PB[pb]], writes=[BuT[b]])
                        if c0 >= 256:
                            S.op("dve", ("tensor_scalar", dict(out=yacc3[b][:, r, c0 - 256:c0 - 256 + N], in0=uT3[b][:, r, c0:c0 + N], scalar1=sd[:, r:r + 1], scalar2=None, op0=ALU.mult)),
                                 reads=[BuT[b], Bsd], writes=[Byacc[b]])
            S.barrier()
            A.off = mark
            if S5_DEBUG_STAGE < 2:
                return Byacc[0]
            par = A.f32(96)
            par3 = par.rearrange("p (c t) -> p c t", t=3)
            Bpar = Buf()
            S.dma("sp", ("dma_start", dict(out=par, in_=s5par_d)), writes=[Bpar])
            iot = A.f32(SEQ)
            S.dma("sp", ("dma_start", dict(out=iot, in_=s5iota_d)), writes=[Bpar])
            NCB = 32
            pr_ = A.f32(NCB * 16)
            P3 = r3(pr_, 16)
            dtv, rho, tht, frv, sn, cs, nr, ni, inv, cfr, cfi, ncfr, ncfi, tmpa, tmpb, tmpc = [P3[:, i, :] for i in range(16)]
            are, aim, ldt = par3[:, :, 0], par3[:, :, 1], par3[:, :, 2]
            pv = lambda name, **kw: S.op("dve", (name, kw), reads=[Bpar], writes=[Bpar])
            pa = lambda **kw: S.op("act", ("activation", kw), reads=[Bpar], writes=[Bpar])
            pa(out=dtv, in_=ldt, func=AF.Exp)
            pv("tensor_tensor", out=tmpa, in0=are, in1=dtv, op=ALU.mult)
            pa(out=rho, in_=tmpa, func=AF.Exp)
            pv("tensor_tensor", out=tht, in0=aim, in1=dtv, op=ALU.mult)
            pv("tensor_scalar", out=tht, in0=tht, scalar1=1.0 / TWO_PI, scalar2=None, op0=ALU.mult)
            pv("tensor_scalar", out=tmpa, in0=tht, scalar1=MAGIC, scalar2=None, op0=ALU.add)
            pv("tensor_scalar", out=tmpa, in0=tmpa, scalar1=MAGIC, scalar2=None, op0=ALU.subtract)
            pv("tensor_tensor", out=frv, in0=tht, in1=tmpa, op=ALU.subtract)
            SC = TWO_PI * (1.0 - 1e-6)
            pa(out=sn, in_=frv, func=AF.Sin, scale=SC)
            pa(out=tmpb, in_=frv, func=AF.Sin, scale=SC / 2)
            pv("tensor_tensor", out=tmpb, in0=tmpb, in1=tmpb, op=ALU.mult)
            pv("tensor_scalar", out=cs, in0=tmpb, scalar1=-2.0, scalar2=1.0, op0=ALU.mult, op1=ALU.add)
            pv("tensor_tensor", out=nr, in0=rho, in1=cs, op=ALU.mult)
            pv("tensor_scalar", out=nr, in0=nr, scalar1=-1.0, scalar2=None, op0=ALU.add)
            pv("tensor_tensor", out=ni, in0=rho, in1=sn, op=ALU.mult)
            pv("tensor_tensor", out=tmpa, in0=are, in1=are, op=ALU.mult)
            pv("tensor_tensor", out=tmpb, in0=aim, in1=aim, op=ALU.mult)
            pv("tensor_tensor", out=inv, in0=tmpa, in1=tmpb, op=ALU.add)
            pv("reciprocal", out=inv, in_=inv)
            pv("tensor_tensor", out=tmpa, in0=nr, in1=are, op=ALU.mult)
            pv("tensor_tensor", out=tmpb, in0=ni, in1=aim, op=ALU.mult)
            pv("tensor_tensor", out=tmpa, in0=tmpa, in1=tmpb, op=ALU.add)
            pv("tensor_tensor", out=cfr, in0=tmpa, in1=inv, op=ALU.mult)
            pv("tensor_tensor", out=tmpa, in0=ni, in1=are, op=ALU.mult)
            pv("tensor_tensor", out=tmpb, in0=nr, in1=aim, op=ALU.mult)
            pv("tensor_tensor", out=tmpa, in0=tmpa, in1=tmpb, op=ALU.subtract)
            pv("tensor_tensor", out=cfi, in0=tmpa, in1=inv, op=ALU.mult)
            pv("tensor_scalar", out=ncfr, in0=cfr, scalar1=-1.0, scalar2=None, op0=ALU.mult)
            pv("tensor_scalar", out=ncfi, in0=cfi, scalar1=-1.0, scalar2=None, op0=ALU.mult)

            if S5_DEBUG_STAGE < 3:
                return Bpar
            cosT = A.f32(SEQ)
            sinT = A.f32(SEQ)
            tA = A.f32(SEQ)
            tB = A.f32(SEQ)
            Btab, BtA, BtB = Buf(), Buf(), Buf()
            dr = A.f32(SEQ)
            di = A.f32(SEQ)
            qr = A.f32(SEQ)
            qi = A.f32(SEQ)
            Bdr, Bdi, Bqr, Bqi = Buf(), Buf(), Buf(), Buf()
            m1 = [A.f32(512) for _ in range(2)]
            m2 = [A.f32(512) for _ in range(2)]
            Bm1, Bm2 = [Buf(), Buf()], [Buf(), Buf()]
            hr = [A.bf16(512) for _ in range(2)]
            hi = [A.bf16(512) for _ in range(2)]
            Bhr, Bhi = [Buf(), Buf()], [Buf(), Buf()]
            bt = [(A.bf16(128), A.bf16(128)) for _ in range(2)]
            Bbt = [Buf(), Buf()]
            cst_ = [(A.f32(128), A.f32(128)) for _ in range(2)]
            Bcst = [Buf(), Buf()]
            cw = [(A.bf16(128), A.bf16(128)) for _ in range(2)]
            Bcw = [Buf(), Buf()]
            ctmp = A.f32(128)
            Bctmp = Buf()
            mi = 0
            for d_ in range(2):
                for pr in range(16):
                    ci_ = d_ * 16 + pr
                    r = pr // 4
                    k2 = ci_ % 2
                    btr, bti = bt[k2]
                    S.dma("pool", ("dma_start", dict(out=btr, in_=s5bT_d[d_, 0, pr])), writes=[Bbt[k2]])
                    S.dma("pool", ("dma_start", dict(out=bti, in_=s5bT_d[d_, 1, pr])), writes=[Bbt[k2]])
                    c_r, c_i = cst_[k2]
                    S.dma("sp", ("dma_start", dict(out=c_r, in_=s5c_d[d_, 0, pr])), writes=[Bcst[k2]])
                    S.dma("sp", ("dma_start", dict(out=c_i, in_=s5c_d[d_, 1, pr])), writes=[Bcst[k2]])
                    cwr, cwi = cw[k2]
                    col1 = lambda v, ci_=ci_: v[:, ci_:ci_ + 1]
                    S.op("dve", ("tensor_scalar", dict(out=ctmp, in0=c_r, scalar1=col1(cfr), scalar2=None, op0=ALU.mult)), reads=[Bcst[k2], Bpar], writes=[Bctmp])
                    S.op("dve", ("scalar_tensor_tensor", dict(out=cwr, in0=c_i, scalar=col1(ncfi), in1=ctmp, op0=ALU.mult, op1=ALU.add)), reads=[Bcst[k2], Bpar, Bctmp], writes=[Bcw[k2]])
                    S.op("dve", ("tensor_scalar", dict(out=ctmp, in0=c_r, scalar1=col1(ncfi), scalar2=None, op0=ALU.mult)), reads=[Bcst[k2], Bpar, Bcw[k2]], writes=[Bctmp])
                    S.op("dve", ("scalar_tensor_tensor", dict(out=cwi, in0=c_i, scalar=col1(ncfr), in1=ctmp, op0=ALU.mult, op1=ALU.add)), reads=[Bcst[k2], Bpar, Bctmp], writes=[Bcw[k2]])
                    S.op("dve", ("tensor_scalar", dict(out=tA, in0=iot, scalar1=col1(tht), scalar2=None, op0=ALU.mult)), reads=[Bpar], writes=[BtA])
                    S.op("dve", ("tensor_scalar", dict(out=tB, in0=tA, scalar1=MAGIC, scalar2=None, op0=ALU.add)), reads=[BtA], writes=[BtB])
                    S.op("dve", ("tensor_scalar", dict(out=tB, in0=tB, scalar1=MAGIC, scalar2=None, op0=ALU.subtract)), reads=[BtB], writes=[BtB])
                    S.op("dve", ("tensor_tensor", dict(out=tA, in0=tA, in1=tB, op=ALU.subtract)), reads=[BtA, BtB], writes=[BtA])
                    S.op("act", ("activation", dict(out=sinT, in_=tA, func=AF.Sin, scale=SC)), reads=[BtA], writes=[Btab])
                    S.op("act", ("activation", dict(out=tB, in_=tA, func=AF.Sin, scale=SC / 2)), reads=[BtA], writes=[BtB])
                    S.op("act", ("activation", dict(out=tB, in_=tB, func=AF.Square, scale=1.4142135623730951)), reads=[BtB], writes=[BtB])
                    S.op("act", ("activation", dict(out=cosT, in_=tB, func=AF.Identity, scale=-1.0, bias=1.0)), reads=[BtB], writes=[Btab])
                    rho_c = col1(rho)
                    for b in range(NB):
                        blocks = [(0, 256)] + [(256 + 512 * i, 512) for i in range(4)]
                        for bi, (s0, N) in enumerate(blocks):
                            if d_ == 0:
                                ucols = uT3[b][:, r, s0:s0 + N]
                            else:
                                if bi == 0:
                                    ucols = uT3[b][:, r, 255::-1]
                                else:
                                    hi_c = SEQ - 1 - (bi - 1) * 512
                                    ucols = uT3[b][:, r, hi_c:hi_c - 512:-1]
                            pbr, pbi = (bi % 2) * 2, (bi % 2) * 2 + 1
                            S.op("pe", ("matmul", dict(out=bank(pbr)[:, 0:N], lhsT=btr, rhs=ucols, start=True, stop=True)), reads=[Bbt[k2], BuT[b]], writes=[PB[pbr]])
                            S.op("pe", ("matmul", dict(out=bank(pbi)[:, 0:N], lhsT=bti, rhs=ucols, start=True, stop=True)), reads=[Bbt[k2], BuT[b]], writes=[PB[pbi]])
                            a1, a2 = m1[mi % 2], m2[mi % 2]
                            Ba1, Ba2 = Bm1[mi % 2], Bm2[mi % 2]
                            mi += 1
                            cS, sS = cosT[:, s0:s0 + N], sinT[:, s0:s0 + N]
                            S.op("dve", ("tensor_tensor", dict(out=a1[:, 0:N], in0=bank(pbr)[:, 0:N], in1=cS, op=ALU.mult)), reads=[PB[pbr], Btab], writes=[Ba1])
                            S.op("dve", ("tensor_tensor", dict(out=a2[:, 0:N], in0=bank(pbi)[:, 0:N], in1=sS, op=ALU.mult)), reads=[PB[pbi], Btab], writes=[Ba2])
                            S.op("dve", ("tensor_tensor", dict(out=dr[:, s0:s0 + N], in0=a1[:, 0:N], in1=a2[:, 0:N], op=ALU.add)), reads=[Ba1, Ba2], writes=[Bdr])
                            a1, a2 = m1[mi % 2], m2[mi % 2]
                            Ba1, Ba2 = Bm1[mi % 2], Bm2[mi % 2]
                            mi += 1
                            S.op("dve", ("tensor_tensor", dict(out=a1[:, 0:N], in0=bank(pbi)[:, 0:N], in1=cS, op=ALU.mult)), reads=[PB[pbi], Btab], writes=[Ba1])
                            S.op("dve", ("tensor_tensor", dict(out=a2[:, 0:N], in0=bank(pbr)[:, 0:N], in1=sS, op=ALU.mult)), reads=[PB[pbr], Btab], writes=[Ba2])
                            S.op("dve", ("tensor_tensor", dict(out=di[:, s0:s0 + N], in0=a1[:, 0:N], in1=a2[:, 0:N], op=ALU.subtract)), reads=[Ba1, Ba2], writes=[Bdi])
                        rb = rho_c.broadcast_to([128, SEQ])
                        S.op("dve", ("tensor_tensor_scan", dict(out=qr, data0=rb, data1=dr, initial=0.0, op0=ALU.mult, op1=ALU.add)), reads=[Bdr, Bpar], writes=[Bqr])
                        S.op("dve", ("tensor_tensor_scan", dict(out=qi, data0=rb, data1=di, initial=0.0, op0=ALU.mult, op1=ALU.add)), reads=[Bdi, Bpar], writes=[Bqi])
                        for bi in range(1, 5):
                            s0, N = blocks[bi]
                            cS, sS = cosT[:, s0:s0 + N], sinT[:, s0:s0 + N]
                            a1, a2 = m1[mi % 2], m2[mi % 2]
                            Ba1, Ba2 = Bm1[mi % 2], Bm2[mi % 2]
                            mi += 1
                            h_r, h_i = hr[bi % 2], hi[bi % 2]
                            S.op("dve", ("tensor_tensor", dict(out=a1, in0=qr[:, s0:s0 + N], in1=cS, op=ALU.mult)), reads=[Bqr, Btab], writes=[Ba1])
                            S.op("dve", ("tensor_tensor", dict(out=a2, in0=qi[:, s0:s0 + N], in1=sS, op=ALU.mult)), reads=[Bqi, Btab], writes=[Ba2])
                            S.op("dve", ("tensor_tensor", dict(out=h_r, in0=a1, in1=a2, op=ALU.subtract)), reads=[Ba1, Ba2], writes=[Bhr[bi % 2]])
                            a1, a2 = m1[mi % 2], m2[mi % 2]
                            Ba1, Ba2 = Bm1[mi % 2], Bm2[mi % 2]
                            mi += 1
                            S.op("dve", ("tensor_tensor", dict(out=a1, in0=qr[:, s0:s0 + N], in1=sS, op=ALU.mult)), reads=[Bqr, Btab], writes=[Ba1])
                            S.op("dve", ("tensor_tensor", dict(out=a2, in0=qi[:, s0:s0 + N], in1=cS, op=ALU.mult)), reads=[Bqi, Btab], writes=[Ba2])
                            S.op("dve", ("tensor_tensor", dict(out=h_i, in0=a1, in1=a2, op=ALU.add)), reads=[Ba1, Ba2], writes=[Bhi[bi % 2]])
                            pby = 4 + bi % 2
                            S.op("pe", ("matmul", dict(out=bank(pby), lhsT=cwr, rhs=h_r, start=True, stop=False)), reads=[Bcw[k2], Bhr[bi % 2]], writes=[PB[pby]])
                            S.op("pe", ("matmul", dict(out=bank(pby), lhsT=cwi, rhs=h_i, start=False, stop=True)), reads=[Bcw[k2], Bhi[bi % 2]], writes=[PB[pby]])
                            if d_ == 0:
                                j0 = s0 - 256
                                ycols = yacc3[b][:, r, j0:j0 + 512]
                            else:
                                hj = S_LAT - 1 - (bi - 1) * 512
                                stop = hj - 512
                                ycols = yacc3[b][:, r, hj::-1] if stop < 0 else yacc3[b][:, r, hj:stop:-1]
                            S.op("dve", ("tensor_tensor", dict(out=ycols, in0=bank(pby), in1=ycols, op=ALU.add)), reads=[PB[pby], Byacc[b]], writes=[Byacc[b]])
            S.barrier()
            A.off = mark
            if "yacc" in dbg_d:
                for b in range(NB):
                    S.dma("sp", ("dma_start", dict(out=dbg_d["yacc"][b * 128:(b + 1) * 128, :], in_=yacc[b])), reads=[Byacc[b]])
            wg = A.bf16(4 * 512)
            wg3 = r3(wg, 4)
            wo = A.bf16(4 * 1024)
            wo3 = r3(wo, 4)
            BwC = Buf()
            S.dma("pool", ("dma_start", dict(out=wg3, in_=s5glu_d.rearrange("(k p) n -> p k n", p=128))), writes=[BwC])
            S.dma("pool", ("dma_start", dict(out=wo3, in_=s5wout_d.rearrange("(k p) n -> p k n", p=128))), writes=[BwC])
            gate_bc = [A.f32(D) for _ in range(2)]
            for c in range(2):
                S.dma("sp", ("dma_start", dict(out=gate_bc[c], in_=modrows_d[L, 2, c:c + 1, :].broadcast_to([128, D]))), reads=[Bmodrows], writes=[BwC])
            gT = A.bf16(4 * 512)
            gT3 = r3(gT, 4)
            vT = A.bf16(4 * 512)
            vT3 = r3(vT, 4)
            BgT, BvT = Buf(), Buf()
            sg = [A.f32(512) for _ in range(2)]
            Bsg = [Buf(), Buf()]
            yt = [A.f32(D) for _ in range(2)]
            xt2 = [A.f32(D) for _ in range(2)]
            Byt, Bxt2 = [Buf(), Buf()], [Buf(), Buf()]
            Bxr = Buf()
            oi = 0
            for b in range(NB):
                for i in range(4):
                    j0 = i * 512
                    S.op("act", ("activation", dict(out=gT3, in_=yacc3[b][:, :, j0:j0 + 512], func=AF.Gelu_apprx_tanh)), reads=[Byacc[b]], writes=[BgT])
                    for m in range(4):
                        pb = 2 + m % 2
                        for k in range(4):
                            S.op("pe", ("matmul", dict(out=bank(pb), lhsT=wg3[:, k, m * 128:(m + 1) * 128], rhs=gT3[:, k, :], start=(k == 0), stop=(k == 3))),
                                 reads=[BwC, BgT], writes=[PB[pb]])
                        S.op("act", ("activation", dict(out=sg[m % 2], in_=bank(pb), func=AF.Sigmoid, bias=sd[:, 4 + m:5 + m], scale=1.0)), reads=[PB[pb], Bsd], writes=[Bsg[m % 2]])
                        S.op("dve", ("tensor_tensor", dict(out=vT3[:, m, :], in0=gT3[:, m, :], in1=sg[m % 2], op=ALU.mult)), reads=[BgT, Bsg[m % 2]], writes=[BvT])
                    for t in range(4):
                        ti = 16 * b + 4 * i + t
                        for hf in range(2):
                            for k in range(4):
                                S.op("pe", ("matmul", dict(out=bank(6 + hf), lhsT=vT3[:, k, t * 128:(t + 1) * 128], rhs=wo3[:, k, hf * 512:(hf + 1) * 512], start=(k == 0), stop=(k == 3))),
                                     reads=[BvT, BwC], writes=[PB[6 + hf]])
                        o2 = oi % 2
                        oi += 1
                        S.dma("sp", ("dma_start", dict(out=xt2[o2], in_=xr2_d[ti * 128:(ti + 1) * 128, :])), reads=[Bsrc], writes=[Bxt2[o2]])
                        S.op("dve", ("tensor_tensor", dict(out=yt[o2], in0=psum_t[:, 6 * 512:8 * 512], in1=gate_bc[b], op=ALU.mult)), reads=[PB[6], PB[7], BwC], writes=[Byt[o2]])
                        S.op("dve", ("tensor_tensor", dict(out=yt[o2], in0=yt[o2], in1=xt2[o2], op=ALU.add)), reads=[Byt[o2], Bxt2[o2]], writes=[Byt[o2]])
                        S.dma("sp", ("dma_start", dict(out=xr_d[ti * 128:(ti + 1) * 128, :], in_=yt[o2])), reads=[Byt[o2]], writes=[Bxr])
            return Bxr

        if stop_after >= 3:
            Bxr_b = s5_mixer(Bxr2)
            S.barrier()
            A.off = persist_off
            if "xr3" in dbg_d:
                S.dma("sp", ("dma_start", dict(out=dbg_d["xr3"], in_=xr_d[0:T_LAT, :])), reads=[Bxr_b])
        if stop_after >= 4:
            Bout = moe(1, 32, Bxr_b)

        S.barrier()
        with nc.Block() as block:
            S.emit(block)
    return nc


def _rope_tables():
    inv = np.power(10000.0, -np.arange(0, 32, 2, dtype=np.float32) / 32).astype(np.float32)
    t = np.arange(S_LAT)
    row = (t // 64).astype(np.float32)
    colp = (t % 64).astype(np.float32)
    ang_r = row[:, None] * inv[None, :]
    ang_c = colp[:, None] * inv[None, :]
    cos64 = np.ones((64, SEQ), np.float32)
    sin64 = np.zeros((64, SEQ), np.float32)
    for d in range(64):
        ang = ang_r if d < 32 else ang_c
        i = d % 16
        sgn = -1.0 if (d % 32) < 16 else 1.0
        cos64[d, C_CTX:] = np.cos(ang[:, i])
        sin64[d, C_CTX:] = sgn * np.sin(ang[:, i])
    return np.concatenate([np.concatenate([cos64, cos64], 0), np.concatenate([sin64, sin64], 0)], 1).astype(np.float32)


_PERM64 = np.array([(d // 32) * 32 + ((d % 32) + 16) % 32 for d in range(64)])


def _consts():
    c = np.zeros((128, NCONST), np.float32)
    c[:, 0:128] = np.eye(128)
    c[:, 128:256] = np.triu(np.ones((128, 128)), 1)
    c[:, 256:384] = 1.0
    c[0:64, 384:448] = 1.0 / 64
    c[64:128, 448:512] = 1.0 / 64
    c[:, 512:544] = (np.arange(32) * CAP)[None, :]
    return c


def make_in_maps(inp, cores):
    f = lambda a: np.ascontiguousarray(np.asarray(a, dtype=np.float32))
    shared = {
        "consts": _consts(), "rope": _rope_tables(),
        "w_mod": f(inp["w_mod"]), "b_mod": f(inp["b_mod"]).reshape(2, 1, 6 * D),
        "norm_mix_g": f(inp["norm_mix_g"]).reshape(2, 1, D), "norm_ffn_g": f(inp["norm_ffn_g"]).reshape(2, 1, D),
        "mix_w_in": f(inp["mix_w_in"][0]), "mix_w_out": f(inp["mix_w_out"][0]),
        "gmlp_norm_g": f(inp["gmlp_norm_g"]).reshape(1, 512), "gmlp_w_spatial": f(inp["gmlp_w_spatial"][0]),
        "gmlp_b_spatial": f(inp["gmlp_b_spatial"][0]).reshape(1, 1024),
        "moe_w_group": f(inp["moe_w_group"]), "moe_b_group": f(inp["moe_b_group"]).reshape(2, 1, 4),
        "moe_w_router": f(inp["moe_w_router"]), "moe_b_router": f(inp["moe_b_router"]).reshape(2, 1, 32),
        "moe_w1": f(inp["moe_w1"]), "moe_w3": f(inp["moe_w3"]), "moe_w2": f(inp["moe_w2"]),
        "final_norm_g": f(inp["final_norm_g"]).reshape(1, D),
        "s5_w_in": f(inp["s5_w_in"][0]), "s5_w_glu": f(inp["s5_w_glu"][0]), "s5_w_out": f(inp["s5_w_out"][0]),
    }
    qg = f(inp["q_norm_g"][0]); kg = f(inp["k_norm_g"][0])
    idx = np.arange(128) % 64
    shared["qkg"] = np.stack([qg[idx], qg[_PERM64[idx]], kg[idx], kg[_PERM64[idx]]], 1).astype(np.float32)
    a_re = f(inp["s5_a_re"][0]); a_im = f(inp["s5_a_im"][0]); ldt = f(inp["s5_log_dt"][0])
    par = np.zeros((128, 2, 16, 3), np.float32)
    for d in range(2):
        for pr in range(16):
            for gl in range(2):
                g = 2 * pr + gl
                par[gl * 64:(gl + 1) * 64, d, pr, 0] = a_re[d, g]
                par[gl * 64:(gl + 1) * 64, d, pr, 1] = a_im[d, g]
                par[gl * 64:(gl + 1) * 64, d, pr, 2] = ldt[d, g]
    shared["s5_par"] = par.reshape(128, 96)
    b_re = f(inp["s5_b_re"][0]); b_im = f(inp["s5_b_im"][0]); c_re = f(inp["s5_c_re"][0]); c_im = f(inp["s5_c_im"][0])
    bT = np.zeros((2, 2, 16, 128, 128), np.float32)
    cc = np.zeros((2, 2, 16, 128, 128), np.float32)
    for d in range(2):
        for pr in range(16):
            for gl in range(2):
                g = 2 * pr + gl
                gic = g % 8
                for ri, (bsrc, csrc) in enumerate(((b_re, c_re), (b_im, c_im))):
                    bT[d, ri, pr, gic * 16:(gic + 1) * 16, gl * 64:(gl + 1) * 64] = bsrc[d, g].T
                    cc[d, ri, pr, gl * 64:(gl + 1) * 64, gic * 16:(gic + 1) * 16] = csrc[d, g].T
    shared["s5_bT"] = bT
    shared["s5_iota"] = np.tile(np.arange(SEQ, dtype=np.float32)[None, :], (128, 1))
    shared["s5_c"] = cc
    shared["s5_d"] = f(inp["s5_d"][0]).reshape(4, 128).T.copy()
    shared["s5_b_glu"] = f(inp["s5_b_glu"][0]).reshape(4, 128).T.copy()
    maps = []
    x = np.asarray(inp["x"]); ctx = np.asarray(inp["ctx"]); c = np.asarray(inp["c"]); cc_ = np.asarray(inp["c_ctx"])
    for core in cores:
        m = dict(shared)
        m["x"] = f(x[2 * core:2 * core + 2]).reshape(T_LAT, D)
        m["ctx"] = f(ctx[2 * core:2 * core + 2]).reshape(T_CTX, D)
        cvec = np.stack([c[2 * core], c[2 * core + 1], cc_], 0).astype(np.float32)
        m["cT"] = np.ascontiguousarray(cvec.reshape(3, 8, 128).transpose(2, 1, 0).reshape(128, 24))
        maps.append(m)
    return maps


_NC_CACHE = {}


def kernel(**inputs):
    if "nc" not in _NC_CACHE:
        _NC_CACHE["nc"] = build_program()
    nc = _NC_CACHE["nc"]
    cores = list(range(8))
    maps = make_in_maps(inputs, cores)
    res = run_bass_kernel_spmd(nc, maps, core_ids=cores)
    outs = [np.asarray(r["out"]).reshape(NB, S_LAT, D) for r in res.results]
    return np.concatenate(outs, 0).astype(np.float32)
```

```python
import numpy as np
from contextlib import ExitStack
import concourse.bass as bass
import concourse.mybir as mybir
from concourse.alu_op_type import AluOpType as ALU
from concourse.bass_utils import run_bass_kernel_spmd

F32 = mybir.dt.float32
BF16 = mybir.dt.bfloat16
I32 = mybir.dt.int32
AF = mybir.ActivationFunctionType
AX = mybir.AxisListType

D = 1024
S_LAT = 2048
C_CTX = 256
NB = 2
T_LAT = NB * S_LAT
T_CTX = NB * C_CTX
SEQ = C_CTX + S_LAT
EPS = 1e-6
CAP = 640
NSLOT = 32 * CAP
TRASH = NSLOT
NCONST = 128 * 4 + 32
S5_DEBUG_STAGE = 9


class Buf:
    __slots__ = ("w", "r")

    def __init__(self):
        self.w = None
        self.r = []


class Sched:
    COMPUTE = ("pe", "act", "dve", "pool")
    NDMA = 8

    def __init__(self, nc, es):
        self.nc = nc
        self.streams = {e: [] for e in ("pe", "act", "dve", "pool", "sp")}
        self.sem = {}
        self.cnt = {}
        for e in self.COMPUTE:
            self.sem[e] = es.enter_context(nc.semaphore("s_" + e))
            self.cnt[e] = 0
        self.drr = {}
        for q in ("sp", "act", "pool"):
            for k in range(self.NDMA):
                key = "d_%s%d" % (q, k)
                self.sem[key] = es.enter_context(nc.semaphore(key))
                self.cnt[key] = 0
            self.drr[q] = 0
        self.waited = {e: {} for e in self.streams}
        self.nops = 0

    def _deps(self, reads, writes):
        deps = []
        for b in reads:
            if b.w is not None:
                deps.append(b.w)
        for b in writes:
            if b.w is not None:
                deps.append(b.w)
            deps.extend(b.r)
        return deps

    def _emit_waits(self, eng, deps, skip_self=None):
        best = {}
        for (k, v) in deps:
            if k == skip_self:
                continue
            if v > best.get(k, 0):
                best[k] = v
        w = self.waited[eng]
        for k, v in best.items():
            if w.get(k, 0) < v:
                w[k] = v
                self.streams[eng].append(("wait", k, v))

    def _mark(self, tok, reads, writes):
        for b in writes:
            b.w = tok
            b.r = []
        for b in reads:
            if b.w is tok:
                continue
            b.r.append(tok)
            if len(b.r) > 16:
                best = {}
                for (k, v) in b.r:
                    if v > best.get(k, 0):
                        best[k] = v
                b.r = list(best.items())

    def op(self, eng, fn, reads=(), writes=()):
        deps = self._deps(reads, writes)
        self._emit_waits(eng, deps, skip_self=("pe" if eng == "pe" else None))
        self.cnt[eng] += 1
        tok = (eng, self.cnt[eng])
        self.streams[eng].append(("op", fn, eng, 1))
        self._mark(tok, reads, writes)
        self.nops += 1
        return tok

    def dma(self, q, fn, reads=(), writes=()):
        k = self.drr[q]
        self.drr[q] = (k + 1) % self.NDMA
        key = "d_%s%d" % (q, k)
        deps = self._deps(reads, writes)
        if self.cnt[key] > 0:
            deps.append((key, self.cnt[key]))
        self._emit_waits(q, deps)
        self.cnt[key] += 16
        tok = (key, self.cnt[key])
        self.streams[q].append(("op", fn, key, 16))
        self._mark(tok, reads, writes)
        self.nops += 1
        return tok

    def barrier(self):
        toks = [(k, v) for k, v in self.cnt.items() if v > 0]
        for e in self.streams:
            self._emit_waits(e, toks)

    def emit(self, block):
        sem = self.sem
        streams = self.streams

        def run(e, lst):
            for it in lst:
                if it[0] == "wait":
                    e.wait_ge(sem[it[1]], it[2])
                else:
                    getattr(e, it[1][0])(**it[1][1]).then_inc(sem[it[2]], it[3])

        @block.sync
        def _(e):
            run(e, streams["sp"])

        @block.scalar
        def _(e):
            run(e, streams["act"])

        @block.vector
        def _(e):
            run(e, streams["dve"])

        @block.gpsimd
        def _(e):
            run(e, streams["pool"])

        @block.tensor
        def _(e):
            run(e, streams["pe"])


class Arena:
    def __init__(self, t, size):
        self.t = t
        self.size = size
        self.off = 0

    def f32(self, n):
        a = self.t[:, self.off:self.off + n]
        self.off += n
        assert self.off <= self.size, ("arena overflow", self.off, self.size)
        return a

    def bf16(self, n):
        return self.f32((n + 1) // 2).bitcast(BF16)[:, 0:n]

    def i32(self, n):
        return self.f32(n).bitcast(I32)


def r3(ap, a):
    return ap.rearrange("p (a b) -> p a b", a=a)


def build_program(stop_after=99, dbg=()):
    nc = bass.Bass("TRN2", target_bir_lowering=False)
    dt = nc.dram_tensor

    def din(name, shape, dtype=F32):
        return dt(name, list(shape), dtype, kind="ExternalInput").ap()

    x_d = din("x", [T_LAT, D])
    ctx_d = din("ctx", [T_CTX, D])
    cT_d = din("cT", [128, 24])
    consts_d = din("consts", [128, NCONST])
    rope_d = din("rope", [128, 2 * SEQ])
    qkg_d = din("qkg", [128, 4])
    w_mod_d = din("w_mod", [2, D, 6 * D])
    b_mod_d = din("b_mod", [2, 1, 6 * D])
    gmix_d = din("norm_mix_g", [2, 1, D])
    gffn_d = din("norm_ffn_g", [2, 1, D])
    w_in_d = din("mix_w_in", [D, 1792])
    w_out_d = din("mix_w_out", [D, D])
    lng_d = din("gmlp_norm_g", [1, 512])
    wsp_d = din("gmlp_w_spatial", [8, 128, 128])
    bsp_d = din("gmlp_b_spatial", [1, 1024])
    wgrp_d = din("moe_w_group", [2, D, 4])
    bgrp_d = din("moe_b_group", [2, 1, 4])
    wrt_d = din("moe_w_router", [2, D, 32])
    brt_d = din("moe_b_router", [2, 1, 32])
    w1_d = din("moe_w1", [2, 32, D, 512])
    w3_d = din("moe_w3", [2, 32, D, 512])
    w2_d = din("moe_w2", [2, 32, 512, D])
    fng_d = din("final_norm_g", [1, D])
    s5win_d = din("s5_w_in", [D, 512])
    s5par_d = din("s5_par", [128, 2 * 16 * 3])
    s5iota_d = din("s5_iota", [128, SEQ])
    s5bT_d = din("s5_bT", [2, 2, 16, 128, 128])
    s5c_d = din("s5_c", [2, 2, 16, 128, 128])
    s5d_d = din("s5_d", [128, 4])
    s5glu_d = din("s5_w_glu", [512, 512])
    s5bglu_d = din("s5_b_glu", [128, 4])
    s5wout_d = din("s5_w_out", [512, D])

    out_d = dt("out", [T_LAT, D], F32, kind="ExternalOutput").ap()
    modrows_d = dt("modrows", [2, 6, 3, D], F32, kind="Internal").ap()
    xr_d = dt("xr", [T_LAT + T_CTX, D], F32, kind="Internal").ap()
    xr2_d = dt("xr2", [T_LAT + T_CTX, D], F32, kind="Internal").ap()
    xall_d = dt("xall", [NSLOT + 128, D], BF16, kind="Internal").ap()
    yall_d = dt("yall", [NSLOT + 128, D], F32, kind="Internal").ap()
    dbg_d = {}
    for name, shape in dbg:
        dbg_d[name] = dt("dbg_" + name, list(shape), F32, kind="ExternalOutput").ap()

    with ExitStack() as es:
        S = Sched(nc, es)
        ARENA_WORDS = 53100
        arena_t = es.enter_context(nc.sbuf_tensor("arena", [128, ARENA_WORDS], F32))
        psum_t = es.enter_context(nc.psum_tensor("psum", [128, 4096], F32))
        A = Arena(arena_t, ARENA_WORDS)

        def bank(i, n=512, off=0):
            return psum_t[:, i * 512 + off:i * 512 + off + n]

        def bank_bf(i):
            return psum_t[:, i * 512:(i + 1) * 512].bitcast(BF16)

        PB = [Buf() for _ in range(8)]

        cst = A.f32(NCONST)
        Bc = Buf()
        S.dma("sp", ("dma_start", dict(out=cst, in_=consts_d)), writes=[Bc])
        ident_f = cst[:, 0:128]
        utri_f = cst[:, 128:256]
        ones_f = cst[:, 256:384]
        blk_f = cst[:, 384:512]
        iotaC = cst[:, 512:544]
        cbf = A.bf16(512)
        S.op("dve", ("tensor_copy", dict(out=cbf, in_=cst[:, 0:512])), reads=[Bc], writes=[Bc])
        ident_b = cbf[:, 0:128]
        utri_b = cbf[:, 128:256]
        ones_b = cbf[:, 256:384]
        blk_b = cbf[:, 384:512]
        modT = A.f32(2 * 2 * 8 * 3)
        BmodT = Buf()
        persist_off = A.off

        cT = A.f32(24)
        sc = A.f32(24)
        Bsc = Buf()
        S.dma("sp", ("dma_start", dict(out=cT, in_=cT_d)), writes=[Bsc])
        S.op("act", ("activation", dict(out=sc, in_=cT, func=AF.Silu)), reads=[Bsc], writes=[Bsc])
        sc3 = r3(sc, 8)
        wblk = [A.f32(8 * 512) for _ in range(2)]
        Bwblk = [Buf(), Buf()]
        mrow = A.f32(6 * D)
        gb = A.f32(2 * D)
        bb = A.f32(6 * D)
        Bmrow, Bgb, Bbb = Buf(), Buf(), Buf()
        Bmodrows = Buf()
        for l in range(2):
            S.dma("sp", ("dma_start", dict(out=bb[0:3, :], in_=b_mod_d[l].broadcast_to([3, 6 * D]))), writes=[Bbb])
            S.dma("sp", ("dma_start", dict(out=gb[0:3, 0:D], in_=gmix_d[l].broadcast_to([3, D]))), writes=[Bgb])
            S.dma("sp", ("dma_start", dict(out=gb[0:3, D:2 * D], in_=gffn_d[l].broadcast_to([3, D]))), writes=[Bgb])
            for nb in range(12):
                wb = wblk[nb % 2]
                Bw = Bwblk[nb % 2]
                S.dma("sp", ("dma_start", dict(
                    out=r3(wb, 8), in_=w_mod_d[l][:, nb * 512:(nb + 1) * 512].rearrange("(k p) n -> p k n", p=128))), writes=[Bw])
                pb = nb % 2
                for k in range(8):
                    S.op("pe", ("matmul", dict(out=bank(pb)[0:3, :], lhsT=sc3[:, k, :], rhs=r3(wb, 8)[:, k, :],
                                                                       start=(k == 0), stop=(k == 7))), reads=[Bsc, Bw], writes=[PB[pb]])
                S.op("dve", ("tensor_tensor", dict(out=mrow[0:3, nb * 512:(nb + 1) * 512], in0=bank(pb)[0:3, :],
                                                                      in1=bb[0:3, nb * 512:(nb + 1) * 512], op=ALU.add)),
                     reads=[PB[pb], Bbb], writes=[Bmrow])
            for (kind, goff) in ((1, 0), (4, D)):
                S.op("dve", ("scalar_tensor_tensor", dict(
                    out=mrow[0:3, kind * D:(kind + 1) * D], in0=mrow[0:3, kind * D:(kind + 1) * D], scalar=1.0,
                    in1=gb[0:3, goff:goff + D], op0=ALU.add, op1=ALU.mult)), reads=[Bmrow, Bgb], writes=[Bmrow])
            S.dma("sp", ("dma_start", dict(out=modrows_d[l].rearrange("k c d -> c k d"), in_=r3(mrow[0:3, :], 6))),
                  reads=[Bmrow], writes=[Bmodrows])
        modT5 = modT.rearrange("p (l m k c) -> p l m k c", l=2, m=2, k=8)
        for l in range(2):
            for m in range(2):
                for c in range(3):
                    S.dma("sp", ("dma_start", dict(
                        out=modT5[:, l, m, :, c], in_=modrows_d[l, m, c].rearrange("(k p) -> p k", p=128),
                        allow_slow_non_contiguous=True)), reads=[Bmodrows], writes=[BmodT])
        S.barrier()
        A.off = persist_off
        if "modrows" in dbg_d:
            S.dma("sp", ("dma_start", dict(out=dbg_d["modrows"], in_=modrows_d.rearrange("l k c d -> (l k c) d"))), reads=[Bmodrows])

        def tile_src(layer, ti):
            if layer == 0:
                if ti < 32:
                    return x_d[ti * 128:(ti + 1) * 128, :]
                return ctx_d[(ti - 32) * 128:(ti - 31) * 128, :]
            return xr2_d[ti * 128:(ti + 1) * 128, :]

        def tile_col(ti):
            if ti < 32:
                return ti // 16
            return 2

        class NormT:
            def __init__(self):
                self.xt = [A.f32(D) for _ in range(2)]
                self.Bxt = [Buf(), Buf()]
                self.junk = A.bf16(D)
                self.Bjunk = Buf()
                self.xn = [A.bf16(D) for _ in range(2)]
                self.Bxn = [Buf(), Buf()]
                self.ss = [A.f32(1) for _ in range(2)]
                self.Bss = [Buf(), Buf()]
                self.i = 0

            def run(self, layer, ti, hT3, BhT, c0, psb):
                i = self.i
                self.i += 1
                xt, Bxt = self.xt[i % 2], self.Bxt[i % 2]
                xn, Bxn = self.xn[i % 2], self.Bxn[i % 2]
                ss, Bss = self.ss[i % 2], self.Bss[i % 2]
                junk, Bjunk = self.junk, self.Bjunk
                src = tile_src(layer, ti)
                col = tile_col(ti)
                S.dma("sp", ("dma_start", dict(out=xt, in_=src)), writes=[Bxt])
                S.op("act", ("activation", dict(out=junk, in_=xt, func=AF.Square, accum_out=ss)), reads=[Bxt], writes=[Bjunk, Bss])
                S.op("act", ("activation", dict(out=ss, in_=ss, func=AF.Sqrt, scale=1.0 / D, bias=EPS)), reads=[Bss], writes=[Bss])
                S.op("dve", ("reciprocal", dict(out=ss, in_=ss)), reads=[Bss], writes=[Bss])
                S.op("dve", ("tensor_scalar", dict(out=xn, in0=xt, scalar1=ss, scalar2=None, op0=ALU.mult)), reads=[Bxt, Bss], writes=[Bxn])
                pT = r3(bank_bf(psb), 8)
                for k in range(8):
                    S.op("pe", ("transpose", dict(out=pT[:, k, :], in_=xn[:, k * 128:(k + 1) * 128], identity=ident_b)),
                         reads=[Bxn, Bc], writes=[PB[psb]])
                for k in range(8):
                    eng = "act" if k % 2 == 0 else "dve"
                    if eng == "act":
                        S.op("act", ("activation", dict(out=hT3[:, k, c0:c0 + 128], in_=pT[:, k, :], func=AF.Identity,
                                                                  scale=modT5[:, layer, 1, k, col:col + 1], bias=modT5[:, layer, 0, k, col:col + 1])),
                             reads=[PB[psb], BmodT], writes=[BhT])
                    else:
                        S.op("dve", ("tensor_scalar", dict(out=hT3[:, k, c0:c0 + 128], in0=pT[:, k, :],
                                                                     scalar1=modT5[:, layer, 1, k, col:col + 1], scalar2=modT5[:, layer, 0, k, col:col + 1],
                                                                     op0=ALU.mult, op1=ALU.add)),
                             reads=[PB[psb], BmodT], writes=[BhT])

        def phase1():
            L = 0
            GELU = AF.Gelu_apprx_tanh
            WC = 1280
            w_in = A.bf16(8 * WC)
            w_in3 = r3(w_in, 8)
            Bwin = Buf()
            for k in range(8):
                S.dma("pool", ("dma_start", dict(out=w_in3[:, k, :], in_=w_in_d[k * 128:(k + 1) * 128, 512:1792])), writes=[Bwin])
            wst = A.bf16(8 * 512)
            wst5 = wst.rearrange("p (k j two d) -> p k j two d", k=8, j=4, two=2)
            for k in range(8):
                for two in range(2):
                    S.dma("pool", ("dma_start", dict(out=wst5[:, k, :, two, :],
                                                     in_=w_in_d[k * 128:(k + 1) * 128, two * 256:(two + 1) * 256].rearrange("p (j d) -> p j d", j=4))), writes=[Bwin])
            wst3 = r3(wst, 8)
            wpst = A.bf16(8 * 512)
            wpst3 = r3(wpst, 8)
            wkp = A.bf16(8 * 128)
            wkp3 = r3(wkp, 8)
            Bwperm = Buf()
            sv = wst.rearrange("p (k x b i) -> p k x b i", k=8, b=2, i=16)
            dv = wpst.rearrange("p (k x b i) -> p k x b i", k=8, b=2, i=16)
            svk = w_in3[:, :, 0:128].rearrange("p k (x b i) -> p k x b i", b=2, i=16)
            dvk = wkp3.rearrange("p k (x b i) -> p k x b i", b=2, i=16)
            for b_ in range(2):
                S.op("dve", ("tensor_copy", dict(out=dv[:, :, :, b_, :], in_=sv[:, :, :, 1 - b_, :])), reads=[Bwin], writes=[Bwperm])
                S.op("dve", ("tensor_copy", dict(out=dvk[:, :, :, b_, :], in_=svk[:, :, :, 1 - b_, :])), reads=[Bwin], writes=[Bwperm])
            wout = A.bf16(16 * 1024)
            wout3 = r3(wout, 16)
            Bwout = Buf()
            for c4 in range(4):
                S.dma("pool", ("dma_start", dict(out=wout3[0:64, c4 * 4:(c4 + 1) * 4, :],
                                                            in_=w_out_d[c4 * 256:(c4 + 1) * 256, :].rearrange("(c r) n -> r c n", r=64))), writes=[Bwout])
            yt = A.f32(D)
            wspn = yt
            Bwspn = Buf()
            S.dma("sp", ("dma_start", dict(out=r3(wspn, 8), in_=wsp_d.rearrange("g p q -> p g q"))), writes=[Bwspn])
            wspT = A.bf16(1024)
            wspT3 = r3(wspT, 8)
            BwspT = Buf()
            pw = r3(psum_t[:, 0:1024], 8)
            for g in range(8):
                S.op("pe", ("transpose", dict(out=pw[:, g, :], in_=r3(wspn, 8)[:, g, :], identity=ident_f)),
                     reads=[Bwspn, Bc], writes=[PB[0], PB[1]])
            S.op("act", ("copy", dict(out=wspT, in_=psum_t[:, 0:1024])), reads=[PB[0], PB[1]], writes=[BwspT])
            lng_bc = A.f32(512)
            bsp_bc = A.f32(1024)
            qkg = A.f32(4)
            cosT = A.f32(SEQ)
            sinT = A.f32(SEQ)
            gate_bc = [A.f32(D) for _ in range(3)]
            Bsm = Buf()
            S.dma("sp", ("dma_start", dict(out=lng_bc, in_=lng_d.broadcast_to([128, 512]))), writes=[Bsm])
            S.dma("sp", ("dma_start", dict(out=bsp_bc[0:64, :], in_=bsp_d.broadcast_to([64, 1024]))), writes=[Bsm])
            S.dma("sp", ("dma_start", dict(out=qkg, in_=qkg_d)), writes=[Bsm])
            S.dma("sp", ("dma_start", dict(out=cosT, in_=rope_d[:, 0:SEQ])), writes=[Bsm])
            S.dma("sp", ("dma_start", dict(out=sinT, in_=rope_d[:, SEQ:2 * SEQ])), writes=[Bsm])
            for c in range(3):
                S.dma("sp", ("dma_start", dict(out=gate_bc[c], in_=modrows_d[L, 2, c:c + 1, :].broadcast_to([128, D]))),
                      reads=[Bmodrows], writes=[Bsm])
            bsp3 = r3(bsp_bc[0:64, :], 8)

            QT = A.bf16(4 * SEQ)
            QT3 = r3(QT, 4)
            KT = A.bf16(SEQ)
            Vt = A.bf16(18 * 128)
            V3 = r3(Vt, 18)
            BQ, BK, BV = Buf(), Buf(), Buf()
            hT1 = A.bf16(8 * 512)
            hT = [hT1, hT1]
            BhT1 = Buf()
            BhT = [BhT1, BhT1]
            NT = NormT()
            sqb = A.bf16(512)
            rs = A.f32(512)
            t1 = A.f32(512)
            t2 = A.f32(512)
            Bsq, Brs, Bt1, Bt2 = Buf(), Buf(), Buf(), Buf()
            mx = A.f32(1024)
            Bmx = Buf()
            rec = A.f32(512)
            Brec = Buf()
            sqb2 = [sqb, A.bf16(512)]
            rs2 = [rs, rec]
            t12 = [t1, mx[:, 0:512]]
            t22 = [t2, mx[:, 512:1024]]
            Bsq2, Brs2, Bt12, Bt22 = [Bsq, Buf()], [Brs, Brec], [Bt1, Bmx], [Bt2, Bmx]
            UG = A.bf16(8 * 512)
            UG3 = r3(UG, 8)
            GT3 = UG3
            OT = A.bf16(8 * 512)
            OT3 = r3(OT, 8)
            BUG = Buf()
            BGT = BUG
            BOT = Buf()
            gv = t1
            vnf = t2
            vnb = A.bf16(512)
            st6 = A.f32(6)
            mv = A.f32(2)
            Bgv, Bvnf = Bt1, Bt2
            Bvnb, Bst, Bmv = Buf(), Buf(), Buf()
            PT = [A.bf16(512) for _ in range(4)]
            BPT = [Buf(), Buf(), Buf(), Buf()]
            rec = A.f32(512)
            Brec = Buf()
            xt2 = A.f32(D)
            Byt, Bxt2 = Buf(), Buf()
            Bxr = Buf()
            print('phase1 arena words', A.off)
            hcount = [0]

            def make_hT(blk):
                tiles, c0seq, N = blk
                i = hcount[0]
                hcount[0] += 1
                h3 = r3(hT[i % 2], 8)
                for t, ti in enumerate(tiles):
                    NT.run(L, ti, h3, BhT[i % 2], t * 128, t % 2)
                return h3, BhT[i % 2]

            for b in range(NB):
                blocks = [([32 + 2 * b, 33 + 2 * b], 0, 256)]
                for i in range(4):
                    blocks.append(([16 * b + 4 * i + t for t in range(4)], 256 + 512 * i, 512))
                for blk in blocks:
                    tiles, c0, N = blk
                    h3, Bh = make_hT(blk)
                    for j in range(5):
                        bq, bqp, bms = (2, 3, 4) if j % 2 == 0 else (5, 6, 7)
                        sqb_, rs_, t1_, t2_ = sqb2[j % 2], rs2[j % 2], t12[j % 2], t22[j % 2]
                        Bsq_, Brs_, Bt1_, Bt2_ = Bsq2[j % 2], Brs2[j % 2], Bt12[j % 2], Bt22[j % 2]
                        for (pb, wq, wk) in ((bq, wst3, w_in3), (bqp, wpst3, wkp3)):
                            for k in range(8):
                                lw = wq[:, k, j * 128:(j + 1) * 128] if j < 4 else wk[:, k, 0:128]
                                S.op("pe", ("matmul", dict(out=bank(pb)[:, 0:N], lhsT=lw, rhs=h3[:, k, 0:N], start=(k == 0), stop=(k == 7))),
                                     reads=[Bwin, Bwperm, Bh], writes=[PB[pb]])
                        gi = 0 if j < 4 else 2
                        S.op("act", ("activation", dict(out=sqb_[:, 0:N], in_=bank(bq)[:, 0:N], func=AF.Square)), reads=[PB[bq]], writes=[Bsq_])
                        S.op("pe", ("matmul", dict(out=bank(bms)[:, 0:N], lhsT=blk_b, rhs=sqb_[:, 0:N], start=True, stop=True)), reads=[Bsq_, Bc], writes=[PB[bms]])
                        S.op("act", ("activation", dict(out=rs_[:, 0:N], in_=bank(bms)[:, 0:N], func=AF.Sqrt, bias=EPS, scale=1.0)), reads=[PB[bms]], writes=[Brs_])
                        S.op("dve", ("reciprocal", dict(out=rs_[:, 0:N], in_=rs_[:, 0:N])), reads=[Brs_], writes=[Brs_])
                        S.op("dve", ("scalar_tensor_tensor", dict(out=t1_[:, 0:N], in0=bank(bq)[:, 0:N], scalar=qkg[:, gi:gi + 1], in1=cosT[:, c0:c0 + N],
                                                                             op0=ALU.mult, op1=ALU.mult)), reads=[PB[bq], Bsm], writes=[Bt1_])
                        S.op("dve", ("scalar_tensor_tensor", dict(out=t2_[:, 0:N], in0=bank(bqp)[:, 0:N], scalar=qkg[:, gi + 1:gi + 2], in1=sinT[:, c0:c0 + N],
                                                                             op0=ALU.mult, op1=ALU.mult)), reads=[PB[bqp], Bsm], writes=[Bt2_])
                        S.op("dve", ("tensor_tensor", dict(out=t1_[:, 0:N], in0=t1_[:, 0:N], in1=t2_[:, 0:N], op=ALU.add)), reads=[Bt1_, Bt2_], writes=[Bt1_])
                        if j < 4:
                            dst, Bd = QT3[:, j, c0:c0 + N], BQ
                        else:
                            dst, Bd = KT[:, c0:c0 + N], BK
                        S.op("dve", ("tensor_tensor", dict(out=dst, in0=t1_[:, 0:N], in1=rs_[:, 0:N], op=ALU.mult)), reads=[Bt1_, Brs_], writes=[Bd])
                    for t in range(len(tiles)):
                        kt = c0 // 128 + t
                        for k in range(8):
                            S.op("pe", ("matmul", dict(out=bank(5)[:, 0:128], lhsT=h3[:, k, t * 128:(t + 1) * 128], rhs=w_in3[:, k, 128:256],
                                                                      start=(k == 0), stop=(k == 7))), reads=[Bwin, Bh], writes=[PB[5]])
                        S.op("act", ("copy", dict(out=V3[:, kt, :], in_=bank(5)[:, 0:128])), reads=[PB[5]], writes=[BV])
                for bi, blk in enumerate(blocks):
                    tiles, c0, N = blk
                    h3, Bh = make_hT(blk)
                    for g in range(8):
                        pb = 2 + g % 2
                        for k in range(8):
                            S.op("pe", ("matmul", dict(out=bank(pb)[0:64, 0:N], lhsT=w_in3[:, k, 256 + g * 64:320 + g * 64], rhs=h3[:, k, 0:N],
                                                                             start=(k == 0), stop=(k == 7))), reads=[Bwin, Bh], writes=[PB[pb]])
                        S.op("act", ("activation", dict(out=UG3[0:64, g, 0:N], in_=bank(pb)[0:64, 0:N], func=GELU)), reads=[PB[pb]], writes=[BUG])
                    for t in range(len(tiles)):
                        tc0 = t * 128
                        for k in range(8):
                            S.op("pe", ("matmul", dict(out=bank(4), lhsT=h3[:, k, tc0:tc0 + 128], rhs=w_in3[:, k, 768:1280],
                                                                          start=(k == 0), stop=(k == 7))), reads=[Bwin, Bh], writes=[PB[4]])
                        S.op("act", ("activation", dict(out=gv, in_=bank(4), func=GELU)), reads=[PB[4]], writes=[Bgv])
                        S.op("dve", ("bn_stats", dict(out=st6, in_=gv)), reads=[Bgv], writes=[Bst])
                        S.op("dve", ("bn_aggr", dict(out=mv, in_=st6)), reads=[Bst], writes=[Bmv])
                        S.op("act", ("activation", dict(out=mv[:, 1:2], in_=mv[:, 1:2], func=AF.Sqrt, bias=EPS, scale=1.0)), reads=[Bmv], writes=[Bmv])
                        S.op("dve", ("reciprocal", dict(out=mv[:, 1:2], in_=mv[:, 1:2])), reads=[Bmv], writes=[Bmv])
                        S.op("dve", ("tensor_scalar", dict(out=vnf, in0=gv, scalar1=mv[:, 0:1], scalar2=mv[:, 1:2], op0=ALU.subtract, op1=ALU.mult)),
                             reads=[Bgv, Bmv], writes=[Bvnf])
                        S.op("dve", ("tensor_tensor", dict(out=vnb, in0=vnf, in1=lng_bc, op=ALU.mult)), reads=[Bvnf, Bsm], writes=[Bvnb])
                        pm = r3(psum_t[0:64, 6 * 512:8 * 512], 8)
                        for g in range(8):
                            S.op("pe", ("matmul", dict(out=pm[:, g, :], lhsT=vnb[:, g * 64:(g + 1) * 64], rhs=wspT3[:, g, :], start=True, stop=True)),
                                 reads=[Bvnb, BwspT], writes=[PB[6], PB[7]])
                        S.op("dve", ("tensor_tensor", dict(out=r3(mx[0:64, :], 8), in0=pm, in1=bsp3, op=ALU.add)), reads=[PB[6], PB[7], Bsm], writes=[Bmx])
                        S.op("dve", ("tensor_tensor", dict(out=GT3[0:64, :, tc0:tc0 + 128], in0=r3(mx[0:64, :], 8), in1=UG3[0:64, :, tc0:tc0 + 128], op=ALU.mult)),
                             reads=[Bmx, BUG], writes=[BGT])
                    kts = list(range(2)) if bi == 0 else list(range(18))
                    steps = [(h, ki, kt) for h in range(8) for ki, kt in enumerate(kts)]
                    nk = len(kts)

                    def issue_S(idx):
                        h, ki, kt = steps[idx]
                        half, j = h // 4, h % 4
                        p0 = half * 64
                        sb_ = (0, 1, 6)[idx % 3]
                        S.op("pe", ("matmul", dict(out=bank(sb_)[:, 0:N], lhsT=KT[p0:p0 + 64, kt * 128:(kt + 1) * 128],
                                                   rhs=QT3[p0:p0 + 64, j, c0:c0 + N], start=True, stop=True)), reads=[BK, BQ], writes=[PB[sb_]])
                        pt, Bpt = PT[idx % 4], BPT[idx % 4]
                        S.op("act", ("activation", dict(out=pt[:, 0:N], in_=bank(sb_)[:, 0:N], func=AF.Exp, scale=0.125)), reads=[PB[sb_]], writes=[Bpt])

                    def issue_PV(idx):
                        h, ki, kt = steps[idx]
                        half = h // 4
                        p0 = half * 64
                        bo, bd = 2 + (h % 2) * 2, 3 + (h % 2) * 2
                        pt, Bpt = PT[idx % 4], BPT[idx % 4]
                        S.op("pe", ("matmul", dict(out=bank(bo)[0:64, 0:N], lhsT=V3[:, kt, p0:p0 + 64], rhs=pt[:, 0:N],
                                                   start=(ki == 0), stop=(ki == nk - 1))), reads=[BV, Bpt], writes=[PB[bo]])
                        S.op("pe", ("matmul", dict(out=bank(bd)[0:64, 0:N], lhsT=ones_b[:, 0:64], rhs=pt[:, 0:N],
                                                   start=(ki == 0), stop=(ki == nk - 1))), reads=[Bc, Bpt], writes=[PB[bd]])
                        if ki == nk - 1:
                            S.op("dve", ("reciprocal", dict(out=rec[0:64, 0:N], in_=bank(bd)[0:64, 0:N])), reads=[PB[bd]], writes=[Brec])
                            S.op("dve", ("tensor_tensor", dict(out=OT3[0:64, h, 0:N], in0=bank(bo)[0:64, 0:N], in1=rec[0:64, 0:N], op=ALU.mult)),
                                 reads=[PB[bo], Brec], writes=[BOT])

                    for idx in range(len(steps) + 2):
                        if idx < len(steps):
                            issue_S(idx)
                        if idx >= 2:
                            issue_PV(idx - 2)
                    for t, ti in enumerate(tiles):
                        tc0 = t * 128
                        col = tile_col(ti)
                        for hf in range(2):
                            for c in range(16):
                                lw = OT3[0:64, c, tc0:tc0 + 128] if c < 8 else GT3[0:64, c - 8, tc0:tc0 + 128]
                                S.op("pe", ("matmul", dict(out=bank(6 + hf), lhsT=lw, rhs=wout3[0:64, c, hf * 512:(hf + 1) * 512],
                                                                                 start=(c == 0), stop=(c == 15))), reads=[BOT, BGT, Bwout], writes=[PB[6 + hf]])
                        S.dma("sp", ("dma_start", dict(out=xt2, in_=tile_src(L, ti))), writes=[Bxt2])
                        S.op("dve", ("tensor_tensor", dict(out=yt, in0=psum_t[:, 6 * 512:8 * 512], in1=gate_bc[col], op=ALU.mult)),
                             reads=[PB[6], PB[7], Bsm], writes=[Byt])
                        S.op("dve", ("tensor_tensor", dict(out=yt, in0=yt, in1=xt2, op=ALU.add)), reads=[Byt, Bxt2], writes=[Byt])
                        S.dma("sp", ("dma_start", dict(out=xr_d[ti * 128:(ti + 1) * 128, :], in_=yt)), reads=[Byt], writes=[Bxr])
            return Bxr

        if stop_after >= 1:
            Bxr = phase1()
            S.barrier()
            A.off = persist_off
            if "xr" in dbg_d:
                S.dma("sp", ("dma_start", dict(out=dbg_d["xr"], in_=xr_d)), reads=[Bxr])
        def moe(L, ntiles, Bsrc):
            last = (L == 1)
            ncol = 2 if last else 3
            Abc = [A.f32(D) for _ in range(ncol)]
            Sbc = [A.f32(D) for _ in range(ncol)]
            Gbc = [A.f32(D) for _ in range(ncol)]
            Bbc = Buf()
            for c in range(ncol):
                for (kind, dst) in ((4, Abc), (3, Sbc), (5, Gbc)):
                    S.dma("sp", ("dma_start", dict(out=dst[c], in_=modrows_d[L, kind, c:c + 1, :].broadcast_to([128, D]))), reads=[Bmodrows], writes=[Bbc])
            fng = A.f32(D)
            if last:
                S.dma("sp", ("dma_start", dict(out=fng, in_=fng_d.broadcast_to([128, D]))), writes=[Bbc])
            w36 = A.f32(8 * 36)
            w36_3 = r3(w36, 8)
            b36 = A.f32(36)
            S.dma("sp", ("dma_start", dict(out=w36_3[:, :, 0:4], in_=wgrp_d[L].rearrange("(k p) n -> p k n", p=128), allow_slow_non_contiguous=True)), writes=[Bbc])
            S.dma("sp", ("dma_start", dict(out=w36_3[:, :, 4:36], in_=wrt_d[L].rearrange("(k p) n -> p k n", p=128), allow_slow_non_contiguous=True)), writes=[Bbc])
            S.dma("sp", ("dma_start", dict(out=b36[:, 0:4], in_=bgrp_d[L].broadcast_to([128, 4]))), writes=[Bbc])
            S.dma("sp", ("dma_start", dict(out=b36[:, 4:36], in_=brt_d[L].broadcast_to([128, 32]))), writes=[Bbc])
            slot_i = A.i32(ntiles * 2)
            slot_i3 = r3(slot_i, ntiles)
            wts = A.f32(ntiles * 2)
            wts3 = r3(wts, ntiles)
            Bslot = Buf()
            Bwts = Buf()
            run = A.f32(32)
            Brun = Buf()
            S.op("dve", ("memset", dict(ap=run, constant=0.0)), writes=[Brun])
            ztile = A.f32(D)
            Bz = Buf()
            Byall = Buf()
            Bxall = Buf()
            S.op("dve", ("memset", dict(ap=ztile, constant=0.0)), writes=[Bz])
            S.dma("sp", ("dma_start", dict(out=yall_d[NSLOT:NSLOT + 128, :], in_=ztile)), reads=[Bz], writes=[Byall])
            mark = A.off
            xt = [A.f32(D) for _ in range(2)]
            Bxt = [Buf(), Buf()]
            ff = [A.f32(D) for _ in range(2)]
            Bff = [Buf(), Buf()]
            fb = [A.bf16(D) for _ in range(2)]
            Bfb = [Buf(), Buf()]
            fTs = A.f32(D)
            BfTs = Buf()
            junk = A.bf16(D)
            Bjunk = Buf()
            sm = A.f32(256)
            Bsmall = Buf()
            ss = sm[:, 0:1]
            lg = sm[:, 4:40]
            gmax = sm[:, 40:41]
            ngmax = sm[:, 41:42]
            gsum = sm[:, 42:43]
            eg = sm[:, 44:48]
            gmask = sm[:, 48:52]
            pen = sm[:, 52:56]
            masked = sm[:, 56:88]
            m8 = sm[:, 88:96]
            ntop1 = sm[:, 96:97]
            e2 = sm[:, 97:98]
            wa = sm[:, 98:99]
            wb = sm[:, 99:100]
            slotf = sm[:, 100:102]
            vab = sm[:, 102:104]
            sel1 = sm[:, 104:136]
            sel = sm[:, 136:168]
            pos = sm[:, 168:200]
            valid = sm[:, 200:232]
            tmp32 = A.f32(32)
            selb = A.bf16(32)
            fTs2 = [fTs, A.f32(D)]
            BfTs2 = [BfTs, Buf()]
            ssA = [A.f32(1), A.f32(1)]
            BssA = [Buf(), Buf()]
            lgb = [A.f32(36), A.f32(36)]
            Blg = [Buf(), Buf()]

            def stageA(ti):
                col = tile_col(ti)
                x_, Bx_ = xt[ti % 2], Bxt[ti % 2]
                f_, Bf_ = ff[ti % 2], Bff[ti % 2]
                fb_, Bfb_ = fb[ti % 2], Bfb[ti % 2]
                ss_, Bss_ = ssA[ti % 2], BssA[ti % 2]
                fT_, BfT_ = fTs2[ti % 2], BfTs2[ti % 2]
                S.dma("sp", ("dma_start", dict(out=x_, in_=xr_d[ti * 128:(ti + 1) * 128, :])), reads=[Bsrc], writes=[Bx_])
                S.op("act", ("activation", dict(out=junk, in_=x_, func=AF.Square, accum_out=ss_)), reads=[Bx_], writes=[Bjunk, Bss_])
                S.op("act", ("activation", dict(out=ss_, in_=ss_, func=AF.Sqrt, scale=1.0 / D, bias=EPS)), reads=[Bss_], writes=[Bss_])
                S.op("dve", ("reciprocal", dict(out=ss_, in_=ss_)), reads=[Bss_], writes=[Bss_])
                S.op("dve", ("scalar_tensor_tensor", dict(out=f_, in0=x_, scalar=ss_, in1=Abc[col], op0=ALU.mult, op1=ALU.mult)), reads=[Bx_, Bss_, Bbc], writes=[Bf_])
                S.op("dve", ("tensor_tensor", dict(out=f_, in0=f_, in1=Sbc[col], op=ALU.add)), reads=[Bf_, Bbc], writes=[Bf_])
                S.op("act", ("copy", dict(out=fb_, in_=f_)), reads=[Bf_], writes=[Bfb_])
                pT = r3(psum_t[:, 0:1024], 8)
                for k in range(8):
                    S.op("pe", ("transpose", dict(out=pT[:, k, :], in_=f_[:, k * 128:(k + 1) * 128], identity=ident_f)), reads=[Bf_, Bc], writes=[PB[0], PB[1]])
                S.op("act", ("copy", dict(out=fT_, in_=psum_t[:, 0:1024])), reads=[PB[0], PB[1]], writes=[BfT_])
                pl = 2 + ti % 2
                for k in range(8):
                    S.op("pe", ("matmul", dict(out=bank(pl)[:, 0:36], lhsT=fT_[:, k * 128:(k + 1) * 128], rhs=w36_3[:, k, :], start=(k == 0), stop=(k == 7))),
                         reads=[BfT_, Bbc], writes=[PB[pl]])

            def stageB(ti):
                lg = lgb[ti % 2]
                fb_, Bfb_ = fb[ti % 2], Bfb[ti % 2]
                pl = 2 + ti % 2
                S.op("dve", ("tensor_tensor", dict(out=lgb[ti % 2], in0=bank(pl)[:, 0:36], in1=b36, op=ALU.add)), reads=[PB[pl], Bbc], writes=[Blg[ti % 2]])
                dv = lambda name, **kw: S.op("dve", (name, kw), reads=[Bsmall, Brun, Blg[ti % 2]], writes=[Bsmall])
                dv("tensor_reduce", out=ngmax, in_=lg[:, 0:4], axis=AX.X, op=ALU.max, negate=True)
                dv("tensor_scalar", out=gmask, in0=lg[:, 0:4], scalar1=ngmax, scalar2=0.0, op0=ALU.add, op1=ALU.is_ge)
                dv("tensor_scalar", out=pen, in0=gmask, scalar1=1e30, scalar2=-1e30, op0=ALU.mult, op1=ALU.add)
                dv("tensor_tensor", out=r3(masked, 4), in0=r3(lg[:, 4:36], 4), in1=pen.unsqueeze(2).broadcast_to([128, 4, 8]), op=ALU.add)
                dv("max", out=m8, in_=masked)
                dv("tensor_scalar", out=ntop1, in0=m8[:, 0:1], scalar1=-1.0, scalar2=None, op0=ALU.mult)
                S.op("act", ("activation", dict(out=eg, in_=lg[:, 0:4], func=AF.Exp, bias=ngmax, scale=1.0, accum_out=gsum)), reads=[Bsmall, Blg[ti % 2]], writes=[Bact])
                S.op("act", ("activation", dict(out=e2, in_=m8[:, 1:2], func=AF.Exp, bias=ntop1, scale=1.0)), reads=[Bsmall], writes=[Bact])
                dv("tensor_scalar", out=sel1, in0=masked, scalar1=m8[:, 0:1], scalar2=None, op0=ALU.is_ge)
                dv("tensor_scalar", out=sel, in0=masked, scalar1=m8[:, 1:2], scalar2=None, op0=ALU.is_ge)
                dv("tensor_copy", out=selb, in_=sel)
                S.op("pe", ("matmul", dict(out=bank(4)[:, 0:32], lhsT=utri_b, rhs=selb, start=True, stop=True)), reads=[Bsmall, Bc], writes=[PB[4]])
                S.op("pe", ("matmul", dict(out=bank(4)[:, 32:64], lhsT=ones_b, rhs=selb, start=True, stop=True)), reads=[Bsmall, Bc], writes=[PB[4]])
                dv("tensor_tensor", out=sel, in0=sel, in1=sel1, op=ALU.subtract)
                S.op("dve", ("tensor_tensor", dict(out=pos, in0=bank(4)[:, 0:32], in1=run, op=ALU.add)), reads=[PB[4], Brun, Bsmall], writes=[Bsmall])
                S.op("dve", ("tensor_tensor", dict(out=run, in0=bank(4)[:, 32:64], in1=run, op=ALU.add)), reads=[PB[4], Brun, Bsmall], writes=[Brun])
                dv("tensor_scalar", out=valid, in0=pos, scalar1=float(CAP), scalar2=None, op0=ALU.is_lt)
                dv("tensor_tensor", out=pos, in0=pos, in1=iotaC, op=ALU.add)
                dv("scalar_tensor_tensor", out=pos, in0=pos, scalar=-float(TRASH), in1=valid, op0=ALU.add, op1=ALU.mult)
                selcat = r3(sm[:, 104:168], 2)
                pvcat = r3(sm[:, 168:232], 2)
                t4 = tmp128.rearrange("p (a c e) -> p a c e", a=2, c=2)
                dv("tensor_tensor", out=t4, in0=selcat.unsqueeze(2).broadcast_to([128, 2, 2, 32]), in1=pvcat.unsqueeze(1).broadcast_to([128, 2, 2, 32]), op=ALU.mult)
                dv("tensor_reduce", out=r4, in_=r3(tmp128, 4), axis=AX.X, op=ALU.add)
                S.op("dve", ("tensor_scalar", dict(out=slot_i3[:, ti, :], in0=r4[:, 0:4:2], scalar1=float(TRASH), scalar2=None, op0=ALU.add)), reads=[Bsmall], writes=[Bslot])
                for a_ in range(2):
                    S.dma("pool", ("indirect_dma_start", dict(out=xall_d, out_offset=bass.IndirectOffsetOnAxis(ap=slot_i3[:, ti, a_:a_ + 1], axis=0),
                                                             in_=fb_, in_offset=None)), reads=[Bfb_, Bslot], writes=[])
                S.op("dve", ("tensor_scalar", dict(out=wa, in0=e2, scalar1=1.0, scalar2=gsum, op0=ALU.add, op1=ALU.mult)), reads=[Bact, Bsmall], writes=[Bsmall])
                dv("reciprocal", out=wa, in_=wa)
                S.op("dve", ("tensor_tensor", dict(out=wb, in0=wa, in1=e2, op=ALU.mult)), reads=[Bact, Bsmall], writes=[Bsmall])
                S.op("dve", ("tensor_tensor", dict(out=wts3[:, ti, :], in0=sm[:, 98:100], in1=r4[:, 1:4:2], op=ALU.mult)), reads=[Bsmall], writes=[Bwts])

            tmp128 = A.f32(128)
            r4 = sm[:, 240:244]
            Bact = Buf()
            sma = A.f32(8)
            eg = sma[:, 0:4]
            gsum = sma[:, 4:5]
            e2 = sma[:, 5:6]
            for step in range(ntiles + 1):
                if step < ntiles:
                    stageA(step)
                if step >= 1:
                    stageB(step - 1)
            S.barrier()
            A.off = mark
            if last is False and "slots" in dbg_d:
                pass
            NJ = CAP // 128
            wbuf = [(A.bf16(8 * 512), A.bf16(8 * 512), A.bf16(4 * 1024)) for _ in range(2)]
            Bwb = [Buf(), Buf()]
            xrows = [A.bf16(NJ * D) for _ in range(2)]
            Bxrows = [Buf(), Buf()]
            XT = A.bf16(8 * CAP)
            XT3 = r3(XT, 8)
            BXT = Buf()
            sl = [A.f32(512) for _ in range(2)]
            Bsl = [Buf(), Buf()]
            hs = A.bf16(4 * CAP)
            hs3 = r3(hs, 4)
            Bhs = Buf()
            ysb = [A.f32(D) for _ in range(2)]
            Bysb = [Buf(), Buf()]
            yi = 0
            stg = (A.f32(8 * 512), A.f32(8 * 512), A.f32(4 * 1024))
            Bstg = [Buf(), Buf(), Buf()]

            def load_w(e_):
                S.dma("sp", ("dma_start", dict(out=r3(stg[0], 8), in_=w1_d[L, e_].rearrange("(k p) n -> p k n", p=128))), writes=[Bstg[0]])
                S.dma("sp", ("dma_start", dict(out=r3(stg[1], 8), in_=w3_d[L, e_].rearrange("(k p) n -> p k n", p=128))), writes=[Bstg[1]])
                S.dma("sp", ("dma_start", dict(out=r3(stg[2], 4), in_=w2_d[L, e_].rearrange("(k p) n -> p k n", p=128))), writes=[Bstg[2]])

            def cast_w(e_, which):
                dst = wbuf[e_ % 2][which]
                Bw_ = Bwb[e_ % 2]
                if which == 0:
                    S.op("act", ("copy", dict(out=dst, in_=stg[0])), reads=[Bstg[0]], writes=[Bw_])
                elif which == 1:
                    S.op("dve", ("tensor_copy", dict(out=dst, in_=stg[1])), reads=[Bstg[1]], writes=[Bw_])
                else:
                    S.op("act", ("copy", dict(out=dst[:, 0:2048], in_=stg[2][:, 0:2048])), reads=[Bstg[2]], writes=[Bw_])
                    S.op("dve", ("tensor_copy", dict(out=dst[:, 2048:4096], in_=stg[2][:, 2048:4096])), reads=[Bstg[2]], writes=[Bw_])

            def load_x(e_):
                S.dma("sp", ("dma_start", dict(out=r3(xrows[e_ % 2], NJ), in_=xall_d[e_ * CAP:(e_ + 1) * CAP, :].rearrange("(j p) d -> p j d", p=128))),
                      reads=[Bxall], writes=[Bxrows[e_ % 2]])

            load_w(0)
            load_x(0)
            for w_ in range(3):
                cast_w(0, w_)
            for e_ in range(32):
                w1b, w3b, w2b = wbuf[e_ % 2]
                Bw = Bwb[e_ % 2]
                xr_, Bxr_ = xrows[e_ % 2], Bxrows[e_ % 2]
                if e_ + 1 < 32:
                    load_w(e_ + 1)
                    load_x(e_ + 1)
                for j in range(NJ):
                    pb = j % 2
                    pT = r3(bank_bf(pb), 8)
                    for k in range(8):
                        S.op("pe", ("transpose", dict(out=pT[:, k, :], in_=r3(xr_, NJ)[:, j, k * 128:(k + 1) * 128], identity=ident_b)),
                             reads=[Bxr_, Bc], writes=[PB[pb]])
                    S.op("act" if j % 2 == 0 else "dve", ("tensor_copy" if j % 2 else "copy", dict(out=XT3[:, :, j * 128:(j + 1) * 128], in_=pT)),
                         reads=[PB[pb]], writes=[BXT])
                w1v, w3v, w2v = r3(w1b, 8), r3(w3b, 8), r3(w2b, 4)
                for bi_, (c0, n) in enumerate(((0, 512), (512, CAP - 512))):
                    for m in range(4):
                        for (pb, wv) in ((2 + (m % 2) * 2, w1v), (3 + (m % 2) * 2, w3v)):
                            for k in range(8):
                                S.op("pe", ("matmul", dict(out=bank(pb)[:, 0:n], lhsT=wv[:, k, m * 128:(m + 1) * 128], rhs=XT3[:, k, c0:c0 + n], start=(k == 0), stop=(k == 7))),
                                     reads=[Bw, BXT], writes=[PB[pb]])
                        p1, p3 = 2 + (m % 2) * 2, 3 + (m % 2) * 2
                        s_, Bs_ = sl[m % 2], Bsl[m % 2]
                        S.op("act", ("activation", dict(out=s_[:, 0:n], in_=bank(p1)[:, 0:n], func=AF.Silu)), reads=[PB[p1]], writes=[Bs_])
                        S.op("dve", ("tensor_tensor", dict(out=hs3[:, m, c0:c0 + n], in0=bank(p3)[:, 0:n], in1=s_[:, 0:n], op=ALU.mult)), reads=[PB[p3], Bs_], writes=[Bhs])
                    if e_ + 1 < 32:
                        cast_w(e_ + 1, bi_)
                for j in range(NJ):
                    for hf in range(2):
                        for m in range(4):
                            S.op("pe", ("matmul", dict(out=bank(6 + hf), lhsT=hs3[:, m, j * 128:(j + 1) * 128], rhs=w2v[:, m, hf * 512:(hf + 1) * 512], start=(m == 0), stop=(m == 3))),
                                 reads=[Bhs, Bw], writes=[PB[6 + hf]])
                    y_, By_ = ysb[yi % 2], Bysb[yi % 2]
                    yi += 1
                    S.op("act", ("copy", dict(out=y_[:, 0:512], in_=bank(6))), reads=[PB[6]], writes=[By_])
                    S.op("dve", ("tensor_copy", dict(out=y_[:, 512:1024], in_=bank(7))), reads=[PB[7]], writes=[By_])
                    r0 = e_ * CAP + j * 128
                    S.dma("sp", ("dma_start", dict(out=yall_d[r0:r0 + 128, :], in_=y_)), reads=[By_], writes=[Byall])
                    if j == 1 and e_ + 1 < 32:
                        cast_w(e_ + 1, 2)
            S.barrier()
            A.off = mark
            ya = [A.f32(D) for _ in range(2)]
            yb = [A.f32(D) for _ in range(2)]
            x1 = [A.f32(D) for _ in range(2)]
            Bya, Byb, Bx1 = [Buf(), Buf()], [Buf(), Buf()], [Buf(), Buf()]
            junk2 = A.bf16(D)
            Bj2 = Buf()
            ss2 = [A.f32(1) for _ in range(2)]
            Bss2 = [Buf(), Buf()]
            Bdst = Buf()
            for ti in range(ntiles):
                col = tile_col(ti)
                i2 = ti % 2
                S.dma("pool", ("indirect_dma_start", dict(out=ya[i2], out_offset=None, in_=yall_d, in_offset=bass.IndirectOffsetOnAxis(ap=slot_i3[:, ti, 0:1], axis=0))),
                      reads=[Byall, Bslot], writes=[Bya[i2]])
                S.dma("pool", ("indirect_dma_start", dict(out=yb[i2], out_offset=None, in_=yall_d, in_offset=bass.IndirectOffsetOnAxis(ap=slot_i3[:, ti, 1:2], axis=0))),
                      reads=[Byall, Bslot], writes=[Byb[i2]])
                S.dma("sp", ("dma_start", dict(out=x1[i2], in_=xr_d[ti * 128:(ti + 1) * 128, :])), reads=[Bsrc], writes=[Bx1[i2]])
                S.op("act", ("activation", dict(out=ya[i2], in_=ya[i2], func=AF.Copy, scale=wts3[:, ti, 0:1])), reads=[Bya[i2], Bwts], writes=[Bya[i2]])
                S.op("dve", ("scalar_tensor_tensor", dict(out=yb[i2], in0=yb[i2], scalar=wts3[:, ti, 1:2], in1=ya[i2], op0=ALU.mult, op1=ALU.add)),
                     reads=[Byb[i2], Bya[i2], Bwts], writes=[Byb[i2]])
                S.op("dve", ("tensor_tensor", dict(out=yb[i2], in0=yb[i2], in1=Gbc[col], op=ALU.mult)), reads=[Byb[i2], Bbc], writes=[Byb[i2]])
                S.op("dve", ("tensor_tensor", dict(out=x1[i2], in0=x1[i2], in1=yb[i2], op=ALU.add)), reads=[Bx1[i2], Byb[i2]], writes=[Bx1[i2]])
                if not last:
                    S.dma("sp", ("dma_start", dict(out=xr2_d[ti * 128:(ti + 1) * 128, :], in_=x1[i2])), reads=[Bx1[i2]], writes=[Bdst])
                else:
                    S.op("act", ("activation", dict(out=junk2, in_=x1[i2], func=AF.Square, accum_out=ss2[i2])), reads=[Bx1[i2]], writes=[Bj2, Bss2[i2]])
                    S.op("act", ("activation", dict(out=ss2[i2], in_=ss2[i2], func=AF.Sqrt, scale=1.0 / D, bias=EPS)), reads=[Bss2[i2]], writes=[Bss2[i2]])
                    S.op("dve", ("reciprocal", dict(out=ss2[i2], in_=ss2[i2])), reads=[Bss2[i2]], writes=[Bss2[i2]])
                    S.op("dve", ("scalar_tensor_tensor", dict(out=x1[i2], in0=x1[i2], scalar=ss2[i2], in1=fng, op0=ALU.mult, op1=ALU.mult)),
                         reads=[Bx1[i2], Bss2[i2], Bbc], writes=[Bx1[i2]])
                    S.dma("sp", ("dma_start", dict(out=out_d[ti * 128:(ti + 1) * 128, :], in_=x1[i2])), reads=[Bx1[i2]], writes=[Bdst])
            return Bdst

        if stop_after >= 2:
            Bxr2 = moe(0, 36, Bxr)
            S.barrier()
            A.off = persist_off
            if "xr2" in dbg_d:
                S.dma("sp", ("dma_start", dict(out=dbg_d["xr2"], in_=xr2_d)), reads=[Bxr2])
        def s5_mixer(Bsrc):
            L = 1
            TWO_PI = 6.283185307179586
            MAGIC = 12582912.0
            yacc = [A.f32(4 * S_LAT) for _ in range(NB)]
            yacc3 = [r3(y, 4) for y in yacc]
            Byacc = [Buf(), Buf()]
            uT = [A.bf16(4 * SEQ) for _ in range(NB)]
            uT3 = [r3(u, 4) for u in uT]
            BuT = [Buf(), Buf()]
            sd = A.f32(8)
            Bsd = Buf()
            S.dma("sp", ("dma_start", dict(out=sd[:, 0:4], in_=s5d_d)), writes=[Bsd])
            S.dma("sp", ("dma_start", dict(out=sd[:, 4:8], in_=s5bglu_d)), writes=[Bsd])
            mark = A.off
            if S5_DEBUG_STAGE < 1:
                return Byacc[0]
            w5 = A.bf16(8 * 512)
            w5_3 = r3(w5, 8)
            Bw5 = Buf()
            S.dma("pool", ("dma_start", dict(out=w5_3, in_=s5win_d.rearrange("(k p) n -> p k n", p=128))), writes=[Bw5])
            hT1 = A.bf16(8 * 512)
            h3 = r3(hT1, 8)
            Bh = Buf()
            NT = NormT()
            for b in range(NB):
                blocks = [([32 + 2 * b, 33 + 2 * b], 0, 256)]
                for i in range(4):
                    blocks.append(([16 * b + 4 * i + t for t in range(4)], 256 + 512 * i, 512))
                for (tiles, c0, N) in blocks:
                    for t, ti in enumerate(tiles):
                        NT.run(L, ti, h3, Bh, t * 128, t % 2)
                    for r in range(4 if S5_DEBUG_STAGE >= 1.5 else 0):
                        pb = 2 + r % 2
                        for k in range(8):
                            S.op("pe", ("matmul", dict(out=bank(pb)[:, 0:N], lhsT=w5_3[:, k, r * 128:(r + 1) * 128], rhs=h3[:, k, 0:N], start=(k == 0), stop=(k == 7))),
                                 reads=[Bw5, Bh], writes=[PB[pb]])
                        S.op("act", ("copy", dict(out=uT3[b][:, r, c0:c0 + N], in_=bank(pb)[:, 0:N])), reads=[PB[pb]], writes=[BuT[b]])
                        if c0 >= 256:
                            S.op("dve", ("tensor_scalar", dict(out=yacc3[b][:, r, c0 - 256:c0 - 256 + N], in0=uT3[b][:, r, c0:c0 + N], scalar1=sd[:, r:r + 1], scalar2=None, op0=ALU.mult)),
                                 reads=[BuT[b], Bsd], writes=[Byacc[b]])
            S.barrier()
            A.off = mark
            if S5_DEBUG_STAGE < 2:
                return Byacc[0]
            par = A.f32(96)
            par3 = par.rearrange("p (c t) -> p c t", t=3)
            Bpar = Buf()
            S.dma("sp", ("dma_start", dict(out=par, in_=s5par_d)), writes=[Bpar])
            iot = A.f32(SEQ)
            S.dma("sp", ("dma_start", dict(out=iot, in_=s5iota_d)), writes=[Bpar])
            NCB = 32
            pr_ = A.f32(NCB * 16)
            P3 = r3(pr_, 16)
            dtv, rho, tht, frv, sn, cs, nr, ni, inv, cfr, cfi, ncfr, ncfi, tmpa, tmpb, tmpc = [P3[:, i, :] for i in range(16)]
            are, aim, ldt = par3[:, :, 0], par3[:, :, 1], par3[:, :, 2]
            pv = lambda name, **kw: S.op("dve", (name, kw), reads=[Bpar], writes=[Bpar])
            pa = lambda **kw: S.op("act", ("activation", kw), reads=[Bpar], writes=[Bpar])
            pa(out=dtv, in_=ldt, func=AF.Exp)
            pv("tensor_tensor", out=tmpa, in0=are, in1=dtv, op=ALU.mult)
            pa(out=rho, in_=tmpa, func=AF.Exp)
            pv("tensor_tensor", out=tht, in0=aim, in1=dtv, op=ALU.mult)
            pv("tensor_scalar", out=tht, in0=tht, scalar1=1.0 / TWO_PI, scalar2=None, op0=ALU.mult)
            pv("tensor_scalar", out=tmpa, in0=tht, scalar1=MAGIC, scalar2=None, op0=ALU.add)
            pv("tensor_scalar", out=tmpa, in0=tmpa, scalar1=MAGIC, scalar2=None, op0=ALU.subtract)
            pv("tensor_tensor", out=frv, in0=tht, in1=tmpa, op=ALU.subtract)
            SC = TWO_PI * (1.0 - 1e-6)
            pa(out=sn, in_=frv, func=AF.Sin, scale=SC)
            pa(out=tmpb, in_=frv, func=AF.Sin, scale=SC / 2)
            pv("tensor_tensor", out=tmpb, in0=tmpb, in1=tmpb, op=ALU.mult)
            pv("tensor_scalar", out=cs, in0=tmpb, scalar1=-2.0, scalar2=1.0, op0=ALU.mult, op1=ALU.add)
            pv("tensor_tensor", out=nr, in0=rho, in1=cs, op=ALU.mult)
            pv("tensor_scalar", out=nr, in0=nr, scalar1=-1.0, scalar2=None, op0=ALU.add)
            pv("tensor_tensor", out=ni, in0=rho, in1=sn, op=ALU.mult)
            pv("tensor_tensor", out=tmpa, in0=are, in1=are, op=ALU.mult)
            pv("tensor_tensor", out=tmpb, in0=aim, in1=aim, op=ALU.mult)
            pv("tensor_tensor", out=inv, in0=tmpa, in1=tmpb, op=ALU.add)
            pv("reciprocal", out=inv, in_=inv)
            pv("tensor_tensor", out=tmpa, in0=nr, in1=are, op=ALU.mult)
            pv("tensor_tensor", out=tmpb, in0=ni, in1=aim, op=ALU.mult)
            pv("tensor_tensor", out=tmpa, in0=tmpa, in1=tmpb, op=ALU.add)
            pv("tensor_tensor", out=cfr, in0=tmpa, in1=inv, op=ALU.mult)
            pv("tensor_tensor", out=tmpa, in0=ni, in1=are, op=ALU.mult)
            pv("tensor_tensor", out=tmpb, in0=nr, in1=aim, op=ALU.mult)
            pv("tensor_tensor", out=tmpa, in0=tmpa, in1=tmpb, op=ALU.subtract)
            pv("tensor_tensor", out=cfi, in0=tmpa, in1=inv, op=ALU.mult)
            pv("tensor_scalar", out=ncfr, in0=cfr, scalar1=-1.0, scalar2=None, op0=ALU.mult)
            pv("tensor_scalar", out=ncfi, in0=cfi, scalar1=-1.0, scalar2=None, op0=ALU.mult)

            if S5_DEBUG_STAGE < 3:
                return Bpar
            cosT = A.f32(SEQ)
            sinT = A.f32(SEQ)
            tA = A.f32(SEQ)
            tB = A.f32(SEQ)
            Btab, BtA, BtB = Buf(), Buf(), Buf()
            dr = A.f32(SEQ)
            di = A.f32(SEQ)
            qr = A.bf16(SEQ)
            qi = A.bf16(SEQ)
            cosb = A.bf16(SEQ)
            sinb = A.bf16(SEQ)
            Btabb = Buf()
            m1b = [A.bf16(512) for _ in range(2)]
            m2b = [A.bf16(512) for _ in range(2)]
            Bdr, Bdi, Bqr, Bqi = Buf(), Buf(), Buf(), Buf()
            m1 = [A.f32(512) for _ in range(2)]
            m2 = [A.f32(512) for _ in range(2)]
            Bm1, Bm2 = [Buf(), Buf()], [Buf(), Buf()]
            hr = [A.bf16(512) for _ in range(2)]
            hi = [A.bf16(512) for _ in range(2)]
            Bhr, Bhi = [Buf(), Buf()], [Buf(), Buf()]
            bt = [(A.bf16(128), A.bf16(128)) for _ in range(2)]
            Bbt = [Buf(), Buf()]
            cst_ = [(A.f32(128), A.f32(128)) for _ in range(2)]
            Bcst = [Buf(), Buf()]
            cw = [(A.bf16(128), A.bf16(128)) for _ in range(2)]
            Bcw = [Buf(), Buf()]
            ctmp = A.f32(128)
            Bctmp = Buf()
            mi = 0
            for d_ in range(2):
                for pr in range(16):
                    ci_ = d_ * 16 + pr
                    r = pr // 4
                    k2 = ci_ % 2
                    btr, bti = bt[k2]
                    S.dma("pool", ("dma_start", dict(out=btr, in_=s5bT_d[d_, 0, pr])), writes=[Bbt[k2]])
                    S.dma("pool", ("dma_start", dict(out=bti, in_=s5bT_d[d_, 1, pr])), writes=[Bbt[k2]])
                    c_r, c_i = cst_[k2]
                    S.dma("sp", ("dma_start", dict(out=c_r, in_=s5c_d[d_, 0, pr])), writes=[Bcst[k2]])
                    S.dma("sp", ("dma_start", dict(out=c_i, in_=s5c_d[d_, 1, pr])), writes=[Bcst[k2]])
                    cwr, cwi = cw[k2]
                    col1 = lambda v, ci_=ci_: v[:, ci_:ci_ + 1]
                    S.op("dve", ("tensor_scalar", dict(out=ctmp, in0=c_r, scalar1=col1(cfr), scalar2=None, op0=ALU.mult)), reads=[Bcst[k2], Bpar], writes=[Bctmp])
                    S.op("dve", ("scalar_tensor_tensor", dict(out=cwr, in0=c_i, scalar=col1(ncfi), in1=ctmp, op0=ALU.mult, op1=ALU.add)), reads=[Bcst[k2], Bpar, Bctmp], writes=[Bcw[k2]])
                    S.op("dve", ("tensor_scalar", dict(out=ctmp, in0=c_r, scalar1=col1(ncfi), scalar2=None, op0=ALU.mult)), reads=[Bcst[k2], Bpar, Bcw[k2]], writes=[Bctmp])
                    S.op("dve", ("scalar_tensor_tensor", dict(out=cwi, in0=c_i, scalar=col1(ncfr), in1=ctmp, op0=ALU.mult, op1=ALU.add)), reads=[Bcst[k2], Bpar, Bctmp], writes=[Bcw[k2]])
                    S.op("act", ("activation", dict(out=tA, in_=iot, func=AF.Copy, scale=col1(tht))), reads=[Bpar], writes=[BtA])
                    S.op("act", ("activation", dict(out=tB, in_=tA, func=AF.Identity, bias=MAGIC, scale=1.0)), reads=[BtA], writes=[BtB])
                    S.op("act", ("activation", dict(out=tB, in_=tB, func=AF.Identity, bias=-MAGIC, scale=1.0)), reads=[BtB], writes=[BtB])
                    S.op("dve", ("tensor_tensor", dict(out=tA, in0=tA, in1=tB, op=ALU.subtract)), reads=[BtA, BtB], writes=[BtA])
                    S.op("act", ("activation", dict(out=sinT, in_=tA, func=AF.Sin, scale=SC)), reads=[BtA], writes=[Btab])
                    S.op("act", ("activation", dict(out=tB, in_=tA, func=AF.Sin, scale=SC / 2)), reads=[BtA], writes=[BtB])
                    S.op("act", ("activation", dict(out=tB, in_=tB, func=AF.Square, scale=1.4142135623730951)), reads=[BtB], writes=[BtB])
                    S.op("act", ("activation", dict(out=cosT, in_=tB, func=AF.Identity, scale=-1.0, bias=1.0)), reads=[BtB], writes=[Btab])
                    S.op("act", ("copy", dict(out=cosb, in_=cosT)), reads=[Btab], writes=[Btabb])
                    S.op("act", ("copy", dict(out=sinb, in_=sinT)), reads=[Btab], writes=[Btabb])
                    rho_c = col1(rho)
                    for b in range(NB):
                        blocks = [(0, 256)] + [(256 + 512 * i, 512) for i in range(4)]
                        for bi, (s0, N) in enumerate(blocks):
                            if d_ == 0:
                                ucols = uT3[b][:, r, s0:s0 + N]
                            else:
                                if bi == 0:
                                    ucols = uT3[b][:, r, 255::-1]
                                else:
                                    hi_c = SEQ - 1 - (bi - 1) * 512
                                    ucols = uT3[b][:, r, hi_c:hi_c - 512:-1]
                            pbr, pbi = (bi % 2) * 2, (bi % 2) * 2 + 1
                            S.op("pe", ("matmul", dict(out=bank(pbr)[:, 0:N], lhsT=btr, rhs=ucols, start=True, stop=True)), reads=[Bbt[k2], BuT[b]], writes=[PB[pbr]])
                            S.op("pe", ("matmul", dict(out=bank(pbi)[:, 0:N], lhsT=bti, rhs=ucols, start=True, stop=True)), reads=[Bbt[k2], BuT[b]], writes=[PB[pbi]])
                            a1, a2 = m1[mi % 2], m2[mi % 2]
                            Ba1, Ba2 = Bm1[mi % 2], Bm2[mi % 2]
                            mi += 1
                            cS, sS = cosT[:, s0:s0 + N], sinT[:, s0:s0 + N]
                            S.op("dve", ("tensor_tensor", dict(out=a1[:, 0:N], in0=bank(pbr)[:, 0:N], in1=cS, op=ALU.mult)), reads=[PB[pbr], Btab], writes=[Ba1])
                            S.op("dve", ("tensor_tensor", dict(out=a2[:, 0:N], in0=bank(pbi)[:, 0:N], in1=sS, op=ALU.mult)), reads=[PB[pbi], Btab], writes=[Ba2])
                            S.op("dve", ("tensor_tensor", dict(out=dr[:, s0:s0 + N], in0=a1[:, 0:N], in1=a2[:, 0:N], op=ALU.add)), reads=[Ba1, Ba2], writes=[Bdr])
                            a1, a2 = m1[mi % 2], m2[mi % 2]
                            Ba1, Ba2 = Bm1[mi % 2], Bm2[mi % 2]
                            mi += 1
                            S.op("dve", ("tensor_tensor", dict(out=a1[:, 0:N], in0=bank(pbi)[:, 0:N], in1=cS, op=ALU.mult)), reads=[PB[pbi], Btab], writes=[Ba1])
                            S.op("dve", ("tensor_tensor", dict(out=a2[:, 0:N], in0=bank(pbr)[:, 0:N], in1=sS, op=ALU.mult)), reads=[PB[pbr], Btab], writes=[Ba2])
                            S.op("dve", ("tensor_tensor", dict(out=di[:, s0:s0 + N], in0=a1[:, 0:N], in1=a2[:, 0:N], op=ALU.subtract)), reads=[Ba1, Ba2], writes=[Bdi])
                        rb = rho_c.broadcast_to([128, SEQ])
                        S.op("dve", ("tensor_tensor_scan", dict(out=qr, data0=rb, data1=dr, initial=0.0, op0=ALU.mult, op1=ALU.add)), reads=[Bdr, Bpar], writes=[Bqr])
                        S.op("dve", ("tensor_tensor_scan", dict(out=qi, data0=rb, data1=di, initial=0.0, op0=ALU.mult, op1=ALU.add)), reads=[Bdi, Bpar], writes=[Bqi])
                        for bi in range(1, 5):
                            s0, N = blocks[bi]
                            cS, sS = cosb[:, s0:s0 + N], sinb[:, s0:s0 + N]
                            a1, a2 = m1b[mi % 2], m2b[mi % 2]
                            Ba1, Ba2 = Bm1[mi % 2], Bm2[mi % 2]
                            mi += 1
                            h_r, h_i = hr[bi % 2], hi[bi % 2]
                            S.op("dve", ("tensor_tensor", dict(out=a1, in0=qr[:, s0:s0 + N], in1=cS, op=ALU.mult)), reads=[Bqr, Btabb], writes=[Ba1])
                            S.op("dve", ("tensor_tensor", dict(out=a2, in0=qi[:, s0:s0 + N], in1=sS, op=ALU.mult)), reads=[Bqi, Btabb], writes=[Ba2])
                            S.op("dve", ("tensor_tensor", dict(out=h_r, in0=a1, in1=a2, op=ALU.subtract)), reads=[Ba1, Ba2], writes=[Bhr[bi % 2]])
                            a1, a2 = m1b[mi % 2], m2b[mi % 2]
                            Ba1, Ba2 = Bm1[mi % 2], Bm2[mi % 2]
                            mi += 1
                            S.op("dve", ("tensor_tensor", dict(out=a1, in0=qr[:, s0:s0 + N], in1=sS, op=ALU.mult)), reads=[Bqr, Btabb], writes=[Ba1])
                            S.op("dve", ("tensor_tensor", dict(out=a2, in0=qi[:, s0:s0 + N], in1=cS, op=ALU.mult)), reads=[Bqi, Btabb], writes=[Ba2])
                            S.op("dve", ("tensor_tensor", dict(out=h_i, in0=a1, in1=a2, op=ALU.add)), reads=[Ba1, Ba2], writes=[Bhi[bi % 2]])
                            pby = 4 + bi % 2
                            S.op("pe", ("matmul", dict(out=bank(pby), lhsT=cwr, rhs=h_r, start=True, stop=False)), reads=[Bcw[k2], Bhr[bi % 2]], writes=[PB[pby]])
                            S.op("pe", ("matmul", dict(out=bank(pby), lhsT=cwi, rhs=h_i, start=False, stop=True)), reads=[Bcw[k2], Bhi[bi % 2]], writes=[PB[pby]])
                            if d_ == 0:
                                j0 = s0 - 256
                                ycols = yacc3[b][:, r, j0:j0 + 512]
                            else:
                                hj = S_LAT - 1 - (bi - 1) * 512
                                stop = hj - 512
                                ycols = yacc3[b][:, r, hj::-1] if stop < 0 else yacc3[b][:, r, hj:stop:-1]
                            S.op("dve", ("tensor_tensor", dict(out=ycols, in0=bank(pby), in1=ycols, op=ALU.add)), reads=[PB[pby], Byacc[b]], writes=[Byacc[b]])
            S.barrier()
            A.off = mark
            if "yacc" in dbg_d:
                for b in range(NB):
                    S.dma("sp", ("dma_start", dict(out=dbg_d["yacc"][b * 128:(b + 1) * 128, :], in_=yacc[b])), reads=[Byacc[b]])
            wg = A.bf16(4 * 512)
            wg3 = r3(wg, 4)
            wo = A.bf16(4 * 1024)
            wo3 = r3(wo, 4)
            BwC = Buf()
            S.dma("pool", ("dma_start", dict(out=wg3, in_=s5glu_d.rearrange("(k p) n -> p k n", p=128))), writes=[BwC])
            S.dma("pool", ("dma_start", dict(out=wo3, in_=s5wout_d.rearrange("(k p) n -> p k n", p=128))), writes=[BwC])
            gate_bc = [A.f32(D) for _ in range(2)]
            for c in range(2):
                S.dma("sp", ("dma_start", dict(out=gate_bc[c], in_=modrows_d[L, 2, c:c + 1, :].broadcast_to([128, D]))), reads=[Bmodrows], writes=[BwC])
            gT = A.bf16(4 * 512)
            gT3 = r3(gT, 4)
            vT = A.bf16(4 * 512)
            vT3 = r3(vT, 4)
            BgT, BvT = Buf(), Buf()
            sg = [A.f32(512) for _ in range(2)]
            Bsg = [Buf(), Buf()]
            yt = [A.f32(D) for _ in range(2)]
            xt2 = [A.f32(D) for _ in range(2)]
            Byt, Bxt2 = [Buf(), Buf()], [Buf(), Buf()]
            Bxr = Buf()
            oi = 0
            for b in range(NB):
                for i in range(4):
                    j0 = i * 512
                    S.op("act", ("activation", dict(out=gT3, in_=yacc3[b][:, :, j0:j0 + 512], func=AF.Gelu_apprx_tanh)), reads=[Byacc[b]], writes=[BgT])
                    for m in range(4):
                        pb = 2 + m % 2
                        for k in range(4):
                            S.op("pe", ("matmul", dict(out=bank(pb), lhsT=wg3[:, k, m * 128:(m + 1) * 128], rhs=gT3[:, k, :], start=(k == 0), stop=(k == 3))),
                                 reads=[BwC, BgT], writes=[PB[pb]])
                        S.op("act", ("activation", dict(out=sg[m % 2], in_=bank(pb), func=AF.Sigmoid, bias=sd[:, 4 + m:5 + m], scale=1.0)), reads=[PB[pb], Bsd], writes=[Bsg[m % 2]])
                        S.op("dve", ("tensor_tensor", dict(out=vT3[:, m, :], in0=gT3[:, m, :], in1=sg[m % 2], op=ALU.mult)), reads=[BgT, Bsg[m % 2]], writes=[BvT])
                    for t in range(4):
                        ti = 16 * b + 4 * i + t
                        for hf in range(2):
                            for k in range(4):
                                S.op("pe", ("matmul", dict(out=bank(6 + hf), lhsT=vT3[:, k, t * 128:(t + 1) * 128], rhs=wo3[:, k, hf * 512:(hf + 1) * 512], start=(k == 0), stop=(k == 3))),
                                     reads=[BvT, BwC], writes=[PB[6 + hf]])
                        o2 = oi % 2
                        oi += 1
                        S.dma("sp", ("dma_start", dict(out=xt2[o2], in_=xr2_d[ti * 128:(ti + 1) * 128, :])), reads=[Bsrc], writes=[Bxt2[o2]])
                        S.op("dve", ("tensor_tensor", dict(out=yt[o2], in0=psum_t[:, 6 * 512:8 * 512], in1=gate_bc[b], op=ALU.mult)), reads=[PB[6], PB[7], BwC], writes=[Byt[o2]])
                        S.op("dve", ("tensor_tensor", dict(out=yt[o2], in0=yt[o2], in1=xt2[o2], op=ALU.add)), reads=[Byt[o2], Bxt2[o2]], writes=[Byt[o2]])
                        S.dma("sp", ("dma_start", dict(out=xr_d[ti * 128:(ti + 1) * 128, :], in_=yt[o2])), reads=[Byt[o2]], writes=[Bxr])
            return Bxr

        if stop_after >= 3:
            Bxr_b = s5_mixer(Bxr2)
            S.barrier()
            A.off = persist_off
            if "xr3" in dbg_d:
                S.dma("sp", ("dma_start", dict(out=dbg_d["xr3"], in_=xr_d[0:T_LAT, :])), reads=[Bxr_b])
        if stop_after >= 4:
            Bout = moe(1, 32, Bxr_b)

        S.barrier()
        with nc.Block() as block:
            S.emit(block)
    return nc


def _rope_tables():
    inv = np.power(10000.0, -np.arange(0, 32, 2, dtype=np.float32) / 32).astype(np.float32)
    t = np.arange(S_LAT)
    row = (t // 64).astype(np.float32)
    colp = (t % 64).astype(np.float32)
    ang_r = row[:, None] * inv[None, :]
    ang_c = colp[:, None] * inv[None, :]
    cos64 = np.ones((64, SEQ), np.float32)
    sin64 = np.zeros((64, SEQ), np.float32)
    for d in range(64):
        ang = ang_r if d < 32 else ang_c
        i = d % 16
        sgn = -1.0 if (d % 32) < 16 else 1.0
        cos64[d, C_CTX:] = np.cos(ang[:, i])
        sin64[d, C_CTX:] = sgn * np.sin(ang[:, i])
    return np.concatenate([np.concatenate([cos64, cos64], 0), np.concatenate([sin64, sin64], 0)], 1).astype(np.float32)


_PERM64 = np.array([(d // 32) * 32 + ((d % 32) + 16) % 32 for d in range(64)])


def _consts():
    c = np.zeros((128, NCONST), np.float32)
    c[:, 0:128] = np.eye(128)
    c[:, 128:256] = np.triu(np.ones((128, 128)), 1)
    c[:, 256:384] = 1.0
    c[0:64, 384:448] = 1.0 / 64
    c[64:128, 448:512] = 1.0 / 64
    c[:, 512:544] = (np.arange(32) * CAP)[None, :]
    return c


def make_in_maps(inp, cores):
    f = lambda a: np.ascontiguousarray(np.asarray(a, dtype=np.float32))
    shared = {
        "consts": _consts(), "rope": _rope_tables(),
        "w_mod": f(inp["w_mod"]), "b_mod": f(inp["b_mod"]).reshape(2, 1, 6 * D),
        "norm_mix_g": f(inp["norm_mix_g"]).reshape(2, 1, D), "norm_ffn_g": f(inp["norm_ffn_g"]).reshape(2, 1, D),
        "mix_w_in": f(inp["mix_w_in"][0]), "mix_w_out": f(inp["mix_w_out"][0]),
        "gmlp_norm_g": f(inp["gmlp_norm_g"]).reshape(1, 512), "gmlp_w_spatial": f(inp["gmlp_w_spatial"][0]),
        "gmlp_b_spatial": f(inp["gmlp_b_spatial"][0]).reshape(1, 1024),
        "moe_w_group": f(inp["moe_w_group"]), "moe_b_group": f(inp["moe_b_group"]).reshape(2, 1, 4),
        "moe_w_router": f(inp["moe_w_router"]), "moe_b_router": f(inp["moe_b_router"]).reshape(2, 1, 32),
        "moe_w1": f(inp["moe_w1"]), "moe_w3": f(inp["moe_w3"]), "moe_w2": f(inp["moe_w2"]),
        "final_norm_g": f(inp["final_norm_g"]).reshape(1, D),
        "s5_w_in": f(inp["s5_w_in"][0]), "s5_w_glu": f(inp["s5_w_glu"][0]), "s5_w_out": f(inp["s5_w_out"][0]),
    }
    qg = f(inp["q_norm_g"][0]); kg = f(inp["k_norm_g"][0])
    idx = np.arange(128) % 64
    shared["qkg"] = np.stack([qg[idx], qg[_PERM64[idx]], kg[idx], kg[_PERM64[idx]]], 1).astype(np.float32)
    a_re = f(inp["s5_a_re"][0]); a_im = f(inp["s5_a_im"][0]); ldt = f(inp["s5_log_dt"][0])
    par = np.zeros((128, 2, 16, 3), np.float32)
    for d in range(2):
        for pr in range(16):
            for gl in range(2):
                g = 2 * pr + gl
                par[gl * 64:(gl + 1) * 64, d, pr, 0] = a_re[d, g]
                par[gl * 64:(gl + 1) * 64, d, pr, 1] = a_im[d, g]
                par[gl * 64:(gl + 1) * 64, d, pr, 2] = ldt[d, g]
    shared["s5_par"] = par.reshape(128, 96)
    b_re = f(inp["s5_b_re"][0]); b_im = f(inp["s5_b_im"][0]); c_re = f(inp["s5_c_re"][0]); c_im = f(inp["s5_c_im"][0])
    bT = np.zeros((2, 2, 16, 128, 128), np.float32)
    cc = np.zeros((2, 2, 16, 128, 128), np.float32)
    for d in range(2):
        for pr in range(16):
            for gl in range(2):
                g = 2 * pr + gl
                gic = g % 8
                for ri, (bsrc, csrc) in enumerate(((b_re, c_re), (b_im, c_im))):
                    bT[d, ri, pr, gic * 16:(gic + 1) * 16, gl * 64:(gl + 1) * 64] = bsrc[d, g].T
                    cc[d, ri, pr, gl * 64:(gl + 1) * 64, gic * 16:(gic + 1) * 16] = csrc[d, g].T
    shared["s5_bT"] = bT
    shared["s5_iota"] = np.tile(np.arange(SEQ, dtype=np.float32)[None, :], (128, 1))
    shared["s5_c"] = cc
    shared["s5_d"] = f(inp["s5_d"][0]).reshape(4, 128).T.copy()
    shared["s5_b_glu"] = f(inp["s5_b_glu"][0]).reshape(4, 128).T.copy()
    maps = []
    x = np.asarray(inp["x"]); ctx = np.asarray(inp["ctx"]); c = np.asarray(inp["c"]); cc_ = np.asarray(inp["c_ctx"])
    for core in cores:
        m = dict(shared)
        m["x"] = f(x[2 * core:2 * core + 2]).reshape(T_LAT, D)
        m["ctx"] = f(ctx[2 * core:2 * core + 2]).reshape(T_CTX, D)
        cvec = np.stack([c[2 * core], c[2 * core + 1], cc_], 0).astype(np.float32)
        m["cT"] = np.ascontiguousarray(cvec.reshape(3, 8, 128).transpose(2, 1, 0).reshape(128, 24))
        maps.append(m)
    return maps


_NC_CACHE = {}


def kernel(**inputs):
    if "nc" not in _NC_CACHE:
        _NC_CACHE["nc"] = build_program()
    nc = _NC_CACHE["nc"]
    cores = list(range(8))
    maps = make_in_maps(inputs, cores)
    res = run_bass_kernel_spmd(nc, maps, core_ids=cores)
    outs = [np.asarray(r["out"]).reshape(NB, S_LAT, D) for r in res.results]
    return np.concatenate(outs, 0).astype(np.float32)
```

```python
import numpy as np
from contextlib import ExitStack
import concourse.bass as bass
import concourse.mybir as mybir
from concourse.alu_op_type import AluOpType as ALU
from concourse.bass_utils import run_bass_kernel_spmd

F32 = mybir.dt.float32
BF16 = mybir.dt.bfloat16
I32 = mybir.dt.int32
AF = mybir.ActivationFunctionType
AX = mybir.AxisListType

D = 1024
S_LAT = 2048
C_CTX = 256
NB = 2
T_LAT = NB * S_LAT
T_CTX = NB * C_CTX
SEQ = C_CTX + S_LAT
EPS = 1e-6
CAP = 640
NSLOT = 32 * CAP
TRASH = NSLOT
NCONST = 128 * 4 + 32
S5_DEBUG_STAGE = 9


class Buf:
    __slots__ = ("w", "r")

    def __init__(self):
        self.w = None
        self.r = []


class Sched:
    COMPUTE = ("pe", "act", "dve", "pool")
    NDMA = 8

    def __init__(self, nc, es):
        self.nc = nc
        self.streams = {e: [] for e in ("pe", "act", "dve", "pool", "sp")}
        self.sem = {}
        self.cnt = {}
        for e in self.COMPUTE:
            self.sem[e] = es.enter_context(nc.semaphore("s_" + e))
            self.cnt[e] = 0
        self.drr = {}
        for q in ("sp", "act", "pool"):
            for k in range(self.NDMA):
                key = "d_%s%d" % (q, k)
                self.sem[key] = es.enter_context(nc.semaphore(key))
                self.cnt[key] = 0
            self.drr[q] = 0
        self.waited = {e: {} for e in self.streams}
        self.nops = 0

    def _deps(self, reads, writes):
        deps = []
        for b in reads:
            if b.w is not None:
                deps.append(b.w)
        for b in writes:
            if b.w is not None:
                deps.append(b.w)
            deps.extend(b.r)
        return deps

    def _emit_waits(self, eng, deps, skip_self=None):
        best = {}
        for (k, v) in deps:
            if k == skip_self:
                continue
            if v > best.get(k, 0):
                best[k] = v
        w = self.waited[eng]
        for k, v in best.items():
            if w.get(k, 0) < v:
                w[k] = v
                self.streams[eng].append(("wait", k, v))

    def _mark(self, tok, reads, writes):
        for b in writes:
            b.w = tok
            b.r = []
        for b in reads:
            if b.w is tok:
                continue
            b.r.append(tok)
            if len(b.r) > 16:
                best = {}
                for (k, v) in b.r:
                    if v > best.get(k, 0):
                        best[k] = v
                b.r = list(best.items())

    def op(self, eng, fn, reads=(), writes=()):
        deps = self._deps(reads, writes)
        self._emit_waits(eng, deps, skip_self=("pe" if eng == "pe" else None))
        self.cnt[eng] += 1
        tok = (eng, self.cnt[eng])
        self.streams[eng].append(("op", fn, eng, 1))
        self._mark(tok, reads, writes)
        self.nops += 1
        return tok

    def dma(self, q, fn, reads=(), writes=()):
        k = self.drr[q]
        self.drr[q] = (k + 1) % self.NDMA
        key = "d_%s%d" % (q, k)
        deps = self._deps(reads, writes)
        if self.cnt[key] > 0:
            deps.append((key, self.cnt[key]))
        self._emit_waits(q, deps)
        self.cnt[key] += 16
        tok = (key, self.cnt[key])
        self.streams[q].append(("op", fn, key, 16))
        self._mark(tok, reads, writes)
        self.nops += 1
        return tok

    def barrier(self):
        toks = [(k, v) for k, v in self.cnt.items() if v > 0]
        for e in self.streams:
            self._emit_waits(e, toks)

    def emit(self, block):
        sem = self.sem
        streams = self.streams

        def run(e, lst):
            for it in lst:
                if it[0] == "wait":
                    e.wait_ge(sem[it[1]], it[2])
                else:
                    getattr(e, it[1][0])(**it[1][1]).then_inc(sem[it[2]], it[3])

        @block.sync
        def _(e):
            run(e, streams["sp"])

        @block.scalar
        def _(e):
            run(e, streams["act"])

        @block.vector
        def _(e):
            run(e, streams["dve"])

        @block.gpsimd
        def _(e):
            run(e, streams["pool"])

        @block.tensor
        def _(e):
            run(e, streams["pe"])


class Arena:
    def __init__(self, t, size):
        self.t = t
        self.size = size
        self.off = 0

    def f32(self, n):
        a = self.t[:, self.off:self.off + n]
        self.off += n
        assert self.off <= self.size, ("arena overflow", self.off, self.size)
        return a

    def bf16(self, n):
        return self.f32((n + 1) // 2).bitcast(BF16)[:, 0:n]

    def i32(self, n):
        return self.f32(n).bitcast(I32)


def r3(ap, a):
    return ap.rearrange("p (a b) -> p a b", a=a)


def build_program(stop_after=99, dbg=()):
    nc = bass.Bass("TRN2", target_bir_lowering=False)
    dt = nc.dram_tensor

    def din(name, shape, dtype=F32):
        return dt(name, list(shape), dtype, kind="ExternalInput").ap()

    x_d = din("x", [T_LAT, D])
    ctx_d = din("ctx", [T_CTX, D])
    cT_d = din("cT", [128, 24])
    consts_d = din("consts", [128, NCONST])
    rope_d = din("rope", [128, 2 * SEQ])
    qkg_d = din("qkg", [128, 4])
    w_mod_d = din("w_mod", [2, D, 6 * D])
    b_mod_d = din("b_mod", [2, 1, 6 * D])
    gmix_d = din("norm_mix_g", [2, 1, D])
    gffn_d = din("norm_ffn_g", [2, 1, D])
    w_in_d = din("mix_w_in", [D, 1792])
    w_out_d = din("mix_w_out", [D, D])
    lng_d = din("gmlp_norm_g", [1, 512])
    wsp_d = din("gmlp_w_spatial", [8, 128, 128])
    bsp_d = din("gmlp_b_spatial", [1, 1024])
    wgrp_d = din("moe_w_group", [2, D, 4])
    bgrp_d = din("moe_b_group", [2, 1, 4])
    wrt_d = din("moe_w_router", [2, D, 32])
    brt_d = din("moe_b_router", [2, 1, 32])
    w1_d = din("moe_w1", [2, 32, D, 512])
    w3_d = din("moe_w3", [2, 32, D, 512])
    w2_d = din("moe_w2", [2, 32, 512, D])
    fng_d = din("final_norm_g", [1, D])
    s5win_d = din("s5_w_in", [D, 512])
    s5par_d = din("s5_par", [128, 2 * 16 * 3])
    s5iota_d = din("s5_iota", [128, SEQ])
    s5bT_d = din("s5_bT", [2, 2, 16, 128, 128])
    s5c_d = din("s5_c", [2, 2, 16, 128, 128])
    s5d_d = din("s5_d", [128, 4])
    s5glu_d = din("s5_w_glu", [512, 512])
    s5bglu_d = din("s5_b_glu", [128, 4])
    s5wout_d = din("s5_w_out", [512, D])

    out_d = dt("out", [T_LAT, D], F32, kind="ExternalOutput").ap()
    modrows_d = dt("modrows", [2, 6, 3, D], F32, kind="Internal").ap()
    xr_d = dt("xr", [T_LAT + T_CTX, D], F32, kind="Internal").ap()
    xr2_d = dt("xr2", [T_LAT + T_CTX, D], F32, kind="Internal").ap()
    xall_d = dt("xall", [NSLOT + 128, D], BF16, kind="Internal").ap()
    yall_d = dt("yall", [NSLOT + 128, D], F32, kind="Internal").ap()
    dbg_d = {}
    for name, shape in dbg:
        dbg_d[name] = dt("dbg_" + name, list(shape), F32, kind="ExternalOutput").ap()

    with ExitStack() as es:
        S = Sched(nc, es)
        ARENA_WORDS = 53100
        arena_t = es.enter_context(nc.sbuf_tensor("arena", [128, ARENA_WORDS], F32))
        psum_t = es.enter_context(nc.psum_tensor("psum", [128, 4096], F32))
        A = Arena(arena_t, ARENA_WORDS)

        def bank(i, n=512, off=0):
            return psum_t[:, i * 512 + off:i * 512 + off + n]

        def bank_bf(i):
            return psum_t[:, i * 512:(i + 1) * 512].bitcast(BF16)

        PB = [Buf() for _ in range(8)]

        cst = A.f32(NCONST)
        Bc = Buf()
        S.dma("sp", ("dma_start", dict(out=cst, in_=consts_d)), writes=[Bc])
        ident_f = cst[:, 0:128]
        utri_f = cst[:, 128:256]
        ones_f = cst[:, 256:384]
        blk_f = cst[:, 384:512]
        iotaC = cst[:, 512:544]
        cbf = A.bf16(512)
        S.op("dve", ("tensor_copy", dict(out=cbf, in_=cst[:, 0:512])), reads=[Bc], writes=[Bc])
        ident_b = cbf[:, 0:128]
        utri_b = cbf[:, 128:256]
        ones_b = cbf[:, 256:384]
        blk_b = cbf[:, 384:512]
        modT = A.f32(2 * 2 * 8 * 3)
        BmodT = Buf()
        persist_off = A.off

        cT = A.f32(24)
        sc = A.f32(24)
        Bsc = Buf()
        S.dma("sp", ("dma_start", dict(out=cT, in_=cT_d)), writes=[Bsc])
        S.op("act", ("activation", dict(out=sc, in_=cT, func=AF.Silu)), reads=[Bsc], writes=[Bsc])
        sc3 = r3(sc, 8)
        wblk = [A.f32(8 * 512) for _ in range(2)]
        Bwblk = [Buf(), Buf()]
        mrow = A.f32(6 * D)
        gb = A.f32(2 * D)
        bb = A.f32(6 * D)
        Bmrow, Bgb, Bbb = Buf(), Buf(), Buf()
        Bmodrows = Buf()
        for l in range(2):
            S.dma("sp", ("dma_start", dict(out=bb[0:3, :], in_=b_mod_d[l].broadcast_to([3, 6 * D]))), writes=[Bbb])
            S.dma("sp", ("dma_start", dict(out=gb[0:3, 0:D], in_=gmix_d[l].broadcast_to([3, D]))), writes=[Bgb])
            S.dma("sp", ("dma_start", dict(out=gb[0:3, D:2 * D], in_=gffn_d[l].broadcast_to([3, D]))), writes=[Bgb])
            for nb in range(12):
                wb = wblk[nb % 2]
                Bw = Bwblk[nb % 2]
                S.dma("sp", ("dma_start", dict(
                    out=r3(wb, 8), in_=w_mod_d[l][:, nb * 512:(nb + 1) * 512].rearrange("(k p) n -> p k n", p=128))), writes=[Bw])
                pb = nb % 2
                for k in range(8):
                    S.op("pe", ("matmul", dict(out=bank(pb)[0:3, :], lhsT=sc3[:, k, :], rhs=r3(wb, 8)[:, k, :],
                                                                       start=(k == 0), stop=(k == 7))), reads=[Bsc, Bw], writes=[PB[pb]])
                S.op("dve", ("tensor_tensor", dict(out=mrow[0:3, nb * 512:(nb + 1) * 512], in0=bank(pb)[0:3, :],
                                                                      in1=bb[0:3, nb * 512:(nb + 1) * 512], op=ALU.add)),
                     reads=[PB[pb], Bbb], writes=[Bmrow])
            for (kind, goff) in ((1, 0), (4, D)):
                S.op("dve", ("scalar_tensor_tensor", dict(
                    out=mrow[0:3, kind * D:(kind + 1) * D], in0=mrow[0:3, kind * D:(kind + 1) * D], scalar=1.0,
                    in1=gb[0:3, goff:goff + D], op0=ALU.add, op1=ALU.mult)), reads=[Bmrow, Bgb], writes=[Bmrow])
            S.dma("sp", ("dma_start", dict(out=modrows_d[l].rearrange("k c d -> c k d"), in_=r3(mrow[0:3, :], 6))),
                  reads=[Bmrow], writes=[Bmodrows])
        modT5 = modT.rearrange("p (l m k c) -> p l m k c", l=2, m=2, k=8)
        for l in range(2):
            for m in range(2):
                for c in range(3):
                    S.dma("sp", ("dma_start", dict(
                        out=modT5[:, l, m, :, c], in_=modrows_d[l, m, c].rearrange("(k p) -> p k", p=128),
                        allow_slow_non_contiguous=True)), reads=[Bmodrows], writes=[BmodT])
        S.barrier()
        A.off = persist_off
        if "modrows" in dbg_d:
            S.dma("sp", ("dma_start", dict(out=dbg_d["modrows"], in_=modrows_d.rearrange("l k c d -> (l k c) d"))), reads=[Bmodrows])

        def tile_src(layer, ti):
            if layer == 0:
                if ti < 32:
                    return x_d[ti * 128:(ti + 1) * 128, :]
                return ctx_d[(ti - 32) * 128:(ti - 31) * 128, :]
            return xr2_d[ti * 128:(ti + 1) * 128, :]

        def tile_col(ti):
            if ti < 32:
                return ti // 16
            return 2

        class NormT:
            def __init__(self):
                self.xt = [A.f32(D) for _ in range(2)]
                self.Bxt = [Buf(), Buf()]
                self.junk = A.bf16(D)
                self.Bjunk = Buf()
                self.xn = [A.bf16(D) for _ in range(2)]
                self.Bxn = [Buf(), Buf()]
                self.ss = [A.f32(1) for _ in range(2)]
                self.Bss = [Buf(), Buf()]
                self.i = 0

            def run(self, layer, ti, hT3, BhT, c0, psb):
                i = self.i
                self.i += 1
                xt, Bxt = self.xt[i % 2], self.Bxt[i % 2]
                xn, Bxn = self.xn[i % 2], self.Bxn[i % 2]
                ss, Bss = self.ss[i % 2], self.Bss[i % 2]
                junk, Bjunk = self.junk, self.Bjunk
                src = tile_src(layer, ti)
                col = tile_col(ti)
                S.dma("sp", ("dma_start", dict(out=xt, in_=src)), writes=[Bxt])
                S.op("act", ("activation", dict(out=junk, in_=xt, func=AF.Square, accum_out=ss)), reads=[Bxt], writes=[Bjunk, Bss])
                S.op("act", ("activation", dict(out=ss, in_=ss, func=AF.Sqrt, scale=1.0 / D, bias=EPS)), reads=[Bss], writes=[Bss])
                S.op("dve", ("reciprocal", dict(out=ss, in_=ss)), reads=[Bss], writes=[Bss])
                S.op("dve", ("tensor_scalar", dict(out=xn, in0=xt, scalar1=ss, scalar2=None, op0=ALU.mult)), reads=[Bxt, Bss], writes=[Bxn])
                pT = r3(bank_bf(psb), 8)
                for k in range(8):
                    S.op("pe", ("transpose", dict(out=pT[:, k, :], in_=xn[:, k * 128:(k + 1) * 128], identity=ident_b)),
                         reads=[Bxn, Bc], writes=[PB[psb]])
                for k in range(8):
                    eng = "act" if k % 2 == 0 else "dve"
                    if eng == "act":
                        S.op("act", ("activation", dict(out=hT3[:, k, c0:c0 + 128], in_=pT[:, k, :], func=AF.Identity,
                                                                  scale=modT5[:, layer, 1, k, col:col + 1], bias=modT5[:, layer, 0, k, col:col + 1])),
                             reads=[PB[psb], BmodT], writes=[BhT])
                    else:
                        S.op("dve", ("tensor_scalar", dict(out=hT3[:, k, c0:c0 + 128], in0=pT[:, k, :],
                                                                     scalar1=modT5[:, layer, 1, k, col:col + 1], scalar2=modT5[:, layer, 0, k, col:col + 1],
                                                                     op0=ALU.mult, op1=ALU.add)),
                             reads=[PB[psb], BmodT], writes=[BhT])

        def phase1():
            L = 0
            GELU = AF.Gelu_apprx_tanh
            WC = 1280
            w_in = A.bf16(8 * WC)
            w_in3 = r3(w_in, 8)
            Bwin = Buf()
            for k in range(8):
                S.dma("pool", ("dma_start", dict(out=w_in3[:, k, :], in_=w_in_d[k * 128:(k + 1) * 128, 512:1792])), writes=[Bwin])
            wst = A.bf16(8 * 512)
            wst5 = wst.rearrange("p (k j two d) -> p k j two d", k=8, j=4, two=2)
            for k in range(8):
                for two in range(2):
                    S.dma("pool", ("dma_start", dict(out=wst5[:, k, :, two, :],
                                                     in_=w_in_d[k * 128:(k + 1) * 128, two * 256:(two + 1) * 256].rearrange("p (j d) -> p j d", j=4))), writes=[Bwin])
            wst3 = r3(wst, 8)
            wpst = A.bf16(8 * 512)
            wpst3 = r3(wpst, 8)
            wkp = A.bf16(8 * 128)
            wkp3 = r3(wkp, 8)
            Bwperm = Buf()
            sv = wst.rearrange("p (k x b i) -> p k x b i", k=8, b=2, i=16)
            dv = wpst.rearrange("p (k x b i) -> p k x b i", k=8, b=2, i=16)
            svk = w_in3[:, :, 0:128].rearrange("p k (x b i) -> p k x b i", b=2, i=16)
            dvk = wkp3.rearrange("p k (x b i) -> p k x b i", b=2, i=16)
            for b_ in range(2):
                S.op("dve", ("tensor_copy", dict(out=dv[:, :, :, b_, :], in_=sv[:, :, :, 1 - b_, :])), reads=[Bwin], writes=[Bwperm])
                S.op("dve", ("tensor_copy", dict(out=dvk[:, :, :, b_, :], in_=svk[:, :, :, 1 - b_, :])), reads=[Bwin], writes=[Bwperm])
            wout = A.bf16(12 * 1024)
            wout3 = r3(wout, 12)
            Bwout = Buf()
            S.dma("pool", ("dma_start", dict(out=wout3[0:64, 0:4, :], in_=w_out_d[0:256, :].rearrange("(c r) n -> r c n", r=64))), writes=[Bwout])
            S.dma("pool", ("dma_start", dict(out=wout3[64:128, 0:4, :], in_=w_out_d[256:512, :].rearrange("(c r) n -> r c n", r=64))), writes=[Bwout])
            S.dma("pool", ("dma_start", dict(out=wout3[0:64, 4:8, :], in_=w_out_d[512:768, :].rearrange("(c r) n -> r c n", r=64))), writes=[Bwout])
            S.dma("pool", ("dma_start", dict(out=wout3[0:64, 8:12, :], in_=w_out_d[768:1024, :].rearrange("(c r) n -> r c n", r=64))), writes=[Bwout])
            yt = A.f32(D)
            wspn = yt
            Bwspn = Buf()
            S.dma("sp", ("dma_start", dict(out=r3(wspn, 8), in_=wsp_d.rearrange("g p q -> p g q"))), writes=[Bwspn])
            wspT = A.bf16(1024)
            wspT3 = r3(wspT, 8)
            BwspT = Buf()
            pw = r3(psum_t[:, 0:1024], 8)
            for g in range(8):
                S.op("pe", ("transpose", dict(out=pw[:, g, :], in_=r3(wspn, 8)[:, g, :], identity=ident_f)),
                     reads=[Bwspn, Bc], writes=[PB[0], PB[1]])
            S.op("act", ("copy", dict(out=wspT, in_=psum_t[:, 0:1024])), reads=[PB[0], PB[1]], writes=[BwspT])
            lng_bc = A.f32(512)
            bsp_bc = A.f32(1024)
            qkg = A.f32(4)
            cosT = A.f32(SEQ)
            sinT = A.f32(SEQ)
            gate_bc = [A.f32(D) for _ in range(3)]
            Bsm = Buf()
            S.dma("sp", ("dma_start", dict(out=lng_bc, in_=lng_d.broadcast_to([128, 512]))), writes=[Bsm])
            S.dma("sp", ("dma_start", dict(out=bsp_bc[0:64, :], in_=bsp_d.broadcast_to([64, 1024]))), writes=[Bsm])
            S.dma("sp", ("dma_start", dict(out=qkg, in_=qkg_d)), writes=[Bsm])
            S.dma("sp", ("dma_start", dict(out=cosT, in_=rope_d[:, 0:SEQ])), writes=[Bsm])
            S.dma("sp", ("dma_start", dict(out=sinT, in_=rope_d[:, SEQ:2 * SEQ])), writes=[Bsm])
            for c in range(3):
                S.dma("sp", ("dma_start", dict(out=gate_bc[c], in_=modrows_d[L, 2, c:c + 1, :].broadcast_to([128, D]))),
                      reads=[Bmodrows], writes=[Bsm])
            bsp3 = r3(bsp_bc[0:64, :], 8)

            BK0 = Buf()
            QT = A.bf16(4 * SEQ)
            QT3 = r3(QT, 4)
            KTz = [A.bf16(SEQ), A.bf16(SEQ)]
            S.op("dve", ("memset", dict(ap=KTz[0][64:128, :], constant=0.0)), writes=[BK0])
            S.op("dve", ("memset", dict(ap=KTz[1][0:64, :], constant=0.0)), writes=[BK0])
            Vt = A.bf16(18 * 128)
            V3 = r3(Vt, 18)
            BQ, BK, BV = Buf(), Buf(), Buf()
            hT1 = A.bf16(8 * 512)
            hT = [hT1, hT1]
            BhT1 = Buf()
            BhT = [BhT1, BhT1]
            NT = NormT()
            sqb = A.bf16(512)
            rs = A.f32(512)
            t1 = A.f32(512)
            t2 = A.f32(512)
            Bsq, Brs, Bt1, Bt2 = Buf(), Buf(), Buf(), Buf()
            mx = A.f32(1024)
            Bmx = Buf()
            rec = A.f32(512)
            Brec = Buf()
            sqb2 = [sqb, A.bf16(512)]
            rs2 = [rs, rec]
            t12 = [t1, mx[:, 0:512]]
            t22 = [t2, mx[:, 512:1024]]
            Bsq2, Brs2, Bt12, Bt22 = [Bsq, Buf()], [Brs, Brec], [Bt1, Bmx], [Bt2, Bmx]
            UG = A.bf16(8 * 512)
            UG3 = r3(UG, 8)
            GT3 = UG3
            OT = A.bf16(4 * 512)
            OT3 = r3(OT, 4)
            BUG = Buf()
            BGT = BUG
            BOT = Buf()
            gv = t1
            vnf = t2
            vnb = A.bf16(512)
            st6 = A.f32(6)
            mv = A.f32(2)
            Bgv, Bvnf = Bt1, Bt2
            Bvnb, Bst, Bmv = Buf(), Buf(), Buf()
            PT = [A.bf16(512) for _ in range(4)]
            BPT = [Buf(), Buf(), Buf(), Buf()]
            rec = A.f32(512)
            Brec = Buf()
            xt2 = A.f32(D)
            Byt, Bxt2 = Buf(), Buf()
            Bxr = Buf()
            print('phase1 arena words', A.off)
            hcount = [0]

            def make_hT(blk):
                tiles, c0seq, N = blk
                i = hcount[0]
                hcount[0] += 1
                h3 = r3(hT[i % 2], 8)
                for t, ti in enumerate(tiles):
                    NT.run(L, ti, h3, BhT[i % 2], t * 128, t % 2)
                return h3, BhT[i % 2]

            for b in range(NB):
                blocks = [([32 + 2 * b, 33 + 2 * b], 0, 256)]
                for i in range(4):
                    blocks.append(([16 * b + 4 * i + t for t in range(4)], 256 + 512 * i, 512))
                for blk in blocks:
                    tiles, c0, N = blk
                    h3, Bh = make_hT(blk)
                    for j in range(5):
                        bq, bqp, bms = (2, 3, 4) if j % 2 == 0 else (5, 6, 7)
                        sqb_, rs_, t1_, t2_ = sqb2[j % 2], rs2[j % 2], t12[j % 2], t22[j % 2]
                        Bsq_, Brs_, Bt1_, Bt2_ = Bsq2[j % 2], Brs2[j % 2], Bt12[j % 2], Bt22[j % 2]
                        for (pb, wq, wk) in ((bq, wst3, w_in3), (bqp, wpst3, wkp3)):
                            for k in range(8):
                                lw = wq[:, k, j * 128:(j + 1) * 128] if j < 4 else wk[:, k, 0:128]
                                S.op("pe", ("matmul", dict(out=bank(pb)[:, 0:N], lhsT=lw, rhs=h3[:, k, 0:N], start=(k == 0), stop=(k == 7))),
                                     reads=[Bwin, Bwperm, Bh], writes=[PB[pb]])
                        gi = 0 if j < 4 else 2
                        S.op("act", ("activation", dict(out=sqb_[:, 0:N], in_=bank(bq)[:, 0:N], func=AF.Square)), reads=[PB[bq]], writes=[Bsq_])
                        S.op("pe", ("matmul", dict(out=bank(bms)[:, 0:N], lhsT=blk_b, rhs=sqb_[:, 0:N], start=True, stop=True)), reads=[Bsq_, Bc], writes=[PB[bms]])
                        S.op("act", ("activation", dict(out=rs_[:, 0:N], in_=bank(bms)[:, 0:N], func=AF.Sqrt, bias=EPS, scale=1.0)), reads=[PB[bms]], writes=[Brs_])
                        S.op("dve", ("reciprocal", dict(out=rs_[:, 0:N], in_=rs_[:, 0:N])), reads=[Brs_], writes=[Brs_])
                        S.op("dve", ("scalar_tensor_tensor", dict(out=t1_[:, 0:N], in0=bank(bq)[:, 0:N], scalar=qkg[:, gi:gi + 1], in1=cosT[:, c0:c0 + N],
                                                                             op0=ALU.mult, op1=ALU.mult)), reads=[PB[bq], Bsm], writes=[Bt1_])
                        S.op("dve", ("scalar_tensor_tensor", dict(out=t2_[:, 0:N], in0=bank(bqp)[:, 0:N], scalar=qkg[:, gi + 1:gi + 2], in1=sinT[:, c0:c0 + N],
                                                                             op0=ALU.mult, op1=ALU.mult)), reads=[PB[bqp], Bsm], writes=[Bt2_])
                        S.op("dve", ("tensor_tensor", dict(out=t1_[:, 0:N], in0=t1_[:, 0:N], in1=t2_[:, 0:N], op=ALU.add)), reads=[Bt1_, Bt2_], writes=[Bt1_])
                        if j < 4:
                            S.op("dve", ("tensor_tensor", dict(out=QT3[:, j, c0:c0 + N], in0=t1_[:, 0:N], in1=rs_[:, 0:N], op=ALU.mult)), reads=[Bt1_, Brs_], writes=[BQ])
                        else:
                            for hf_ in range(2):
                                ps_ = slice(hf_ * 64, hf_ * 64 + 64)
                                S.op("dve", ("tensor_tensor", dict(out=KTz[hf_][ps_, c0:c0 + N], in0=t1_[ps_, 0:N], in1=rs_[ps_, 0:N], op=ALU.mult)),
                                     reads=[Bt1_, Brs_, BK0], writes=[BK])
                    for t in range(len(tiles)):
                        kt = c0 // 128 + t
                        for k in range(8):
                            S.op("pe", ("matmul", dict(out=bank(5)[:, 0:128], lhsT=h3[:, k, t * 128:(t + 1) * 128], rhs=w_in3[:, k, 128:256],
                                                                      start=(k == 0), stop=(k == 7))), reads=[Bwin, Bh], writes=[PB[5]])
                        S.op("act", ("copy", dict(out=V3[:, kt, :], in_=bank(5)[:, 0:128])), reads=[PB[5]], writes=[BV])
                for bi, blk in enumerate(blocks):
                    tiles, c0, N = blk
                    h3, Bh = make_hT(blk)
                    for g in range(8):
                        pb = 2 + g % 2
                        for k in range(8):
                            S.op("pe", ("matmul", dict(out=bank(pb)[0:64, 0:N], lhsT=w_in3[:, k, 256 + g * 64:320 + g * 64], rhs=h3[:, k, 0:N],
                                                                             start=(k == 0), stop=(k == 7))), reads=[Bwin, Bh], writes=[PB[pb]])
                        S.op("act", ("activation", dict(out=UG3[0:64, g, 0:N], in_=bank(pb)[0:64, 0:N], func=GELU)), reads=[PB[pb]], writes=[BUG])
                    for t in range(len(tiles)):
                        tc0 = t * 128
                        for k in range(8):
                            S.op("pe", ("matmul", dict(out=bank(4), lhsT=h3[:, k, tc0:tc0 + 128], rhs=w_in3[:, k, 768:1280],
                                                                          start=(k == 0), stop=(k == 7))), reads=[Bwin, Bh], writes=[PB[4]])
                        S.op("act", ("activation", dict(out=gv, in_=bank(4), func=GELU)), reads=[PB[4]], writes=[Bgv])
                        S.op("dve", ("bn_stats", dict(out=st6, in_=gv)), reads=[Bgv], writes=[Bst])
                        S.op("dve", ("bn_aggr", dict(out=mv, in_=st6)), reads=[Bst], writes=[Bmv])
                        S.op("act", ("activation", dict(out=mv[:, 1:2], in_=mv[:, 1:2], func=AF.Sqrt, bias=EPS, scale=1.0)), reads=[Bmv], writes=[Bmv])
                        S.op("dve", ("reciprocal", dict(out=mv[:, 1:2], in_=mv[:, 1:2])), reads=[Bmv], writes=[Bmv])
                        S.op("dve", ("tensor_scalar", dict(out=vnf, in0=gv, scalar1=mv[:, 0:1], scalar2=mv[:, 1:2], op0=ALU.subtract, op1=ALU.mult)),
                             reads=[Bgv, Bmv], writes=[Bvnf])
                        S.op("dve", ("tensor_tensor", dict(out=vnb, in0=vnf, in1=lng_bc, op=ALU.mult)), reads=[Bvnf, Bsm], writes=[Bvnb])
                        pm = r3(psum_t[0:64, 6 * 512:8 * 512], 8)
                        for g in range(8):
                            S.op("pe", ("matmul", dict(out=pm[:, g, :], lhsT=vnb[:, g * 64:(g + 1) * 64], rhs=wspT3[:, g, :], start=True, stop=True)),
                                 reads=[Bvnb, BwspT], writes=[PB[6], PB[7]])
                        S.op("dve", ("tensor_tensor", dict(out=r3(mx[0:64, :], 8), in0=pm, in1=bsp3, op=ALU.add)), reads=[PB[6], PB[7], Bsm], writes=[Bmx])
                        S.op("dve", ("tensor_tensor", dict(out=GT3[0:64, :, tc0:tc0 + 128], in0=r3(mx[0:64, :], 8), in1=UG3[0:64, :, tc0:tc0 + 128], op=ALU.mult)),
                             reads=[Bmx, BUG], writes=[BGT])
                    kts = list(range(2)) if bi == 0 else list(range(18))
                    steps = [(h, ki, kt) for h in range(8) for ki, kt in enumerate(kts)]
                    nk = len(kts)

                    def issue_S(idx):
                        h, ki, kt = steps[idx]
                        half, j = h // 4, h % 4
                        sb_ = (0, 1, 6)[idx % 3]
                        S.op("pe", ("matmul", dict(out=bank(sb_)[:, 0:N], lhsT=KTz[half][:, kt * 128:(kt + 1) * 128],
                                                   rhs=QT3[:, j, c0:c0 + N], start=True, stop=True)), reads=[BK, BQ], writes=[PB[sb_]])
                        pt, Bpt = PT[idx % 4], BPT[idx % 4]
                        S.op("act", ("activation", dict(out=pt[:, 0:N], in_=bank(sb_)[:, 0:N], func=AF.Exp, scale=0.125)), reads=[PB[sb_]], writes=[Bpt])

                    def issue_PV(idx):
                        h, ki, kt = steps[idx]
                        half, j = h // 4, h % 4
                        p0 = half * 64
                        bo, bd = 2 + (h % 2) * 2, 3 + (h % 2) * 2
                        pt, Bpt = PT[idx % 4], BPT[idx % 4]
                        S.op("pe", ("matmul", dict(out=bank(bo)[:, 0:N], lhsT=V3[:, kt, :], rhs=pt[:, 0:N],
                                                   start=(ki == 0), stop=(ki == nk - 1))), reads=[BV, Bpt], writes=[PB[bo]])
                        S.op("pe", ("matmul", dict(out=bank(bd)[:, 0:N], lhsT=ones_b, rhs=pt[:, 0:N],
                                                   start=(ki == 0), stop=(ki == nk - 1))), reads=[Bc, Bpt], writes=[PB[bd]])
                        if ki == nk - 1:
                            S.op("dve", ("reciprocal", dict(out=rec[p0:p0 + 64, 0:N], in_=bank(bd)[p0:p0 + 64, 0:N])), reads=[PB[bd]], writes=[Brec])
                            S.op("dve", ("tensor_tensor", dict(out=OT3[p0:p0 + 64, j, 0:N], in0=bank(bo)[p0:p0 + 64, 0:N], in1=rec[p0:p0 + 64, 0:N], op=ALU.mult)),
                                 reads=[PB[bo], Brec], writes=[BOT])

                    for idx in range(len(steps) + 2):
                        if idx < len(steps):
                            issue_S(idx)
                        if idx >= 2:
                            issue_PV(idx - 2)
                    for t, ti in enumerate(tiles):
                        tc0 = t * 128
                        col = tile_col(ti)
                        for hf in range(2):
                            for c in range(12):
                                if c < 4:
                                    lw, rw = OT3[:, c, tc0:tc0 + 128], wout3[:, c, hf * 512:(hf + 1) * 512]
                                else:
                                    lw, rw = GT3[0:64, c - 4, tc0:tc0 + 128], wout3[0:64, c, hf * 512:(hf + 1) * 512]
                                S.op("pe", ("matmul", dict(out=bank(6 + hf), lhsT=lw, rhs=rw,
                                                                                 start=(c == 0), stop=(c == 11))), reads=[BOT, BGT, Bwout], writes=[PB[6 + hf]])
                        S.dma("sp", ("dma_start", dict(out=xt2, in_=tile_src(L, ti))), writes=[Bxt2])
                        S.op("dve", ("tensor_tensor", dict(out=yt, in0=psum_t[:, 6 * 512:8 * 512], in1=gate_bc[col], op=ALU.mult)),
                             reads=[PB[6], PB[7], Bsm], writes=[Byt])
                        S.op("dve", ("tensor_tensor", dict(out=yt, in0=yt, in1=xt2, op=ALU.add)), reads=[Byt, Bxt2], writes=[Byt])
                        S.dma("sp", ("dma_start", dict(out=xr_d[ti * 128:(ti + 1) * 128, :], in_=yt)), reads=[Byt], writes=[Bxr])
            return Bxr

        if stop_after >= 1:
            Bxr = phase1()
            S.barrier()
            A.off = persist_off
            if "xr" in dbg_d:
                S.dma("sp", ("dma_start", dict(out=dbg_d["xr"], in_=xr_d)), reads=[Bxr])
        def moe(L, ntiles, Bsrc):
            last = (L == 1)
            ncol = 2 if last else 3
            Abc = [A.f32(D) for _ in range(ncol)]
            Sbc = [A.f32(D) for _ in range(ncol)]
            Gbc = [A.f32(D) for _ in range(ncol)]
            Bbc = Buf()
            for c in range(ncol):
                for (kind, dst) in ((4, Abc), (3, Sbc), (5, Gbc)):
                    S.dma("sp", ("dma_start", dict(out=dst[c], in_=modrows_d[L, kind, c:c + 1, :].broadcast_to([128, D]))), reads=[Bmodrows], writes=[Bbc])
            fng = A.f32(D)
            if last:
                S.dma("sp", ("dma_start", dict(out=fng, in_=fng_d.broadcast_to([128, D]))), writes=[Bbc])
            w36 = A.f32(8 * 36)
            w36_3 = r3(w36, 8)
            b36 = A.f32(36)
            S.dma("sp", ("dma_start", dict(out=w36_3[:, :, 0:4], in_=wgrp_d[L].rearrange("(k p) n -> p k n", p=128), allow_slow_non_contiguous=True)), writes=[Bbc])
            S.dma("sp", ("dma_start", dict(out=w36_3[:, :, 4:36], in_=wrt_d[L].rearrange("(k p) n -> p k n", p=128), allow_slow_non_contiguous=True)), writes=[Bbc])
            S.dma("sp", ("dma_start", dict(out=b36[:, 0:4], in_=bgrp_d[L].broadcast_to([128, 4]))), writes=[Bbc])
            S.dma("sp", ("dma_start", dict(out=b36[:, 4:36], in_=brt_d[L].broadcast_to([128, 32]))), writes=[Bbc])
            slot_i = A.i32(ntiles * 2)
            slot_i3 = r3(slot_i, ntiles)
            wts = A.f32(ntiles * 2)
            wts3 = r3(wts, ntiles)
            Bslot = Buf()
            Bwts = Buf()
            run = A.f32(32)
            Brun = Buf()
            S.op("dve", ("memset", dict(ap=run, constant=0.0)), writes=[Brun])
            ztile = A.f32(D)
            Bz = Buf()
            Byall = Buf()
            Bxall = Buf()
            S.op("dve", ("memset", dict(ap=ztile, constant=0.0)), writes=[Bz])
            S.dma("sp", ("dma_start", dict(out=yall_d[NSLOT:NSLOT + 128, :], in_=ztile)), reads=[Bz], writes=[Byall])
            mark = A.off
            xt = [A.f32(D) for _ in range(2)]
            Bxt = [Buf(), Buf()]
            ff = [A.f32(D) for _ in range(2)]
            Bff = [Buf(), Buf()]
            fb = [A.bf16(D) for _ in range(2)]
            Bfb = [Buf(), Buf()]
            fTs = A.f32(D)
            BfTs = Buf()
            junk = A.bf16(D)
            Bjunk = Buf()
            sm = A.f32(256)
            Bsmall = Buf()
            ss = sm[:, 0:1]
            lg = sm[:, 4:40]
            gmax = sm[:, 40:41]
            ngmax = sm[:, 41:42]
            gsum = sm[:, 42:43]
            eg = sm[:, 44:48]
            gmask = sm[:, 48:52]
            pen = sm[:, 52:56]
            masked = sm[:, 56:88]
            m8 = sm[:, 88:96]
            ntop1 = sm[:, 96:97]
            e2 = sm[:, 97:98]
            wa = sm[:, 98:99]
            wb = sm[:, 99:100]
            slotf = sm[:, 100:102]
            vab = sm[:, 102:104]
            sel1 = sm[:, 104:136]
            sel = sm[:, 136:168]
            pos = sm[:, 168:200]
            valid = sm[:, 200:232]
            tmp32 = A.f32(32)
            selb = A.bf16(32)
            fTs2 = [fTs, A.f32(D)]
            BfTs2 = [BfTs, Buf()]
            ssA = [A.f32(1), A.f32(1)]
            BssA = [Buf(), Buf()]
            lgb = [A.f32(36), A.f32(36)]
            Blg = [Buf(), Buf()]

            def stageA(ti):
                col = tile_col(ti)
                x_, Bx_ = xt[ti % 2], Bxt[ti % 2]
                f_, Bf_ = ff[ti % 2], Bff[ti % 2]
                fb_, Bfb_ = fb[ti % 2], Bfb[ti % 2]
                ss_, Bss_ = ssA[ti % 2], BssA[ti % 2]
                fT_, BfT_ = fTs2[ti % 2], BfTs2[ti % 2]
                S.dma("sp", ("dma_start", dict(out=x_, in_=xr_d[ti * 128:(ti + 1) * 128, :])), reads=[Bsrc], writes=[Bx_])
                S.op("act", ("activation", dict(out=junk, in_=x_, func=AF.Square, accum_out=ss_)), reads=[Bx_], writes=[Bjunk, Bss_])
                S.op("act", ("activation", dict(out=ss_, in_=ss_, func=AF.Sqrt, scale=1.0 / D, bias=EPS)), reads=[Bss_], writes=[Bss_])
                S.op("dve", ("reciprocal", dict(out=ss_, in_=ss_)), reads=[Bss_], writes=[Bss_])
                S.op("dve", ("scalar_tensor_tensor", dict(out=f_, in0=x_, scalar=ss_, in1=Abc[col], op0=ALU.mult, op1=ALU.mult)), reads=[Bx_, Bss_, Bbc], writes=[Bf_])
                S.op("dve", ("tensor_tensor", dict(out=f_, in0=f_, in1=Sbc[col], op=ALU.add)), reads=[Bf_, Bbc], writes=[Bf_])
                S.op("act", ("copy", dict(out=fb_, in_=f_)), reads=[Bf_], writes=[Bfb_])
                pT = r3(psum_t[:, 0:1024], 8)
                for k in range(8):
                    S.op("pe", ("transpose", dict(out=pT[:, k, :], in_=f_[:, k * 128:(k + 1) * 128], identity=ident_f)), reads=[Bf_, Bc], writes=[PB[0], PB[1]])
                S.op("act", ("copy", dict(out=fT_, in_=psum_t[:, 0:1024])), reads=[PB[0], PB[1]], writes=[BfT_])
                pl = 2 + ti % 2
                for k in range(8):
                    S.op("pe", ("matmul", dict(out=bank(pl)[:, 0:36], lhsT=fT_[:, k * 128:(k + 1) * 128], rhs=w36_3[:, k, :], start=(k == 0), stop=(k == 7))),
                         reads=[BfT_, Bbc], writes=[PB[pl]])

            def stageB(ti):
                lg = lgb[ti % 2]
                fb_, Bfb_ = fb[ti % 2], Bfb[ti % 2]
                pl = 2 + ti % 2
                S.op("dve", ("tensor_tensor", dict(out=lgb[ti % 2], in0=bank(pl)[:, 0:36], in1=b36, op=ALU.add)), reads=[PB[pl], Bbc], writes=[Blg[ti % 2]])
                dv = lambda name, **kw: S.op("dve", (name, kw), reads=[Bsmall, Brun, Blg[ti % 2]], writes=[Bsmall])
                dv("tensor_reduce", out=ngmax, in_=lg[:, 0:4], axis=AX.X, op=ALU.max, negate=True)
                dv("tensor_scalar", out=gmask, in0=lg[:, 0:4], scalar1=ngmax, scalar2=0.0, op0=ALU.add, op1=ALU.is_ge)
                dv("tensor_scalar", out=pen, in0=gmask, scalar1=1e30, scalar2=-1e30, op0=ALU.mult, op1=ALU.add)
                dv("tensor_tensor", out=r3(masked, 4), in0=r3(lg[:, 4:36], 4), in1=pen.unsqueeze(2).broadcast_to([128, 4, 8]), op=ALU.add)
                dv("max", out=m8, in_=masked)
                dv("tensor_scalar", out=ntop1, in0=m8[:, 0:1], scalar1=-1.0, scalar2=None, op0=ALU.mult)
                S.op("act", ("activation", dict(out=eg, in_=lg[:, 0:4], func=AF.Exp, bias=ngmax, scale=1.0, accum_out=gsum)), reads=[Bsmall, Blg[ti % 2]], writes=[Bact])
                S.op("act", ("activation", dict(out=e2, in_=m8[:, 1:2], func=AF.Exp, bias=ntop1, scale=1.0)), reads=[Bsmall], writes=[Bact])
                dv("tensor_scalar", out=sel1, in0=masked, scalar1=m8[:, 0:1], scalar2=None, op0=ALU.is_ge)
                dv("tensor_scalar", out=sel, in0=masked, scalar1=m8[:, 1:2], scalar2=None, op0=ALU.is_ge)
                dv("tensor_copy", out=selb, in_=sel)
                S.op("pe", ("matmul", dict(out=bank(4)[:, 0:32], lhsT=utri_b, rhs=selb, start=True, stop=True)), reads=[Bsmall, Bc], writes=[PB[4]])
                S.op("pe", ("matmul", dict(out=bank(4)[:, 32:64], lhsT=ones_b, rhs=selb, start=True, stop=True)), reads=[Bsmall, Bc], writes=[PB[4]])
                dv("tensor_tensor", out=sel, in0=sel, in1=sel1, op=ALU.subtract)
                S.op("dve", ("tensor_tensor", dict(out=pos, in0=bank(4)[:, 0:32], in1=run, op=ALU.add)), reads=[PB[4], Brun, Bsmall], writes=[Bsmall])
                S.op("dve", ("tensor_tensor", dict(out=run, in0=bank(4)[:, 32:64], in1=run, op=ALU.add)), reads=[PB[4], Brun, Bsmall], writes=[Brun])
                dv("tensor_scalar", out=valid, in0=pos, scalar1=float(CAP), scalar2=None, op0=ALU.is_lt)
                dv("tensor_tensor", out=pos, in0=pos, in1=iotaC, op=ALU.add)
                dv("scalar_tensor_tensor", out=pos, in0=pos, scalar=-float(TRASH), in1=valid, op0=ALU.add, op1=ALU.mult)
                selcat = r3(sm[:, 104:168], 2)
                pvcat = r3(sm[:, 168:232], 2)
                t4 = tmp128.rearrange("p (a c e) -> p a c e", a=2, c=2)
                dv("tensor_tensor", out=t4, in0=selcat.unsqueeze(2).broadcast_to([128, 2, 2, 32]), in1=pvcat.unsqueeze(1).broadcast_to([128, 2, 2, 32]), op=ALU.mult)
                dv("tensor_reduce", out=r4, in_=r3(tmp128, 4), axis=AX.X, op=ALU.add)
                S.op("dve", ("tensor_scalar", dict(out=slot_i3[:, ti, :], in0=r4[:, 0:4:2], scalar1=float(TRASH), scalar2=None, op0=ALU.add)), reads=[Bsmall], writes=[Bslot])
                for a_ in range(2):
                    S.dma("pool", ("indirect_dma_start", dict(out=xall_d, out_offset=bass.IndirectOffsetOnAxis(ap=slot_i3[:, ti, a_:a_ + 1], axis=0),
                                                             in_=fb_, in_offset=None)), reads=[Bfb_, Bslot], writes=[])
                S.op("dve", ("tensor_scalar", dict(out=wa, in0=e2, scalar1=1.0, scalar2=gsum, op0=ALU.add, op1=ALU.mult)), reads=[Bact, Bsmall], writes=[Bsmall])
                dv("reciprocal", out=wa, in_=wa)
                S.op("dve", ("tensor_tensor", dict(out=wb, in0=wa, in1=e2, op=ALU.mult)), reads=[Bact, Bsmall], writes=[Bsmall])
                S.op("dve", ("tensor_tensor", dict(out=wts3[:, ti, :], in0=sm[:, 98:100], in1=r4[:, 1:4:2], op=ALU.mult)), reads=[Bsmall], writes=[Bwts])

            tmp128 = A.f32(128)
            r4 = sm[:, 240:244]
            Bact = Buf()
            sma = A.f32(8)
            eg = sma[:, 0:4]
            gsum = sma[:, 4:5]
            e2 = sma[:, 5:6]
            for step in range(ntiles + 1):
                if step < ntiles:
                    stageA(step)
                if step >= 1:
                    stageB(step - 1)
            S.barrier()
            A.off = mark
            if last is False and "slots" in dbg_d:
                pass
            NJ = CAP // 128
            wbuf = [(A.bf16(8 * 512), A.bf16(8 * 512), A.bf16(4 * 1024)) for _ in range(2)]
            Bwb = [Buf(), Buf()]
            xrows = [A.bf16(NJ * D) for _ in range(2)]
            Bxrows = [Buf(), Buf()]
            XT = A.bf16(8 * CAP)
            XT3 = r3(XT, 8)
            BXT = Buf()
            sl = [A.f32(512) for _ in range(2)]
            Bsl = [Buf(), Buf()]
            hs = A.bf16(4 * CAP)
            hs3 = r3(hs, 4)
            Bhs = Buf()
            ysb = [A.f32(D) for _ in range(2)]
            Bysb = [Buf(), Buf()]
            yi = 0
            stg = (A.f32(8 * 512), A.f32(8 * 512), A.f32(4 * 1024))
            Bstg = [Buf(), Buf(), Buf()]

            def load_w(e_):
                S.dma("sp", ("dma_start", dict(out=r3(stg[0], 8), in_=w1_d[L, e_].rearrange("(k p) n -> p k n", p=128))), writes=[Bstg[0]])
                S.dma("sp", ("dma_start", dict(out=r3(stg[1], 8), in_=w3_d[L, e_].rearrange("(k p) n -> p k n", p=128))), writes=[Bstg[1]])
                S.dma("sp", ("dma_start", dict(out=r3(stg[2], 4), in_=w2_d[L, e_].rearrange("(k p) n -> p k n", p=128))), writes=[Bstg[2]])

            def cast_w(e_, which):
                dst = wbuf[e_ % 2][which]
                Bw_ = Bwb[e_ % 2]
                if which == 0:
                    S.op("act", ("copy", dict(out=dst, in_=stg[0])), reads=[Bstg[0]], writes=[Bw_])
                elif which == 1:
                    S.op("dve", ("tensor_copy", dict(out=dst, in_=stg[1])), reads=[Bstg[1]], writes=[Bw_])
                else:
                    S.op("act", ("copy", dict(out=dst[:, 0:2048], in_=stg[2][:, 0:2048])), reads=[Bstg[2]], writes=[Bw_])
                    S.op("dve", ("tensor_copy", dict(out=dst[:, 2048:4096], in_=stg[2][:, 2048:4096])), reads=[Bstg[2]], writes=[Bw_])

            def load_x(e_):
                S.dma("sp", ("dma_start", dict(out=r3(xrows[e_ % 2], NJ), in_=xall_d[e_ * CAP:(e_ + 1) * CAP, :].rearrange("(j p) d -> p j d", p=128))),
                      reads=[Bxall], writes=[Bxrows[e_ % 2]])

            load_w(0)
            load_x(0)
            for w_ in range(3):
                cast_w(0, w_)
            for e_ in range(32):
                w1b, w3b, w2b = wbuf[e_ % 2]
                Bw = Bwb[e_ % 2]
                xr_, Bxr_ = xrows[e_ % 2], Bxrows[e_ % 2]
                if e_ + 1 < 32:
                    load_w(e_ + 1)
                    load_x(e_ + 1)
                for j in range(NJ):
                    pb = j % 2
                    pT = r3(bank_bf(pb), 8)
                    for k in range(8):
                        S.op("pe", ("transpose", dict(out=pT[:, k, :], in_=r3(xr_, NJ)[:, j, k * 128:(k + 1) * 128], identity=ident_b)),
                             reads=[Bxr_, Bc], writes=[PB[pb]])
                    S.op("act" if j % 2 == 0 else "dve", ("tensor_copy" if j % 2 else "copy", dict(out=XT3[:, :, j * 128:(j + 1) * 128], in_=pT)),
                         reads=[PB[pb]], writes=[BXT])
                w1v, w3v, w2v = r3(w1b, 8), r3(w3b, 8), r3(w2b, 4)
                for bi_, (c0, n) in enumerate(((0, 512), (512, CAP - 512))):
                    for m in range(4):
                        for (pb, wv) in ((2 + (m % 2) * 2, w1v), (3 + (m % 2) * 2, w3v)):
                            for k in range(8):
                                S.op("pe", ("matmul", dict(out=bank(pb)[:, 0:n], lhsT=wv[:, k, m * 128:(m + 1) * 128], rhs=XT3[:, k, c0:c0 + n], start=(k == 0), stop=(k == 7))),
                                     reads=[Bw, BXT], writes=[PB[pb]])
                        p1, p3 = 2 + (m % 2) * 2, 3 + (m % 2) * 2
                        s_, Bs_ = sl[m % 2], Bsl[m % 2]
                        S.op("act", ("activation", dict(out=s_[:, 0:n], in_=bank(p1)[:, 0:n], func=AF.Silu)), reads=[PB[p1]], writes=[Bs_])
                        S.op("dve", ("tensor_tensor", dict(out=hs3[:, m, c0:c0 + n], in0=bank(p3)[:, 0:n], in1=s_[:, 0:n], op=ALU.mult)), reads=[PB[p3], Bs_], writes=[Bhs])
                    if e_ + 1 < 32:
                        cast_w(e_ + 1, bi_)
                for j in range(NJ):
                    for hf in range(2):
                        for m in range(4):
                            S.op("pe", ("matmul", dict(out=bank(6 + hf), lhsT=hs3[:, m, j * 128:(j + 1) * 128], rhs=w2v[:, m, hf * 512:(hf + 1) * 512], start=(m == 0), stop=(m == 3))),
                                 reads=[Bhs, Bw], writes=[PB[6 + hf]])
                    y_, By_ = ysb[yi % 2], Bysb[yi % 2]
                    yi += 1
                    S.op("act", ("copy", dict(out=y_[:, 0:512], in_=bank(6))), reads=[PB[6]], writes=[By_])
                    S.op("dve", ("tensor_copy", dict(out=y_[:, 512:1024], in_=bank(7))), reads=[PB[7]], writes=[By_])
                    r0 = e_ * CAP + j * 128
                    S.dma("sp", ("dma_start", dict(out=yall_d[r0:r0 + 128, :], in_=y_)), reads=[By_], writes=[Byall])
                    if j == 1 and e_ + 1 < 32:
                        cast_w(e_ + 1, 2)
            S.barrier()
            A.off = mark
            ya = [A.f32(D) for _ in range(2)]
            yb = [A.f32(D) for _ in range(2)]
            x1 = [A.f32(D) for _ in range(2)]
            Bya, Byb, Bx1 = [Buf(), Buf()], [Buf(), Buf()], [Buf(), Buf()]
            junk2 = A.bf16(D)
            Bj2 = Buf()
            ss2 = [A.f32(1) for _ in range(2)]
            Bss2 = [Buf(), Buf()]
            Bdst = Buf()
            for ti in range(ntiles):
                col = tile_col(ti)
                i2 = ti % 2
                S.dma("pool", ("indirect_dma_start", dict(out=ya[i2], out_offset=None, in_=yall_d, in_offset=bass.IndirectOffsetOnAxis(ap=slot_i3[:, ti, 0:1], axis=0))),
                      reads=[Byall, Bslot], writes=[Bya[i2]])
                S.dma("pool", ("indirect_dma_start", dict(out=yb[i2], out_offset=None, in_=yall_d, in_offset=bass.IndirectOffsetOnAxis(ap=slot_i3[:, ti, 1:2], axis=0))),
                      reads=[Byall, Bslot], writes=[Byb[i2]])
                S.dma("sp", ("dma_start", dict(out=x1[i2], in_=xr_d[ti * 128:(ti + 1) * 128, :])), reads=[Bsrc], writes=[Bx1[i2]])
                S.op("act", ("activation", dict(out=ya[i2], in_=ya[i2], func=AF.Copy, scale=wts3[:, ti, 0:1])), reads=[Bya[i2], Bwts], writes=[Bya[i2]])
                S.op("dve", ("scalar_tensor_tensor", dict(out=yb[i2], in0=yb[i2], scalar=wts3[:, ti, 1:2], in1=ya[i2], op0=ALU.mult, op1=ALU.add)),
                     reads=[Byb[i2], Bya[i2], Bwts], writes=[Byb[i2]])
                S.op("dve", ("tensor_tensor", dict(out=yb[i2], in0=yb[i2], in1=Gbc[col], op=ALU.mult)), reads=[Byb[i2], Bbc], writes=[Byb[i2]])
                S.op("dve", ("tensor_tensor", dict(out=x1[i2], in0=x1[i2], in1=yb[i2], op=ALU.add)), reads=[Bx1[i2], Byb[i2]], writes=[Bx1[i2]])
                if not last:
                    S.dma("sp", ("dma_start", dict(out=xr2_d[ti * 128:(ti + 1) * 128, :], in_=x1[i2])), reads=[Bx1[i2]], writes=[Bdst])
                else:
                    S.op("act", ("activation", dict(out=junk2, in_=x1[i2], func=AF.Square, accum_out=ss2[i2])), reads=[Bx1[i2]], writes=[Bj2, Bss2[i2]])
                    S.op("act", ("activation", dict(out=ss2[i2], in_=ss2[i2], func=AF.Sqrt, scale=1.0 / D, bias=EPS)), reads=[Bss2[i2]], writes=[Bss2[i2]])
                    S.op("dve", ("reciprocal", dict(out=ss2[i2], in_=ss2[i2])), reads=[Bss2[i2]], writes=[Bss2[i2]])
                    S.op("dve", ("scalar_tensor_tensor", dict(out=x1[i2], in0=x1[i2], scalar=ss2[i2], in1=fng, op0=ALU.mult, op1=ALU.mult)),
                         reads=[Bx1[i2], Bss2[i2], Bbc], writes=[Bx1[i2]])
                    S.dma("sp", ("dma_start", dict(out=out_d[ti * 128:(ti + 1) * 128, :], in_=x1[i2])), reads=[Bx1[i2]], writes=[Bdst])
            return Bdst

        if stop_after >= 2:
            Bxr2 = moe(0, 36, Bxr)
            S.barrier()
            A.off = persist_off
            if "xr2" in dbg_d:
                S.dma("sp", ("dma_start", dict(out=dbg_d["xr2"], in_=xr2_d)), reads=[Bxr2])
        def s5_mixer(Bsrc):
            L = 1
            TWO_PI = 6.283185307179586
            MAGIC = 12582912.0
            yacc = [A.f32(4 * S_LAT) for _ in range(NB)]
            yacc3 = [r3(y, 4) for y in yacc]
            Byacc = [Buf(), Buf()]
            uT = [A.bf16(4 * SEQ) for _ in range(NB)]
            uT3 = [r3(u, 4) for u in uT]
            BuT = [Buf(), Buf()]
            sd = A.f32(8)
            Bsd = Buf()
            S.dma("sp", ("dma_start", dict(out=sd[:, 0:4], in_=s5d_d)), writes=[Bsd])
            S.dma("sp", ("dma_start", dict(out=sd[:, 4:8], in_=s5bglu_d)), writes=[Bsd])
            mark = A.off
            if S5_DEBUG_STAGE < 1:
                return Byacc[0]
            w5 = A.bf16(8 * 512)
            w5_3 = r3(w5, 8)
            Bw5 = Buf()
            S.dma("pool", ("dma_start", dict(out=w5_3, in_=s5win_d.rearrange("(k p) n -> p k n", p=128))), writes=[Bw5])
            hT1 = A.bf16(8 * 512)
            h3 = r3(hT1, 8)
            Bh = Buf()
            NT = NormT()
            for b in range(NB):
                blocks = [([32 + 2 * b, 33 + 2 * b], 0, 256)]
                for i in range(4):
                    blocks.append(([16 * b + 4 * i + t for t in range(4)], 256 + 512 * i, 512))
                for (tiles, c0, N) in blocks:
                    for t, ti in enumerate(tiles):
                        NT.run(L, ti, h3, Bh, t * 128, t % 2)
                    for r in range(4 if S5_DEBUG_STAGE >= 1.5 else 0):
                        pb = 2 + r % 2
                        for k in range(8):
                            S.op("pe", ("matmul", dict(out=bank(pb)[:, 0:N], lhsT=w5_3[:, k, r * 128:(r + 1) * 128], rhs=h3[:, k, 0:N], start=(k == 0), stop=(k == 7))),
                                 reads=[Bw5, Bh], writes=[PB[pb]])
                        S.op("act", ("copy", dict(out=uT3[b][:, r, c0:c0 + N], in_=bank(pb)[:, 0:N])), reads=[PB[pb]], writes=[BuT[b]])
                        if c0 >= 256:
                            S.op("dve", ("tensor_scalar", dict(out=yacc3[b][:, r, c0 - 256:c0 - 256 + N], in0=uT3[b][:, r, c0:c0 + N], scalar1=sd[:, r:r + 1], scalar2=None, op0=ALU.mult)),
                                 reads=[BuT[b], Bsd], writes=[Byacc[b]])
            S.barrier()
            A.off = mark
            if S5_DEBUG_STAGE < 2:
                return Byacc[0]
            par = A.f32(96)
            par3 = par.rearrange("p (c t) -> p c t", t=3)
            Bpar = Buf()
            S.dma("sp", ("dma_start", dict(out=par, in_=s5par_d)), writes=[Bpar])
            iot = A.f32(SEQ)
            S.dma("sp", ("dma_start", dict(out=iot, in_=s5iota_d)), writes=[Bpar])
            NCB = 32
            pr_ = A.f32(NCB * 16)
            P3 = r3(pr_, 16)
            dtv, rho, tht, frv, sn, cs, nr, ni, inv, cfr, cfi, ncfr, ncfi, tmpa, tmpb, tmpc = [P3[:, i, :] for i in range(16)]
            are, aim, ldt = par3[:, :, 0], par3[:, :, 1], par3[:, :, 2]
            pv = lambda name, **kw: S.op("dve", (name, kw), reads=[Bpar], writes=[Bpar])
            pa = lambda **kw: S.op("act", ("activation", kw), reads=[Bpar], writes=[Bpar])
            pa(out=dtv, in_=ldt, func=AF.Exp)
            pv("tensor_tensor", out=tmpa, in0=are, in1=dtv, op=ALU.mult)
            pa(out=rho, in_=tmpa, func=AF.Exp)
            pv("tensor_tensor", out=tht, in0=aim, in1=dtv, op=ALU.mult)
            pv("tensor_scalar", out=tht, in0=tht, scalar1=1.0 / TWO_PI, scalar2=None, op0=ALU.mult)
            pv("tensor_scalar", out=tmpa, in0=tht, scalar1=MAGIC, scalar2=None, op0=ALU.add)
            pv("tensor_scalar", out=tmpa, in0=tmpa, scalar1=MAGIC, scalar2=None, op0=ALU.subtract)
            pv("tensor_tensor", out=frv, in0=tht, in1=tmpa, op=ALU.subtract)
            SC = TWO_PI * (1.0 - 1e-6)
            pa(out=sn, in_=frv, func=AF.Sin, scale=SC)
            pa(out=tmpb, in_=frv, func=AF.Sin, scale=SC / 2)
            pv("tensor_tensor", out=tmpb, in0=tmpb, in1=tmpb, op=ALU.mult)
            pv("tensor_scalar", out=cs, in0=tmpb, scalar1=-2.0, scalar2=1.0, op0=ALU.mult, op1=ALU.add)
            pv("tensor_tensor", out=nr, in0=rho, in1=cs, op=ALU.mult)
            pv("tensor_scalar", out=nr, in0=nr, scalar1=-1.0, scalar2=None, op0=ALU.add)
            pv("tensor_tensor", out=ni, in0=rho, in1=sn, op=ALU.mult)
            pv("tensor_tensor", out=tmpa, in0=are, in1=are, op=ALU.mult)
            pv("tensor_tensor", out=tmpb, in0=aim, in1=aim, op=ALU.mult)
            pv("tensor_tensor", out=inv, in0=tmpa, in1=tmpb, op=ALU.add)
            pv("reciprocal", out=inv, in_=inv)
            pv("tensor_tensor", out=tmpa, in0=nr, in1=are, op=ALU.mult)
            pv("tensor_tensor", out=tmpb, in0=ni, in1=aim, op=ALU.mult)
            pv("tensor_tensor", out=tmpa, in0=tmpa, in1=tmpb, op=ALU.add)
            pv("tensor_tensor", out=cfr, in0=tmpa, in1=inv, op=ALU.mult)
            pv("tensor_tensor", out=tmpa, in0=ni, in1=are, op=ALU.mult)
            pv("tensor_tensor", out=tmpb, in0=nr, in1=aim, op=ALU.mult)
            pv("tensor_tensor", out=tmpa, in0=tmpa, in1=tmpb, op=ALU.subtract)
            pv("tensor_tensor", out=cfi, in0=tmpa, in1=inv, op=ALU.mult)
            pv("tensor_scalar", out=ncfr, in0=cfr, scalar1=-1.0, scalar2=None, op0=ALU.mult)
            pv("tensor_scalar", out=ncfi, in0=cfi, scalar1=-1.0, scalar2=None, op0=ALU.mult)

            if S5_DEBUG_STAGE < 3:
                return Bpar
            cosT = A.f32(SEQ)
            sinT = A.f32(SEQ)
            tA = A.f32(SEQ)
            tB = A.f32(SEQ)
            Btab, BtA, BtB = Buf(), Buf(), Buf()
            dr = A.f32(SEQ)
            di = A.f32(SEQ)
            qr = A.bf16(SEQ)
            qi = A.bf16(SEQ)
            cosb = A.bf16(SEQ)
            sinb = A.bf16(SEQ)
            Btabb = Buf()
            m1b = [A.bf16(512) for _ in range(2)]
            m2b = [A.bf16(512) for _ in range(2)]
            Bdr, Bdi, Bqr, Bqi = Buf(), Buf(), Buf(), Buf()
            m1 = [A.f32(512) for _ in range(2)]
            m2 = [A.f32(512) for _ in range(2)]
            Bm1, Bm2 = [Buf(), Buf()], [Buf(), Buf()]
            hr = [A.bf16(512) for _ in range(2)]
            hi = [A.bf16(512) for _ in range(2)]
            Bhr, Bhi = [Buf(), Buf()], [Buf(), Buf()]
            bt = [(A.bf16(128), A.bf16(128)) for _ in range(2)]
            Bbt = [Buf(), Buf()]
            cst_ = [(A.f32(128), A.f32(128)) for _ in range(2)]
            Bcst = [Buf(), Buf()]
            cw = [(A.bf16(128), A.bf16(128)) for _ in range(2)]
            Bcw = [Buf(), Buf()]
            ctmp = A.f32(128)
            Bctmp = Buf()
            mi = 0
            for d_ in range(2):
                for pr in range(16):
                    ci_ = d_ * 16 + pr
                    r = pr // 4
                    k2 = ci_ % 2
                    btr, bti = bt[k2]
                    S.dma("pool", ("dma_start", dict(out=btr, in_=s5bT_d[d_, 0, pr])), writes=[Bbt[k2]])
                    S.dma("pool", ("dma_start", dict(out=bti, in_=s5bT_d[d_, 1, pr])), writes=[Bbt[k2]])
                    c_r, c_i = cst_[k2]
                    S.dma("sp", ("dma_start", dict(out=c_r, in_=s5c_d[d_, 0, pr])), writes=[Bcst[k2]])
                    S.dma("sp", ("dma_start", dict(out=c_i, in_=s5c_d[d_, 1, pr])), writes=[Bcst[k2]])
                    cwr, cwi = cw[k2]
                    col1 = lambda v, ci_=ci_: v[:, ci_:ci_ + 1]
                    S.op("dve", ("tensor_scalar", dict(out=ctmp, in0=c_r, scalar1=col1(cfr), scalar2=None, op0=ALU.mult)), reads=[Bcst[k2], Bpar], writes=[Bctmp])
                    S.op("dve", ("scalar_tensor_tensor", dict(out=cwr, in0=c_i, scalar=col1(ncfi), in1=ctmp, op0=ALU.mult, op1=ALU.add)), reads=[Bcst[k2], Bpar, Bctmp], writes=[Bcw[k2]])
                    S.op("dve", ("tensor_scalar", dict(out=ctmp, in0=c_r, scalar1=col1(ncfi), scalar2=None, op0=ALU.mult)), reads=[Bcst[k2], Bpar, Bcw[k2]], writes=[Bctmp])
                    S.op("dve", ("scalar_tensor_tensor", dict(out=cwi, in0=c_i, scalar=col1(ncfr), in1=ctmp, op0=ALU.mult, op1=ALU.add)), reads=[Bcst[k2], Bpar, Bctmp], writes=[Bcw[k2]])
                    S.op("act", ("activation", dict(out=tA, in_=iot, func=AF.Copy, scale=col1(tht))), reads=[Bpar], writes=[BtA])
                    S.op("act", ("activation", dict(out=tB, in_=tA, func=AF.Identity, bias=MAGIC, scale=1.0)), reads=[BtA], writes=[BtB])
                    S.op("act", ("activation", dict(out=tB, in_=tB, func=AF.Identity, bias=-MAGIC, scale=1.0)), reads=[BtB], writes=[BtB])
                    S.op("dve", ("tensor_tensor", dict(out=tA, in0=tA, in1=tB, op=ALU.subtract)), reads=[BtA, BtB], writes=[BtA])
                    S.op("act", ("activation", dict(out=sinT, in_=tA, func=AF.Sin, scale=SC)), reads=[BtA], writes=[Btab])
                    S.op("act", ("activation", dict(out=tB, in_=tA, func=AF.Sin, scale=SC / 2)), reads=[BtA], writes=[BtB])
                    S.op("act", ("activation", dict(out=tB, in_=tB, func=AF.Square, scale=1.4142135623730951)), reads=[BtB], writes=[BtB])
                    S.op("act", ("activation", dict(out=cosT, in_=tB, func=AF.Identity, scale=-1.0, bias=1.0)), reads=[BtB], writes=[Btab])
                    S.op("act", ("copy", dict(out=cosb, in_=cosT)), reads=[Btab], writes=[Btabb])
                    S.op("act", ("copy", dict(out=sinb, in_=sinT)), reads=[Btab], writes=[Btabb])
                    rho_c = col1(rho)
                    for b in range(NB):
                        blocks = [(0, 256)] + [(256 + 512 * i, 512) for i in range(4)]
                        for bi, (s0, N) in enumerate(blocks):
                            if d_ == 0:
                                ucols = uT3[b][:, r, s0:s0 + N]
                            else:
                                if bi == 0:
                                    ucols = uT3[b][:, r, 255::-1]
                                else:
                                    hi_c = SEQ - 1 - (bi - 1) * 512
                                    ucols = uT3[b][:, r, hi_c:hi_c - 512:-1]
                            pbr, pbi = (bi % 2) * 2, (bi % 2) * 2 + 1
                            S.op("pe", ("matmul", dict(out=bank(pbr)[:, 0:N], lhsT=btr, rhs=ucols, start=True, stop=True)), reads=[Bbt[k2], BuT[b]], writes=[PB[pbr]])
                            S.op("pe", ("matmul", dict(out=bank(pbi)[:, 0:N], lhsT=bti, rhs=ucols, start=True, stop=True)), reads=[Bbt[k2], BuT[b]], writes=[PB[pbi]])
                            a1, a2 = m1[mi % 2], m2[mi % 2]
                            Ba1, Ba2 = Bm1[mi % 2], Bm2[mi % 2]
                            mi += 1
                            cS, sS = cosT[:, s0:s0 + N], sinT[:, s0:s0 + N]
                            S.op("dve", ("tensor_tensor", dict(out=a1[:, 0:N], in0=bank(pbr)[:, 0:N], in1=cS, op=ALU.mult)), reads=[PB[pbr], Btab], writes=[Ba1])
                            S.op("dve", ("tensor_tensor", dict(out=a2[:, 0:N], in0=bank(pbi)[:, 0:N], in1=sS, op=ALU.mult)), reads=[PB[pbi], Btab], writes=[Ba2])
                            S.op("dve", ("tensor_tensor", dict(out=dr[:, s0:s0 + N], in0=a1[:, 0:N], in1=a2[:, 0:N], op=ALU.add)), reads=[Ba1, Ba2], writes=[Bdr])
                            a1, a2 = m1[mi % 2], m2[mi % 2]
                            Ba1, Ba2 = Bm1[mi % 2], Bm2[mi % 2]
                            mi += 1
                            S.op("dve", ("tensor_tensor", dict(out=a1[:, 0:N], in0=bank(pbi)[:, 0:N], in1=cS, op=ALU.mult)), reads=[PB[pbi], Btab], writes=[Ba1])
                            S.op("dve", ("tensor_tensor", dict(out=a2[:, 0:N], in0=bank(pbr)[:, 0:N], in1=sS, op=ALU.mult)), reads=[PB[pbr], Btab], writes=[Ba2])
                            S.op("dve", ("tensor_tensor", dict(out=di[:, s0:s0 + N], in0=a1[:, 0:N], in1=a2[:, 0:N], op=ALU.subtract)), reads=[Ba1, Ba2], writes=[Bdi])
                        rb = rho_c.broadcast_to([128, SEQ])
                        S.op("dve", ("tensor_tensor_scan", dict(out=qr, data0=rb, data1=dr, initial=0.0, op0=ALU.mult, op1=ALU.add)), reads=[Bdr, Bpar], writes=[Bqr])
                        S.op("dve", ("tensor_tensor_scan", dict(out=qi, data0=rb, data1=di, initial=0.0, op0=ALU.mult, op1=ALU.add)), reads=[Bdi, Bpar], writes=[Bqi])
                        for bi in range(1, 5):
                            s0, N = blocks[bi]
                            cS, sS = cosb[:, s0:s0 + N], sinb[:, s0:s0 + N]
                            a1, a2 = m1b[mi % 2], m2b[mi % 2]
                            Ba1, Ba2 = Bm1[mi % 2], Bm2[mi % 2]
                            mi += 1
                            h_r, h_i = hr[bi % 2], hi[bi % 2]
                            S.op("dve", ("tensor_tensor", dict(out=a1, in0=qr[:, s0:s0 + N], in1=cS, op=ALU.mult)), reads=[Bqr, Btabb], writes=[Ba1])
                            S.op("dve", ("tensor_tensor", dict(out=a2, in0=qi[:, s0:s0 + N], in1=sS, op=ALU.mult)), reads=[Bqi, Btabb], writes=[Ba2])
                            S.op("dve", ("tensor_tensor", dict(out=h_r, in0=a1, in1=a2, op=ALU.subtract)), reads=[Ba1, Ba2], writes=[Bhr[bi % 2]])
                            a1, a2 = m1b[mi % 2], m2b[mi % 2]
                            Ba1, Ba2 = Bm1[mi % 2], Bm2[mi % 2]
                            mi += 1
                            S.op("dve", ("tensor_tensor", dict(out=a1, in0=qr[:, s0:s0 + N], in1=sS, op=ALU.mult)), reads=[Bqr, Btabb], writes=[Ba1])
                            S.op("dve", ("tensor_tensor", dict(out=a2, in0=qi[:, s0:s0 + N], in1=cS, op=ALU.mult)), reads=[Bqi, Btabb], writes=[Ba2])
                            S.op("dve", ("tensor_tensor", dict(out=h_i, in0=a1, in1=a2, op=ALU.add)), reads=[Ba1, Ba2], writes=[Bhi[bi % 2]])
                            pby = 4 + bi % 2
                            S.op("pe", ("matmul", dict(out=bank(pby), lhsT=cwr, rhs=h_r, start=True, stop=False)), reads=[Bcw[k2], Bhr[bi % 2]], writes=[PB[pby]])
                            S.op("pe", ("matmul", dict(out=bank(pby), lhsT=cwi, rhs=h_i, start=False, stop=True)), reads=[Bcw[k2], Bhi[bi % 2]], writes=[PB[pby]])
                            if d_ == 0:
                                j0 = s0 - 256
                                ycols = yacc3[b][:, r, j0:j0 + 512]
                            else:
                                hj = S_LAT - 1 - (bi - 1) * 512
                                stop = hj - 512
                                ycols = yacc3[b][:, r, hj::-1] if stop < 0 else yacc3[b][:, r, hj:stop:-1]
                            S.op("dve", ("tensor_tensor", dict(out=ycols, in0=bank(pby), in1=ycols, op=ALU.add)), reads=[PB[pby], Byacc[b]], writes=[Byacc[b]])
            S.barrier()
            A.off = mark
            if "yacc" in dbg_d:
                for b in range(NB):
                    S.dma("sp", ("dma_start", dict(out=dbg_d["yacc"][b * 128:(b + 1) * 128, :], in_=yacc[b])), reads=[Byacc[b]])
            wg = A.bf16(4 * 512)
            wg3 = r3(wg, 4)
            wo = A.bf16(4 * 1024)
            wo3 = r3(wo, 4)
            BwC = Buf()
            S.dma("pool", ("dma_start", dict(out=wg3, in_=s5glu_d.rearrange("(k p) n -> p k n", p=128))), writes=[BwC])
            S.dma("pool", ("dma_start", dict(out=wo3, in_=s5wout_d.rearrange("(k p) n -> p k n", p=128))), writes=[BwC])
            gate_bc = [A.f32(D) for _ in range(2)]
            for c in range(2):
                S.dma("sp", ("dma_start", dict(out=gate_bc[c], in_=modrows_d[L, 2, c:c + 1, :].broadcast_to([128, D]))), reads=[Bmodrows], writes=[BwC])
            gT = A.bf16(4 * 512)
            gT3 = r3(gT, 4)
            vT = A.bf16(4 * 512)
            vT3 = r3(vT, 4)
            BgT, BvT = Buf(), Buf()
            sg = [A.f32(512) for _ in range(2)]
            Bsg = [Buf(), Buf()]
            yt = [A.f32(D) for _ in range(2)]
            xt2 = [A.f32(D) for _ in range(2)]
            Byt, Bxt2 = [Buf(), Buf()], [Buf(), Buf()]
            Bxr = Buf()
            oi = 0
            for b in range(NB):
                for i in range(4):
                    j0 = i * 512
                    S.op("act", ("activation", dict(out=gT3, in_=yacc3[b][:, :, j0:j0 + 512], func=AF.Gelu_apprx_tanh)), reads=[Byacc[b]], writes=[BgT])
                    for m in range(4):
                        pb = 2 + m % 2
                        for k in range(4):
                            S.op("pe", ("matmul", dict(out=bank(pb), lhsT=wg3[:, k, m * 128:(m + 1) * 128], rhs=gT3[:, k, :], start=(k == 0), stop=(k == 3))),
                                 reads=[BwC, BgT], writes=[PB[pb]])
                        S.op("act", ("activation", dict(out=sg[m % 2], in_=bank(pb), func=AF.Sigmoid, bias=sd[:, 4 + m:5 + m], scale=1.0)), reads=[PB[pb], Bsd], writes=[Bsg[m % 2]])
                        S.op("dve", ("tensor_tensor", dict(out=vT3[:, m, :], in0=gT3[:, m, :], in1=sg[m % 2], op=ALU.mult)), reads=[BgT, Bsg[m % 2]], writes=[BvT])
                    for t in range(4):
                        ti = 16 * b + 4 * i + t
                        for hf in range(2):
                            for k in range(4):
                                S.op("pe", ("matmul", dict(out=bank(6 + hf), lhsT=vT3[:, k, t * 128:(t + 1) * 128], rhs=wo3[:, k, hf * 512:(hf + 1) * 512], start=(k == 0), stop=(k == 3))),
                                     reads=[BvT, BwC], writes=[PB[6 + hf]])
                        o2 = oi % 2
                        oi += 1
                        S.dma("sp", ("dma_start", dict(out=xt2[o2], in_=xr2_d[ti * 128:(ti + 1) * 128, :])), reads=[Bsrc], writes=[Bxt2[o2]])
                        S.op("dve", ("tensor_tensor", dict(out=yt[o2], in0=psum_t[:, 6 * 512:8 * 512], in1=gate_bc[b], op=ALU.mult)), reads=[PB[6], PB[7], BwC], writes=[Byt[o2]])
                        S.op("dve", ("tensor_tensor", dict(out=yt[o2], in0=yt[o2], in1=xt2[o2], op=ALU.add)), reads=[Byt[o2], Bxt2[o2]], writes=[Byt[o2]])
                        S.dma("sp", ("dma_start", dict(out=xr_d[ti * 128:(ti + 1) * 128, :], in_=yt[o2])), reads=[Byt[o2]], writes=[Bxr])
            return Bxr

        if stop_after >= 3:
            Bxr_b = s5_mixer(Bxr2)
            S.barrier()
            A.off = persist_off
            if "xr3" in dbg_d:
                S.dma("sp", ("dma_start", dict(out=dbg_d["xr3"], in_=xr_d[0:T_LAT, :])), reads=[Bxr_b])
        if stop_after >= 4:
            Bout = moe(1, 32, Bxr_b)

        S.barrier()
        with nc.Block() as block:
            S.emit(block)
    return nc


def _rope_tables():
    inv = np.power(10000.0, -np.arange(0, 32, 2, dtype=np.float32) / 32).astype(np.float32)
    t = np.arange(S_LAT)
    row = (t // 64).astype(np.float32)
    colp = (t % 64).astype(np.float32)
    ang_r = row[:, None] * inv[None, :]
    ang_c = colp[:, None] * inv[None, :]
    cos64 = np.ones((64, SEQ), np.float32)
    sin64 = np.zeros((64, SEQ), np.float32)
    for d in range(64):
        ang = ang_r if d < 32 else ang_c
        i = d % 16
        sgn = -1.0 if (d % 32) < 16 else 1.0
        cos64[d, C_CTX:] = np.cos(ang[:, i])
        sin64[d, C_CTX:] = sgn * np.sin(ang[:, i])
    return np.concatenate([np.concatenate([cos64, cos64], 0), np.concatenate([sin64, sin64], 0)], 1).astype(np.float32)


_PERM64 = np.array([(d // 32) * 32 + ((d % 32) + 16) % 32 for d in range(64)])


def _consts():
    c = np.zeros((128, NCONST), np.float32)
    c[:, 0:128] = np.eye(128)
    c[:, 128:256] = np.triu(np.ones((128, 128)), 1)
    c[:, 256:384] = 1.0
    c[0:64, 384:448] = 1.0 / 64
    c[64:128, 448:512] = 1.0 / 64
    c[:, 512:544] = (np.arange(32) * CAP)[None, :]
    return c


def make_in_maps(inp, cores):
    f = lambda a: np.ascontiguousarray(np.asarray(a, dtype=np.float32))
    shared = {
        "consts": _consts(), "rope": _rope_tables(),
        "w_mod": f(inp["w_mod"]), "b_mod": f(inp["b_mod"]).reshape(2, 1, 6 * D),
        "norm_mix_g": f(inp["norm_mix_g"]).reshape(2, 1, D), "norm_ffn_g": f(inp["norm_ffn_g"]).reshape(2, 1, D),
        "mix_w_in": f(inp["mix_w_in"][0]), "mix_w_out": f(inp["mix_w_out"][0]),
        "gmlp_norm_g": f(inp["gmlp_norm_g"]).reshape(1, 512), "gmlp_w_spatial": f(inp["gmlp_w_spatial"][0]),
        "gmlp_b_spatial": f(inp["gmlp_b_spatial"][0]).reshape(1, 1024),
        "moe_w_group": f(inp["moe_w_group"]), "moe_b_group": f(inp["moe_b_group"]).reshape(2, 1, 4),
        "moe_w_router": f(inp["moe_w_router"]), "moe_b_router": f(inp["moe_b_router"]).reshape(2, 1, 32),
        "moe_w1": f(inp["moe_w1"]), "moe_w3": f(inp["moe_w3"]), "moe_w2": f(inp["moe_w2"]),
        "final_norm_g": f(inp["final_norm_g"]).reshape(1, D),
        "s5_w_in": f(inp["s5_w_in"][0]), "s5_w_glu": f(inp["s5_w_glu"][0]), "s5_w_out": f(inp["s5_w_out"][0]),
    }
    qg = f(inp["q_norm_g"][0]); kg = f(inp["k_norm_g"][0])
    idx = np.arange(128) % 64
    shared["qkg"] = np.stack([qg[idx], qg[_PERM64[idx]], kg[idx], kg[_PERM64[idx]]], 1).astype(np.float32)
    a_re = f(inp["s5_a_re"][0]); a_im = f(inp["s5_a_im"][0]); ldt = f(inp["s5_log_dt"][0])
    par = np.zeros((128, 2, 16, 3), np.float32)
    for d in range(2):
        for pr in range(16):
            for gl in range(2):
                g = 2 * pr + gl
                par[gl * 64:(gl + 1) * 64, d, pr, 0] = a_re[d, g]
                par[gl * 64:(gl + 1) * 64, d, pr, 1] = a_im[d, g]
                par[gl * 64:(gl + 1) * 64, d, pr, 2] = ldt[d, g]
    shared["s5_par"] = par.reshape(128, 96)
    b_re = f(inp["s5_b_re"][0]); b_im = f(inp["s5_b_im"][0]); c_re = f(inp["s5_c_re"][0]); c_im = f(inp["s5_c_im"][0])
    bT = np.zeros((2, 2, 16, 128, 128), np.float32)
    cc = np.zeros((2, 2, 16, 128, 128), np.float32)
    for d in range(2):
        for pr in range(16):
            for gl in range(2):
                g = 2 * pr + gl
                gic = g % 8
                for ri, (bsrc, csrc) in enumerate(((b_re, c_re), (b_im, c_im))):
                    bT[d, ri, pr, gic * 16:(gic + 1) * 16, gl * 64:(gl + 1) * 64] = bsrc[d, g].T
                    cc[d, ri, pr, gl * 64:(gl + 1) * 64, gic * 16:(gic + 1) * 16] = csrc[d, g].T
    shared["s5_bT"] = bT
    shared["s5_iota"] = np.tile(np.arange(SEQ, dtype=np.float32)[None, :], (128, 1))
    shared["s5_c"] = cc
    shared["s5_d"] = f(inp["s5_d"][0]).reshape(4, 128).T.copy()
    shared["s5_b_glu"] = f(inp["s5_b_glu"][0]).reshape(4, 128).T.copy()
    maps = []
    x = np.asarray(inp["x"]); ctx = np.asarray(inp["ctx"]); c = np.asarray(inp["c"]); cc_ = np.asarray(inp["c_ctx"])
    for core in cores:
        m = dict(shared)
        m["x"] = f(x[2 * core:2 * core + 2]).reshape(T_LAT, D)
        m["ctx"] = f(ctx[2 * core:2 * core + 2]).reshape(T_CTX, D)
        cvec = np.stack([c[2 * core], c[2 * core + 1], cc_], 0).astype(np.float32)
        m["cT"] = np.ascontiguousarray(cvec.reshape(3, 8, 128).transpose(2, 1, 0).reshape(128, 24))
        maps.append(m)
    return maps


_NC_CACHE = {}


def kernel(**inputs):
    if "nc" not in _NC_CACHE:
        _NC_CACHE["nc"] = build_program()
    nc = _NC_CACHE["nc"]
    cores = list(range(8))
    maps = make_in_maps(inputs, cores)
    res = run_bass_kernel_spmd(nc, maps, core_ids=cores)
    outs = [np.asarray(r["out"]).reshape(NB, S_LAT, D) for r in res.results]
    return np.concatenate(outs, 0).astype(np.float32)
```

```python
import numpy as np
from contextlib import ExitStack
import concourse.bass as bass
import concourse.mybir as mybir
from concourse.alu_op_type import AluOpType as ALU
from concourse.bass_utils import run_bass_kernel_spmd

F32 = mybir.dt.float32
BF16 = mybir.dt.bfloat16
I32 = mybir.dt.int32
AF = mybir.ActivationFunctionType
AX = mybir.AxisListType

D = 1024
S_LAT = 2048
C_CTX = 256
NB = 2
T_LAT = NB * S_LAT
T_CTX = NB * C_CTX
SEQ = C_CTX + S_LAT
EPS = 1e-6
CAP = 640
NSLOT = 32 * CAP
TRASH = NSLOT
NCONST = 128 * 4 + 32
S5_DEBUG_STAGE = 9


class Buf:
    __slots__ = ("w", "r")

    def __init__(self):
        self.w = None
        self.r = []


class Sched:
    COMPUTE = ("pe", "act", "dve", "pool")
    NDMA = 8

    def __init__(self, nc, es):
        self.nc = nc
        self.streams = {e: [] for e in ("pe", "act", "dve", "pool", "sp")}
        self.sem = {}
        self.cnt = {}
        for e in self.COMPUTE:
            self.sem[e] = es.enter_context(nc.semaphore("s_" + e))
            self.cnt[e] = 0
        self.drr = {}
        for q in ("sp", "act", "pool"):
            for k in range(self.NDMA):
                key = "d_%s%d" % (q, k)
                self.sem[key] = es.enter_context(nc.semaphore(key))
                self.cnt[key] = 0
            self.drr[q] = 0
        self.waited = {e: {} for e in self.streams}
        self.nops = 0

    def _deps(self, reads, writes):
        deps = []
        for b in reads:
            if b.w is not None:
                deps.append(b.w)
        for b in writes:
            if b.w is not None:
                deps.append(b.w)
            deps.extend(b.r)
        return deps

    def _emit_waits(self, eng, deps, skip_self=None):
        best = {}
        for (k, v) in deps:
            if k == skip_self:
                continue
            if v > best.get(k, 0):
                best[k] = v
        w = self.waited[eng]
        for k, v in best.items():
            if w.get(k, 0) < v:
                w[k] = v
                self.streams[eng].append(("wait", k, v))

    def _mark(self, tok, reads, writes):
        for b in writes:
            b.w = tok
            b.r = []
        for b in reads:
            if b.w is tok:
                continue
            b.r.append(tok)
            if len(b.r) > 16:
                best = {}
                for (k, v) in b.r:
                    if v > best.get(k, 0):
                        best[k] = v
                b.r = list(best.items())

    def op(self, eng, fn, reads=(), writes=()):
        deps = self._deps(reads, writes)
        self._emit_waits(eng, deps, skip_self=("pe" if eng == "pe" else None))
        self.cnt[eng] += 1
        tok = (eng, self.cnt[eng])
        self.streams[eng].append(("op", fn, eng, 1))
        self._mark(tok, reads, writes)
        self.nops += 1
        return tok

    def dma(self, q, fn, reads=(), writes=()):
        k = self.drr[q]
        self.drr[q] = (k + 1) % self.NDMA
        key = "d_%s%d" % (q, k)
        deps = self._deps(reads, writes)
        if self.cnt[key] > 0:
            deps.append((key, self.cnt[key]))
        self._emit_waits(q, deps)
        self.cnt[key] += 16
        tok = (key, self.cnt[key])
        self.streams[q].append(("op", fn, key, 16))
        self._mark(tok, reads, writes)
        self.nops += 1
        return tok

    def barrier(self):
        toks = [(k, v) for k, v in self.cnt.items() if v > 0]
        for e in self.streams:
            self._emit_waits(e, toks)

    def emit(self, block):
        sem = self.sem
        streams = self.streams

        def run(e, lst):
            for it in lst:
                if it[0] == "wait":
                    e.wait_ge(sem[it[1]], it[2])
                else:
                    getattr(e, it[1][0])(**it[1][1]).then_inc(sem[it[2]], it[3])

        @block.sync
        def _(e):
            run(e, streams["sp"])

        @block.scalar
        def _(e):
            run(e, streams["act"])

        @block.vector
        def _(e):
            run(e, streams["dve"])

        @block.gpsimd
        def _(e):
            run(e, streams["pool"])

        @block.tensor
        def _(e):
            run(e, streams["pe"])


class Arena:
    def __init__(self, t, size):
        self.t = t
        self.size = size
        self.off = 0

    def f32(self, n):
        a = self.t[:, self.off:self.off + n]
        self.off += n
        assert self.off <= self.size, ("arena overflow", self.off, self.size)
        return a

    def bf16(self, n):
        return self.f32((n + 1) // 2).bitcast(BF16)[:, 0:n]

    def i32(self, n):
        return self.f32(n).bitcast(I32)


def r3(ap, a):
    return ap.rearrange("p (a b) -> p a b", a=a)


def build_program(stop_after=99, dbg=()):
    nc = bass.Bass("TRN2", target_bir_lowering=False)
    dt = nc.dram_tensor

    def din(name, shape, dtype=F32):
        return dt(name, list(shape), dtype, kind="ExternalInput").ap()

    x_d = din("x", [T_LAT, D])
    ctx_d = din("ctx", [T_CTX, D])
    cT_d = din("cT", [128, 24])
    consts_d = din("consts", [128, NCONST])
    rope_d = din("rope", [128, 2 * SEQ])
    qkg_d = din("qkg", [128, 4])
    w_mod_d = din("w_mod", [2, D, 6 * D])
    b_mod_d = din("b_mod", [2, 1, 6 * D])
    gmix_d = din("norm_mix_g", [2, 1, D])
    gffn_d = din("norm_ffn_g", [2, 1, D])
    w_in_d = din("mix_w_in", [D, 1792])
    w_out_d = din("mix_w_out", [D, D])
    lng_d = din("gmlp_norm_g", [1, 512])
    wsp_d = din("gmlp_w_spatial", [8, 128, 128])
    bsp_d = din("gmlp_b_spatial", [1, 1024])
    wgrp_d = din("moe_w_group", [2, D, 4])
    bgrp_d = din("moe_b_group", [2, 1, 4])
    wrt_d = din("moe_w_router", [2, D, 32])
    brt_d = din("moe_b_router", [2, 1, 32])
    w1_d = din("moe_w1", [2, 32, D, 512])
    w3_d = din("moe_w3", [2, 32, D, 512])
    w2_d = din("moe_w2", [2, 32, 512, D])
    fng_d = din("final_norm_g", [1, D])
    s5win_d = din("s5_w_in", [D, 512])
    s5par_d = din("s5_par", [128, 2 * 16 * 3])
    s5iota_d = din("s5_iota", [128, SEQ])
    s5bT_d = din("s5_bT", [2, 2, 16, 128, 128])
    s5c_d = din("s5_c", [2, 2, 16, 128, 128])
    s5d_d = din("s5_d", [128, 4])
    s5glu_d = din("s5_w_glu", [512, 512])
    s5bglu_d = din("s5_b_glu", [128, 4])
    s5wout_d = din("s5_w_out", [512, D])

    out_d = dt("out", [T_LAT, D], F32, kind="ExternalOutput").ap()
    modrows_d = dt("modrows", [2, 6, 3, D], F32, kind="Internal").ap()
    xr_d = dt("xr", [T_LAT + T_CTX, D], F32, kind="Internal").ap()
    xr2_d = dt("xr2", [T_LAT + T_CTX, D], F32, kind="Internal").ap()
    xall_d = dt("xall", [NSLOT + 128, D], BF16, kind="Internal").ap()
    yall_d = dt("yall", [NSLOT + 128, D], F32, kind="Internal").ap()
    dbg_d = {}
    for name, shape in dbg:
        dbg_d[name] = dt("dbg_" + name, list(shape), F32, kind="ExternalOutput").ap()

    with ExitStack() as es:
        S = Sched(nc, es)
        ARENA_WORDS = 53100
        arena_t = es.enter_context(nc.sbuf_tensor("arena", [128, ARENA_WORDS], F32))
        psum_t = es.enter_context(nc.psum_tensor("psum", [128, 4096], F32))
        A = Arena(arena_t, ARENA_WORDS)

        def bank(i, n=512, off=0):
            return psum_t[:, i * 512 + off:i * 512 + off + n]

        def bank_bf(i):
            return psum_t[:, i * 512:(i + 1) * 512].bitcast(BF16)

        PB = [Buf() for _ in range(8)]

        cst = A.f32(NCONST)
        Bc = Buf()
        S.dma("sp", ("dma_start", dict(out=cst, in_=consts_d)), writes=[Bc])
        ident_f = cst[:, 0:128]
        utri_f = cst[:, 128:256]
        ones_f = cst[:, 256:384]
        blk_f = cst[:, 384:512]
        iotaC = cst[:, 512:544]
        cbf = A.bf16(512)
        S.op("dve", ("tensor_copy", dict(out=cbf, in_=cst[:, 0:512])), reads=[Bc], writes=[Bc])
        ident_b = cbf[:, 0:128]
        utri_b = cbf[:, 128:256]
        ones_b = cbf[:, 256:384]
        blk_b = cbf[:, 384:512]
        modT = A.f32(2 * 2 * 8 * 3)
        BmodT = Buf()
        persist_off = A.off

        cT = A.f32(24)
        sc = A.f32(24)
        Bsc = Buf()
        S.dma("sp", ("dma_start", dict(out=cT, in_=cT_d)), writes=[Bsc])
        S.op("act", ("activation", dict(out=sc, in_=cT, func=AF.Silu)), reads=[Bsc], writes=[Bsc])
        sc3 = r3(sc, 8)
        wblk = [A.f32(8 * 512) for _ in range(2)]
        Bwblk = [Buf(), Buf()]
        mrow = A.f32(6 * D)
        gb = A.f32(2 * D)
        bb = A.f32(6 * D)
        Bmrow, Bgb, Bbb = Buf(), Buf(), Buf()
        Bmodrows = Buf()
        for l in range(2):
            S.dma("sp", ("dma_start", dict(out=bb[0:3, :], in_=b_mod_d[l].broadcast_to([3, 6 * D]))), writes=[Bbb])
            S.dma("sp", ("dma_start", dict(out=gb[0:3, 0:D], in_=gmix_d[l].broadcast_to([3, D]))), writes=[Bgb])
            S.dma("sp", ("dma_start", dict(out=gb[0:3, D:2 * D], in_=gffn_d[l].broadcast_to([3, D]))), writes=[Bgb])
            for nb in range(12):
                wb = wblk[nb % 2]
                Bw = Bwblk[nb % 2]
                S.dma("sp", ("dma_start", dict(
                    out=r3(wb, 8), in_=w_mod_d[l][:, nb * 512:(nb + 1) * 512].rearrange("(k p) n -> p k n", p=128))), writes=[Bw])
                pb = nb % 2
                for k in range(8):
                    S.op("pe", ("matmul", dict(out=bank(pb)[0:3, :], lhsT=sc3[:, k, :], rhs=r3(wb, 8)[:, k, :],
                                                                       start=(k == 0), stop=(k == 7))), reads=[Bsc, Bw], writes=[PB[pb]])
                S.op("dve", ("tensor_tensor", dict(out=mrow[0:3, nb * 512:(nb + 1) * 512], in0=bank(pb)[0:3, :],
                                                                      in1=bb[0:3, nb * 512:(nb + 1) * 512], op=ALU.add)),
                     reads=[PB[pb], Bbb], writes=[Bmrow])
            for (kind, goff) in ((1, 0), (4, D)):
                S.op("dve", ("scalar_tensor_tensor", dict(
                    out=mrow[0:3, kind * D:(kind + 1) * D], in0=mrow[0:3, kind * D:(kind + 1) * D], scalar=1.0,
                    in1=gb[0:3, goff:goff + D], op0=ALU.add, op1=ALU.mult)), reads=[Bmrow, Bgb], writes=[Bmrow])
            S.dma("sp", ("dma_start", dict(out=modrows_d[l].rearrange("k c d -> c k d"), in_=r3(mrow[0:3, :], 6))),
                  reads=[Bmrow], writes=[Bmodrows])
        modT5 = modT.rearrange("p (l m k c) -> p l m k c", l=2, m=2, k=8)
        for l in range(2):
            for m in range(2):
                for c in range(3):
                    S.dma("sp", ("dma_start", dict(
                        out=modT5[:, l, m, :, c], in_=modrows_d[l, m, c].rearrange("(k p) -> p k", p=128),
                        allow_slow_non_contiguous=True)), reads=[Bmodrows], writes=[BmodT])
        S.barrier()
        A.off = persist_off
        if "modrows" in dbg_d:
            S.dma("sp", ("dma_start", dict(out=dbg_d["modrows"], in_=modrows_d.rearrange("l k c d -> (l k c) d"))), reads=[Bmodrows])

        def tile_src(layer, ti):
            if layer == 0:
                if ti < 32:
                    return x_d[ti * 128:(ti + 1) * 128, :]
                return ctx_d[(ti - 32) * 128:(ti - 31) * 128, :]
            return xr2_d[ti * 128:(ti + 1) * 128, :]

        def tile_col(ti):
            if ti < 32:
                return ti // 16
            return 2

        class NormT:
            def __init__(self):
                self.xt = [A.f32(D) for _ in range(2)]
                self.Bxt = [Buf(), Buf()]
                self.junk = A.bf16(D)
                self.Bjunk = Buf()
                self.xn = [A.bf16(D) for _ in range(2)]
                self.Bxn = [Buf(), Buf()]
                self.ss = [A.f32(1) for _ in range(2)]
                self.Bss = [Buf(), Buf()]
                self.i = 0

            def run(self, layer, ti, hT3, BhT, c0, psb):
                i = self.i
                self.i += 1
                xt, Bxt = self.xt[i % 2], self.Bxt[i % 2]
                xn, Bxn = self.xn[i % 2], self.Bxn[i % 2]
                ss, Bss = self.ss[i % 2], self.Bss[i % 2]
                junk, Bjunk = self.junk, self.Bjunk
                src = tile_src(layer, ti)
                col = tile_col(ti)
                S.dma("sp", ("dma_start", dict(out=xt, in_=src)), writes=[Bxt])
                S.op("act", ("activation", dict(out=junk, in_=xt, func=AF.Square, accum_out=ss)), reads=[Bxt], writes=[Bjunk, Bss])
                S.op("act", ("activation", dict(out=ss, in_=ss, func=AF.Sqrt, scale=1.0 / D, bias=EPS)), reads=[Bss], writes=[Bss])
                S.op("dve", ("reciprocal", dict(out=ss, in_=ss)), reads=[Bss], writes=[Bss])
                S.op("dve", ("tensor_scalar", dict(out=xn, in0=xt, scalar1=ss, scalar2=None, op0=ALU.mult)), reads=[Bxt, Bss], writes=[Bxn])
                pT = r3(bank_bf(psb), 8)
                for k in range(8):
                    S.op("pe", ("transpose", dict(out=pT[:, k, :], in_=xn[:, k * 128:(k + 1) * 128], identity=ident_b)),
                         reads=[Bxn, Bc], writes=[PB[psb]])
                for k in range(8):
                    eng = "act" if k % 2 == 0 else "dve"
                    if eng == "act":
                        S.op("act", ("activation", dict(out=hT3[:, k, c0:c0 + 128], in_=pT[:, k, :], func=AF.Identity,
                                                                  scale=modT5[:, layer, 1, k, col:col + 1], bias=modT5[:, layer, 0, k, col:col + 1])),
                             reads=[PB[psb], BmodT], writes=[BhT])
                    else:
                        S.op("dve", ("tensor_scalar", dict(out=hT3[:, k, c0:c0 + 128], in0=pT[:, k, :],
                                                                     scalar1=modT5[:, layer, 1, k, col:col + 1], scalar2=modT5[:, layer, 0, k, col:col + 1],
                                                                     op0=ALU.mult, op1=ALU.add)),
                             reads=[PB[psb], BmodT], writes=[BhT])

        def phase1():
            L = 0
            GELU = AF.Gelu_apprx_tanh
            WC = 1280
            w_in = A.bf16(8 * WC)
            w_in3 = r3(w_in, 8)
            Bwin = Buf()
            for k in range(8):
                S.dma("pool", ("dma_start", dict(out=w_in3[:, k, :], in_=w_in_d[k * 128:(k + 1) * 128, 512:1792])), writes=[Bwin])
            wst = A.bf16(8 * 512)
            wst5 = wst.rearrange("p (k j two d) -> p k j two d", k=8, j=4, two=2)
            for k in range(8):
                for two in range(2):
                    S.dma("pool", ("dma_start", dict(out=wst5[:, k, :, two, :],
                                                     in_=w_in_d[k * 128:(k + 1) * 128, two * 256:(two + 1) * 256].rearrange("p (j d) -> p j d", j=4))), writes=[Bwin])
            wst3 = r3(wst, 8)
            wpst = A.bf16(8 * 512)
            wpst3 = r3(wpst, 8)
            wkp = A.bf16(8 * 128)
            wkp3 = r3(wkp, 8)
            Bwperm = Buf()
            sv = wst.rearrange("p (k x b i) -> p k x b i", k=8, b=2, i=16)
            dv = wpst.rearrange("p (k x b i) -> p k x b i", k=8, b=2, i=16)
            svk = w_in3[:, :, 0:128].rearrange("p k (x b i) -> p k x b i", b=2, i=16)
            dvk = wkp3.rearrange("p k (x b i) -> p k x b i", b=2, i=16)
            for b_ in range(2):
                S.op("dve", ("tensor_copy", dict(out=dv[:, :, :, b_, :], in_=sv[:, :, :, 1 - b_, :])), reads=[Bwin], writes=[Bwperm])
                S.op("dve", ("tensor_copy", dict(out=dvk[:, :, :, b_, :], in_=svk[:, :, :, 1 - b_, :])), reads=[Bwin], writes=[Bwperm])
            wout = A.bf16(12 * 1024)
            wout3 = r3(wout, 12)
            Bwout = Buf()
            S.dma("pool", ("dma_start", dict(out=wout3[0:64, 0:4, :], in_=w_out_d[0:256, :].rearrange("(c r) n -> r c n", r=64))), writes=[Bwout])
            S.dma("pool", ("dma_start", dict(out=wout3[64:128, 0:4, :], in_=w_out_d[256:512, :].rearrange("(c r) n -> r c n", r=64))), writes=[Bwout])
            S.dma("pool", ("dma_start", dict(out=wout3[0:64, 4:8, :], in_=w_out_d[512:768, :].rearrange("(c r) n -> r c n", r=64))), writes=[Bwout])
            S.dma("pool", ("dma_start", dict(out=wout3[0:64, 8:12, :], in_=w_out_d[768:1024, :].rearrange("(c r) n -> r c n", r=64))), writes=[Bwout])
            yt = A.f32(D)
            wspn = yt
            Bwspn = Buf()
            S.dma("sp", ("dma_start", dict(out=r3(wspn, 8), in_=wsp_d.rearrange("g p q -> p g q"))), writes=[Bwspn])
            wspT = A.bf16(1024)
            wspT3 = r3(wspT, 8)
            BwspT = Buf()
            pw = r3(psum_t[:, 0:1024], 8)
            for g in range(8):
                S.op("pe", ("transpose", dict(out=pw[:, g, :], in_=r3(wspn, 8)[:, g, :], identity=ident_f)),
                     reads=[Bwspn, Bc], writes=[PB[0], PB[1]])
            S.op("act", ("copy", dict(out=wspT, in_=psum_t[:, 0:1024])), reads=[PB[0], PB[1]], writes=[BwspT])
            lng_bc = A.f32(512)
            bsp_bc = A.f32(1024)
            qkg = A.f32(4)
            cosT = A.f32(SEQ)
            sinT = A.f32(SEQ)
            gate_bc = [A.f32(D) for _ in range(3)]
            Bsm = Buf()
            S.dma("sp", ("dma_start", dict(out=lng_bc, in_=lng_d.broadcast_to([128, 512]))), writes=[Bsm])
            S.dma("sp", ("dma_start", dict(out=bsp_bc[0:64, :], in_=bsp_d.broadcast_to([64, 1024]))), writes=[Bsm])
            S.dma("sp", ("dma_start", dict(out=qkg, in_=qkg_d)), writes=[Bsm])
            S.dma("sp", ("dma_start", dict(out=cosT, in_=rope_d[:, 0:SEQ])), writes=[Bsm])
            S.dma("sp", ("dma_start", dict(out=sinT, in_=rope_d[:, SEQ:2 * SEQ])), writes=[Bsm])
            for c in range(3):
                S.dma("sp", ("dma_start", dict(out=gate_bc[c], in_=modrows_d[L, 2, c:c + 1, :].broadcast_to([128, D]))),
                      reads=[Bmodrows], writes=[Bsm])
            bsp3 = r3(bsp_bc[0:64, :], 8)

            BK0 = Buf()
            QT = A.bf16(4 * SEQ)
            QT3 = r3(QT, 4)
            KTz = [A.bf16(SEQ), A.bf16(SEQ)]
            S.op("dve", ("memset", dict(ap=KTz[0][64:128, :], constant=0.0)), writes=[BK0])
            S.op("dve", ("memset", dict(ap=KTz[1][0:64, :], constant=0.0)), writes=[BK0])
            Vt = A.bf16(18 * 128)
            V3 = r3(Vt, 18)
            BQ, BK, BV = Buf(), Buf(), Buf()
            hT1 = A.bf16(8 * 512)
            hT = [hT1, hT1]
            BhT1 = Buf()
            BhT = [BhT1, BhT1]
            NT = NormT()
            sqb = A.bf16(512)
            rs = A.f32(512)
            t1 = A.f32(512)
            t2 = A.f32(512)
            Bsq, Brs, Bt1, Bt2 = Buf(), Buf(), Buf(), Buf()
            mx = A.f32(1024)
            Bmx = Buf()
            rec = A.f32(512)
            Brec = Buf()
            sqb2 = [sqb, A.bf16(512)]
            rs2 = [rs, rec]
            t12 = [t1, mx[:, 0:512]]
            t22 = [t2, mx[:, 512:1024]]
            Bsq2, Brs2, Bt12, Bt22 = [Bsq, Buf()], [Brs, Brec], [Bt1, Bmx], [Bt2, Bmx]
            UG = A.bf16(8 * 512)
            UG3 = r3(UG, 8)
            GT3 = UG3
            OT = A.bf16(4 * 512)
            OT3 = r3(OT, 4)
            BUG = Buf()
            BGT = BUG
            BOT = Buf()
            gv = t1
            vnf = t2
            vnb = A.bf16(512)
            st6 = A.f32(6)
            mv = A.f32(2)
            Bgv, Bvnf = Bt1, Bt2
            Bvnb, Bst, Bmv = Buf(), Buf(), Buf()
            PT = [A.bf16(512) for _ in range(4)]
            BPT = [Buf(), Buf(), Buf(), Buf()]
            rec = A.f32(512)
            Brec = Buf()
            xt2 = A.f32(D)
            Byt, Bxt2 = Buf(), Buf()
            Bxr = Buf()
            print('phase1 arena words', A.off)
            hcount = [0]

            def make_hT(blk):
                tiles, c0seq, N = blk
                i = hcount[0]
                hcount[0] += 1
                h3 = r3(hT[i % 2], 8)
                for t, ti in enumerate(tiles):
                    NT.run(L, ti, h3, BhT[i % 2], t * 128, t % 2)
                return h3, BhT[i % 2]

            for b in range(NB):
                blocks = [([32 + 2 * b, 33 + 2 * b], 0, 256)]
                for i in range(4):
                    blocks.append(([16 * b + 4 * i + t for t in range(4)], 256 + 512 * i, 512))
                for blk in blocks:
                    tiles, c0, N = blk
                    h3, Bh = make_hT(blk)
                    for j in range(5):
                        bq, bqp, bms = (2, 3, 4) if j % 2 == 0 else (5, 6, 7)
                        sqb_, rs_, t1_, t2_ = sqb2[j % 2], rs2[j % 2], t12[j % 2], t22[j % 2]
                        Bsq_, Brs_, Bt1_, Bt2_ = Bsq2[j % 2], Brs2[j % 2], Bt12[j % 2], Bt22[j % 2]
                        for (pb, wq, wk) in ((bq, wst3, w_in3), (bqp, wpst3, wkp3)):
                            for k in range(8):
                                lw = wq[:, k, j * 128:(j + 1) * 128] if j < 4 else wk[:, k, 0:128]
                                S.op("pe", ("matmul", dict(out=bank(pb)[:, 0:N], lhsT=lw, rhs=h3[:, k, 0:N], start=(k == 0), stop=(k == 7))),
                                     reads=[Bwin, Bwperm, Bh], writes=[PB[pb]])
                        gi = 0 if j < 4 else 2
                        S.op("act", ("activation", dict(out=sqb_[:, 0:N], in_=bank(bq)[:, 0:N], func=AF.Square)), reads=[PB[bq]], writes=[Bsq_])
                        S.op("pe", ("matmul", dict(out=bank(bms)[:, 0:N], lhsT=blk_b, rhs=sqb_[:, 0:N], start=True, stop=True)), reads=[Bsq_, Bc], writes=[PB[bms]])
                        S.op("act", ("activation", dict(out=rs_[:, 0:N], in_=bank(bms)[:, 0:N], func=AF.Sqrt, bias=EPS, scale=1.0)), reads=[PB[bms]], writes=[Brs_])
                        S.op("dve", ("reciprocal", dict(out=rs_[:, 0:N], in_=rs_[:, 0:N])), reads=[Brs_], writes=[Brs_])
                        S.op("dve", ("scalar_tensor_tensor", dict(out=t1_[:, 0:N], in0=bank(bq)[:, 0:N], scalar=qkg[:, gi:gi + 1], in1=cosT[:, c0:c0 + N],
                                                                             op0=ALU.mult, op1=ALU.mult)), reads=[PB[bq], Bsm], writes=[Bt1_])
                        S.op("dve", ("scalar_tensor_tensor", dict(out=t2_[:, 0:N], in0=bank(bqp)[:, 0:N], scalar=qkg[:, gi + 1:gi + 2], in1=sinT[:, c0:c0 + N],
                                                                             op0=ALU.mult, op1=ALU.mult)), reads=[PB[bqp], Bsm], writes=[Bt2_])
                        S.op("dve", ("tensor_tensor", dict(out=t1_[:, 0:N], in0=t1_[:, 0:N], in1=t2_[:, 0:N], op=ALU.add)), reads=[Bt1_, Bt2_], writes=[Bt1_])
                        if j < 4:
                            S.op("dve", ("tensor_tensor", dict(out=QT3[:, j, c0:c0 + N], in0=t1_[:, 0:N], in1=rs_[:, 0:N], op=ALU.mult)), reads=[Bt1_, Brs_], writes=[BQ])
                        else:
                            for hf_ in range(2):
                                ps_ = slice(hf_ * 64, hf_ * 64 + 64)
                                S.op("dve", ("tensor_tensor", dict(out=KTz[hf_][ps_, c0:c0 + N], in0=t1_[ps_, 0:N], in1=rs_[ps_, 0:N], op=ALU.mult)),
                                     reads=[Bt1_, Brs_, BK0], writes=[BK])
                    for t in range(len(tiles)):
                        kt = c0 // 128 + t
                        for k in range(8):
                            S.op("pe", ("matmul", dict(out=bank(5)[:, 0:128], lhsT=h3[:, k, t * 128:(t + 1) * 128], rhs=w_in3[:, k, 128:256],
                                                                      start=(k == 0), stop=(k == 7))), reads=[Bwin, Bh], writes=[PB[5]])
                        S.op("act", ("copy", dict(out=V3[:, kt, :], in_=bank(5)[:, 0:128])), reads=[PB[5]], writes=[BV])
                for bi, blk in enumerate(blocks):
                    tiles, c0, N = blk
                    h3, Bh = make_hT(blk)
                    for g in range(8):
                        pb = 2 + g % 2
                        for k in range(8):
                            S.op("pe", ("matmul", dict(out=bank(pb)[0:64, 0:N], lhsT=w_in3[:, k, 256 + g * 64:320 + g * 64], rhs=h3[:, k, 0:N],
                                                                             start=(k == 0), stop=(k == 7))), reads=[Bwin, Bh], writes=[PB[pb]])
                        S.op("act", ("activation", dict(out=UG3[0:64, g, 0:N], in_=bank(pb)[0:64, 0:N], func=GELU)), reads=[PB[pb]], writes=[BUG])
                    for t in range(len(tiles)):
                        tc0 = t * 128
                        for k in range(8):
                            S.op("pe", ("matmul", dict(out=bank(4), lhsT=h3[:, k, tc0:tc0 + 128], rhs=w_in3[:, k, 768:1280],
                                                                          start=(k == 0), stop=(k == 7))), reads=[Bwin, Bh], writes=[PB[4]])
                        S.op("act", ("activation", dict(out=gv, in_=bank(4), func=GELU)), reads=[PB[4]], writes=[Bgv])
                        S.op("dve", ("bn_stats", dict(out=st6, in_=gv)), reads=[Bgv], writes=[Bst])
                        S.op("dve", ("bn_aggr", dict(out=mv, in_=st6)), reads=[Bst], writes=[Bmv])
                        S.op("act", ("activation", dict(out=mv[:, 1:2], in_=mv[:, 1:2], func=AF.Sqrt, bias=EPS, scale=1.0)), reads=[Bmv], writes=[Bmv])
                        S.op("dve", ("reciprocal", dict(out=mv[:, 1:2], in_=mv[:, 1:2])), reads=[Bmv], writes=[Bmv])
                        S.op("dve", ("tensor_scalar", dict(out=vnf, in0=gv, scalar1=mv[:, 0:1], scalar2=mv[:, 1:2], op0=ALU.subtract, op1=ALU.mult)),
                             reads=[Bgv, Bmv], writes=[Bvnf])
                        S.op("dve", ("tensor_tensor", dict(out=vnb, in0=vnf, in1=lng_bc, op=ALU.mult)), reads=[Bvnf, Bsm], writes=[Bvnb])
                        pm = r3(psum_t[0:64, 6 * 512:8 * 512], 8)
                        for g in range(8):
                            S.op("pe", ("matmul", dict(out=pm[:, g, :], lhsT=vnb[:, g * 64:(g + 1) * 64], rhs=wspT3[:, g, :], start=True, stop=True)),
                                 reads=[Bvnb, BwspT], writes=[PB[6], PB[7]])
                        S.op("dve", ("tensor_tensor", dict(out=r3(mx[0:64, :], 8), in0=pm, in1=bsp3, op=ALU.add)), reads=[PB[6], PB[7], Bsm], writes=[Bmx])
                        S.op("dve", ("tensor_tensor", dict(out=GT3[0:64, :, tc0:tc0 + 128], in0=r3(mx[0:64, :], 8), in1=UG3[0:64, :, tc0:tc0 + 128], op=ALU.mult)),
                             reads=[Bmx, BUG], writes=[BGT])
                    kts = list(range(2)) if bi == 0 else list(range(18))
                    steps = [(h, ki, kt) for h in range(8) for ki, kt in enumerate(kts)]
                    nk = len(kts)

                    def issue_S(idx):
                        h, ki, kt = steps[idx]
                        half, j = h // 4, h % 4
                        sb_ = (0, 1, 6)[idx % 3]
                        S.op("pe", ("matmul", dict(out=bank(sb_)[:, 0:N], lhsT=KTz[half][:, kt * 128:(kt + 1) * 128],
                                                   rhs=QT3[:, j, c0:c0 + N], start=True, stop=True)), reads=[BK, BQ], writes=[PB[sb_]])
                        pt, Bpt = PT[idx % 4], BPT[idx % 4]
                        S.op("act", ("activation", dict(out=pt[:, 0:N], in_=bank(sb_)[:, 0:N], func=AF.Exp, scale=0.125)), reads=[PB[sb_]], writes=[Bpt])

                    def issue_PV(idx):
                        h, ki, kt = steps[idx]
                        half, j = h // 4, h % 4
                        p0 = half * 64
                        bo, bd = 2 + (h % 2) * 2, 3 + (h % 2) * 2
                        pt, Bpt = PT[idx % 4], BPT[idx % 4]
                        S.op("pe", ("matmul", dict(out=bank(bo)[:, 0:N], lhsT=V3[:, kt, :], rhs=pt[:, 0:N],
                                                   start=(ki == 0), stop=(ki == nk - 1))), reads=[BV, Bpt], writes=[PB[bo]])
                        S.op("pe", ("matmul", dict(out=bank(bd)[:, 0:N], lhsT=ones_b, rhs=pt[:, 0:N],
                                                   start=(ki == 0), stop=(ki == nk - 1))), reads=[Bc, Bpt], writes=[PB[bd]])
                        if ki == nk - 1:
                            S.op("dve", ("reciprocal", dict(out=rec[p0:p0 + 64, 0:N], in_=bank(bd)[p0:p0 + 64, 0:N])), reads=[PB[bd]], writes=[Brec])
                            S.op("dve", ("tensor_tensor", dict(out=OT3[p0:p0 + 64, j, 0:N], in0=bank(bo)[p0:p0 + 64, 0:N], in1=rec[p0:p0 + 64, 0:N], op=ALU.mult)),
                                 reads=[PB[bo], Brec], writes=[BOT])

                    for idx in range(len(steps) + 2):
                        if idx < len(steps):
                            issue_S(idx)
                        if idx >= 2:
                            issue_PV(idx - 2)
                    for t, ti in enumerate(tiles):
                        tc0 = t * 128
                        col = tile_col(ti)
                        for hf in range(2):
                            for c in range(12):
                                if c < 4:
                                    lw, rw = OT3[:, c, tc0:tc0 + 128], wout3[:, c, hf * 512:(hf + 1) * 512]
                                else:
                                    lw, rw = GT3[0:64, c - 4, tc0:tc0 + 128], wout3[0:64, c, hf * 512:(hf + 1) * 512]
                                S.op("pe", ("matmul", dict(out=bank(6 + hf), lhsT=lw, rhs=rw,
                                                                                 start=(c == 0), stop=(c == 11))), reads=[BOT, BGT, Bwout], writes=[PB[6 + hf]])
                        S.dma("sp", ("dma_start", dict(out=xt2, in_=tile_src(L, ti))), writes=[Bxt2])
                        S.op("dve", ("tensor_tensor", dict(out=yt, in0=psum_t[:, 6 * 512:8 * 512], in1=gate_bc[col], op=ALU.mult)),
                             reads=[PB[6], PB[7], Bsm], writes=[Byt])
                        S.op("dve", ("tensor_tensor", dict(out=yt, in0=yt, in1=xt2, op=ALU.add)), reads=[Byt, Bxt2], writes=[Byt])
                        S.dma("sp", ("dma_start", dict(out=xr_d[ti * 128:(ti + 1) * 128, :], in_=yt)), reads=[Byt], writes=[Bxr])
            return Bxr

        if stop_after >= 1:
            Bxr = phase1()
            S.barrier()
            A.off = persist_off
            if "xr" in dbg_d:
                S.dma("sp", ("dma_start", dict(out=dbg_d["xr"], in_=xr_d)), reads=[Bxr])
        def moe(L, ntiles, Bsrc):
            last = (L == 1)
            ncol = 2 if last else 3
            Abc = [A.f32(D) for _ in range(ncol)]
            Sbc = [A.f32(D) for _ in range(ncol)]
            Gbc = [A.f32(D) for _ in range(ncol)]
            Bbc = Buf()
            for c in range(ncol):
                for (kind, dst) in ((4, Abc), (3, Sbc), (5, Gbc)):
                    S.dma("sp", ("dma_start", dict(out=dst[c], in_=modrows_d[L, kind, c:c + 1, :].broadcast_to([128, D]))), reads=[Bmodrows], writes=[Bbc])
            fng = A.f32(D)
            if last:
                S.dma("sp", ("dma_start", dict(out=fng, in_=fng_d.broadcast_to([128, D]))), writes=[Bbc])
            w36 = A.f32(8 * 36)
            w36_3 = r3(w36, 8)
            b36 = A.f32(36)
            S.dma("sp", ("dma_start", dict(out=w36_3[:, :, 0:4], in_=wgrp_d[L].rearrange("(k p) n -> p k n", p=128), allow_slow_non_contiguous=True)), writes=[Bbc])
            S.dma("sp", ("dma_start", dict(out=w36_3[:, :, 4:36], in_=wrt_d[L].rearrange("(k p) n -> p k n", p=128), allow_slow_non_contiguous=True)), writes=[Bbc])
            S.dma("sp", ("dma_start", dict(out=b36[:, 0:4], in_=bgrp_d[L].broadcast_to([128, 4]))), writes=[Bbc])
            S.dma("sp", ("dma_start", dict(out=b36[:, 4:36], in_=brt_d[L].broadcast_to([128, 32]))), writes=[Bbc])
            slot_i = A.i32(ntiles * 2)
            slot_i3 = r3(slot_i, ntiles)
            wts = A.f32(ntiles * 2)
            wts3 = r3(wts, ntiles)
            Bslot = Buf()
            Bwts = Buf()
            run = A.f32(32)
            Brun = Buf()
            S.op("dve", ("memset", dict(ap=run, constant=0.0)), writes=[Brun])
            ztile = A.f32(D)
            Bz = Buf()
            Byall = Buf()
            Bxall = Buf()
            S.op("dve", ("memset", dict(ap=ztile, constant=0.0)), writes=[Bz])
            S.dma("sp", ("dma_start", dict(out=yall_d[NSLOT:NSLOT + 128, :], in_=ztile)), reads=[Bz], writes=[Byall])
            mark = A.off
            xt = [A.f32(D) for _ in range(2)]
            Bxt = [Buf(), Buf()]
            ff = [A.f32(D) for _ in range(2)]
            Bff = [Buf(), Buf()]
            fb = [A.bf16(D) for _ in range(3)]
            Bfb = [Buf(), Buf(), Buf()]
            fTs = A.f32(D)
            BfTs = Buf()
            junk = A.bf16(D)
            Bjunk = Buf()
            sm = A.f32(256)
            Bsmall = Buf()
            ss = sm[:, 0:1]
            lg = sm[:, 4:40]
            gmax = sm[:, 40:41]
            ngmax = sm[:, 41:42]
            gsum = sm[:, 42:43]
            eg = sm[:, 44:48]
            gmask = sm[:, 48:52]
            pen = sm[:, 52:56]
            masked = sm[:, 56:88]
            m8 = sm[:, 88:96]
            ntop1 = sm[:, 96:97]
            e2 = sm[:, 97:98]
            wa = sm[:, 98:99]
            wb = sm[:, 99:100]
            slotf = sm[:, 100:102]
            vab = sm[:, 102:104]
            sel1 = sm[:, 104:136]
            sel = sm[:, 136:168]
            pos = sm[:, 168:200]
            valid = sm[:, 200:232]
            tmp32 = A.f32(32)
            selb = A.bf16(32)
            fTs2 = [fTs, A.f32(D)]
            BfTs2 = [BfTs, Buf()]
            ssA = [A.f32(1), A.f32(1)]
            BssA = [Buf(), Buf()]
            lgb = [A.f32(36), A.f32(36)]
            Blg = [Buf(), Buf()]

            def stageA(ti):
                col = tile_col(ti)
                x_, Bx_ = xt[ti % 2], Bxt[ti % 2]
                f_, Bf_ = ff[ti % 2], Bff[ti % 2]
                fb_, Bfb_ = fb[ti % 3], Bfb[ti % 3]
                ss_, Bss_ = ssA[ti % 2], BssA[ti % 2]
                fT_, BfT_ = fTs2[ti % 2], BfTs2[ti % 2]
                S.dma("sp", ("dma_start", dict(out=x_, in_=xr_d[ti * 128:(ti + 1) * 128, :])), reads=[Bsrc], writes=[Bx_])
                S.op("act", ("activation", dict(out=junk, in_=x_, func=AF.Square, accum_out=ss_)), reads=[Bx_], writes=[Bjunk, Bss_])
                S.op("act", ("activation", dict(out=ss_, in_=ss_, func=AF.Sqrt, scale=1.0 / D, bias=EPS)), reads=[Bss_], writes=[Bss_])
                S.op("dve", ("reciprocal", dict(out=ss_, in_=ss_)), reads=[Bss_], writes=[Bss_])
                S.op("dve", ("scalar_tensor_tensor", dict(out=f_, in0=x_, scalar=ss_, in1=Abc[col], op0=ALU.mult, op1=ALU.mult)), reads=[Bx_, Bss_, Bbc], writes=[Bf_])
                S.op("dve", ("tensor_tensor", dict(out=f_, in0=f_, in1=Sbc[col], op=ALU.add)), reads=[Bf_, Bbc], writes=[Bf_])
                pT = r3(psum_t[:, 0:1024], 8)
                for k in range(8):
                    S.op("pe", ("transpose", dict(out=pT[:, k, :], in_=f_[:, k * 128:(k + 1) * 128], identity=ident_f)), reads=[Bf_, Bc], writes=[PB[0], PB[1]])
                S.op("act", ("copy", dict(out=fT_, in_=psum_t[:, 0:1024])), reads=[PB[0], PB[1]], writes=[BfT_])
                S.op("act", ("copy", dict(out=fb_, in_=f_)), reads=[Bf_], writes=[Bfb_])

            def stageA2(ti):
                fT_, BfT_ = fTs2[ti % 2], BfTs2[ti % 2]
                pl = 2 + ti % 2
                for k in range(8):
                    S.op("pe", ("matmul", dict(out=bank(pl)[:, 0:36], lhsT=fT_[:, k * 128:(k + 1) * 128], rhs=w36_3[:, k, :], start=(k == 0), stop=(k == 7))),
                         reads=[BfT_, Bbc], writes=[PB[pl]])

            def stageB(ti):
                lg = lgb[ti % 2]
                fb_, Bfb_ = fb[ti % 3], Bfb[ti % 3]
                pl = 2 + ti % 2
                S.op("dve", ("tensor_tensor", dict(out=lgb[ti % 2], in0=bank(pl)[:, 0:36], in1=b36, op=ALU.add)), reads=[PB[pl], Bbc], writes=[Blg[ti % 2]])
                dv = lambda name, **kw: S.op("dve", (name, kw), reads=[Bsmall, Brun, Blg[ti % 2]], writes=[Bsmall])
                exv = ex2[ti % 2]
                Bex = Bex2[ti % 2]
                ngm, nt1, tp2 = exv[:, 0:1], exv[:, 1:2], exv[:, 2:3]
                S.op("dve", ("tensor_reduce", dict(out=ngm, in_=lg[:, 0:4], axis=AX.X, op=ALU.max, negate=True)), reads=[Blg[ti % 2]], writes=[Bex])
                S.op("dve", ("tensor_scalar", dict(out=gmask, in0=lg[:, 0:4], scalar1=ngm, scalar2=0.0, op0=ALU.add, op1=ALU.is_ge)), reads=[Blg[ti % 2], Bex, Bsmall], writes=[Bsmall])
                dv("tensor_scalar", out=pen, in0=gmask, scalar1=1e30, scalar2=-1e30, op0=ALU.mult, op1=ALU.add)
                dv("tensor_tensor", out=r3(masked, 4), in0=r3(lg[:, 4:36], 4), in1=pen.unsqueeze(2).broadcast_to([128, 4, 8]), op=ALU.add)
                dv("max", out=m8, in_=masked)
                S.op("dve", ("tensor_copy", dict(out=exv[:, 1:3], in_=m8[:, 0:2])), reads=[Bsmall], writes=[Bex])
                S.op("dve", ("tensor_scalar", dict(out=nt1, in0=nt1, scalar1=-1.0, scalar2=None, op0=ALU.mult)), reads=[Bex], writes=[Bex])
                S.op("act", ("activation", dict(out=eg, in_=lg[:, 0:4], func=AF.Exp, bias=ngm, scale=1.0, accum_out=gsum)), reads=[Bex, Blg[ti % 2]], writes=[Bact])
                S.op("act", ("activation", dict(out=e2, in_=tp2, func=AF.Exp, bias=nt1, scale=1.0)), reads=[Bex], writes=[Bact])
                dv("tensor_scalar", out=sel1, in0=masked, scalar1=m8[:, 0:1], scalar2=None, op0=ALU.is_ge)
                dv("tensor_scalar", out=sel, in0=masked, scalar1=m8[:, 1:2], scalar2=None, op0=ALU.is_ge)
                dv("tensor_copy", out=selb, in_=sel)
                S.op("pe", ("matmul", dict(out=bank(4)[:, 0:32], lhsT=utri_b, rhs=selb, start=True, stop=True)), reads=[Bsmall, Bc], writes=[PB[4]])
                S.op("pe", ("matmul", dict(out=bank(4)[:, 32:64], lhsT=ones_b, rhs=selb, start=True, stop=True)), reads=[Bsmall, Bc], writes=[PB[4]])

            def stageB2(ti):
                fb_, Bfb_ = fb[ti % 3], Bfb[ti % 3]
                dv = lambda name, **kw: S.op("dve", (name, kw), reads=[Bsmall, Brun, Blg[ti % 2]], writes=[Bsmall])
                dv("tensor_tensor", out=sel, in0=sel, in1=sel1, op=ALU.subtract)
                S.op("dve", ("tensor_tensor", dict(out=pos, in0=bank(4)[:, 0:32], in1=run, op=ALU.add)), reads=[PB[4], Brun, Bsmall], writes=[Bsmall])
                S.op("dve", ("tensor_tensor", dict(out=run, in0=bank(4)[:, 32:64], in1=run, op=ALU.add)), reads=[PB[4], Brun, Bsmall], writes=[Brun])
                dv("tensor_scalar", out=valid, in0=pos, scalar1=float(CAP), scalar2=None, op0=ALU.is_lt)
                dv("tensor_tensor", out=pos, in0=pos, in1=iotaC, op=ALU.add)
                dv("scalar_tensor_tensor", out=pos, in0=pos, scalar=-float(TRASH), in1=valid, op0=ALU.add, op1=ALU.mult)
                selcat = r3(sm[:, 104:168], 2)
                pvcat = r3(sm[:, 168:232], 2)
                t4 = tmp128.rearrange("p (a c e) -> p a c e", a=2, c=2)
                dv("tensor_tensor", out=t4, in0=selcat.unsqueeze(2).broadcast_to([128, 2, 2, 32]), in1=pvcat.unsqueeze(1).broadcast_to([128, 2, 2, 32]), op=ALU.mult)
                dv("tensor_reduce", out=r4, in_=r3(tmp128, 4), axis=AX.X, op=ALU.add)
                S.op("dve", ("tensor_scalar", dict(out=slot_i3[:, ti, :], in0=r4[:, 0:4:2], scalar1=float(TRASH), scalar2=None, op0=ALU.add)), reads=[Bsmall], writes=[Bslot])
                for a_ in range(2):
                    S.dma("pool", ("indirect_dma_start", dict(out=xall_d, out_offset=bass.IndirectOffsetOnAxis(ap=slot_i3[:, ti, a_:a_ + 1], axis=0),
                                                             in_=fb_, in_offset=None)), reads=[Bfb_, Bslot], writes=[])
                S.op("dve", ("tensor_scalar", dict(out=wa, in0=e2, scalar1=1.0, scalar2=gsum, op0=ALU.add, op1=ALU.mult)), reads=[Bact, Bsmall], writes=[Bsmall])
                dv("reciprocal", out=wa, in_=wa)
                S.op("dve", ("tensor_tensor", dict(out=wb, in0=wa, in1=e2, op=ALU.mult)), reads=[Bact, Bsmall], writes=[Bsmall])
                S.op("dve", ("tensor_tensor", dict(out=wts3[:, ti, :], in0=sm[:, 98:100], in1=r4[:, 1:4:2], op=ALU.mult)), reads=[Bsmall], writes=[Bwts])

            tmp128 = A.f32(128)
            r4 = sm[:, 240:244]
            Bact = Buf()
            ex2 = [A.f32(4), A.f32(4)]
            Bex2 = [Buf(), Buf()]
            sma = A.f32(8)
            eg = sma[:, 0:4]
            gsum = sma[:, 4:5]
            e2 = sma[:, 5:6]
            for step in range(ntiles + 1):
                if step < ntiles:
                    stageA(step)
                if step >= 1:
                    stageB(step - 1)
                if step < ntiles:
                    stageA2(step)
                if step >= 1:
                    stageB2(step - 1)
            S.barrier()
            A.off = mark
            if last is False and "slots" in dbg_d:
                pass
            NJ = CAP // 128
            wbuf = [(A.bf16(8 * 512), A.bf16(8 * 512), A.bf16(4 * 1024)) for _ in range(2)]
            Bwb = [Buf(), Buf()]
            xrows = [A.bf16(NJ * D) for _ in range(2)]
            Bxrows = [Buf(), Buf()]
            XT = A.bf16(8 * CAP)
            XT3 = r3(XT, 8)
            BXT = Buf()
            sl = [A.f32(512) for _ in range(2)]
            Bsl = [Buf(), Buf()]
            hs = A.bf16(4 * CAP)
            hs3 = r3(hs, 4)
            Bhs = Buf()
            ysb = [A.f32(D) for _ in range(2)]
            Bysb = [Buf(), Buf()]
            yi = 0
            stg = (A.f32(8 * 512), A.f32(8 * 512), A.f32(4 * 1024))
            Bstg = [Buf(), Buf(), Buf()]

            def load_w(e_):
                S.dma("sp", ("dma_start", dict(out=r3(stg[0], 8), in_=w1_d[L, e_].rearrange("(k p) n -> p k n", p=128))), writes=[Bstg[0]])
                S.dma("sp", ("dma_start", dict(out=r3(stg[1], 8), in_=w3_d[L, e_].rearrange("(k p) n -> p k n", p=128))), writes=[Bstg[1]])
                S.dma("sp", ("dma_start", dict(out=r3(stg[2], 4), in_=w2_d[L, e_].rearrange("(k p) n -> p k n", p=128))), writes=[Bstg[2]])

            def cast_w(e_, which):
                dst = wbuf[e_ % 2][which]
                Bw_ = Bwb[e_ % 2]
                if which == 0:
                    S.op("act", ("copy", dict(out=dst, in_=stg[0])), reads=[Bstg[0]], writes=[Bw_])
                elif which == 1:
                    S.op("dve", ("tensor_copy", dict(out=dst, in_=stg[1])), reads=[Bstg[1]], writes=[Bw_])
                else:
                    S.op("act", ("copy", dict(out=dst[:, 0:2048], in_=stg[2][:, 0:2048])), reads=[Bstg[2]], writes=[Bw_])
                    S.op("dve", ("tensor_copy", dict(out=dst[:, 2048:4096], in_=stg[2][:, 2048:4096])), reads=[Bstg[2]], writes=[Bw_])

            def load_x(e_):
                S.dma("sp", ("dma_start", dict(out=r3(xrows[e_ % 2], NJ), in_=xall_d[e_ * CAP:(e_ + 1) * CAP, :].rearrange("(j p) d -> p j d", p=128))),
                      reads=[Bxall], writes=[Bxrows[e_ % 2]])

            load_w(0)
            load_x(0)
            for w_ in range(3):
                cast_w(0, w_)
            for e_ in range(32):
                w1b, w3b, w2b = wbuf[e_ % 2]
                Bw = Bwb[e_ % 2]
                xr_, Bxr_ = xrows[e_ % 2], Bxrows[e_ % 2]
                if e_ + 1 < 32:
                    load_w(e_ + 1)
                    load_x(e_ + 1)
                for j in range(NJ):
                    pb = j % 2
                    pT = r3(bank_bf(pb), 8)
                    for k in range(8):
                        S.op("pe", ("transpose", dict(out=pT[:, k, :], in_=r3(xr_, NJ)[:, j, k * 128:(k + 1) * 128], identity=ident_b)),
                             reads=[Bxr_, Bc], writes=[PB[pb]])
                    S.op("act" if j % 2 == 0 else "dve", ("tensor_copy" if j % 2 else "copy", dict(out=XT3[:, :, j * 128:(j + 1) * 128], in_=pT)),
                         reads=[PB[pb]], writes=[BXT])
                w1v, w3v, w2v = r3(w1b, 8), r3(w3b, 8), r3(w2b, 4)
                for bi_, (c0, n) in enumerate(((0, 512), (512, CAP - 512))):
                    for m in range(4):
                        for (pb, wv) in ((2 + (m % 2) * 2, w1v), (3 + (m % 2) * 2, w3v)):
                            for k in range(8):
                                S.op("pe", ("matmul", dict(out=bank(pb)[:, 0:n], lhsT=wv[:, k, m * 128:(m + 1) * 128], rhs=XT3[:, k, c0:c0 + n], start=(k == 0), stop=(k == 7))),
                                     reads=[Bw, BXT], writes=[PB[pb]])
                        p1, p3 = 2 + (m % 2) * 2, 3 + (m % 2) * 2
                        s_, Bs_ = sl[m % 2], Bsl[m % 2]
                        S.op("act", ("activation", dict(out=s_[:, 0:n], in_=bank(p1)[:, 0:n], func=AF.Silu)), reads=[PB[p1]], writes=[Bs_])
                        S.op("dve", ("tensor_tensor", dict(out=hs3[:, m, c0:c0 + n], in0=bank(p3)[:, 0:n], in1=s_[:, 0:n], op=ALU.mult)), reads=[PB[p3], Bs_], writes=[Bhs])
                    if e_ + 1 < 32:
                        cast_w(e_ + 1, bi_)
                for j in range(NJ):
                    for hf in range(2):
                        for m in range(4):
                            S.op("pe", ("matmul", dict(out=bank(6 + hf), lhsT=hs3[:, m, j * 128:(j + 1) * 128], rhs=w2v[:, m, hf * 512:(hf + 1) * 512], start=(m == 0), stop=(m == 3))),
                                 reads=[Bhs, Bw], writes=[PB[6 + hf]])
                    y_, By_ = ysb[yi % 2], Bysb[yi % 2]
                    yi += 1
                    S.op("act", ("copy", dict(out=y_[:, 0:512], in_=bank(6))), reads=[PB[6]], writes=[By_])
                    S.op("dve", ("tensor_copy", dict(out=y_[:, 512:1024], in_=bank(7))), reads=[PB[7]], writes=[By_])
                    r0 = e_ * CAP + j * 128
                    S.dma("sp", ("dma_start", dict(out=yall_d[r0:r0 + 128, :], in_=y_)), reads=[By_], writes=[Byall])
                    if j == 1 and e_ + 1 < 32:
                        cast_w(e_ + 1, 2)
            S.barrier()
            A.off = mark
            ya = [A.f32(D) for _ in range(2)]
            yb = [A.f32(D) for _ in range(2)]
            x1 = [A.f32(D) for _ in range(2)]
            Bya, Byb, Bx1 = [Buf(), Buf()], [Buf(), Buf()], [Buf(), Buf()]
            junk2 = A.bf16(D)
            Bj2 = Buf()
            ss2 = [A.f32(1) for _ in range(2)]
            Bss2 = [Buf(), Buf()]
            Bdst = Buf()
            for ti in range(ntiles):
                col = tile_col(ti)
                i2 = ti % 2
                S.dma("pool", ("indirect_dma_start", dict(out=ya[i2], out_offset=None, in_=yall_d, in_offset=bass.IndirectOffsetOnAxis(ap=slot_i3[:, ti, 0:1], axis=0))),
                      reads=[Byall, Bslot], writes=[Bya[i2]])
                S.dma("pool", ("indirect_dma_start", dict(out=yb[i2], out_offset=None, in_=yall_d, in_offset=bass.IndirectOffsetOnAxis(ap=slot_i3[:, ti, 1:2], axis=0))),
                      reads=[Byall, Bslot], writes=[Byb[i2]])
                S.dma("sp", ("dma_start", dict(out=x1[i2], in_=xr_d[ti * 128:(ti + 1) * 128, :])), reads=[Bsrc], writes=[Bx1[i2]])
                S.op("act", ("activation", dict(out=ya[i2], in_=ya[i2], func=AF.Copy, scale=wts3[:, ti, 0:1])), reads=[Bya[i2], Bwts], writes=[Bya[i2]])
                S.op("dve", ("scalar_tensor_tensor", dict(out=yb[i2], in0=yb[i2], scalar=wts3[:, ti, 1:2], in1=ya[i2], op0=ALU.mult, op1=ALU.add)),
                     reads=[Byb[i2], Bya[i2], Bwts], writes=[Byb[i2]])
                S.op("dve", ("tensor_tensor", dict(out=yb[i2], in0=yb[i2], in1=Gbc[col], op=ALU.mult)), reads=[Byb[i2], Bbc], writes=[Byb[i2]])
                S.op("dve", ("tensor_tensor", dict(out=x1[i2], in0=x1[i2], in1=yb[i2], op=ALU.add)), reads=[Bx1[i2], Byb[i2]], writes=[Bx1[i2]])
                if not last:
                    S.dma("sp", ("dma_start", dict(out=xr2_d[ti * 128:(ti + 1) * 128, :], in_=x1[i2])), reads=[Bx1[i2]], writes=[Bdst])
                else:
                    S.op("act", ("activation", dict(out=junk2, in_=x1[i2], func=AF.Square, accum_out=ss2[i2])), reads=[Bx1[i2]], writes=[Bj2, Bss2[i2]])
                    S.op("act", ("activation", dict(out=ss2[i2], in_=ss2[i2], func=AF.Sqrt, scale=1.0 / D, bias=EPS)), reads=[Bss2[i2]], writes=[Bss2[i2]])
                    S.op("dve", ("reciprocal", dict(out=ss2[i2], in_=ss2[i2])), reads=[Bss2[i2]], writes=[Bss2[i2]])
                    S.op("dve", ("scalar_tensor_tensor", dict(out=x1[i2], in0=x1[i2], scalar=ss2[i2], in1=fng, op0=ALU.mult, op1=ALU.mult)),
                         reads=[Bx1[i2], Bss2[i2], Bbc], writes=[Bx1[i2]])
                    S.dma("sp", ("dma_start", dict(out=out_d[ti * 128:(ti + 1) * 128, :], in_=x1[i2])), reads=[Bx1[i2]], writes=[Bdst])
            return Bdst

        if stop_after >= 2:
            Bxr2 = moe(0, 36, Bxr)
            S.barrier()
            A.off = persist_off
            if "xr2" in dbg_d:
                S.dma("sp", ("dma_start", dict(out=dbg_d["xr2"], in_=xr2_d)), reads=[Bxr2])
        def s5_mixer(Bsrc):
            L = 1
            TWO_PI = 6.283185307179586
            MAGIC = 12582912.0
            yacc = [A.f32(4 * S_LAT) for _ in range(NB)]
            yacc3 = [r3(y, 4) for y in yacc]
            Byacc = [Buf(), Buf()]
            uT = [A.bf16(4 * SEQ) for _ in range(NB)]
            uT3 = [r3(u, 4) for u in uT]
            BuT = [Buf(), Buf()]
            sd = A.f32(8)
            Bsd = Buf()
            S.dma("sp", ("dma_start", dict(out=sd[:, 0:4], in_=s5d_d)), writes=[Bsd])
            S.dma("sp", ("dma_start", dict(out=sd[:, 4:8], in_=s5bglu_d)), writes=[Bsd])
            mark = A.off
            if S5_DEBUG_STAGE < 1:
                return Byacc[0]
            w5 = A.bf16(8 * 512)
            w5_3 = r3(w5, 8)
            Bw5 = Buf()
            S.dma("pool", ("dma_start", dict(out=w5_3, in_=s5win_d.rearrange("(k p) n -> p k n", p=128))), writes=[Bw5])
            hT1 = A.bf16(8 * 512)
            h3 = r3(hT1, 8)
            Bh = Buf()
            NT = NormT()
            for b in range(NB):
                blocks = [([32 + 2 * b, 33 + 2 * b], 0, 256)]
                for i in range(4):
                    blocks.append(([16 * b + 4 * i + t for t in range(4)], 256 + 512 * i, 512))
                for (tiles, c0, N) in blocks:
                    for t, ti in enumerate(tiles):
                        NT.run(L, ti, h3, Bh, t * 128, t % 2)
                    for r in range(4 if S5_DEBUG_STAGE >= 1.5 else 0):
                        pb = 2 + r % 2
                        for k in range(8):
                            S.op("pe", ("matmul", dict(out=bank(pb)[:, 0:N], lhsT=w5_3[:, k, r * 128:(r + 1) * 128], rhs=h3[:, k, 0:N], start=(k == 0), stop=(k == 7))),
                                 reads=[Bw5, Bh], writes=[PB[pb]])
                        S.op("act", ("copy", dict(out=uT3[b][:, r, c0:c0 + N], in_=bank(pb)[:, 0:N])), reads=[PB[pb]], writes=[BuT[b]])
                        if c0 >= 256:
                            S.op("dve", ("tensor_scalar", dict(out=yacc3[b][:, r, c0 - 256:c0 - 256 + N], in0=uT3[b][:, r, c0:c0 + N], scalar1=sd[:, r:r + 1], scalar2=None, op0=ALU.mult)),
                                 reads=[BuT[b], Bsd], writes=[Byacc[b]])
            S.barrier()
            A.off = mark
            if S5_DEBUG_STAGE < 2:
                return Byacc[0]
            par = A.f32(96)
            par3 = par.rearrange("p (c t) -> p c t", t=3)
            Bpar = Buf()
            S.dma("sp", ("dma_start", dict(out=par, in_=s5par_d)), writes=[Bpar])
            iot = A.f32(SEQ)
            S.dma("sp", ("dma_start", dict(out=iot, in_=s5iota_d)), writes=[Bpar])
            NCB = 32
            pr_ = A.f32(NCB * 16)
            P3 = r3(pr_, 16)
            dtv, rho, tht, frv, sn, cs, nr, ni, inv, cfr, cfi, ncfr, ncfi, tmpa, tmpb, tmpc = [P3[:, i, :] for i in range(16)]
            are, aim, ldt = par3[:, :, 0], par3[:, :, 1], par3[:, :, 2]
            pv = lambda name, **kw: S.op("dve", (name, kw), reads=[Bpar], writes=[Bpar])
            pa = lambda **kw: S.op("act", ("activation", kw), reads=[Bpar], writes=[Bpar])
            pa(out=dtv, in_=ldt, func=AF.Exp)
            pv("tensor_tensor", out=tmpa, in0=are, in1=dtv, op=ALU.mult)
            pa(out=rho, in_=tmpa, func=AF.Exp)
            pv("tensor_tensor", out=tht, in0=aim, in1=dtv, op=ALU.mult)
            pv("tensor_scalar", out=tht, in0=tht, scalar1=1.0 / TWO_PI, scalar2=None, op0=ALU.mult)
            pv("tensor_scalar", out=tmpa, in0=tht, scalar1=MAGIC, scalar2=None, op0=ALU.add)
            pv("tensor_scalar", out=tmpa, in0=tmpa, scalar1=MAGIC, scalar2=None, op0=ALU.subtract)
            pv("tensor_tensor", out=frv, in0=tht, in1=tmpa, op=ALU.subtract)
            SC = TWO_PI * (1.0 - 1e-6)
            pa(out=sn, in_=frv, func=AF.Sin, scale=SC)
            pa(out=tmpb, in_=frv, func=AF.Sin, scale=SC / 2)
            pv("tensor_tensor", out=tmpb, in0=tmpb, in1=tmpb, op=ALU.mult)
            pv("tensor_scalar", out=cs, in0=tmpb, scalar1=-2.0, scalar2=1.0, op0=ALU.mult, op1=ALU.add)
            pv("tensor_tensor", out=nr, in0=rho, in1=cs, op=ALU.mult)
            pv("tensor_scalar", out=nr, in0=nr, scalar1=-1.0, scalar2=None, op0=ALU.add)
            pv("tensor_tensor", out=ni, in0=rho, in1=sn, op=ALU.mult)
            pv("tensor_tensor", out=tmpa, in0=are, in1=are, op=ALU.mult)
            pv("tensor_tensor", out=tmpb, in0=aim, in1=aim, op=ALU.mult)
            pv("tensor_tensor", out=inv, in0=tmpa, in1=tmpb, op=ALU.add)
            pv("reciprocal", out=inv, in_=inv)
            pv("tensor_tensor", out=tmpa, in0=nr, in1=are, op=ALU.mult)
            pv("tensor_tensor", out=tmpb, in0=ni, in1=aim, op=ALU.mult)
            pv("tensor_tensor", out=tmpa, in0=tmpa, in1=tmpb, op=ALU.add)
            pv("tensor_tensor", out=cfr, in0=tmpa, in1=inv, op=ALU.mult)
            pv("tensor_tensor", out=tmpa, in0=ni, in1=are, op=ALU.mult)
            pv("tensor_tensor", out=tmpb, in0=nr, in1=aim, op=ALU.mult)
            pv("tensor_tensor", out=tmpa, in0=tmpa, in1=tmpb, op=ALU.subtract)
            pv("tensor_tensor", out=cfi, in0=tmpa, in1=inv, op=ALU.mult)
            pv("tensor_scalar", out=ncfr, in0=cfr, scalar1=-1.0, scalar2=None, op0=ALU.mult)
            pv("tensor_scalar", out=ncfi, in0=cfi, scalar1=-1.0, scalar2=None, op0=ALU.mult)

            if S5_DEBUG_STAGE < 3:
                return Bpar
            cosT = A.f32(SEQ)
            sinT = A.f32(SEQ)
            tA = A.f32(SEQ)
            tB = A.f32(SEQ)
            Btab, BtA, BtB = Buf(), Buf(), Buf()
            dr = A.f32(SEQ)
            di = A.f32(SEQ)
            qr = A.bf16(SEQ)
            qi = A.bf16(SEQ)
            cosb = A.bf16(SEQ)
            sinb = A.bf16(SEQ)
            Btabb = Buf()
            m1b = [A.bf16(512) for _ in range(2)]
            m2b = [A.bf16(512) for _ in range(2)]
            Bdr, Bdi, Bqr, Bqi = Buf(), Buf(), Buf(), Buf()
            m1 = [A.f32(512) for _ in range(2)]
            m2 = [A.f32(512) for _ in range(2)]
            Bm1, Bm2 = [Buf(), Buf()], [Buf(), Buf()]
            hr = [A.bf16(512) for _ in range(2)]
            hi = [A.bf16(512) for _ in range(2)]
            Bhr, Bhi = [Buf(), Buf()], [Buf(), Buf()]
            bt = [(A.bf16(128), A.bf16(128)) for _ in range(2)]
            Bbt = [Buf(), Buf()]
            cst_ = [(A.f32(128), A.f32(128)) for _ in range(2)]
            Bcst = [Buf(), Buf()]
            cw = [(A.bf16(128), A.bf16(128)) for _ in range(2)]
            Bcw = [Buf(), Buf()]
            ctmp = A.f32(128)
            Bctmp = Buf()
            mi = 0
            for d_ in range(2):
                for pr in range(16):
                    ci_ = d_ * 16 + pr
                    r = pr // 4
                    k2 = ci_ % 2
                    btr, bti = bt[k2]
                    S.dma("pool", ("dma_start", dict(out=btr, in_=s5bT_d[d_, 0, pr])), writes=[Bbt[k2]])
                    S.dma("pool", ("dma_start", dict(out=bti, in_=s5bT_d[d_, 1, pr])), writes=[Bbt[k2]])
                    c_r, c_i = cst_[k2]
                    S.dma("sp", ("dma_start", dict(out=c_r, in_=s5c_d[d_, 0, pr])), writes=[Bcst[k2]])
                    S.dma("sp", ("dma_start", dict(out=c_i, in_=s5c_d[d_, 1, pr])), writes=[Bcst[k2]])
                    cwr, cwi = cw[k2]
                    col1 = lambda v, ci_=ci_: v[:, ci_:ci_ + 1]
                    S.op("dve", ("tensor_scalar", dict(out=ctmp, in0=c_r, scalar1=col1(cfr), scalar2=None, op0=ALU.mult)), reads=[Bcst[k2], Bpar], writes=[Bctmp])
                    S.op("dve", ("scalar_tensor_tensor", dict(out=cwr, in0=c_i, scalar=col1(ncfi), in1=ctmp, op0=ALU.mult, op1=ALU.add)), reads=[Bcst[k2], Bpar, Bctmp], writes=[Bcw[k2]])
                    S.op("dve", ("tensor_scalar", dict(out=ctmp, in0=c_r, scalar1=col1(ncfi), scalar2=None, op0=ALU.mult)), reads=[Bcst[k2], Bpar, Bcw[k2]], writes=[Bctmp])
                    S.op("dve", ("scalar_tensor_tensor", dict(out=cwi, in0=c_i, scalar=col1(ncfr), in1=ctmp, op0=ALU.mult, op1=ALU.add)), reads=[Bcst[k2], Bpar, Bctmp], writes=[Bcw[k2]])
                    S.op("act", ("activation", dict(out=tA, in_=iot, func=AF.Copy, scale=col1(tht))), reads=[Bpar], writes=[BtA])
                    S.op("act", ("activation", dict(out=tB, in_=tA, func=AF.Identity, bias=MAGIC, scale=1.0)), reads=[BtA], writes=[BtB])
                    S.op("act", ("activation", dict(out=tB, in_=tB, func=AF.Identity, bias=-MAGIC, scale=1.0)), reads=[BtB], writes=[BtB])
                    S.op("dve", ("tensor_tensor", dict(out=tA, in0=tA, in1=tB, op=ALU.subtract)), reads=[BtA, BtB], writes=[BtA])
                    S.op("act", ("activation", dict(out=sinT, in_=tA, func=AF.Sin, scale=SC)), reads=[BtA], writes=[Btab])
                    S.op("act", ("activation", dict(out=tB, in_=tA, func=AF.Sin, scale=SC / 2)), reads=[BtA], writes=[BtB])
                    S.op("act", ("activation", dict(out=tB, in_=tB, func=AF.Square, scale=1.4142135623730951)), reads=[BtB], writes=[BtB])
                    S.op("act", ("activation", dict(out=cosT, in_=tB, func=AF.Identity, scale=-1.0, bias=1.0)), reads=[BtB], writes=[Btab])
                    S.op("act", ("copy", dict(out=cosb, in_=cosT)), reads=[Btab], writes=[Btabb])
                    S.op("act", ("copy", dict(out=sinb, in_=sinT)), reads=[Btab], writes=[Btabb])
                    rho_c = col1(rho)
                    for b in range(NB):
                        blocks = [(0, 256)] + [(256 + 512 * i, 512) for i in range(4)]
                        for bi, (s0, N) in enumerate(blocks):
                            if d_ == 0:
                                ucols = uT3[b][:, r, s0:s0 + N]
                            else:
                                if bi == 0:
                                    ucols = uT3[b][:, r, 255::-1]
                                else:
                                    hi_c = SEQ - 1 - (bi - 1) * 512
                                    ucols = uT3[b][:, r, hi_c:hi_c - 512:-1]
                            pbr, pbi = (bi % 2) * 2, (bi % 2) * 2 + 1
                            S.op("pe", ("matmul", dict(out=bank(pbr)[:, 0:N], lhsT=btr, rhs=ucols, start=True, stop=True)), reads=[Bbt[k2], BuT[b]], writes=[PB[pbr]])
                            S.op("pe", ("matmul", dict(out=bank(pbi)[:, 0:N], lhsT=bti, rhs=ucols, start=True, stop=True)), reads=[Bbt[k2], BuT[b]], writes=[PB[pbi]])
                            a1, a2 = m1[mi % 2], m2[mi % 2]
                            Ba1, Ba2 = Bm1[mi % 2], Bm2[mi % 2]
                            mi += 1
                            cS, sS = cosT[:, s0:s0 + N], sinT[:, s0:s0 + N]
                            S.op("dve", ("tensor_tensor", dict(out=a1[:, 0:N], in0=bank(pbr)[:, 0:N], in1=cS, op=ALU.mult)), reads=[PB[pbr], Btab], writes=[Ba1])
                            S.op("dve", ("tensor_tensor", dict(out=a2[:, 0:N], in0=bank(pbi)[:, 0:N], in1=sS, op=ALU.mult)), reads=[PB[pbi], Btab], writes=[Ba2])
                            S.op("dve", ("tensor_tensor", dict(out=dr[:, s0:s0 + N], in0=a1[:, 0:N], in1=a2[:, 0:N], op=ALU.add)), reads=[Ba1, Ba2], writes=[Bdr])
                            a1, a2 = m1[mi % 2], m2[mi % 2]
                            Ba1, Ba2 = Bm1[mi % 2], Bm2[mi % 2]
                            mi += 1
                            S.op("dve", ("tensor_tensor", dict(out=a1[:, 0:N], in0=bank(pbi)[:, 0:N], in1=cS, op=ALU.mult)), reads=[PB[pbi], Btab], writes=[Ba1])
                            S.op("dve", ("tensor_tensor", dict(out=a2[:, 0:N], in0=bank(pbr)[:, 0:N], in1=sS, op=ALU.mult)), reads=[PB[pbr], Btab], writes=[Ba2])
                            S.op("dve", ("tensor_tensor", dict(out=di[:, s0:s0 + N], in0=a1[:, 0:N], in1=a2[:, 0:N], op=ALU.subtract)), reads=[Ba1, Ba2], writes=[Bdi])
                        rb = rho_c.broadcast_to([128, SEQ])
                        S.op("dve", ("tensor_tensor_scan", dict(out=qr, data0=rb, data1=dr, initial=0.0, op0=ALU.mult, op1=ALU.add)), reads=[Bdr, Bpar], writes=[Bqr])
                        S.op("dve", ("tensor_tensor_scan", dict(out=qi, data0=rb, data1=di, initial=0.0, op0=ALU.mult, op1=ALU.add)), reads=[Bdi, Bpar], writes=[Bqi])
                        for bi in range(1, 5):
                            s0, N = blocks[bi]
                            cS, sS = cosb[:, s0:s0 + N], sinb[:, s0:s0 + N]
                            a1, a2 = m1b[mi % 2], m2b[mi % 2]
                            Ba1, Ba2 = Bm1[mi % 2], Bm2[mi % 2]
                            mi += 1
                            h_r, h_i = hr[bi % 2], hi[bi % 2]
                            S.op("dve", ("tensor_tensor", dict(out=a1, in0=qr[:, s0:s0 + N], in1=cS, op=ALU.mult)), reads=[Bqr, Btabb], writes=[Ba1])
                            S.op("dve", ("tensor_tensor", dict(out=a2, in0=qi[:, s0:s0 + N], in1=sS, op=ALU.mult)), reads=[Bqi, Btabb], writes=[Ba2])
                            S.op("dve", ("tensor_tensor", dict(out=h_r, in0=a1, in1=a2, op=ALU.subtract)), reads=[Ba1, Ba2], writes=[Bhr[bi % 2]])
                            a1, a2 = m1b[mi % 2], m2b[mi % 2]
                            Ba1, Ba2 = Bm1[mi % 2], Bm2[mi % 2]
                            mi += 1
                            S.op("dve", ("tensor_tensor", dict(out=a1, in0=qr[:, s0:s0 + N], in1=sS, op=ALU.mult)), reads=[Bqr, Btabb], writes=[Ba1])
                            S.op("dve", ("tensor_tensor", dict(out=a2, in0=qi[:, s0:s0 + N], in1=cS, op=ALU.mult)), reads=[Bqi, Btabb], writes=[Ba2])
                            S.op("dve", ("tensor_tensor", dict(out=h_i, in0=a1, in1=a2, op=ALU.add)), reads=[Ba1, Ba2], writes=[Bhi[bi % 2]])
                            pby = 4 + bi % 2
                            S.op("pe", ("matmul", dict(out=bank(pby), lhsT=cwr, rhs=h_r, start=True, stop=False)), reads=[Bcw[k2], Bhr[bi % 2]], writes=[PB[pby]])
                            S.op("pe", ("matmul", dict(out=bank(pby), lhsT=cwi, rhs=h_i, start=False, stop=True)), reads=[Bcw[k2], Bhi[bi % 2]], writes=[PB[pby]])
                            if d_ == 0:
                                j0 = s0 - 256
                                ycols = yacc3[b][:, r, j0:j0 + 512]
                            else:
                                hj = S_LAT - 1 - (bi - 1) * 512
                                stop = hj - 512
                                ycols = yacc3[b][:, r, hj::-1] if stop < 0 else yacc3[b][:, r, hj:stop:-1]
                            S.op("dve", ("tensor_tensor", dict(out=ycols, in0=bank(pby), in1=ycols, op=ALU.add)), reads=[PB[pby], Byacc[b]], writes=[Byacc[b]])
            S.barrier()
            A.off = mark
            if "yacc" in dbg_d:
                for b in range(NB):
                    S.dma("sp", ("dma_start", dict(out=dbg_d["yacc"][b * 128:(b + 1) * 128, :], in_=yacc[b])), reads=[Byacc[b]])
            wg = A.bf16(4 * 512)
            wg3 = r3(wg, 4)
            wo = A.bf16(4 * 1024)
            wo3 = r3(wo, 4)
            BwC = Buf()
            S.dma("pool", ("dma_start", dict(out=wg3, in_=s5glu_d.rearrange("(k p) n -> p k n", p=128))), writes=[BwC])
            S.dma("pool", ("dma_start", dict(out=wo3, in_=s5wout_d.rearrange("(k p) n -> p k n", p=128))), writes=[BwC])
            gate_bc = [A.f32(D) for _ in range(2)]
            for c in range(2):
                S.dma("sp", ("dma_start", dict(out=gate_bc[c], in_=modrows_d[L, 2, c:c + 1, :].broadcast_to([128, D]))), reads=[Bmodrows], writes=[BwC])
            gT = A.bf16(4 * 512)
            gT3 = r3(gT, 4)
            vT = A.bf16(4 * 512)
            vT3 = r3(vT, 4)
            BgT, BvT = Buf(), Buf()
            sg = [A.f32(512) for _ in range(2)]
            Bsg = [Buf(), Buf()]
            yt = [A.f32(D) for _ in range(2)]
            xt2 = [A.f32(D) for _ in range(2)]
            Byt, Bxt2 = [Buf(), Buf()], [Buf(), Buf()]
            Bxr = Buf()
            oi = 0
            for b in range(NB):
                for i in range(4):
                    j0 = i * 512
                    S.op("act", ("activation", dict(out=gT3, in_=yacc3[b][:, :, j0:j0 + 512], func=AF.Gelu_apprx_tanh)), reads=[Byacc[b]], writes=[BgT])
                    for m in range(4):
                        pb = 2 + m % 2
                        for k in range(4):
                            S.op("pe", ("matmul", dict(out=bank(pb), lhsT=wg3[:, k, m * 128:(m + 1) * 128], rhs=gT3[:, k, :], start=(k == 0), stop=(k == 3))),
                                 reads=[BwC, BgT], writes=[PB[pb]])
                        S.op("act", ("activation", dict(out=sg[m % 2], in_=bank(pb), func=AF.Sigmoid, bias=sd[:, 4 + m:5 + m], scale=1.0)), reads=[PB[pb], Bsd], writes=[Bsg[m % 2]])
                        S.op("dve", ("tensor_tensor", dict(out=vT3[:, m, :], in0=gT3[:, m, :], in1=sg[m % 2], op=ALU.mult)), reads=[BgT, Bsg[m % 2]], writes=[BvT])
                    for t in range(4):
                        ti = 16 * b + 4 * i + t
                        for hf in range(2):
                            for k in range(4):
                                S.op("pe", ("matmul", dict(out=bank(6 + hf), lhsT=vT3[:, k, t * 128:(t + 1) * 128], rhs=wo3[:, k, hf * 512:(hf + 1) * 512], start=(k == 0), stop=(k == 3))),
                                     reads=[BvT, BwC], writes=[PB[6 + hf]])
                        o2 = oi % 2
                        oi += 1
                        S.dma("sp", ("dma_start", dict(out=xt2[o2], in_=xr2_d[ti * 128:(ti + 1) * 128, :])), reads=[Bsrc], writes=[Bxt2[o2]])
                        S.op("dve", ("tensor_tensor", dict(out=yt[o2], in0=psum_t[:, 6 * 512:8 * 512], in1=gate_bc[b], op=ALU.mult)), reads=[PB[6], PB[7], BwC], writes=[Byt[o2]])
                        S.op("dve", ("tensor_tensor", dict(out=yt[o2], in0=yt[o2], in1=xt2[o2], op=ALU.add)), reads=[Byt[o2], Bxt2[o2]], writes=[Byt[o2]])
                        S.dma("sp", ("dma_start", dict(out=xr_d[ti * 128:(ti + 1) * 128, :], in_=yt[o2])), reads=[Byt[o2]], writes=[Bxr])
            return Bxr

        if stop_after >= 3:
            Bxr_b = s5_mixer(Bxr2)
            S.barrier()
            A.off = persist_off
            if "xr3" in dbg_d:
                S.dma("sp", ("dma_start", dict(out=dbg_d["xr3"], in_=xr_d[0:T_LAT, :])), reads=[Bxr_b])
        if stop_after >= 4:
            Bout = moe(1, 32, Bxr_b)

        S.barrier()
        with nc.Block() as block:
            S.emit(block)
    return nc


def _rope_tables():
    inv = np.power(10000.0, -np.arange(0, 32, 2, dtype=np.float32) / 32).astype(np.float32)
    t = np.arange(S_LAT)
    row = (t // 64).astype(np.float32)
    colp = (t % 64).astype(np.float32)
    ang_r = row[:, None] * inv[None, :]
    ang_c = colp[:, None] * inv[None, :]
    cos64 = np.ones((64, SEQ), np.float32)
    sin64 = np.zeros((64, SEQ), np.float32)
    for d in range(64):
        ang = ang_r if d < 32 else ang_c
        i = d % 16
        sgn = -1.0 if (d % 32) < 16 else 1.0
        cos64[d, C_CTX:] = np.cos(ang[:, i])
        sin64[d, C_CTX:] = sgn * np.sin(ang[:, i])
    return np.concatenate([np.concatenate([cos64, cos64], 0), np.concatenate([sin64, sin64], 0)], 1).astype(np.float32)


_PERM64 = np.array([(d // 32) * 32 + ((d % 32) + 16) % 32 for d in range(64)])


def _consts():
    c = np.zeros((128, NCONST), np.float32)
    c[:, 0:128] = np.eye(128)
    c[:, 128:256] = np.triu(np.ones((128, 128)), 1)
    c[:, 256:384] = 1.0
    c[0:64, 384:448] = 1.0 / 64
    c[64:128, 448:512] = 1.0 / 64
    c[:, 512:544] = (np.arange(32) * CAP)[None, :]
    return c


def make_in_maps(inp, cores):
    f = lambda a: np.ascontiguousarray(np.asarray(a, dtype=np.float32))
    shared = {
        "consts": _consts(), "rope": _rope_tables(),
        "w_mod": f(inp["w_mod"]), "b_mod": f(inp["b_mod"]).reshape(2, 1, 6 * D),
        "norm_mix_g": f(inp["norm_mix_g"]).reshape(2, 1, D), "norm_ffn_g": f(inp["norm_ffn_g"]).reshape(2, 1, D),
        "mix_w_in": f(inp["mix_w_in"][0]), "mix_w_out": f(inp["mix_w_out"][0]),
        "gmlp_norm_g": f(inp["gmlp_norm_g"]).reshape(1, 512), "gmlp_w_spatial": f(inp["gmlp_w_spatial"][0]),
        "gmlp_b_spatial": f(inp["gmlp_b_spatial"][0]).reshape(1, 1024),
        "moe_w_group": f(inp["moe_w_group"]), "moe_b_group": f(inp["moe_b_group"]).reshape(2, 1, 4),
        "moe_w_router": f(inp["moe_w_router"]), "moe_b_router": f(inp["moe_b_router"]).reshape(2, 1, 32),
        "moe_w1": f(inp["moe_w1"]), "moe_w3": f(inp["moe_w3"]), "moe_w2": f(inp["moe_w2"]),
        "final_norm_g": f(inp["final_norm_g"]).reshape(1, D),
        "s5_w_in": f(inp["s5_w_in"][0]), "s5_w_glu": f(inp["s5_w_glu"][0]), "s5_w_out": f(inp["s5_w_out"][0]),
    }
    qg = f(inp["q_norm_g"][0]); kg = f(inp["k_norm_g"][0])
    idx = np.arange(128) % 64
    shared["qkg"] = np.stack([qg[idx], qg[_PERM64[idx]], kg[idx], kg[_PERM64[idx]]], 1).astype(np.float32)
    a_re = f(inp["s5_a_re"][0]); a_im = f(inp["s5_a_im"][0]); ldt = f(inp["s5_log_dt"][0])
    par = np.zeros((128, 2, 16, 3), np.float32)
    for d in range(2):
        for pr in range(16):
            for gl in range(2):
                g = 2 * pr + gl
                par[gl * 64:(gl + 1) * 64, d, pr, 0] = a_re[d, g]
                par[gl * 64:(gl + 1) * 64, d, pr, 1] = a_im[d, g]
                par[gl * 64:(gl + 1) * 64, d, pr, 2] = ldt[d, g]
    shared["s5_par"] = par.reshape(128, 96)
    b_re = f(inp["s5_b_re"][0]); b_im = f(inp["s5_b_im"][0]); c_re = f(inp["s5_c_re"][0]); c_im = f(inp["s5_c_im"][0])
    bT = np.zeros((2, 2, 16, 128, 128), np.float32)
    cc = np.zeros((2, 2, 16, 128, 128), np.float32)
    for d in range(2):
        for pr in range(16):
            for gl in range(2):
                g = 2 * pr + gl
                gic = g % 8
                for ri, (bsrc, csrc) in enumerate(((b_re, c_re), (b_im, c_im))):
                    bT[d, ri, pr, gic * 16:(gic + 1) * 16, gl * 64:(gl + 1) * 64] = bsrc[d, g].T
                    cc[d, ri, pr, gl * 64:(gl + 1) * 64, gic * 16:(gic + 1) * 16] = csrc[d, g].T
    shared["s5_bT"] = bT
    shared["s5_iota"] = np.tile(np.arange(SEQ, dtype=np.float32)[None, :], (128, 1))
    shared["s5_c"] = cc
    shared["s5_d"] = f(inp["s5_d"][0]).reshape(4, 128).T.copy()
    shared["s5_b_glu"] = f(inp["s5_b_glu"][0]).reshape(4, 128).T.copy()
    maps = []
    x = np.asarray(inp["x"]); ctx = np.asarray(inp["ctx"]); c = np.asarray(inp["c"]); cc_ = np.asarray(inp["c_ctx"])
    for core in cores:
        m = dict(shared)
        m["x"] = f(x[2 * core:2 * core + 2]).reshape(T_LAT, D)
        m["ctx"] = f(ctx[2 * core:2 * core + 2]).reshape(T_CTX, D)
        cvec = np.stack([c[2 * core], c[2 * core + 1], cc_], 0).astype(np.float32)
        m["cT"] = np.ascontiguousarray(cvec.reshape(3, 8, 128).transpose(2, 1, 0).reshape(128, 24))
        maps.append(m)
    return maps


_NC_CACHE = {}


def kernel(**inputs):
    if "nc" not in _NC_CACHE:
        _NC_CACHE["nc"] = build_program()
    nc = _NC_CACHE["nc"]
    cores = list(range(8))
    maps = make_in_maps(inputs, cores)
    res = run_bass_kernel_spmd(nc, maps, core_ids=cores)
    outs = [np.asarray(r["out"]).reshape(NB, S_LAT, D) for r in res.results]
    return np.concatenate(outs, 0).astype(np.float32)
```

```python
import numpy as np
from contextlib import ExitStack
import concourse.bass as bass
import concourse.mybir as mybir
from concourse.alu_op_type import AluOpType as ALU
from concourse.bass_utils import run_bass_kernel_spmd

F32 = mybir.dt.float32
BF16 = mybir.dt.bfloat16
I32 = mybir.dt.int32
AF = mybir.ActivationFunctionType
AX = mybir.AxisListType

D = 1024
S_LAT = 2048
C_CTX = 256
NB = 2
T_LAT = NB * S_LAT
T_CTX = NB * C_CTX
SEQ = C_CTX + S_LAT
EPS = 1e-6
CAP = 640
NSLOT = 32 * CAP
TRASH = NSLOT
NCONST = 128 * 4 + 32
S5_DEBUG_STAGE = 9


class Buf:
    __slots__ = ("w", "r")

    def __init__(self):
        self.w = None
        self.r = []


class Sched:
    COMPUTE = ("pe", "act", "dve", "pool")
    NDMA = 8

    def __init__(self, nc, es):
        self.nc = nc
        self.streams = {e: [] for e in ("pe", "act", "dve", "pool", "sp")}
        self.sem = {}
        self.cnt = {}
        for e in self.COMPUTE:
            self.sem[e] = es.enter_context(nc.semaphore("s_" + e))
            self.cnt[e] = 0
        self.drr = {}
        for q in ("sp", "act", "pool"):
            for k in range(self.NDMA):
                key = "d_%s%d" % (q, k)
                self.sem[key] = es.enter_context(nc.semaphore(key))
                self.cnt[key] = 0
            self.drr[q] = 0
        self.waited = {e: {} for e in self.streams}
        self.nops = 0

    def _deps(self, reads, writes):
        deps = []
        for b in reads:
            if b.w is not None:
                deps.append(b.w)
        for b in writes:
            if b.w is not None:
                deps.append(b.w)
            deps.extend(b.r)
        return deps

    def _emit_waits(self, eng, deps, skip_self=None):
        best = {}
        for (k, v) in deps:
            if k == skip_self:
                continue
            if v > best.get(k, 0):
                best[k] = v
        w = self.waited[eng]
        for k, v in best.items():
            if w.get(k, 0) < v:
                w[k] = v
                self.streams[eng].append(("wait", k, v))

    def _mark(self, tok, reads, writes):
        for b in writes:
            b.w = tok
            b.r = []
        for b in reads:
            if b.w is tok:
                continue
            b.r.append(tok)
            if len(b.r) > 16:
                best = {}
                for (k, v) in b.r:
                    if v > best.get(k, 0):
                        best[k] = v
                b.r = list(best.items())

    def op(self, eng, fn, reads=(), writes=()):
        deps = self._deps(reads, writes)
        self._emit_waits(eng, deps, skip_self=("pe" if eng == "pe" else None))
        self.cnt[eng] += 1
        tok = (eng, self.cnt[eng])
        self.streams[eng].append(("op", fn, eng, 1))
        self._mark(tok, reads, writes)
        self.nops += 1
        return tok

    def dma(self, q, fn, reads=(), writes=()):
        k = self.drr[q]
        self.drr[q] = (k + 1) % self.NDMA
        key = "d_%s%d" % (q, k)
        deps = self._deps(reads, writes)
        if self.cnt[key] > 0:
            deps.append((key, self.cnt[key]))
        self._emit_waits(q, deps)
        self.cnt[key] += 16
        tok = (key, self.cnt[key])
        self.streams[q].append(("op", fn, key, 16))
        self._mark(tok, reads, writes)
        self.nops += 1
        return tok

    def barrier(self):
        toks = [(k, v) for k, v in self.cnt.items() if v > 0]
        for e in self.streams:
            self._emit_waits(e, toks)

    def emit(self, block):
        sem = self.sem
        streams = self.streams

        def run(e, lst):
            for it in lst:
                if it[0] == "wait":
                    e.wait_ge(sem[it[1]], it[2])
                else:
                    getattr(e, it[1][0])(**it[1][1]).then_inc(sem[it[2]], it[3])

        @block.sync
        def _(e):
            run(e, streams["sp"])

        @block.scalar
        def _(e):
            run(e, streams["act"])

        @block.vector
        def _(e):
            run(e, streams["dve"])

        @block.gpsimd
        def _(e):
            run(e, streams["pool"])

        @block.tensor
        def _(e):
            run(e, streams["pe"])


class Arena:
    def __init__(self, t, size):
        self.t = t
        self.size = size
        self.off = 0

    def f32(self, n):
        a = self.t[:, self.off:self.off + n]
        self.off += n
        assert self.off <= self.size, ("arena overflow", self.off, self.size)
        return a

    def bf16(self, n):
        return self.f32((n + 1) // 2).bitcast(BF16)[:, 0:n]

    def i32(self, n):
        return self.f32(n).bitcast(I32)


def r3(ap, a):
    return ap.rearrange("p (a b) -> p a b", a=a)


def build_program(stop_after=99, dbg=()):
    nc = bass.Bass("TRN2", target_bir_lowering=False)
    dt = nc.dram_tensor

    def din(name, shape, dtype=F32):
        return dt(name, list(shape), dtype, kind="ExternalInput").ap()

    x_d = din("x", [T_LAT, D])
    ctx_d = din("ctx", [T_CTX, D])
    cT_d = din("cT", [128, 24])
    consts_d = din("consts", [128, NCONST])
    rope_d = din("rope", [128, 2 * SEQ])
    qkg_d = din("qkg", [128, 4])
    w_mod_d = din("w_mod", [2, D, 6 * D])
    b_mod_d = din("b_mod", [2, 1, 6 * D])
    gmix_d = din("norm_mix_g", [2, 1, D])
    gffn_d = din("norm_ffn_g", [2, 1, D])
    w_in_d = din("mix_w_in", [D, 1792])
    w_out_d = din("mix_w_out", [D, D])
    lng_d = din("gmlp_norm_g", [1, 512])
    wsp_d = din("gmlp_w_spatial", [8, 128, 128])
    bsp_d = din("gmlp_b_spatial", [1, 1024])
    wgrp_d = din("moe_w_group", [2, D, 4])
    bgrp_d = din("moe_b_group", [2, 1, 4])
    wrt_d = din("moe_w_router", [2, D, 32])
    brt_d = din("moe_b_router", [2, 1, 32])
    w1_d = din("moe_w1", [2, 32, D, 512])
    w3_d = din("moe_w3", [2, 32, D, 512])
    w2_d = din("moe_w2", [2, 32, 512, D])
    fng_d = din("final_norm_g", [1, D])
    s5win_d = din("s5_w_in", [D, 512])
    s5par_d = din("s5_par", [128, 2 * 16 * 3])
    s5iota_d = din("s5_iota", [128, SEQ])
    s5bT_d = din("s5_bT", [2, 2, 16, 128, 128])
    s5c_d = din("s5_c", [2, 2, 16, 128, 128])
    s5d_d = din("s5_d", [128, 4])
    s5glu_d = din("s5_w_glu", [512, 512])
    s5bglu_d = din("s5_b_glu", [128, 4])
    s5wout_d = din("s5_w_out", [512, D])

    out_d = dt("out", [T_LAT, D], F32, kind="ExternalOutput").ap()
    modrows_d = dt("modrows", [2, 6, 3, D], F32, kind="Internal").ap()
    xr_d = dt("xr", [T_LAT + T_CTX, D], F32, kind="Internal").ap()
    xr2_d = dt("xr2", [T_LAT + T_CTX, D], F32, kind="Internal").ap()
    xall_d = dt("xall", [NSLOT + 128, D], BF16, kind="Internal").ap()
    yall_d = dt("yall", [NSLOT + 128, D], F32, kind="Internal").ap()
    dbg_d = {}
    for name, shape in dbg:
        dbg_d[name] = dt("dbg_" + name, list(shape), F32, kind="ExternalOutput").ap()

    with ExitStack() as es:
        S = Sched(nc, es)
        ARENA_WORDS = 53100
        arena_t = es.enter_context(nc.sbuf_tensor("arena", [128, ARENA_WORDS], F32))
        psum_t = es.enter_context(nc.psum_tensor("psum", [128, 4096], F32))
        A = Arena(arena_t, ARENA_WORDS)

        def bank(i, n=512, off=0):
            return psum_t[:, i * 512 + off:i * 512 + off + n]

        def bank_bf(i):
            return psum_t[:, i * 512:(i + 1) * 512].bitcast(BF16)

        PB = [Buf() for _ in range(8)]

        cst = A.f32(NCONST)
        Bc = Buf()
        S.dma("sp", ("dma_start", dict(out=cst, in_=consts_d)), writes=[Bc])
        ident_f = cst[:, 0:128]
        utri_f = cst[:, 128:256]
        ones_f = cst[:, 256:384]
        blk_f = cst[:, 384:512]
        iotaC = cst[:, 512:544]
        cbf = A.bf16(512)
        S.op("dve", ("tensor_copy", dict(out=cbf, in_=cst[:, 0:512])), reads=[Bc], writes=[Bc])
        ident_b = cbf[:, 0:128]
        utri_b = cbf[:, 128:256]
        ones_b = cbf[:, 256:384]
        blk_b = cbf[:, 384:512]
        modT = A.f32(2 * 2 * 8 * 3)
        BmodT = Buf()
        persist_off = A.off

        cT = A.f32(24)
        sc = A.f32(24)
        Bsc = Buf()
        S.dma("sp", ("dma_start", dict(out=cT, in_=cT_d)), writes=[Bsc])
        S.op("act", ("activation", dict(out=sc, in_=cT, func=AF.Silu)), reads=[Bsc], writes=[Bsc])
        sc3 = r3(sc, 8)
        wblk = [A.f32(8 * 512) for _ in range(2)]
        Bwblk = [Buf(), Buf()]
        mrow = A.f32(6 * D)
        gb = A.f32(2 * D)
        bb = A.f32(6 * D)
        Bmrow, Bgb, Bbb = Buf(), Buf(), Buf()
        Bmodrows = Buf()
        for l in range(2):
            S.dma("sp", ("dma_start", dict(out=bb[0:3, :], in_=b_mod_d[l].broadcast_to([3, 6 * D]))), writes=[Bbb])
            S.dma("sp", ("dma_start", dict(out=gb[0:3, 0:D], in_=gmix_d[l].broadcast_to([3, D]))), writes=[Bgb])
            S.dma("sp", ("dma_start", dict(out=gb[0:3, D:2 * D], in_=gffn_d[l].broadcast_to([3, D]))), writes=[Bgb])
            for nb in range(12):
                wb = wblk[nb % 2]
                Bw = Bwblk[nb % 2]
                S.dma("sp", ("dma_start", dict(
                    out=r3(wb, 8), in_=w_mod_d[l][:, nb * 512:(nb + 1) * 512].rearrange("(k p) n -> p k n", p=128))), writes=[Bw])
                pb = nb % 2
                for k in range(8):
                    S.op("pe", ("matmul", dict(out=bank(pb)[0:3, :], lhsT=sc3[:, k, :], rhs=r3(wb, 8)[:, k, :],
                                                                       start=(k == 0), stop=(k == 7))), reads=[Bsc, Bw], writes=[PB[pb]])
                S.op("dve", ("tensor_tensor", dict(out=mrow[0:3, nb * 512:(nb + 1) * 512], in0=bank(pb)[0:3, :],
                                                                      in1=bb[0:3, nb * 512:(nb + 1) * 512], op=ALU.add)),
                     reads=[PB[pb], Bbb], writes=[Bmrow])
            for (kind, goff) in ((1, 0), (4, D)):
                S.op("dve", ("scalar_tensor_tensor", dict(
                    out=mrow[0:3, kind * D:(kind + 1) * D], in0=mrow[0:3, kind * D:(kind + 1) * D], scalar=1.0,
                    in1=gb[0:3, goff:goff + D], op0=ALU.add, op1=ALU.mult)), reads=[Bmrow, Bgb], writes=[Bmrow])
            S.dma("sp", ("dma_start", dict(out=modrows_d[l].rearrange("k c d -> c k d"), in_=r3(mrow[0:3, :], 6))),
                  reads=[Bmrow], writes=[Bmodrows])
        modT5 = modT.rearrange("p (l m k c) -> p l m k c", l=2, m=2, k=8)
        for l in range(2):
            for m in range(2):
                for c in range(3):
                    S.dma("sp", ("dma_start", dict(
                        out=modT5[:, l, m, :, c], in_=modrows_d[l, m, c].rearrange("(k p) -> p k", p=128),
                        allow_slow_non_contiguous=True)), reads=[Bmodrows], writes=[BmodT])
        S.barrier()
        A.off = persist_off
        if "modrows" in dbg_d:
            S.dma("sp", ("dma_start", dict(out=dbg_d["modrows"], in_=modrows_d.rearrange("l k c d -> (l k c) d"))), reads=[Bmodrows])

        def tile_src(layer, ti):
            if layer == 0:
                if ti < 32:
                    return x_d[ti * 128:(ti + 1) * 128, :]
                return ctx_d[(ti - 32) * 128:(ti - 31) * 128, :]
            return xr2_d[ti * 128:(ti + 1) * 128, :]

        def tile_col(ti):
            if ti < 32:
                return ti // 16
            return 2

        class NormT:
            def __init__(self):
                self.xt = [A.f32(D) for _ in range(2)]
                self.Bxt = [Buf(), Buf()]
                self.junk = A.bf16(D)
                self.Bjunk = Buf()
                self.xn = [A.bf16(D) for _ in range(2)]
                self.Bxn = [Buf(), Buf()]
                self.ss = [A.f32(1) for _ in range(2)]
                self.Bss = [Buf(), Buf()]
                self.i = 0

            def run(self, layer, ti, hT3, BhT, c0, psb):
                i = self.i
                self.i += 1
                xt, Bxt = self.xt[i % 2], self.Bxt[i % 2]
                xn, Bxn = self.xn[i % 2], self.Bxn[i % 2]
                ss, Bss = self.ss[i % 2], self.Bss[i % 2]
                junk, Bjunk = self.junk, self.Bjunk
                src = tile_src(layer, ti)
                col = tile_col(ti)
                S.dma("sp", ("dma_start", dict(out=xt, in_=src)), writes=[Bxt])
                S.op("act", ("activation", dict(out=junk, in_=xt, func=AF.Square, accum_out=ss)), reads=[Bxt], writes=[Bjunk, Bss])
                S.op("act", ("activation", dict(out=ss, in_=ss, func=AF.Sqrt, scale=1.0 / D, bias=EPS)), reads=[Bss], writes=[Bss])
                S.op("dve", ("reciprocal", dict(out=ss, in_=ss)), reads=[Bss], writes=[Bss])
                S.op("dve", ("tensor_scalar", dict(out=xn, in0=xt, scalar1=ss, scalar2=None, op0=ALU.mult)), reads=[Bxt, Bss], writes=[Bxn])
                pT = r3(bank_bf(psb), 8)
                for k in range(8):
                    S.op("pe", ("transpose", dict(out=pT[:, k, :], in_=xn[:, k * 128:(k + 1) * 128], identity=ident_b)),
                         reads=[Bxn, Bc], writes=[PB[psb]])
                for k in range(8):
                    eng = "act" if k % 2 == 0 else "dve"
                    if eng == "act":
                        S.op("act", ("activation", dict(out=hT3[:, k, c0:c0 + 128], in_=pT[:, k, :], func=AF.Identity,
                                                                  scale=modT5[:, layer, 1, k, col:col + 1], bias=modT5[:, layer, 0, k, col:col + 1])),
                             reads=[PB[psb], BmodT], writes=[BhT])
                    else:
                        S.op("dve", ("tensor_scalar", dict(out=hT3[:, k, c0:c0 + 128], in0=pT[:, k, :],
                                                                     scalar1=modT5[:, layer, 1, k, col:col + 1], scalar2=modT5[:, layer, 0, k, col:col + 1],
                                                                     op0=ALU.mult, op1=ALU.add)),
                             reads=[PB[psb], BmodT], writes=[BhT])

        def phase1():
            L = 0
            GELU = AF.Gelu_apprx_tanh
            WC = 1280
            w_in = A.bf16(8 * WC)
            w_in3 = r3(w_in, 8)
            Bwin = Buf()
            for k in range(8):
                S.dma("pool", ("dma_start", dict(out=w_in3[:, k, :], in_=w_in_d[k * 128:(k + 1) * 128, 512:1792])), writes=[Bwin])
            wst = A.bf16(8 * 512)
            wst5 = wst.rearrange("p (k j two d) -> p k j two d", k=8, j=4, two=2)
            for k in range(8):
                for two in range(2):
                    S.dma("pool", ("dma_start", dict(out=wst5[:, k, :, two, :],
                                                     in_=w_in_d[k * 128:(k + 1) * 128, two * 256:(two + 1) * 256].rearrange("p (j d) -> p j d", j=4))), writes=[Bwin])
            wst3 = r3(wst, 8)
            wpst = A.bf16(8 * 512)
            wpst3 = r3(wpst, 8)
            wkp = A.bf16(8 * 128)
            wkp3 = r3(wkp, 8)
            Bwperm = Buf()
            sv = wst.rearrange("p (k x b i) -> p k x b i", k=8, b=2, i=16)
            dv = wpst.rearrange("p (k x b i) -> p k x b i", k=8, b=2, i=16)
            svk = w_in3[:, :, 0:128].rearrange("p k (x b i) -> p k x b i", b=2, i=16)
            dvk = wkp3.rearrange("p k (x b i) -> p k x b i", b=2, i=16)
            for b_ in range(2):
                S.op("dve", ("tensor_copy", dict(out=dv[:, :, :, b_, :], in_=sv[:, :, :, 1 - b_, :])), reads=[Bwin], writes=[Bwperm])
                S.op("dve", ("tensor_copy", dict(out=dvk[:, :, :, b_, :], in_=svk[:, :, :, 1 - b_, :])), reads=[Bwin], writes=[Bwperm])
            wout = A.bf16(12 * 1024)
            wout3 = r3(wout, 12)
            Bwout = Buf()
            S.dma("pool", ("dma_start", dict(out=wout3[0:64, 0:4, :], in_=w_out_d[0:256, :].rearrange("(c r) n -> r c n", r=64))), writes=[Bwout])
            S.dma("pool", ("dma_start", dict(out=wout3[64:128, 0:4, :], in_=w_out_d[256:512, :].rearrange("(c r) n -> r c n", r=64))), writes=[Bwout])
            S.dma("pool", ("dma_start", dict(out=wout3[0:64, 4:8, :], in_=w_out_d[512:768, :].rearrange("(c r) n -> r c n", r=64))), writes=[Bwout])
            S.dma("pool", ("dma_start", dict(out=wout3[0:64, 8:12, :], in_=w_out_d[768:1024, :].rearrange("(c r) n -> r c n", r=64))), writes=[Bwout])
            yt = A.f32(D)
            wspn = yt
            Bwspn = Buf()
            S.dma("sp", ("dma_start", dict(out=r3(wspn, 8), in_=wsp_d.rearrange("g p q -> p g q"))), writes=[Bwspn])
            wspT = A.bf16(1024)
            wspT3 = r3(wspT, 8)
            BwspT = Buf()
            pw = r3(psum_t[:, 0:1024], 8)
            for g in range(8):
                S.op("pe", ("transpose", dict(out=pw[:, g, :], in_=r3(wspn, 8)[:, g, :], identity=ident_f)),
                     reads=[Bwspn, Bc], writes=[PB[0], PB[1]])
            S.op("act", ("copy", dict(out=wspT, in_=psum_t[:, 0:1024])), reads=[PB[0], PB[1]], writes=[BwspT])
            lng_bc = A.f32(512)
            bsp_bc = A.f32(1024)
            qkg = A.f32(4)
            cosT = A.f32(SEQ)
            sinT = A.f32(SEQ)
            gate_bc = [A.f32(D) for _ in range(3)]
            Bsm = Buf()
            S.dma("sp", ("dma_start", dict(out=lng_bc, in_=lng_d.broadcast_to([128, 512]))), writes=[Bsm])
            S.dma("sp", ("dma_start", dict(out=bsp_bc[0:64, :], in_=bsp_d.broadcast_to([64, 1024]))), writes=[Bsm])
            S.dma("sp", ("dma_start", dict(out=qkg, in_=qkg_d)), writes=[Bsm])
            S.dma("sp", ("dma_start", dict(out=cosT, in_=rope_d[:, 0:SEQ])), writes=[Bsm])
            S.dma("sp", ("dma_start", dict(out=sinT, in_=rope_d[:, SEQ:2 * SEQ])), writes=[Bsm])
            for c in range(3):
                S.dma("sp", ("dma_start", dict(out=gate_bc[c], in_=modrows_d[L, 2, c:c + 1, :].broadcast_to([128, D]))),
                      reads=[Bmodrows], writes=[Bsm])
            bsp3 = r3(bsp_bc[0:64, :], 8)

            BK0 = Buf()
            QT = A.bf16(4 * SEQ)
            QT3 = r3(QT, 4)
            KTz = [A.bf16(SEQ), A.bf16(SEQ)]
            S.op("dve", ("memset", dict(ap=KTz[0][64:128, :], constant=0.0)), writes=[BK0])
            S.op("dve", ("memset", dict(ap=KTz[1][0:64, :], constant=0.0)), writes=[BK0])
            Vt = A.bf16(18 * 128)
            V3 = r3(Vt, 18)
            BQ, BK, BV = Buf(), Buf(), Buf()
            hT1 = A.bf16(8 * 512)
            hT = [hT1, hT1]
            BhT1 = Buf()
            BhT = [BhT1, BhT1]
            NT = NormT()
            sqb = A.bf16(512)
            rs = A.f32(512)
            t1 = A.f32(512)
            t2 = A.f32(512)
            Bsq, Brs, Bt1, Bt2 = Buf(), Buf(), Buf(), Buf()
            mx = A.f32(1024)
            Bmx = Buf()
            rec = A.f32(512)
            Brec = Buf()
            sqb2 = [sqb, A.bf16(512)]
            rs2 = [rs, rec]
            t12 = [t1, mx[:, 0:512]]
            t22 = [t2, mx[:, 512:1024]]
            Bsq2, Brs2, Bt12, Bt22 = [Bsq, Buf()], [Brs, Brec], [Bt1, Bmx], [Bt2, Bmx]
            UG = A.bf16(8 * 512)
            UG3 = r3(UG, 8)
            GT3 = UG3
            OT = A.bf16(4 * 512)
            OT3 = r3(OT, 4)
            BUG = Buf()
            BGT = BUG
            BOT = Buf()
            gv = t1
            vnf = t2
            vnb = A.bf16(512)
            st6 = A.f32(6)
            mv = A.f32(2)
            Bgv, Bvnf = Bt1, Bt2
            Bvnb, Bst, Bmv = Buf(), Buf(), Buf()
            PT = [A.bf16(512) for _ in range(4)]
            BPT = [Buf(), Buf(), Buf(), Buf()]
            rec = A.f32(512)
            Brec = Buf()
            xt2 = A.f32(D)
            Byt, Bxt2 = Buf(), Buf()
            Bxr = Buf()
            print('phase1 arena words', A.off)
            hcount = [0]

            def make_hT(blk):
                tiles, c0seq, N = blk
                i = hcount[0]
                hcount[0] += 1
                h3 = r3(hT[i % 2], 8)
                for t, ti in enumerate(tiles):
                    NT.run(L, ti, h3, BhT[i % 2], t * 128, t % 2)
                return h3, BhT[i % 2]

            for b in range(NB):
                blocks = [([32 + 2 * b, 33 + 2 * b], 0, 256)]
                for i in range(4):
                    blocks.append(([16 * b + 4 * i + t for t in range(4)], 256 + 512 * i, 512))
                for blk in blocks:
                    tiles, c0, N = blk
                    h3, Bh = make_hT(blk)
                    for j in range(5):
                        bq, bqp, bms = (2, 3, 4) if j % 2 == 0 else (5, 6, 7)
                        sqb_, rs_, t1_, t2_ = sqb2[j % 2], rs2[j % 2], t12[j % 2], t22[j % 2]
                        Bsq_, Brs_, Bt1_, Bt2_ = Bsq2[j % 2], Brs2[j % 2], Bt12[j % 2], Bt22[j % 2]
                        for (pb, wq, wk) in ((bq, wst3, w_in3), (bqp, wpst3, wkp3)):
                            for k in range(8):
                                lw = wq[:, k, j * 128:(j + 1) * 128] if j < 4 else wk[:, k, 0:128]
                                S.op("pe", ("matmul", dict(out=bank(pb)[:, 0:N], lhsT=lw, rhs=h3[:, k, 0:N], start=(k == 0), stop=(k == 7))),
                                     reads=[Bwin, Bwperm, Bh], writes=[PB[pb]])
                        gi = 0 if j < 4 else 2
                        S.op("act", ("activation", dict(out=sqb_[:, 0:N], in_=bank(bq)[:, 0:N], func=AF.Square)), reads=[PB[bq]], writes=[Bsq_])
                        S.op("pe", ("matmul", dict(out=bank(bms)[:, 0:N], lhsT=blk_b, rhs=sqb_[:, 0:N], start=True, stop=True)), reads=[Bsq_, Bc], writes=[PB[bms]])
                        S.op("act", ("activation", dict(out=rs_[:, 0:N], in_=bank(bms)[:, 0:N], func=AF.Sqrt, bias=EPS, scale=1.0)), reads=[PB[bms]], writes=[Brs_])
                        S.op("dve", ("reciprocal", dict(out=rs_[:, 0:N], in_=rs_[:, 0:N])), reads=[Brs_], writes=[Brs_])
                        S.op("dve", ("scalar_tensor_tensor", dict(out=t1_[:, 0:N], in0=bank(bq)[:, 0:N], scalar=qkg[:, gi:gi + 1], in1=cosT[:, c0:c0 + N],
                                                                             op0=ALU.mult, op1=ALU.mult)), reads=[PB[bq], Bsm], writes=[Bt1_])
                        S.op("dve", ("scalar_tensor_tensor", dict(out=t2_[:, 0:N], in0=bank(bqp)[:, 0:N], scalar=qkg[:, gi + 1:gi + 2], in1=sinT[:, c0:c0 + N],
                                                                             op0=ALU.mult, op1=ALU.mult)), reads=[PB[bqp], Bsm], writes=[Bt2_])
                        S.op("dve", ("tensor_tensor", dict(out=t1_[:, 0:N], in0=t1_[:, 0:N], in1=t2_[:, 0:N], op=ALU.add)), reads=[Bt1_, Bt2_], writes=[Bt1_])
                        if j < 4:
                            S.op("dve", ("tensor_tensor", dict(out=QT3[:, j, c0:c0 + N], in0=t1_[:, 0:N], in1=rs_[:, 0:N], op=ALU.mult)), reads=[Bt1_, Brs_], writes=[BQ])
                        else:
                            for hf_ in range(2):
                                ps_ = slice(hf_ * 64, hf_ * 64 + 64)
                                S.op("dve", ("tensor_tensor", dict(out=KTz[hf_][ps_, c0:c0 + N], in0=t1_[ps_, 0:N], in1=rs_[ps_, 0:N], op=ALU.mult)),
                                     reads=[Bt1_, Brs_, BK0], writes=[BK])
                    for t in range(len(tiles)):
                        kt = c0 // 128 + t
                        for k in range(8):
                            S.op("pe", ("matmul", dict(out=bank(5)[:, 0:128], lhsT=h3[:, k, t * 128:(t + 1) * 128], rhs=w_in3[:, k, 128:256],
                                                                      start=(k == 0), stop=(k == 7))), reads=[Bwin, Bh], writes=[PB[5]])
                        S.op("act", ("copy", dict(out=V3[:, kt, :], in_=bank(5)[:, 0:128])), reads=[PB[5]], writes=[BV])
                for bi, blk in enumerate(blocks):
                    tiles, c0, N = blk
                    h3, Bh = make_hT(blk)
                    for g in range(8):
                        pb = 2 + g % 2
                        for k in range(8):
                            S.op("pe", ("matmul", dict(out=bank(pb)[0:64, 0:N], lhsT=w_in3[:, k, 256 + g * 64:320 + g * 64], rhs=h3[:, k, 0:N],
                                                                             start=(k == 0), stop=(k == 7))), reads=[Bwin, Bh], writes=[PB[pb]])
                        S.op("act", ("activation", dict(out=UG3[0:64, g, 0:N], in_=bank(pb)[0:64, 0:N], func=GELU)), reads=[PB[pb]], writes=[BUG])
                    for t in range(len(tiles)):
                        tc0 = t * 128
                        for k in range(8):
                            S.op("pe", ("matmul", dict(out=bank(4), lhsT=h3[:, k, tc0:tc0 + 128], rhs=w_in3[:, k, 768:1280],
                                                                          start=(k == 0), stop=(k == 7))), reads=[Bwin, Bh], writes=[PB[4]])
                        S.op("act", ("activation", dict(out=gv, in_=bank(4), func=GELU)), reads=[PB[4]], writes=[Bgv])
                        S.op("dve", ("bn_stats", dict(out=st6, in_=gv)), reads=[Bgv], writes=[Bst])
                        S.op("dve", ("bn_aggr", dict(out=mv, in_=st6)), reads=[Bst], writes=[Bmv])
                        S.op("act", ("activation", dict(out=mv[:, 1:2], in_=mv[:, 1:2], func=AF.Sqrt, bias=EPS, scale=1.0)), reads=[Bmv], writes=[Bmv])
                        S.op("dve", ("reciprocal", dict(out=mv[:, 1:2], in_=mv[:, 1:2])), reads=[Bmv], writes=[Bmv])
                        S.op("dve", ("tensor_scalar", dict(out=vnf, in0=gv, scalar1=mv[:, 0:1], scalar2=mv[:, 1:2], op0=ALU.subtract, op1=ALU.mult)),
                             reads=[Bgv, Bmv], writes=[Bvnf])
                        S.op("dve", ("tensor_tensor", dict(out=vnb, in0=vnf, in1=lng_bc, op=ALU.mult)), reads=[Bvnf, Bsm], writes=[Bvnb])
                        pm = r3(psum_t[0:64, 6 * 512:8 * 512], 8)
                        for g in range(8):
                            S.op("pe", ("matmul", dict(out=pm[:, g, :], lhsT=vnb[:, g * 64:(g + 1) * 64], rhs=wspT3[:, g, :], start=True, stop=True)),
                                 reads=[Bvnb, BwspT], writes=[PB[6], PB[7]])
                        S.op("dve", ("tensor_tensor", dict(out=r3(mx[0:64, :], 8), in0=pm, in1=bsp3, op=ALU.add)), reads=[PB[6], PB[7], Bsm], writes=[Bmx])
                        S.op("dve", ("tensor_tensor", dict(out=GT3[0:64, :, tc0:tc0 + 128], in0=r3(mx[0:64, :], 8), in1=UG3[0:64, :, tc0:tc0 + 128], op=ALU.mult)),
                             reads=[Bmx, BUG], writes=[BGT])
                    kts = list(range(2)) if bi == 0 else list(range(18))
                    steps = [(h, ki, kt) for h in range(8) for ki, kt in enumerate(kts)]
                    nk = len(kts)

                    def issue_S(idx):
                        h, ki, kt = steps[idx]
                        half, j = h // 4, h % 4
                        sb_ = (0, 1, 6)[idx % 3]
                        S.op("pe", ("matmul", dict(out=bank(sb_)[:, 0:N], lhsT=KTz[half][:, kt * 128:(kt + 1) * 128],
                                                   rhs=QT3[:, j, c0:c0 + N], start=True, stop=True)), reads=[BK, BQ], writes=[PB[sb_]])
                        pt, Bpt = PT[idx % 4], BPT[idx % 4]
                        S.op("act", ("activation", dict(out=pt[:, 0:N], in_=bank(sb_)[:, 0:N], func=AF.Exp, scale=0.125)), reads=[PB[sb_]], writes=[Bpt])

                    def issue_PV(idx):
                        h, ki, kt = steps[idx]
                        half, j = h // 4, h % 4
                        p0 = half * 64
                        bo, bd = 2 + (h % 2) * 2, 3 + (h % 2) * 2
                        pt, Bpt = PT[idx % 4], BPT[idx % 4]
                        S.op("pe", ("matmul", dict(out=bank(bo)[:, 0:N], lhsT=V3[:, kt, :], rhs=pt[:, 0:N],
                                                   start=(ki == 0), stop=(ki == nk - 1))), reads=[BV, Bpt], writes=[PB[bo]])
                        S.op("pe", ("matmul", dict(out=bank(bd)[:, 0:N], lhsT=ones_b, rhs=pt[:, 0:N],
                                                   start=(ki == 0), stop=(ki == nk - 1))), reads=[Bc, Bpt], writes=[PB[bd]])
                        if ki == nk - 1:
                            S.op("dve", ("reciprocal", dict(out=rec[p0:p0 + 64, 0:N], in_=bank(bd)[p0:p0 + 64, 0:N])), reads=[PB[bd]], writes=[Brec])
                            S.op("dve", ("tensor_tensor", dict(out=OT3[p0:p0 + 64, j, 0:N], in0=bank(bo)[p0:p0 + 64, 0:N], in1=rec[p0:p0 + 64, 0:N], op=ALU.mult)),
                                 reads=[PB[bo], Brec], writes=[BOT])

                    for idx in range(len(steps) + 2):
                        if idx < len(steps):
                            issue_S(idx)
                        if idx >= 2:
                            issue_PV(idx - 2)
                    for t, ti in enumerate(tiles):
                        tc0 = t * 128
                        col = tile_col(ti)
                        for hf in range(2):
                            for c in range(12):
                                if c < 4:
                                    lw, rw = OT3[:, c, tc0:tc0 + 128], wout3[:, c, hf * 512:(hf + 1) * 512]
                                else:
                                    lw, rw = GT3[0:64, c - 4, tc0:tc0 + 128], wout3[0:64, c, hf * 512:(hf + 1) * 512]
                                S.op("pe", ("matmul", dict(out=bank(6 + hf), lhsT=lw, rhs=rw,
                                                                                 start=(c == 0), stop=(c == 11))), reads=[BOT, BGT, Bwout], writes=[PB[6 + hf]])
                        S.dma("sp", ("dma_start", dict(out=xt2, in_=tile_src(L, ti))), writes=[Bxt2])
                        S.op("dve", ("tensor_tensor", dict(out=yt, in0=psum_t[:, 6 * 512:8 * 512], in1=gate_bc[col], op=ALU.mult)),
                             reads=[PB[6], PB[7], Bsm], writes=[Byt])
                        S.op("dve", ("tensor_tensor", dict(out=yt, in0=yt, in1=xt2, op=ALU.add)), reads=[Byt, Bxt2], writes=[Byt])
                        S.dma("sp", ("dma_start", dict(out=xr_d[ti * 128:(ti + 1) * 128, :], in_=yt)), reads=[Byt], writes=[Bxr])
            return Bxr

        if stop_after >= 1:
            Bxr = phase1()
            S.barrier()
            A.off = persist_off
            if "xr" in dbg_d:
                S.dma("sp", ("dma_start", dict(out=dbg_d["xr"], in_=xr_d)), reads=[Bxr])
        def moe(L, ntiles, Bsrc):
            last = (L == 1)
            ncol = 2 if last else 3
            Abc = [A.f32(D) for _ in range(ncol)]
            Sbc = [A.f32(D) for _ in range(ncol)]
            Gbc = [A.f32(D) for _ in range(ncol)]
            Bbc = Buf()
            for c in range(ncol):
                for (kind, dst) in ((4, Abc), (3, Sbc), (5, Gbc)):
                    S.dma("sp", ("dma_start", dict(out=dst[c], in_=modrows_d[L, kind, c:c + 1, :].broadcast_to([128, D]))), reads=[Bmodrows], writes=[Bbc])
            fng = A.f32(D)
            if last:
                S.dma("sp", ("dma_start", dict(out=fng, in_=fng_d.broadcast_to([128, D]))), writes=[Bbc])
            w36 = A.f32(8 * 36)
            w36_3 = r3(w36, 8)
            b36 = A.f32(36)
            S.dma("sp", ("dma_start", dict(out=w36_3[:, :, 0:4], in_=wgrp_d[L].rearrange("(k p) n -> p k n", p=128), allow_slow_non_contiguous=True)), writes=[Bbc])
            S.dma("sp", ("dma_start", dict(out=w36_3[:, :, 4:36], in_=wrt_d[L].rearrange("(k p) n -> p k n", p=128), allow_slow_non_contiguous=True)), writes=[Bbc])
            S.dma("sp", ("dma_start", dict(out=b36[:, 0:4], in_=bgrp_d[L].broadcast_to([128, 4]))), writes=[Bbc])
            S.dma("sp", ("dma_start", dict(out=b36[:, 4:36], in_=brt_d[L].broadcast_to([128, 32]))), writes=[Bbc])
            slot_i = A.i32(ntiles * 2)
            slot_i3 = r3(slot_i, ntiles)
            wts = A.f32(ntiles * 2)
            wts3 = r3(wts, ntiles)
            Bslot = Buf()
            Bwts = Buf()
            run = A.f32(32)
            Brun = Buf()
            S.op("dve", ("memset", dict(ap=run, constant=0.0)), writes=[Brun])
            ztile = A.f32(D)
            Bz = Buf()
            Byall = Buf()
            Bxall = Buf()
            S.op("dve", ("memset", dict(ap=ztile, constant=0.0)), writes=[Bz])
            S.dma("sp", ("dma_start", dict(out=yall_d[NSLOT:NSLOT + 128, :], in_=ztile)), reads=[Bz], writes=[Byall])
            mark = A.off
            xt = [A.f32(D) for _ in range(2)]
            Bxt = [Buf(), Buf()]
            ff = [A.f32(D) for _ in range(2)]
            Bff = [Buf(), Buf()]
            fb = [A.bf16(D) for _ in range(3)]
            Bfb = [Buf(), Buf(), Buf()]
            fTs = A.f32(D)
            BfTs = Buf()
            junk = A.bf16(D)
            Bjunk = Buf()
            sm = A.f32(256)
            Bsmall = Buf()
            ss = sm[:, 0:1]
            lg = sm[:, 4:40]
            gmax = sm[:, 40:41]
            ngmax = sm[:, 41:42]
            gsum = sm[:, 42:43]
            eg = sm[:, 44:48]
            gmask = sm[:, 48:52]
            pen = sm[:, 52:56]
            masked = sm[:, 56:88]
            m8 = sm[:, 88:96]
            ntop1 = sm[:, 96:97]
            e2 = sm[:, 97:98]
            wa = sm[:, 98:99]
            wb = sm[:, 99:100]
            slotf = sm[:, 100:102]
            vab = sm[:, 102:104]
            sel1 = sm[:, 104:136]
            sel = sm[:, 136:168]
            pos = sm[:, 168:200]
            valid = sm[:, 200:232]
            tmp32 = A.f32(32)
            selb = A.bf16(32)
            fTs2 = [fTs, A.f32(D)]
            BfTs2 = [BfTs, Buf()]
            ssA = [A.f32(1), A.f32(1)]
            BssA = [Buf(), Buf()]
            lgb = [A.f32(36), A.f32(36)]
            Blg = [Buf(), Buf()]

            def stageA(ti):
                col = tile_col(ti)
                x_, Bx_ = xt[ti % 2], Bxt[ti % 2]
                f_, Bf_ = ff[ti % 2], Bff[ti % 2]
                fb_, Bfb_ = fb[ti % 3], Bfb[ti % 3]
                ss_, Bss_ = ssA[ti % 2], BssA[ti % 2]
                fT_, BfT_ = fTs2[ti % 2], BfTs2[ti % 2]
                S.dma("sp", ("dma_start", dict(out=x_, in_=xr_d[ti * 128:(ti + 1) * 128, :])), reads=[Bsrc], writes=[Bx_])
                S.op("act", ("activation", dict(out=junk, in_=x_, func=AF.Square, accum_out=ss_)), reads=[Bx_], writes=[Bjunk, Bss_])
                S.op("act", ("activation", dict(out=ss_, in_=ss_, func=AF.Sqrt, scale=1.0 / D, bias=EPS)), reads=[Bss_], writes=[Bss_])
                S.op("dve", ("reciprocal", dict(out=ss_, in_=ss_)), reads=[Bss_], writes=[Bss_])
                S.op("dve", ("scalar_tensor_tensor", dict(out=f_, in0=x_, scalar=ss_, in1=Abc[col], op0=ALU.mult, op1=ALU.mult)), reads=[Bx_, Bss_, Bbc], writes=[Bf_])
                S.op("dve", ("tensor_tensor", dict(out=f_, in0=f_, in1=Sbc[col], op=ALU.add)), reads=[Bf_, Bbc], writes=[Bf_])
                pT = r3(psum_t[:, 0:1024], 8)
                for k in range(8):
                    S.op("pe", ("transpose", dict(out=pT[:, k, :], in_=f_[:, k * 128:(k + 1) * 128], identity=ident_f)), reads=[Bf_, Bc], writes=[PB[0], PB[1]])
                S.op("act", ("copy", dict(out=fT_, in_=psum_t[:, 0:1024])), reads=[PB[0], PB[1]], writes=[BfT_])
                S.op("act", ("copy", dict(out=fb_, in_=f_)), reads=[Bf_], writes=[Bfb_])

            def stageA2(ti):
                fT_, BfT_ = fTs2[ti % 2], BfTs2[ti % 2]
                pl = 2 + ti % 2
                for k in range(8):
                    S.op("pe", ("matmul", dict(out=bank(pl)[:, 0:36], lhsT=fT_[:, k * 128:(k + 1) * 128], rhs=w36_3[:, k, :], start=(k == 0), stop=(k == 7))),
                         reads=[BfT_, Bbc], writes=[PB[pl]])

            def stageB(ti):
                lg = lgb[ti % 2]
                fb_, Bfb_ = fb[ti % 3], Bfb[ti % 3]
                pl = 2 + ti % 2
                S.op("dve", ("tensor_tensor", dict(out=lgb[ti % 2], in0=bank(pl)[:, 0:36], in1=b36, op=ALU.add)), reads=[PB[pl], Bbc], writes=[Blg[ti % 2]])
                dv = lambda name, **kw: S.op("dve", (name, kw), reads=[Bsmall, Brun, Blg[ti % 2]], writes=[Bsmall])
                exv = ex2[ti % 2]
                Bex = Bex2[ti % 2]
                ngm, nt1, tp2 = exv[:, 0:1], exv[:, 1:2], exv[:, 2:3]
                S.op("dve", ("tensor_reduce", dict(out=ngm, in_=lg[:, 0:4], axis=AX.X, op=ALU.max, negate=True)), reads=[Blg[ti % 2]], writes=[Bex])
                S.op("dve", ("tensor_scalar", dict(out=gmask, in0=lg[:, 0:4], scalar1=ngm, scalar2=0.0, op0=ALU.add, op1=ALU.is_ge)), reads=[Blg[ti % 2], Bex, Bsmall], writes=[Bsmall])
                dv("tensor_scalar", out=pen, in0=gmask, scalar1=1e30, scalar2=-1e30, op0=ALU.mult, op1=ALU.add)
                dv("tensor_tensor", out=r3(masked, 4), in0=r3(lg[:, 4:36], 4), in1=pen.unsqueeze(2).broadcast_to([128, 4, 8]), op=ALU.add)
                dv("max", out=m8, in_=masked)
                S.op("dve", ("tensor_copy", dict(out=exv[:, 1:3], in_=m8[:, 0:2])), reads=[Bsmall], writes=[Bex])
                S.op("dve", ("tensor_scalar", dict(out=nt1, in0=nt1, scalar1=-1.0, scalar2=None, op0=ALU.mult)), reads=[Bex], writes=[Bex])
                S.op("act", ("activation", dict(out=eg, in_=lg[:, 0:4], func=AF.Exp, bias=ngm, scale=1.0, accum_out=gsum)), reads=[Bex, Blg[ti % 2]], writes=[Bact])
                S.op("act", ("activation", dict(out=e2, in_=tp2, func=AF.Exp, bias=nt1, scale=1.0)), reads=[Bex], writes=[Bact])
                dv("tensor_scalar", out=sel1, in0=masked, scalar1=m8[:, 0:1], scalar2=None, op0=ALU.is_ge)
                dv("tensor_scalar", out=sel, in0=masked, scalar1=m8[:, 1:2], scalar2=None, op0=ALU.is_ge)
                dv("tensor_copy", out=selb, in_=sel)
                S.op("pe", ("matmul", dict(out=bank(4)[:, 0:32], lhsT=utri_b, rhs=selb, start=True, stop=True)), reads=[Bsmall, Bc], writes=[PB[4]])
                S.op("pe", ("matmul", dict(out=bank(4)[:, 32:64], lhsT=ones_b, rhs=selb, start=True, stop=True)), reads=[Bsmall, Bc], writes=[PB[4]])

            def stageB2(ti):
                fb_, Bfb_ = fb[ti % 3], Bfb[ti % 3]
                dv = lambda name, **kw: S.op("dve", (name, kw), reads=[Bsmall, Brun, Blg[ti % 2]], writes=[Bsmall])
                dv("tensor_tensor", out=sel, in0=sel, in1=sel1, op=ALU.subtract)
                S.op("dve", ("tensor_tensor", dict(out=pos, in0=bank(4)[:, 0:32], in1=run, op=ALU.add)), reads=[PB[4], Brun, Bsmall], writes=[Bsmall])
                S.op("dve", ("tensor_tensor", dict(out=run, in0=bank(4)[:, 32:64], in1=run, op=ALU.add)), reads=[PB[4], Brun, Bsmall], writes=[Brun])
                dv("tensor_scalar", out=valid, in0=pos, scalar1=float(CAP), scalar2=None, op0=ALU.is_lt)
                dv("tensor_tensor", out=pos, in0=pos, in1=iotaC, op=ALU.add)
                dv("scalar_tensor_tensor", out=pos, in0=pos, scalar=-float(TRASH), in1=valid, op0=ALU.add, op1=ALU.mult)
                selcat = r3(sm[:, 104:168], 2)
                pvcat = r3(sm[:, 168:232], 2)
                t4 = tmp128.rearrange("p (a c e) -> p a c e", a=2, c=2)
                dv("tensor_tensor", out=t4, in0=selcat.unsqueeze(2).broadcast_to([128, 2, 2, 32]), in1=pvcat.unsqueeze(1).broadcast_to([128, 2, 2, 32]), op=ALU.mult)
                dv("tensor_reduce", out=r4, in_=r3(tmp128, 4), axis=AX.X, op=ALU.add)
                S.op("dve", ("tensor_scalar", dict(out=slot_i3[:, ti, :], in0=r4[:, 0:4:2], scalar1=float(TRASH), scalar2=None, op0=ALU.add)), reads=[Bsmall], writes=[Bslot])
                for a_ in range(2):
                    S.dma("pool", ("indirect_dma_start", dict(out=xall_d, out_offset=bass.IndirectOffsetOnAxis(ap=slot_i3[:, ti, a_:a_ + 1], axis=0),
                                                             in_=fb_, in_offset=None)), reads=[Bfb_, Bslot], writes=[])
                S.op("dve", ("tensor_scalar", dict(out=wa, in0=e2, scalar1=1.0, scalar2=gsum, op0=ALU.add, op1=ALU.mult)), reads=[Bact, Bsmall], writes=[Bsmall])
                dv("reciprocal", out=wa, in_=wa)
                S.op("dve", ("tensor_tensor", dict(out=wb, in0=wa, in1=e2, op=ALU.mult)), reads=[Bact, Bsmall], writes=[Bsmall])
                S.op("dve", ("tensor_tensor", dict(out=wts3[:, ti, :], in0=sm[:, 98:100], in1=r4[:, 1:4:2], op=ALU.mult)), reads=[Bsmall], writes=[Bwts])

            tmp128 = A.f32(128)
            r4 = sm[:, 240:244]
            Bact = Buf()
            ex2 = [A.f32(4), A.f32(4)]
            Bex2 = [Buf(), Buf()]
            sma = A.f32(8)
            eg = sma[:, 0:4]
            gsum = sma[:, 4:5]
            e2 = sma[:, 5:6]
            for step in range(ntiles + 1):
                if step < ntiles:
                    stageA(step)
                if step >= 1:
                    stageB(step - 1)
                if step < ntiles:
                    stageA2(step)
                if step >= 1:
                    stageB2(step - 1)
            S.barrier()
            A.off = mark
            if last is False and "slots" in dbg_d:
                pass
            NJ = CAP // 128
            wbuf = [(A.bf16(8 * 512), A.bf16(8 * 512), A.bf16(4 * 1024)) for _ in range(2)]
            Bwb = [Buf(), Buf()]
            xrows = [A.bf16(NJ * D) for _ in range(2)]
            Bxrows = [Buf(), Buf()]
            XT = A.bf16(8 * CAP)
            XT3 = r3(XT, 8)
            BXT = Buf()
            sl = [A.f32(512) for _ in range(2)]
            Bsl = [Buf(), Buf()]
            hs = A.bf16(4 * CAP)
            hs3 = r3(hs, 4)
            Bhs = Buf()
            ysb = [A.f32(D) for _ in range(2)]
            Bysb = [Buf(), Buf()]
            yi = 0
            stg = (A.f32(8 * 512), A.f32(8 * 512), A.f32(4 * 1024))
            Bstg = [Buf(), Buf(), Buf()]

            def load_w(e_):
                S.dma("sp", ("dma_start", dict(out=r3(stg[0], 8), in_=w1_d[L, e_].rearrange("(k p) n -> p k n", p=128))), writes=[Bstg[0]])
                S.dma("sp", ("dma_start", dict(out=r3(stg[1], 8), in_=w3_d[L, e_].rearrange("(k p) n -> p k n", p=128))), writes=[Bstg[1]])
                S.dma("sp", ("dma_start", dict(out=r3(stg[2], 4), in_=w2_d[L, e_].rearrange("(k p) n -> p k n", p=128))), writes=[Bstg[2]])

            def cast_w(e_, which):
                dst = wbuf[e_ % 2][which]
                Bw_ = Bwb[e_ % 2]
                if which == 0:
                    S.op("act", ("copy", dict(out=dst, in_=stg[0])), reads=[Bstg[0]], writes=[Bw_])
                elif which == 1:
                    S.op("dve", ("tensor_copy", dict(out=dst, in_=stg[1])), reads=[Bstg[1]], writes=[Bw_])
                else:
                    S.op("act", ("copy", dict(out=dst[:, 0:2048], in_=stg[2][:, 0:2048])), reads=[Bstg[2]], writes=[Bw_])
                    S.op("dve", ("tensor_copy", dict(out=dst[:, 2048:4096], in_=stg[2][:, 2048:4096])), reads=[Bstg[2]], writes=[Bw_])

            def load_x(e_):
                S.dma("sp", ("dma_start", dict(out=r3(xrows[e_ % 2], NJ), in_=xall_d[e_ * CAP:(e_ + 1) * CAP, :].rearrange("(j p) d -> p j d", p=128))),
                      reads=[Bxall], writes=[Bxrows[e_ % 2]])

            load_w(0)
            load_x(0)
            for w_ in range(3):
                cast_w(0, w_)
            for e_ in range(32):
                w1b, w3b, w2b = wbuf[e_ % 2]
                Bw = Bwb[e_ % 2]
                xr_, Bxr_ = xrows[e_ % 2], Bxrows[e_ % 2]
                if e_ + 1 < 32:
                    load_w(e_ + 1)
                    load_x(e_ + 1)
                for j in range(NJ):
                    pb = j % 2
                    pT = r3(bank_bf(pb), 8)
                    for k in range(8):
                        S.op("pe", ("transpose", dict(out=pT[:, k, :], in_=r3(xr_, NJ)[:, j, k * 128:(k + 1) * 128], identity=ident_b)),
                             reads=[Bxr_, Bc], writes=[PB[pb]])
                    S.op("act" if j % 2 == 0 else "dve", ("tensor_copy" if j % 2 else "copy", dict(out=XT3[:, :, j * 128:(j + 1) * 128], in_=pT)),
                         reads=[PB[pb]], writes=[BXT])
                w1v, w3v, w2v = r3(w1b, 8), r3(w3b, 8), r3(w2b, 4)
                for bi_, (c0, n) in enumerate(((0, 512), (512, CAP - 512))):
                    for m in range(4):
                        for (pb, wv) in ((2 + (m % 2) * 2, w1v), (3 + (m % 2) * 2, w3v)):
                            for k in range(8):
                                S.op("pe", ("matmul", dict(out=bank(pb)[:, 0:n], lhsT=wv[:, k, m * 128:(m + 1) * 128], rhs=XT3[:, k, c0:c0 + n], start=(k == 0), stop=(k == 7))),
                                     reads=[Bw, BXT], writes=[PB[pb]])
                        p1, p3 = 2 + (m % 2) * 2, 3 + (m % 2) * 2
                        s_, Bs_ = sl[m % 2], Bsl[m % 2]
                        S.op("act", ("activation", dict(out=s_[:, 0:n], in_=bank(p1)[:, 0:n], func=AF.Silu)), reads=[PB[p1]], writes=[Bs_])
                        S.op("dve", ("tensor_tensor", dict(out=hs3[:, m, c0:c0 + n], in0=bank(p3)[:, 0:n], in1=s_[:, 0:n], op=ALU.mult)), reads=[PB[p3], Bs_], writes=[Bhs])
                    if e_ + 1 < 32:
                        cast_w(e_ + 1, bi_)
                for j in range(NJ):
                    for hf in range(2):
                        for m in range(4):
                            S.op("pe", ("matmul", dict(out=bank(6 + hf), lhsT=hs3[:, m, j * 128:(j + 1) * 128], rhs=w2v[:, m, hf * 512:(hf + 1) * 512], start=(m == 0), stop=(m == 3))),
                                 reads=[Bhs, Bw], writes=[PB[6 + hf]])
                    y_, By_ = ysb[yi % 2], Bysb[yi % 2]
                    yi += 1
                    S.op("act", ("copy", dict(out=y_[:, 0:512], in_=bank(6))), reads=[PB[6]], writes=[By_])
                    S.op("dve", ("tensor_copy", dict(out=y_[:, 512:1024], in_=bank(7))), reads=[PB[7]], writes=[By_])
                    r0 = e_ * CAP + j * 128
                    S.dma("sp", ("dma_start", dict(out=yall_d[r0:r0 + 128, :], in_=y_)), reads=[By_], writes=[Byall])
                    if j == 1 and e_ + 1 < 32:
                        cast_w(e_ + 1, 2)
            S.barrier()
            A.off = mark
            ya = [A.f32(D) for _ in range(2)]
            yb = [A.f32(D) for _ in range(2)]
            x1 = [A.f32(D) for _ in range(2)]
            Bya, Byb, Bx1 = [Buf(), Buf()], [Buf(), Buf()], [Buf(), Buf()]
            junk2 = A.bf16(D)
            Bj2 = Buf()
            ss2 = [A.f32(1) for _ in range(2)]
            Bss2 = [Buf(), Buf()]
            Bdst = Buf()
            for ti in range(ntiles):
                col = tile_col(ti)
                i2 = ti % 2
                S.dma("pool", ("indirect_dma_start", dict(out=ya[i2], out_offset=None, in_=yall_d, in_offset=bass.IndirectOffsetOnAxis(ap=slot_i3[:, ti, 0:1], axis=0))),
                      reads=[Byall, Bslot], writes=[Bya[i2]])
                S.dma("pool", ("indirect_dma_start", dict(out=yb[i2], out_offset=None, in_=yall_d, in_offset=bass.IndirectOffsetOnAxis(ap=slot_i3[:, ti, 1:2], axis=0))),
                      reads=[Byall, Bslot], writes=[Byb[i2]])
                S.dma("sp", ("dma_start", dict(out=x1[i2], in_=xr_d[ti * 128:(ti + 1) * 128, :])), reads=[Bsrc], writes=[Bx1[i2]])
                S.op("act", ("activation", dict(out=ya[i2], in_=ya[i2], func=AF.Copy, scale=wts3[:, ti, 0:1])), reads=[Bya[i2], Bwts], writes=[Bya[i2]])
                S.op("dve", ("scalar_tensor_tensor", dict(out=yb[i2], in0=yb[i2], scalar=wts3[:, ti, 1:2], in1=ya[i2], op0=ALU.mult, op1=ALU.add)),
                     reads=[Byb[i2], Bya[i2], Bwts], writes=[Byb[i2]])
                S.op("dve", ("tensor_tensor", dict(out=yb[i2], in0=yb[i2], in1=Gbc[col], op=ALU.mult)), reads=[Byb[i2], Bbc], writes=[Byb[i2]])
                S.op("dve", ("tensor_tensor", dict(out=x1[i2], in0=x1[i2], in1=yb[i2], op=ALU.add)), reads=[Bx1[i2], Byb[i2]], writes=[Bx1[i2]])
                if not last:
                    S.dma("sp", ("dma_start", dict(out=xr2_d[ti * 128:(ti + 1) * 128, :], in_=x1[i2])), reads=[Bx1[i2]], writes=[Bdst])
                else:
                    S.op("act", ("activation", dict(out=junk2, in_=x1[i2], func=AF.Square, accum_out=ss2[i2])), reads=[Bx1[i2]], writes=[Bj2, Bss2[i2]])
                    S.op("act", ("activation", dict(out=ss2[i2], in_=ss2[i2], func=AF.Sqrt, scale=1.0 / D, bias=EPS)), reads=[Bss2[i2]], writes=[Bss2[i2]])
                    S.op("dve", ("reciprocal", dict(out=ss2[i2], in_=ss2[i2])), reads=[Bss2[i2]], writes=[Bss2[i2]])
                    S.op("dve", ("scalar_tensor_tensor", dict(out=x1[i2], in0=x1[i2], scalar=ss2[i2], in1=fng, op0=ALU.mult, op1=ALU.mult)),
                         reads=[Bx1[i2], Bss2[i2], Bbc], writes=[Bx1[i2]])
                    S.dma("sp", ("dma_start", dict(out=out_d[ti * 128:(ti + 1) * 128, :], in_=x1[i2])), reads=[Bx1[i2]], writes=[Bdst])
            return Bdst

        if stop_after >= 2:
            Bxr2 = moe(0, 36, Bxr)
            S.barrier()
            A.off = persist_off
            if "xr2" in dbg_d:
                S.dma("sp", ("dma_start", dict(out=dbg_d["xr2"], in_=xr2_d)), reads=[Bxr2])
        def s5_mixer(Bsrc):
            L = 1
            TWO_PI = 6.283185307179586
            MAGIC = 12582912.0
            yacc = [A.f32(4 * S_LAT) for _ in range(NB)]
            yacc3 = [r3(y, 4) for y in yacc]
            Byacc = [Buf(), Buf()]
            uT = [A.bf16(4 * SEQ) for _ in range(NB)]
            uT3 = [r3(u, 4) for u in uT]
            BuT = [Buf(), Buf()]
            sd = A.f32(8)
            Bsd = Buf()
            S.dma("sp", ("dma_start", dict(out=sd[:, 0:4], in_=s5d_d)), writes=[Bsd])
            S.dma("sp", ("dma_start", dict(out=sd[:, 4:8], in_=s5bglu_d)), writes=[Bsd])
            mark = A.off
            if S5_DEBUG_STAGE < 1:
                return Byacc[0]
            w5 = A.bf16(8 * 512)
            w5_3 = r3(w5, 8)
            Bw5 = Buf()
            S.dma("pool", ("dma_start", dict(out=w5_3, in_=s5win_d.rearrange("(k p) n -> p k n", p=128))), writes=[Bw5])
            hT1 = A.bf16(8 * 512)
            h3 = r3(hT1, 8)
            Bh = Buf()
            NT = NormT()
            for b in range(NB):
                blocks = [([32 + 2 * b, 33 + 2 * b], 0, 256)]
                for i in range(4):
                    blocks.append(([16 * b + 4 * i + t for t in range(4)], 256 + 512 * i, 512))
                for (tiles, c0, N) in blocks:
                    for t, ti in enumerate(tiles):
                        NT.run(L, ti, h3, Bh, t * 128, t % 2)
                    for r in range(4 if S5_DEBUG_STAGE >= 1.5 else 0):
                        pb = 2 + r % 2
                        for k in range(8):
                            S.op("pe", ("matmul", dict(out=bank(pb)[:, 0:N], lhsT=w5_3[:, k, r * 128:(r + 1) * 128], rhs=h3[:, k, 0:N], start=(k == 0), stop=(k == 7))),
                                 reads=[Bw5, Bh], writes=[PB[pb]])
                        S.op("act", ("copy", dict(out=uT3[b][:, r, c0:c0 + N], in_=bank(pb)[:, 0:N])), reads=[PB[pb]], writes=[BuT[b]])
                        if c0 >= 256:
                            S.op("dve", ("tensor_scalar", dict(out=yacc3[b][:, r, c0 - 256:c0 - 256 + N], in0=uT3[b][:, r, c0:c0 + N], scalar1=sd[:, r:r + 1], scalar2=None, op0=ALU.mult)),
                                 reads=[BuT[b], Bsd], writes=[Byacc[b]])
            S.barrier()
            A.off = mark
            if S5_DEBUG_STAGE < 2:
                return Byacc[0]
            par = A.f32(96)
            par3 = par.rearrange("p (c t) -> p c t", t=3)
            Bpar = Buf()
            S.dma("sp", ("dma_start", dict(out=par, in_=s5par_d)), writes=[Bpar])
            iot = A.f32(SEQ)
            S.dma("sp", ("dma_start", dict(out=iot, in_=s5iota_d)), writes=[Bpar])
            NCB = 32
            pr_ = A.f32(NCB * 16)
            P3 = r3(pr_, 16)
            dtv, rho, tht, frv, sn, cs, nr, ni, inv, cfr, cfi, ncfr, ncfi, tmpa, tmpb, tmpc = [P3[:, i, :] for i in range(16)]
            are, aim, ldt = par3[:, :, 0], par3[:, :, 1], par3[:, :, 2]
            pv = lambda name, **kw: S.op("dve", (name, kw), reads=[Bpar], writes=[Bpar])
            pa = lambda **kw: S.op("act", ("activation", kw), reads=[Bpar], writes=[Bpar])
            pa(out=dtv, in_=ldt, func=AF.Exp)
            pv("tensor_tensor", out=tmpa, in0=are, in1=dtv, op=ALU.mult)
            pa(out=rho, in_=tmpa, func=AF.Exp)
            pv("tensor_tensor", out=tht, in0=aim, in1=dtv, op=ALU.mult)
            pv("tensor_scalar", out=tht, in0=tht, scalar1=1.0 / TWO_PI, scalar2=None, op0=ALU.mult)
            pv("tensor_scalar", out=tmpa, in0=tht, scalar1=MAGIC, scalar2=None, op0=ALU.add)
            pv("tensor_scalar", out=tmpa, in0=tmpa, scalar1=MAGIC, scalar2=None, op0=ALU.subtract)
            pv("tensor_tensor", out=frv, in0=tht, in1=tmpa, op=ALU.subtract)
            SC = TWO_PI * (1.0 - 1e-6)
            pa(out=sn, in_=frv, func=AF.Sin, scale=SC)
            pa(out=tmpb, in_=frv, func=AF.Sin, scale=SC / 2)
            pv("tensor_tensor", out=tmpb, in0=tmpb, in1=tmpb, op=ALU.mult)
            pv("tensor_scalar", out=cs, in0=tmpb, scalar1=-2.0, scalar2=1.0, op0=ALU.mult, op1=ALU.add)
            pv("tensor_tensor", out=nr, in0=rho, in1=cs, op=ALU.mult)
            pv("tensor_scalar", out=nr, in0=nr, scalar1=-1.0, scalar2=None, op0=ALU.add)
            pv("tensor_tensor", out=ni, in0=rho, in1=sn, op=ALU.mult)
            pv("tensor_tensor", out=tmpa, in0=are, in1=are, op=ALU.mult)
            pv("tensor_tensor", out=tmpb, in0=aim, in1=aim, op=ALU.mult)
            pv("tensor_tensor", out=inv, in0=tmpa, in1=tmpb, op=ALU.add)
            pv("reciprocal", out=inv, in_=inv)
            pv("tensor_tensor", out=tmpa, in0=nr, in1=are, op=ALU.mult)
            pv("tensor_tensor", out=tmpb, in0=ni, in1=aim, op=ALU.mult)
            pv("tensor_tensor", out=tmpa, in0=tmpa, in1=tmpb, op=ALU.add)
            pv("tensor_tensor", out=cfr, in0=tmpa, in1=inv, op=ALU.mult)
            pv("tensor_tensor", out=tmpa, in0=ni, in1=are, op=ALU.mult)
            pv("tensor_tensor", out=tmpb, in0=nr, in1=aim, op=ALU.mult)
            pv("tensor_tensor", out=tmpa, in0=tmpa, in1=tmpb, op=ALU.subtract)
            pv("tensor_tensor", out=cfi, in0=tmpa, in1=inv, op=ALU.mult)
            pv("tensor_scalar", out=ncfr, in0=cfr, scalar1=-1.0, scalar2=None, op0=ALU.mult)
            pv("tensor_scalar", out=ncfi, in0=cfi, scalar1=-1.0, scalar2=None, op0=ALU.mult)

            if S5_DEBUG_STAGE < 3:
                return Bpar
            cosT = A.f32(SEQ)
            sinT = A.f32(SEQ)
            tA = A.f32(SEQ)
            tB = A.f32(SEQ)
            Btab, BtA, BtB = Buf(), Buf(), Buf()
            dr = A.f32(SEQ)
            di = A.f32(SEQ)
            qr = A.bf16(SEQ)
            qi = A.bf16(SEQ)
            cosb = A.bf16(SEQ)
            sinb = A.bf16(SEQ)
            Btabb = Buf()
            m1b = [A.bf16(512) for _ in range(2)]
            m2b = [A.bf16(512) for _ in range(2)]
            Bdr, Bdi, Bqr, Bqi = Buf(), Buf(), Buf(), Buf()
            m1 = [A.f32(512) for _ in range(2)]
            m2 = [A.f32(512) for _ in range(2)]
            Bm1, Bm2 = [Buf(), Buf()], [Buf(), Buf()]
            bt = [(A.bf16(128), A.bf16(128)) for _ in range(2)]
            Bbt = [Buf(), Buf()]
            cst_ = [(A.f32(128), A.f32(128)) for _ in range(2)]
            Bcst = [Buf(), Buf()]
            cw = [(A.bf16(128), A.bf16(128)) for _ in range(2)]
            Bcw = [Buf(), Buf()]
            ctmp = A.f32(128)
            Bctmp = Buf()
            ncwr = [A.bf16(128), A.bf16(128)]
            mi = 0
            for d_ in range(2):
                for pr in range(16):
                    ci_ = d_ * 16 + pr
                    r = pr // 4
                    k2 = ci_ % 2
                    btr, bti = bt[k2]
                    S.dma("pool", ("dma_start", dict(out=btr, in_=s5bT_d[d_, 0, pr])), writes=[Bbt[k2]])
                    S.dma("pool", ("dma_start", dict(out=bti, in_=s5bT_d[d_, 1, pr])), writes=[Bbt[k2]])
                    c_r, c_i = cst_[k2]
                    S.dma("sp", ("dma_start", dict(out=c_r, in_=s5c_d[d_, 0, pr])), writes=[Bcst[k2]])
                    S.dma("sp", ("dma_start", dict(out=c_i, in_=s5c_d[d_, 1, pr])), writes=[Bcst[k2]])
                    cwr, cwi = cw[k2]
                    col1 = lambda v, ci_=ci_: v[:, ci_:ci_ + 1]
                    S.op("dve", ("tensor_scalar", dict(out=ctmp, in0=c_r, scalar1=col1(cfr), scalar2=None, op0=ALU.mult)), reads=[Bcst[k2], Bpar], writes=[Bctmp])
                    S.op("dve", ("scalar_tensor_tensor", dict(out=cwr, in0=c_i, scalar=col1(ncfi), in1=ctmp, op0=ALU.mult, op1=ALU.add)), reads=[Bcst[k2], Bpar, Bctmp], writes=[Bcw[k2]])
                    S.op("dve", ("tensor_scalar", dict(out=ncwr[k2], in0=cwr, scalar1=-1.0, scalar2=None, op0=ALU.mult)), reads=[Bcw[k2]], writes=[Bcw[k2]])
                    S.op("dve", ("tensor_scalar", dict(out=ctmp, in0=c_r, scalar1=col1(ncfi), scalar2=None, op0=ALU.mult)), reads=[Bcst[k2], Bpar, Bcw[k2]], writes=[Bctmp])
                    S.op("dve", ("scalar_tensor_tensor", dict(out=cwi, in0=c_i, scalar=col1(ncfr), in1=ctmp, op0=ALU.mult, op1=ALU.add)), reads=[Bcst[k2], Bpar, Bctmp], writes=[Bcw[k2]])
                    S.op("act", ("activation", dict(out=tA, in_=iot, func=AF.Copy, scale=col1(tht))), reads=[Bpar], writes=[BtA])
                    S.op("act", ("activation", dict(out=tB, in_=tA, func=AF.Identity, bias=MAGIC, scale=1.0)), reads=[BtA], writes=[BtB])
                    S.op("act", ("activation", dict(out=tB, in_=tB, func=AF.Identity, bias=-MAGIC, scale=1.0)), reads=[BtB], writes=[BtB])
                    S.op("dve", ("tensor_tensor", dict(out=tA, in0=tA, in1=tB, op=ALU.subtract)), reads=[BtA, BtB], writes=[BtA])
                    S.op("act", ("activation", dict(out=sinT, in_=tA, func=AF.Sin, scale=SC)), reads=[BtA], writes=[Btab])
                    S.op("act", ("activation", dict(out=tB, in_=tA, func=AF.Sin, scale=SC / 2)), reads=[BtA], writes=[BtB])
                    S.op("act", ("activation", dict(out=tB, in_=tB, func=AF.Square, scale=1.4142135623730951)), reads=[BtB], writes=[BtB])
                    S.op("act", ("activation", dict(out=cosT, in_=tB, func=AF.Identity, scale=-1.0, bias=1.0)), reads=[BtB], writes=[Btab])
                    S.op("act", ("copy", dict(out=cosb, in_=cosT)), reads=[Btab], writes=[Btabb])
                    S.op("act", ("copy", dict(out=sinb, in_=sinT)), reads=[Btab], writes=[Btabb])
                    rho_c = col1(rho)
                    for b in range(NB):
                        blocks = [(0, 256)] + [(256 + 512 * i, 512) for i in range(4)]
                        for bi, (s0, N) in enumerate(blocks):
                            if d_ == 0:
                                ucols = uT3[b][:, r, s0:s0 + N]
                            else:
                                if bi == 0:
                                    ucols = uT3[b][:, r, 255::-1]
                                else:
                                    hi_c = SEQ - 1 - (bi - 1) * 512
                                    ucols = uT3[b][:, r, hi_c:hi_c - 512:-1]
                            pbr, pbi = (bi % 2) * 2, (bi % 2) * 2 + 1
                            S.op("pe", ("matmul", dict(out=bank(pbr)[:, 0:N], lhsT=btr, rhs=ucols, start=True, stop=True)), reads=[Bbt[k2], BuT[b]], writes=[PB[pbr]])
                            S.op("pe", ("matmul", dict(out=bank(pbi)[:, 0:N], lhsT=bti, rhs=ucols, start=True, stop=True)), reads=[Bbt[k2], BuT[b]], writes=[PB[pbi]])
                            a1, a2 = m1[mi % 2], m2[mi % 2]
                            Ba1, Ba2 = Bm1[mi % 2], Bm2[mi % 2]
                            mi += 1
                            cS, sS = cosT[:, s0:s0 + N], sinT[:, s0:s0 + N]
                            S.op("dve", ("tensor_tensor", dict(out=a1[:, 0:N], in0=bank(pbr)[:, 0:N], in1=cS, op=ALU.mult)), reads=[PB[pbr], Btab], writes=[Ba1])
                            S.op("dve", ("tensor_tensor", dict(out=a2[:, 0:N], in0=bank(pbi)[:, 0:N], in1=sS, op=ALU.mult)), reads=[PB[pbi], Btab], writes=[Ba2])
                            S.op("dve", ("tensor_tensor", dict(out=dr[:, s0:s0 + N], in0=a1[:, 0:N], in1=a2[:, 0:N], op=ALU.add)), reads=[Ba1, Ba2], writes=[Bdr])
                            a1, a2 = m1[mi % 2], m2[mi % 2]
                            Ba1, Ba2 = Bm1[mi % 2], Bm2[mi % 2]
                            mi += 1
                            S.op("dve", ("tensor_tensor", dict(out=a1[:, 0:N], in0=bank(pbi)[:, 0:N], in1=cS, op=ALU.mult)), reads=[PB[pbi], Btab], writes=[Ba1])
                            S.op("dve", ("tensor_tensor", dict(out=a2[:, 0:N], in0=bank(pbr)[:, 0:N], in1=sS, op=ALU.mult)), reads=[PB[pbr], Btab], writes=[Ba2])
                            S.op("dve", ("tensor_tensor", dict(out=di[:, s0:s0 + N], in0=a1[:, 0:N], in1=a2[:, 0:N], op=ALU.subtract)), reads=[Ba1, Ba2], writes=[Bdi])
                        rb = rho_c.broadcast_to([128, SEQ])
                        S.op("dve", ("tensor_tensor_scan", dict(out=qr, data0=rb, data1=dr, initial=0.0, op0=ALU.mult, op1=ALU.add)), reads=[Bdr, Bpar], writes=[Bqr])
                        S.op("dve", ("tensor_tensor_scan", dict(out=qi, data0=rb, data1=di, initial=0.0, op0=ALU.mult, op1=ALU.add)), reads=[Bdi, Bpar], writes=[Bqi])
                        for bi in range(1, 5):
                            s0, N = blocks[bi]
                            cS, sS = cosb[:, s0:s0 + N], sinb[:, s0:s0 + N]
                            a1, a2 = m1b[mi % 2], m2b[mi % 2]
                            Ba1, Ba2 = Bm1[mi % 2], Bm2[mi % 2]
                            mi += 1
                            S.op("dve", ("tensor_tensor", dict(out=a1, in0=qr[:, s0:s0 + N], in1=cS, op=ALU.mult)), reads=[Bqr, Btabb], writes=[Ba1])
                            S.op("dve", ("tensor_tensor", dict(out=a2, in0=qi[:, s0:s0 + N], in1=sS, op=ALU.mult)), reads=[Bqi, Btabb], writes=[Ba2])
                            pby = 4 + bi % 2
                            S.op("pe", ("matmul", dict(out=bank(pby), lhsT=cwr, rhs=a1, start=True, stop=False)), reads=[Bcw[k2], Ba1], writes=[PB[pby]])
                            S.op("pe", ("matmul", dict(out=bank(pby), lhsT=ncwr[k2], rhs=a2, start=False, stop=False)), reads=[Bcw[k2], Ba2], writes=[PB[pby]])
                            a1, a2 = m1b[mi % 2], m2b[mi % 2]
                            Ba1, Ba2 = Bm1[mi % 2], Bm2[mi % 2]
                            mi += 1
                            S.op("dve", ("tensor_tensor", dict(out=a1, in0=qr[:, s0:s0 + N], in1=sS, op=ALU.mult)), reads=[Bqr, Btabb], writes=[Ba1])
                            S.op("dve", ("tensor_tensor", dict(out=a2, in0=qi[:, s0:s0 + N], in1=cS, op=ALU.mult)), reads=[Bqi, Btabb], writes=[Ba2])
                            S.op("pe", ("matmul", dict(out=bank(pby), lhsT=cwi, rhs=a1, start=False, stop=False)), reads=[Bcw[k2], Ba1], writes=[PB[pby]])
                            S.op("pe", ("matmul", dict(out=bank(pby), lhsT=cwi, rhs=a2, start=False, stop=True)), reads=[Bcw[k2], Ba2], writes=[PB[pby]])
                            if d_ == 0:
                                j0 = s0 - 256
                                ycols = yacc3[b][:, r, j0:j0 + 512]
                            else:
                                hj = S_LAT - 1 - (bi - 1) * 512
                                stop = hj - 512
                                ycols = yacc3[b][:, r, hj::-1] if stop < 0 else yacc3[b][:, r, hj:stop:-1]
                            S.op("dve", ("tensor_tensor", dict(out=ycols, in0=bank(pby), in1=ycols, op=ALU.add)), reads=[PB[pby], Byacc[b]], writes=[Byacc[b]])
            S.barrier()
            A.off = mark
            if "yacc" in dbg_d:
                for b in range(NB):
                    S.dma("sp", ("dma_start", dict(out=dbg_d["yacc"][b * 128:(b + 1) * 128, :], in_=yacc[b])), reads=[Byacc[b]])
            wg = A.bf16(4 * 512)
            wg3 = r3(wg, 4)
            wo = A.bf16(4 * 1024)
            wo3 = r3(wo, 4)
            BwC = Buf()
            S.dma("pool", ("dma_start", dict(out=wg3, in_=s5glu_d.rearrange("(k p) n -> p k n", p=128))), writes=[BwC])
            S.dma("pool", ("dma_start", dict(out=wo3, in_=s5wout_d.rearrange("(k p) n -> p k n", p=128))), writes=[BwC])
            gate_bc = [A.f32(D) for _ in range(2)]
            for c in range(2):
                S.dma("sp", ("dma_start", dict(out=gate_bc[c], in_=modrows_d[L, 2, c:c + 1, :].broadcast_to([128, D]))), reads=[Bmodrows], writes=[BwC])
            gT = A.bf16(4 * 512)
            gT3 = r3(gT, 4)
            vT = A.bf16(4 * 512)
            vT3 = r3(vT, 4)
            BgT, BvT = Buf(), Buf()
            sg = [A.f32(512) for _ in range(2)]
            Bsg = [Buf(), Buf()]
            yt = [A.f32(D) for _ in range(2)]
            xt2 = [A.f32(D) for _ in range(2)]
            Byt, Bxt2 = [Buf(), Buf()], [Buf(), Buf()]
            Bxr = Buf()
            oi = 0
            for b in range(NB):
                for i in range(4):
                    j0 = i * 512
                    S.op("act", ("activation", dict(out=gT3, in_=yacc3[b][:, :, j0:j0 + 512], func=AF.Gelu_apprx_tanh)), reads=[Byacc[b]], writes=[BgT])
                    for m in range(4):
                        pb = 2 + m % 2
                        for k in range(4):
                            S.op("pe", ("matmul", dict(out=bank(pb), lhsT=wg3[:, k, m * 128:(m + 1) * 128], rhs=gT3[:, k, :], start=(k == 0), stop=(k == 3))),
                                 reads=[BwC, BgT], writes=[PB[pb]])
                        S.op("act", ("activation", dict(out=sg[m % 2], in_=bank(pb), func=AF.Sigmoid, bias=sd[:, 4 + m:5 + m], scale=1.0)), reads=[PB[pb], Bsd], writes=[Bsg[m % 2]])
                        S.op("dve", ("tensor_tensor", dict(out=vT3[:, m, :], in0=gT3[:, m, :], in1=sg[m % 2], op=ALU.mult)), reads=[BgT, Bsg[m % 2]], writes=[BvT])
                    for t in range(4):
                        ti = 16 * b + 4 * i + t
                        for hf in range(2):
                            for k in range(4):
                                S.op("pe", ("matmul", dict(out=bank(6 + hf), lhsT=vT3[:, k, t * 128:(t + 1) * 128], rhs=wo3[:, k, hf * 512:(hf + 1) * 512], start=(k == 0), stop=(k == 3))),
                                     reads=[BvT, BwC], writes=[PB[6 + hf]])
                        o2 = oi % 2
                        oi += 1
                        S.dma("sp", ("dma_start", dict(out=xt2[o2], in_=xr2_d[ti * 128:(ti + 1) * 128, :])), reads=[Bsrc], writes=[Bxt2[o2]])
                        S.op("dve", ("tensor_tensor", dict(out=yt[o2], in0=psum_t[:, 6 * 512:8 * 512], in1=gate_bc[b], op=ALU.mult)), reads=[PB[6], PB[7], BwC], writes=[Byt[o2]])
                        S.op("dve", ("tensor_tensor", dict(out=yt[o2], in0=yt[o2], in1=xt2[o2], op=ALU.add)), reads=[Byt[o2], Bxt2[o2]], writes=[Byt[o2]])
                        S.dma("sp", ("dma_start", dict(out=xr_d[ti * 128:(ti + 1) * 128, :], in_=yt[o2])), reads=[Byt[o2]], writes=[Bxr])
            return Bxr

        if stop_after >= 3:
            Bxr_b = s5_mixer(Bxr2)
            S.barrier()
            A.off = persist_off
            if "xr3" in dbg_d:
                S.dma("sp", ("dma_start", dict(out=dbg_d["xr3"], in_=xr_d[0:T_LAT, :])), reads=[Bxr_b])
        if stop_after >= 4:
            Bout = moe(1, 32, Bxr_b)

        S.barrier()
        with nc.Block() as block:
            S.emit(block)
    return nc


def _rope_tables():
    inv = np.power(10000.0, -np.arange(0, 32, 2, dtype=np.float32) / 32).astype(np.float32)
    t = np.arange(S_LAT)
    row = (t // 64).astype(np.float32)
    colp = (t % 64).astype(np.float32)
    ang_r = row[:, None] * inv[None, :]
    ang_c = colp[:, None] * inv[None, :]
    cos64 = np.ones((64, SEQ), np.float32)
    sin64 = np.zeros((64, SEQ), np.float32)
    for d in range(64):
        ang = ang_r if d < 32 else ang_c
        i = d % 16
        sgn = -1.0 if (d % 32) < 16 else 1.0
        cos64[d, C_CTX:] = np.cos(ang[:, i])
        sin64[d, C_CTX:] = sgn * np.sin(ang[:, i])
    return np.concatenate([np.concatenate([cos64, cos64], 0), np.concatenate([sin64, sin64], 0)], 1).astype(np.float32)


_PERM64 = np.array([(d // 32) * 32 + ((d % 32) + 16) % 32 for d in range(64)])


def _consts():
    c = np.zeros((128, NCONST), np.float32)
    c[:, 0:128] = np.eye(128)
    c[:, 128:256] = np.triu(np.ones((128, 128)), 1)
    c[:, 256:384] = 1.0
    c[0:64, 384:448] = 1.0 / 64
    c[64:128, 448:512] = 1.0 / 64
    c[:, 512:544] = (np.arange(32) * CAP)[None, :]
    return c


def make_in_maps(inp, cores):
    f = lambda a: np.ascontiguousarray(np.asarray(a, dtype=np.float32))
    shared = {
        "consts": _consts(), "rope": _rope_tables(),
        "w_mod": f(inp["w_mod"]), "b_mod": f(inp["b_mod"]).reshape(2, 1, 6 * D),
        "norm_mix_g": f(inp["norm_mix_g"]).reshape(2, 1, D), "norm_ffn_g": f(inp["norm_ffn_g"]).reshape(2, 1, D),
        "mix_w_in": f(inp["mix_w_in"][0]), "mix_w_out": f(inp["mix_w_out"][0]),
        "gmlp_norm_g": f(inp["gmlp_norm_g"]).reshape(1, 512), "gmlp_w_spatial": f(inp["gmlp_w_spatial"][0]),
        "gmlp_b_spatial": f(inp["gmlp_b_spatial"][0]).reshape(1, 1024),
        "moe_w_group": f(inp["moe_w_group"]), "moe_b_group": f(inp["moe_b_group"]).reshape(2, 1, 4),
        "moe_w_router": f(inp["moe_w_router"]), "moe_b_router": f(inp["moe_b_router"]).reshape(2, 1, 32),
        "moe_w1": f(inp["moe_w1"]), "moe_w3": f(inp["moe_w3"]), "moe_w2": f(inp["moe_w2"]),
        "final_norm_g": f(inp["final_norm_g"]).reshape(1, D),
        "s5_w_in": f(inp["s5_w_in"][0]), "s5_w_glu": f(inp["s5_w_glu"][0]), "s5_w_out": f(inp["s5_w_out"][0]),
    }
    qg = f(inp["q_norm_g"][0]); kg = f(inp["k_norm_g"][0])
    idx = np.arange(128) % 64
    shared["qkg"] = np.stack([qg[idx], qg[_PERM64[idx]], kg[idx], kg[_PERM64[idx]]], 1).astype(np.float32)
    a_re = f(inp["s5_a_re"][0]); a_im = f(inp["s5_a_im"][0]); ldt = f(inp["s5_log_dt"][0])
    par = np.zeros((128, 2, 16, 3), np.float32)
    for d in range(2):
        for pr in range(16):
            for gl in range(2):
                g = 2 * pr + gl
                par[gl * 64:(gl + 1) * 64, d, pr, 0] = a_re[d, g]
                par[gl * 64:(gl + 1) * 64, d, pr, 1] = a_im[d, g]
                par[gl * 64:(gl + 1) * 64, d, pr, 2] = ldt[d, g]
    shared["s5_par"] = par.reshape(128, 96)
    b_re = f(inp["s5_b_re"][0]); b_im = f(inp["s5_b_im"][0]); c_re = f(inp["s5_c_re"][0]); c_im = f(inp["s5_c_im"][0])
    bT = np.zeros((2, 2, 16, 128, 128), np.float32)
    cc = np.zeros((2, 2, 16, 128, 128), np.float32)
    for d in range(2):
        for pr in range(16):
            for gl in range(2):
                g = 2 * pr + gl
                gic = g % 8
                for ri, (bsrc, csrc) in enumerate(((b_re, c_re), (b_im, c_im))):
                    bT[d, ri, pr, gic * 16:(gic + 1) * 16, gl * 64:(gl + 1) * 64] = bsrc[d, g].T
                    cc[d, ri, pr, gl * 64:(gl + 1) * 64, gic * 16:(gic + 1) * 16] = csrc[d, g].T
    shared["s5_bT"] = bT
    shared["s5_iota"] = np.tile(np.arange(SEQ, dtype=np.float32)[None, :], (128, 1))
    shared["s5_c"] = cc
    shared["s5_d"] = f(inp["s5_d"][0]).reshape(4, 128).T.copy()
    shared["s5_b_glu"] = f(inp["s5_b_glu"][0]).reshape(4, 128).T.copy()
    maps = []
    x = np.asarray(inp["x"]); ctx = np.asarray(inp["ctx"]); c = np.asarray(inp["c"]); cc_ = np.asarray(inp["c_ctx"])
    for core in cores:
        m = dict(shared)
        m["x"] = f(x[2 * core:2 * core + 2]).reshape(T_LAT, D)
        m["ctx"] = f(ctx[2 * core:2 * core + 2]).reshape(T_CTX, D)
        cvec = np.stack([c[2 * core], c[2 * core + 1], cc_], 0).astype(np.float32)
        m["cT"] = np.ascontiguousarray(cvec.reshape(3, 8, 128).transpose(2, 1, 0).reshape(128, 24))
        maps.append(m)
    return maps


_NC_CACHE = {}


def kernel(**inputs):
    if "nc" not in _NC_CACHE:
        _NC_CACHE["nc"] = build_program()
    nc = _NC_CACHE["nc"]
    cores = list(range(8))
    maps = make_in_maps(inputs, cores)
    res = run_bass_kernel_spmd(nc, maps, core_ids=cores)
    outs = [np.asarray(r["out"]).reshape(NB, S_LAT, D) for r in res.results]
    return np.concatenate(outs, 0).astype(np.float32)
```

```python
import numpy as np
from contextlib import ExitStack
import concourse.bass as bass
import concourse.mybir as mybir
from concourse.alu_op_type import AluOpType as ALU
from concourse.bass_utils import run_bass_kernel_spmd

F32 = mybir.dt.float32
BF16 = mybir.dt.bfloat16
I32 = mybir.dt.int32
AF = mybir.ActivationFunctionType
AX = mybir.AxisListType

D = 1024
S_LAT = 2048
C_CTX = 256
NB = 2
T_LAT = NB * S_LAT
T_CTX = NB * C_CTX
SEQ = C_CTX + S_LAT
EPS = 1e-6
CAP = 640
NSLOT = 32 * CAP
TRASH = NSLOT
NCONST = 128 * 4 + 32
S5_DEBUG_STAGE = 9


class Buf:
    __slots__ = ("w", "r")

    def __init__(self):
        self.w = None
        self.r = []


class Sched:
    COMPUTE = ("pe", "act", "dve", "pool")
    NDMA = 8

    def __init__(self, nc, es):
        self.nc = nc
        self.streams = {e: [] for e in ("pe", "act", "dve", "pool", "sp")}
        self.sem = {}
        self.cnt = {}
        for e in self.COMPUTE:
            self.sem[e] = es.enter_context(nc.semaphore("s_" + e))
            self.cnt[e] = 0
        self.drr = {}
        for q in ("sp", "act", "pool"):
            for k in range(self.NDMA):
                key = "d_%s%d" % (q, k)
                self.sem[key] = es.enter_context(nc.semaphore(key))
                self.cnt[key] = 0
            self.drr[q] = 0
        self.waited = {e: {} for e in self.streams}
        self.nops = 0

    def _deps(self, reads, writes):
        deps = []
        for b in reads:
            if b.w is not None:
                deps.append(b.w)
        for b in writes:
            if b.w is not None:
                deps.append(b.w)
            deps.extend(b.r)
        return deps

    def _emit_waits(self, eng, deps, skip_self=None):
        best = {}
        for (k, v) in deps:
            if k == skip_self:
                continue
            if v > best.get(k, 0):
                best[k] = v
        w = self.waited[eng]
        for k, v in best.items():
            if w.get(k, 0) < v:
                w[k] = v
                self.streams[eng].append(("wait", k, v))

    def _mark(self, tok, reads, writes):
        for b in writes:
            b.w = tok
            b.r = []
        for b in reads:
            if b.w is tok:
                continue
            b.r.append(tok)
            if len(b.r) > 16:
                best = {}
                for (k, v) in b.r:
                    if v > best.get(k, 0):
                        best[k] = v
                b.r = list(best.items())

    def op(self, eng, fn, reads=(), writes=()):
        deps = self._deps(reads, writes)
        self._emit_waits(eng, deps, skip_self=("pe" if eng == "pe" else None))
        self.cnt[eng] += 1
        tok = (eng, self.cnt[eng])
        self.streams[eng].append(("op", fn, eng, 1))
        self._mark(tok, reads, writes)
        self.nops += 1
        return tok

    def dma(self, q, fn, reads=(), writes=()):
        k = self.drr[q]
        self.drr[q] = (k + 1) % self.NDMA
        key = "d_%s%d" % (q, k)
        deps = self._deps(reads, writes)
        if self.cnt[key] > 0:
            deps.append((key, self.cnt[key]))
        self._emit_waits(q, deps)
        self.cnt[key] += 16
        tok = (key, self.cnt[key])
        self.streams[q].append(("op", fn, key, 16))
        self._mark(tok, reads, writes)
        self.nops += 1
        return tok

    def barrier(self):
        toks = [(k, v) for k, v in self.cnt.items() if v > 0]
        for e in self.streams:
            self._emit_waits(e, toks)

    def emit(self, block):
        sem = self.sem
        streams = self.streams

        def run(e, lst):
            for it in lst:
                if it[0] == "wait":
                    e.wait_ge(sem[it[1]], it[2])
                else:
                    getattr(e, it[1][0])(**it[1][1]).then_inc(sem[it[2]], it[3])

        @block.sync
        def _(e):
            run(e, streams["sp"])

        @block.scalar
        def _(e):
            run(e, streams["act"])

        @block.vector
        def _(e):
            run(e, streams["dve"])

        @block.gpsimd
        def _(e):
            run(e, streams["pool"])

        @block.tensor
        def _(e):
            run(e, streams["pe"])


class Arena:
    def __init__(self, t, size):
        self.t = t
        self.size = size
        self.off = 0

    def f32(self, n):
        a = self.t[:, self.off:self.off + n]
        self.off += n
        assert self.off <= self.size, ("arena overflow", self.off, self.size)
        return a

    def bf16(self, n):
        return self.f32((n + 1) // 2).bitcast(BF16)[:, 0:n]

    def i32(self, n):
        return self.f32(n).bitcast(I32)


def r3(ap, a):
    return ap.rearrange("p (a b) -> p a b", a=a)


def build_program(stop_after=99, dbg=()):
    nc = bass.Bass("TRN2", target_bir_lowering=False)
    dt = nc.dram_tensor

    def din(name, shape, dtype=F32):
        return dt(name, list(shape), dtype, kind="ExternalInput").ap()

    x_d = din("x", [T_LAT, D])
    ctx_d = din("ctx", [T_CTX, D])
    cT_d = din("cT", [128, 24])
    consts_d = din("consts", [128, NCONST])
    rope_d = din("rope", [128, 2 * SEQ])
    qkg_d = din("qkg", [128, 4])
    w_mod_d = din("w_mod", [2, D, 6 * D])
    b_mod_d = din("b_mod", [2, 1, 6 * D])
    gmix_d = din("norm_mix_g", [2, 1, D])
    gffn_d = din("norm_ffn_g", [2, 1, D])
    w_in_d = din("mix_w_in", [D, 1792])
    w_out_d = din("mix_w_out", [D, D])
    lng_d = din("gmlp_norm_g", [1, 512])
    wsp_d = din("gmlp_w_spatial", [8, 128, 128])
    bsp_d = din("gmlp_b_spatial", [1, 1024])
    wgrp_d = din("moe_w_group", [2, D, 4])
    bgrp_d = din("moe_b_group", [2, 1, 4])
    wrt_d = din("moe_w_router", [2, D, 32])
    brt_d = din("moe_b_router", [2, 1, 32])
    w1_d = din("moe_w1", [2, 32, D, 512])
    w3_d = din("moe_w3", [2, 32, D, 512])
    w2_d = din("moe_w2", [2, 32, 512, D])
    fng_d = din("final_norm_g", [1, D])
    s5win_d = din("s5_w_in", [D, 512])
    s5par_d = din("s5_par", [128, 2 * 16 * 3])
    s5iota_d = din("s5_iota", [128, SEQ])
    s5bT_d = din("s5_bT", [2, 2, 16, 128, 128])
    s5c_d = din("s5_c", [2, 2, 16, 128, 128])
    s5d_d = din("s5_d", [128, 4])
    s5glu_d = din("s5_w_glu", [512, 512])
    s5bglu_d = din("s5_b_glu", [128, 4])
    s5wout_d = din("s5_w_out", [512, D])

    out_d = dt("out", [T_LAT, D], F32, kind="ExternalOutput").ap()
    modrows_d = dt("modrows", [2, 6, 3, D], F32, kind="Internal").ap()
    xr_d = dt("xr", [T_LAT + T_CTX, D], F32, kind="Internal").ap()
    xr2_d = dt("xr2", [T_LAT + T_CTX, D], F32, kind="Internal").ap()
    xall_d = dt("xall", [NSLOT + 128, D], BF16, kind="Internal").ap()
    yall_d = dt("yall", [NSLOT + 128, D], F32, kind="Internal").ap()
    dbg_d = {}
    for name, shape in dbg:
        dbg_d[name] = dt("dbg_" + name, list(shape), F32, kind="ExternalOutput").ap()

    with ExitStack() as es:
        S = Sched(nc, es)
        ARENA_WORDS = 53100
        arena_t = es.enter_context(nc.sbuf_tensor("arena", [128, ARENA_WORDS], F32))
        psum_t = es.enter_context(nc.psum_tensor("psum", [128, 4096], F32))
        A = Arena(arena_t, ARENA_WORDS)

        def bank(i, n=512, off=0):
            return psum_t[:, i * 512 + off:i * 512 + off + n]

        def bank_bf(i):
            return psum_t[:, i * 512:(i + 1) * 512].bitcast(BF16)

        PB = [Buf() for _ in range(8)]

        cst = A.f32(NCONST)
        Bc = Buf()
        S.dma("sp", ("dma_start", dict(out=cst, in_=consts_d)), writes=[Bc])
        ident_f = cst[:, 0:128]
        utri_f = cst[:, 128:256]
        ones_f = cst[:, 256:384]
        blk_f = cst[:, 384:512]
        iotaC = cst[:, 512:544]
        cbf = A.bf16(512)
        S.op("dve", ("tensor_copy", dict(out=cbf, in_=cst[:, 0:512])), reads=[Bc], writes=[Bc])
        ident_b = cbf[:, 0:128]
        utri_b = cbf[:, 128:256]
        ones_b = cbf[:, 256:384]
        blk_b = cbf[:, 384:512]
        modT = A.f32(2 * 2 * 8 * 3)
        BmodT = Buf()
        persist_off = A.off

        cT = A.f32(24)
        sc = A.f32(24)
        Bsc = Buf()
        S.dma("sp", ("dma_start", dict(out=cT, in_=cT_d)), writes=[Bsc])
        S.op("act", ("activation", dict(out=sc, in_=cT, func=AF.Silu)), reads=[Bsc], writes=[Bsc])
        sc3 = r3(sc, 8)
        wblk = [A.f32(8 * 512) for _ in range(2)]
        Bwblk = [Buf(), Buf()]
        mrow = A.f32(6 * D)
        gb = A.f32(2 * D)
        bb = A.f32(6 * D)
        Bmrow, Bgb, Bbb = Buf(), Buf(), Buf()
        Bmodrows = Buf()
        for l in range(2):
            S.dma("sp", ("dma_start", dict(out=bb[0:3, :], in_=b_mod_d[l].broadcast_to([3, 6 * D]))), writes=[Bbb])
            S.dma("sp", ("dma_start", dict(out=gb[0:3, 0:D], in_=gmix_d[l].broadcast_to([3, D]))), writes=[Bgb])
            S.dma("sp", ("dma_start", dict(out=gb[0:3, D:2 * D], in_=gffn_d[l].broadcast_to([3, D]))), writes=[Bgb])
            for nb in range(12):
                wb = wblk[nb % 2]
                Bw = Bwblk[nb % 2]
                S.dma("sp", ("dma_start", dict(
                    out=r3(wb, 8), in_=w_mod_d[l][:, nb * 512:(nb + 1) * 512].rearrange("(k p) n -> p k n", p=128))), writes=[Bw])
                pb = nb % 2
                for k in range(8):
                    S.op("pe", ("matmul", dict(out=bank(pb)[0:3, :], lhsT=sc3[:, k, :], rhs=r3(wb, 8)[:, k, :],
                                                                       start=(k == 0), stop=(k == 7))), reads=[Bsc, Bw], writes=[PB[pb]])
                S.op("dve", ("tensor_tensor", dict(out=mrow[0:3, nb * 512:(nb + 1) * 512], in0=bank(pb)[0:3, :],
                                                                      in1=bb[0:3, nb * 512:(nb + 1) * 512], op=ALU.add)),
                     reads=[PB[pb], Bbb], writes=[Bmrow])
            for (kind, goff) in ((1, 0), (4, D)):
                S.op("dve", ("scalar_tensor_tensor", dict(
                    out=mrow[0:3, kind * D:(kind + 1) * D], in0=mrow[0:3, kind * D:(kind + 1) * D], scalar=1.0,
                    in1=gb[0:3, goff:goff + D], op0=ALU.add, op1=ALU.mult)), reads=[Bmrow, Bgb], writes=[Bmrow])
            S.dma("sp", ("dma_start", dict(out=modrows_d[l].rearrange("k c d -> c k d"), in_=r3(mrow[0:3, :], 6))),
                  reads=[Bmrow], writes=[Bmodrows])
        modT5 = modT.rearrange("p (l m k c) -> p l m k c", l=2, m=2, k=8)
        for l in range(2):
            for m in range(2):
                for c in range(3):
                    S.dma("sp", ("dma_start", dict(
                        out=modT5[:, l, m, :, c], in_=modrows_d[l, m, c].rearrange("(k p) -> p k", p=128),
                        allow_slow_non_contiguous=True)), reads=[Bmodrows], writes=[BmodT])
        S.barrier()
        A.off = persist_off
        if "modrows" in dbg_d:
            S.dma("sp", ("dma_start", dict(out=dbg_d["modrows"], in_=modrows_d.rearrange("l k c d -> (l k c) d"))), reads=[Bmodrows])

        def tile_src(layer, ti):
            if layer == 0:
                if ti < 32:
                    return x_d[ti * 128:(ti + 1) * 128, :]
                return ctx_d[(ti - 32) * 128:(ti - 31) * 128, :]
            return xr2_d[ti * 128:(ti + 1) * 128, :]

        def tile_col(ti):
            if ti < 32:
                return ti // 16
            return 2

        class NormT:
            def __init__(self):
                self.xt = [A.f32(D) for _ in range(2)]
                self.Bxt = [Buf(), Buf()]
                self.junk = A.bf16(D)
                self.Bjunk = Buf()
                self.xn = [A.bf16(D) for _ in range(2)]
                self.Bxn = [Buf(), Buf()]
                self.ss = [A.f32(1) for _ in range(2)]
                self.Bss = [Buf(), Buf()]
                self.i = 0

            def run(self, layer, ti, hT3, BhT, c0, psb):
                i = self.i
                self.i += 1
                xt, Bxt = self.xt[i % 2], self.Bxt[i % 2]
                xn, Bxn = self.xn[i % 2], self.Bxn[i % 2]
                ss, Bss = self.ss[i % 2], self.Bss[i % 2]
                junk, Bjunk = self.junk, self.Bjunk
                src = tile_src(layer, ti)
                col = tile_col(ti)
                S.dma("sp", ("dma_start", dict(out=xt, in_=src)), writes=[Bxt])
                S.op("act", ("activation", dict(out=junk, in_=xt, func=AF.Square, accum_out=ss)), reads=[Bxt], writes=[Bjunk, Bss])
                S.op("act", ("activation", dict(out=ss, in_=ss, func=AF.Sqrt, scale=1.0 / D, bias=EPS)), reads=[Bss], writes=[Bss])
                S.op("dve", ("reciprocal", dict(out=ss, in_=ss)), reads=[Bss], writes=[Bss])
                S.op("dve", ("tensor_scalar", dict(out=xn, in0=xt, scalar1=ss, scalar2=None, op0=ALU.mult)), reads=[Bxt, Bss], writes=[Bxn])
                pT = r3(bank_bf(psb), 8)
                for k in range(8):
                    S.op("pe", ("transpose", dict(out=pT[:, k, :], in_=xn[:, k * 128:(k + 1) * 128], identity=ident_b)),
                         reads=[Bxn, Bc], writes=[PB[psb]])
                for k in range(8):
                    eng = "act" if k % 2 == 0 else "dve"
                    if eng == "act":
                        S.op("act", ("activation", dict(out=hT3[:, k, c0:c0 + 128], in_=pT[:, k, :], func=AF.Identity,
                                                                  scale=modT5[:, layer, 1, k, col:col + 1], bias=modT5[:, layer, 0, k, col:col + 1])),
                             reads=[PB[psb], BmodT], writes=[BhT])
                    else:
                        S.op("dve", ("tensor_scalar", dict(out=hT3[:, k, c0:c0 + 128], in0=pT[:, k, :],
                                                                     scalar1=modT5[:, layer, 1, k, col:col + 1], scalar2=modT5[:, layer, 0, k, col:col + 1],
                                                                     op0=ALU.mult, op1=ALU.add)),
                             reads=[PB[psb], BmodT], writes=[BhT])

        def phase1():
            L = 0
            GELU = AF.Gelu_apprx_tanh
            WC = 1280
            w_in = A.bf16(8 * WC)
            w_in3 = r3(w_in, 8)
            Bwin = Buf()
            for k in range(8):
                S.dma("pool", ("dma_start", dict(out=w_in3[:, k, :], in_=w_in_d[k * 128:(k + 1) * 128, 512:1792])), writes=[Bwin])
            wst = A.bf16(8 * 512)
            wst5 = wst.rearrange("p (k j two d) -> p k j two d", k=8, j=4, two=2)
            for k in range(8):
                for two in range(2):
                    S.dma("pool", ("dma_start", dict(out=wst5[:, k, :, two, :],
                                                     in_=w_in_d[k * 128:(k + 1) * 128, two * 256:(two + 1) * 256].rearrange("p (j d) -> p j d", j=4))), writes=[Bwin])
            wst3 = r3(wst, 8)
            wpst = A.bf16(8 * 512)
            wpst3 = r3(wpst, 8)
            wkp = A.bf16(8 * 128)
            wkp3 = r3(wkp, 8)
            Bwperm = Buf()
            sv = wst.rearrange("p (k x b i) -> p k x b i", k=8, b=2, i=16)
            dv = wpst.rearrange("p (k x b i) -> p k x b i", k=8, b=2, i=16)
            svk = w_in3[:, :, 0:128].rearrange("p k (x b i) -> p k x b i", b=2, i=16)
            dvk = wkp3.rearrange("p k (x b i) -> p k x b i", b=2, i=16)
            for b_ in range(2):
                S.op("dve", ("tensor_copy", dict(out=dv[:, :, :, b_, :], in_=sv[:, :, :, 1 - b_, :])), reads=[Bwin], writes=[Bwperm])
                S.op("dve", ("tensor_copy", dict(out=dvk[:, :, :, b_, :], in_=svk[:, :, :, 1 - b_, :])), reads=[Bwin], writes=[Bwperm])
            wout = A.bf16(12 * 1024)
            wout3 = r3(wout, 12)
            Bwout = Buf()
            S.dma("pool", ("dma_start", dict(out=wout3[0:64, 0:4, :], in_=w_out_d[0:256, :].rearrange("(c r) n -> r c n", r=64))), writes=[Bwout])
            S.dma("pool", ("dma_start", dict(out=wout3[64:128, 0:4, :], in_=w_out_d[256:512, :].rearrange("(c r) n -> r c n", r=64))), writes=[Bwout])
            S.dma("pool", ("dma_start", dict(out=wout3[0:64, 4:8, :], in_=w_out_d[512:768, :].rearrange("(c r) n -> r c n", r=64))), writes=[Bwout])
            S.dma("pool", ("dma_start", dict(out=wout3[0:64, 8:12, :], in_=w_out_d[768:1024, :].rearrange("(c r) n -> r c n", r=64))), writes=[Bwout])
            yt = A.f32(D)
            wspn = yt
            Bwspn = Buf()
            S.dma("sp", ("dma_start", dict(out=r3(wspn, 8), in_=wsp_d.rearrange("g p q -> p g q"))), writes=[Bwspn])
            wspT = A.bf16(1024)
            wspT3 = r3(wspT, 8)
            BwspT = Buf()
            pw = r3(psum_t[:, 0:1024], 8)
            for g in range(8):
                S.op("pe", ("transpose", dict(out=pw[:, g, :], in_=r3(wspn, 8)[:, g, :], identity=ident_f)),
                     reads=[Bwspn, Bc], writes=[PB[0], PB[1]])
            S.op("act", ("copy", dict(out=wspT, in_=psum_t[:, 0:1024])), reads=[PB[0], PB[1]], writes=[BwspT])
            lng_bc = A.f32(512)
            bsp_bc = A.f32(1024)
            qkg = A.f32(4)
            cosT = A.f32(SEQ)
            sinT = A.f32(SEQ)
            gate_bc = [A.f32(D) for _ in range(3)]
            Bsm = Buf()
            S.dma("sp", ("dma_start", dict(out=lng_bc, in_=lng_d.broadcast_to([128, 512]))), writes=[Bsm])
            S.dma("sp", ("dma_start", dict(out=bsp_bc[0:64, :], in_=bsp_d.broadcast_to([64, 1024]))), writes=[Bsm])
            S.dma("sp", ("dma_start", dict(out=qkg, in_=qkg_d)), writes=[Bsm])
            S.dma("sp", ("dma_start", dict(out=cosT, in_=rope_d[:, 0:SEQ])), writes=[Bsm])
            S.dma("sp", ("dma_start", dict(out=sinT, in_=rope_d[:, SEQ:2 * SEQ])), writes=[Bsm])
            for c in range(3):
                S.dma("sp", ("dma_start", dict(out=gate_bc[c], in_=modrows_d[L, 2, c:c + 1, :].broadcast_to([128, D]))),
                      reads=[Bmodrows], writes=[Bsm])
            bsp3 = r3(bsp_bc[0:64, :], 8)

            BK0 = Buf()
            QT = A.bf16(4 * SEQ)
            QT3 = r3(QT, 4)
            KTz = [A.bf16(SEQ), A.bf16(SEQ)]
            S.op("dve", ("memset", dict(ap=KTz[0][64:128, :], constant=0.0)), writes=[BK0])
            S.op("dve", ("memset", dict(ap=KTz[1][0:64, :], constant=0.0)), writes=[BK0])
            Vt = A.bf16(18 * 128)
            V3 = r3(Vt, 18)
            BQ, BK, BV = Buf(), Buf(), Buf()
            hT = [A.bf16(8 * 512), A.bf16(8 * 512)]
            BhT = [Buf(), Buf()]
            NT = NormT()
            sqb = A.bf16(512)
            rs = A.f32(512)
            t1 = A.f32(512)
            t2 = A.f32(512)
            Bsq, Brs, Bt1, Bt2 = Buf(), Buf(), Buf(), Buf()
            mx = A.f32(1024)
            Bmx = Buf()
            rec = A.f32(512)
            Brec = Buf()
            sqb2 = [sqb, A.bf16(512)]
            rs2 = [rs, rec]
            t12 = [t1, mx[:, 0:512]]
            t22 = [t2, mx[:, 512:1024]]
            Bsq2, Brs2, Bt12, Bt22 = [Bsq, Buf()], [Brs, Brec], [Bt1, Bmx], [Bt2, Bmx]
            UG = A.bf16(8 * 512)
            UG3 = r3(UG, 8)
            GT3 = UG3
            OT = A.bf16(4 * 512)
            OT3 = r3(OT, 4)
            BUG = Buf()
            BGT = BUG
            BOT = Buf()
            gv = t1
            vnf = t2
            vnb = A.bf16(512)
            st6 = A.f32(6)
            mv = A.f32(2)
            Bgv, Bvnf = Bt1, Bt2
            Bvnb, Bst, Bmv = Buf(), Buf(), Buf()
            PT = [A.bf16(512) for _ in range(4)]
            BPT = [Buf(), Buf(), Buf(), Buf()]
            rec = A.f32(512)
            Brec = Buf()
            xt2 = A.f32(D)
            Byt, Bxt2 = Buf(), Buf()
            Bxr = Buf()
            print('phase1 arena words', A.off)
            hcount = [0]

            def make_hT(blk):
                tiles, c0seq, N = blk
                i = hcount[0]
                hcount[0] += 1
                h3 = r3(hT[i % 2], 8)
                for t, ti in enumerate(tiles):
                    NT.run(L, ti, h3, BhT[i % 2], t * 128, t % 2)
                return h3, BhT[i % 2]

            for b in range(NB):
                blocks = [([32 + 2 * b, 33 + 2 * b], 0, 256)]
                for i in range(4):
                    blocks.append(([16 * b + 4 * i + t for t in range(4)], 256 + 512 * i, 512))
                nxt = make_hT(blocks[0])
                for bi1, blk in enumerate(blocks):
                    tiles, c0, N = blk
                    h3, Bh = nxt
                    if bi1 + 1 < len(blocks):
                        nxt = make_hT(blocks[bi1 + 1])
                    for j in range(5):
                        bq, bqp, bms = (2, 3, 4) if j % 2 == 0 else (5, 6, 7)
                        sqb_, rs_, t1_, t2_ = sqb2[j % 2], rs2[j % 2], t12[j % 2], t22[j % 2]
                        Bsq_, Brs_, Bt1_, Bt2_ = Bsq2[j % 2], Brs2[j % 2], Bt12[j % 2], Bt22[j % 2]
                        for (pb, wq, wk) in ((bq, wst3, w_in3), (bqp, wpst3, wkp3)):
                            for k in range(8):
                                lw = wq[:, k, j * 128:(j + 1) * 128] if j < 4 else wk[:, k, 0:128]
                                S.op("pe", ("matmul", dict(out=bank(pb)[:, 0:N], lhsT=lw, rhs=h3[:, k, 0:N], start=(k == 0), stop=(k == 7))),
                                     reads=[Bwin, Bwperm, Bh], writes=[PB[pb]])
                        gi = 0 if j < 4 else 2
                        S.op("act", ("activation", dict(out=sqb_[:, 0:N], in_=bank(bq)[:, 0:N], func=AF.Square)), reads=[PB[bq]], writes=[Bsq_])
                        S.op("pe", ("matmul", dict(out=bank(bms)[:, 0:N], lhsT=blk_b, rhs=sqb_[:, 0:N], start=True, stop=True)), reads=[Bsq_, Bc], writes=[PB[bms]])
                        S.op("act", ("activation", dict(out=rs_[:, 0:N], in_=bank(bms)[:, 0:N], func=AF.Sqrt, bias=EPS, scale=1.0)), reads=[PB[bms]], writes=[Brs_])
                        S.op("dve", ("reciprocal", dict(out=rs_[:, 0:N], in_=rs_[:, 0:N])), reads=[Brs_], writes=[Brs_])
                        S.op("dve", ("scalar_tensor_tensor", dict(out=t1_[:, 0:N], in0=bank(bq)[:, 0:N], scalar=qkg[:, gi:gi + 1], in1=cosT[:, c0:c0 + N],
                                                                             op0=ALU.mult, op1=ALU.mult)), reads=[PB[bq], Bsm], writes=[Bt1_])
                        S.op("dve", ("scalar_tensor_tensor", dict(out=t2_[:, 0:N], in0=bank(bqp)[:, 0:N], scalar=qkg[:, gi + 1:gi + 2], in1=sinT[:, c0:c0 + N],
                                                                             op0=ALU.mult, op1=ALU.mult)), reads=[PB[bqp], Bsm], writes=[Bt2_])
                        S.op("dve", ("tensor_tensor", dict(out=t1_[:, 0:N], in0=t1_[:, 0:N], in1=t2_[:, 0:N], op=ALU.add)), reads=[Bt1_, Bt2_], writes=[Bt1_])
                        if j < 4:
                            S.op("dve", ("tensor_tensor", dict(out=QT3[:, j, c0:c0 + N], in0=t1_[:, 0:N], in1=rs_[:, 0:N], op=ALU.mult)), reads=[Bt1_, Brs_], writes=[BQ])
                        else:
                            for hf_ in range(2):
                                ps_ = slice(hf_ * 64, hf_ * 64 + 64)
                                S.op("dve", ("tensor_tensor", dict(out=KTz[hf_][ps_, c0:c0 + N], in0=t1_[ps_, 0:N], in1=rs_[ps_, 0:N], op=ALU.mult)),
                                     reads=[Bt1_, Brs_, BK0], writes=[BK])
                    for t in range(len(tiles)):
                        kt = c0 // 128 + t
                        for k in range(8):
                            S.op("pe", ("matmul", dict(out=bank(5)[:, 0:128], lhsT=h3[:, k, t * 128:(t + 1) * 128], rhs=w_in3[:, k, 128:256],
                                                                      start=(k == 0), stop=(k == 7))), reads=[Bwin, Bh], writes=[PB[5]])
                        S.op("act", ("copy", dict(out=V3[:, kt, :], in_=bank(5)[:, 0:128])), reads=[PB[5]], writes=[BV])
                nxt = make_hT(blocks[0])
                for bi, blk in enumerate(blocks):
                    tiles, c0, N = blk
                    h3, Bh = nxt
                    if bi + 1 < len(blocks):
                        nxt = make_hT(blocks[bi + 1])
                    for g in range(8):
                        pb = 2 + g % 2
                        for k in range(8):
                            S.op("pe", ("matmul", dict(out=bank(pb)[0:64, 0:N], lhsT=w_in3[:, k, 256 + g * 64:320 + g * 64], rhs=h3[:, k, 0:N],
                                                                             start=(k == 0), stop=(k == 7))), reads=[Bwin, Bh], writes=[PB[pb]])
                        S.op("act", ("activation", dict(out=UG3[0:64, g, 0:N], in_=bank(pb)[0:64, 0:N], func=GELU)), reads=[PB[pb]], writes=[BUG])
                    for t in range(len(tiles)):
                        tc0 = t * 128
                        for k in range(8):
                            S.op("pe", ("matmul", dict(out=bank(4), lhsT=h3[:, k, tc0:tc0 + 128], rhs=w_in3[:, k, 768:1280],
                                                                          start=(k == 0), stop=(k == 7))), reads=[Bwin, Bh], writes=[PB[4]])
                        S.op("act", ("activation", dict(out=gv, in_=bank(4), func=GELU)), reads=[PB[4]], writes=[Bgv])
                        S.op("dve", ("bn_stats", dict(out=st6, in_=gv)), reads=[Bgv], writes=[Bst])
                        S.op("dve", ("bn_aggr", dict(out=mv, in_=st6)), reads=[Bst], writes=[Bmv])
                        S.op("act", ("activation", dict(out=mv[:, 1:2], in_=mv[:, 1:2], func=AF.Sqrt, bias=EPS, scale=1.0)), reads=[Bmv], writes=[Bmv])
                        S.op("dve", ("reciprocal", dict(out=mv[:, 1:2], in_=mv[:, 1:2])), reads=[Bmv], writes=[Bmv])
                        S.op("dve", ("tensor_scalar", dict(out=vnf, in0=gv, scalar1=mv[:, 0:1], scalar2=mv[:, 1:2], op0=ALU.subtract, op1=ALU.mult)),
                             reads=[Bgv, Bmv], writes=[Bvnf])
                        S.op("dve", ("tensor_tensor", dict(out=vnb, in0=vnf, in1=lng_bc, op=ALU.mult)), reads=[Bvnf, Bsm], writes=[Bvnb])
                        pm = r3(psum_t[0:64, 6 * 512:8 * 512], 8)
                        for g in range(8):
                            S.op("pe", ("matmul", dict(out=pm[:, g, :], lhsT=vnb[:, g * 64:(g + 1) * 64], rhs=wspT3[:, g, :], start=True, stop=True)),
                                 reads=[Bvnb, BwspT], writes=[PB[6], PB[7]])
                        S.op("dve", ("tensor_tensor", dict(out=r3(mx[0:64, :], 8), in0=pm, in1=bsp3, op=ALU.add)), reads=[PB[6], PB[7], Bsm], writes=[Bmx])
                        S.op("dve", ("tensor_tensor", dict(out=GT3[0:64, :, tc0:tc0 + 128], in0=r3(mx[0:64, :], 8), in1=UG3[0:64, :, tc0:tc0 + 128], op=ALU.mult)),
                             reads=[Bmx, BUG], writes=[BGT])
                    kts = list(range(2)) if bi == 0 else list(range(18))
                    steps = [(h, ki, kt) for h in range(8) for ki, kt in enumerate(kts)]
                    nk = len(kts)

                    def issue_S(idx):
                        h, ki, kt = steps[idx]
                        half, j = h // 4, h % 4
                        sb_ = (0, 1, 6)[idx % 3]
                        S.op("pe", ("matmul", dict(out=bank(sb_)[:, 0:N], lhsT=KTz[half][:, kt * 128:(kt + 1) * 128],
                                                   rhs=QT3[:, j, c0:c0 + N], start=True, stop=True)), reads=[BK, BQ], writes=[PB[sb_]])
                        pt, Bpt = PT[idx % 4], BPT[idx % 4]
                        S.op("act", ("activation", dict(out=pt[:, 0:N], in_=bank(sb_)[:, 0:N], func=AF.Exp, scale=0.125)), reads=[PB[sb_]], writes=[Bpt])

                    def issue_PV(idx):
                        h, ki, kt = steps[idx]
                        half, j = h // 4, h % 4
                        p0 = half * 64
                        bo, bd = 2 + (h % 2) * 2, 3 + (h % 2) * 2
                        pt, Bpt = PT[idx % 4], BPT[idx % 4]
                        S.op("pe", ("matmul", dict(out=bank(bo)[:, 0:N], lhsT=V3[:, kt, :], rhs=pt[:, 0:N],
                                                   start=(ki == 0), stop=(ki == nk - 1))), reads=[BV, Bpt], writes=[PB[bo]])
                        S.op("pe", ("matmul", dict(out=bank(bd)[:, 0:N], lhsT=ones_b, rhs=pt[:, 0:N],
                                                   start=(ki == 0), stop=(ki == nk - 1))), reads=[Bc, Bpt], writes=[PB[bd]])
                        if ki == nk - 1:
                            S.op("dve", ("reciprocal", dict(out=rec[p0:p0 + 64, 0:N], in_=bank(bd)[p0:p0 + 64, 0:N])), reads=[PB[bd]], writes=[Brec])
                            S.op("dve", ("tensor_tensor", dict(out=OT3[p0:p0 + 64, j, 0:N], in0=bank(bo)[p0:p0 + 64, 0:N], in1=rec[p0:p0 + 64, 0:N], op=ALU.mult)),
                                 reads=[PB[bo], Brec], writes=[BOT])

                    for idx in range(len(steps) + 2):
                        if idx < len(steps):
                            issue_S(idx)
                        if idx >= 2:
                            issue_PV(idx - 2)
                    for t, ti in enumerate(tiles):
                        tc0 = t * 128
                        col = tile_col(ti)
                        for hf in range(2):
                            for c in range(12):
                                if c < 4:
                                    lw, rw = OT3[:, c, tc0:tc0 + 128], wout3[:, c, hf * 512:(hf + 1) * 512]
                                else:
                                    lw, rw = GT3[0:64, c - 4, tc0:tc0 + 128], wout3[0:64, c, hf * 512:(hf + 1) * 512]
                                S.op("pe", ("matmul", dict(out=bank(6 + hf), lhsT=lw, rhs=rw,
                                                                                 start=(c == 0), stop=(c == 11))), reads=[BOT, BGT, Bwout], writes=[PB[6 + hf]])
                        S.dma("sp", ("dma_start", dict(out=xt2, in_=tile_src(L, ti))), writes=[Bxt2])
                        S.op("dve", ("tensor_tensor", dict(out=yt, in0=psum_t[:, 6 * 512:8 * 512], in1=gate_bc[col], op=ALU.mult)),
                             reads=[PB[6], PB[7], Bsm], writes=[Byt])
                        S.op("dve", ("tensor_tensor", dict(out=yt, in0=yt, in1=xt2, op=ALU.add)), reads=[Byt, Bxt2], writes=[Byt])
                        S.dma("sp", ("dma_start", dict(out=xr_d[ti * 128:(ti + 1) * 128, :], in_=yt)), reads=[Byt], writes=[Bxr])
            return Bxr

        if stop_after >= 1:
            Bxr = phase1()
            S.barrier()
            A.off = persist_off
            if "xr" in dbg_d:
                S.dma("sp", ("dma_start", dict(out=dbg_d["xr"], in_=xr_d)), reads=[Bxr])
        def moe(L, ntiles, Bsrc):
            last = (L == 1)
            ncol = 2 if last else 3
            Abc = [A.f32(D) for _ in range(ncol)]
            Sbc = [A.f32(D) for _ in range(ncol)]
            Gbc = [A.f32(D) for _ in range(ncol)]
            Bbc = Buf()
            for c in range(ncol):
                for (kind, dst) in ((4, Abc), (3, Sbc), (5, Gbc)):
                    S.dma("sp", ("dma_start", dict(out=dst[c], in_=modrows_d[L, kind, c:c + 1, :].broadcast_to([128, D]))), reads=[Bmodrows], writes=[Bbc])
            fng = A.f32(D)
            if last:
                S.dma("sp", ("dma_start", dict(out=fng, in_=fng_d.broadcast_to([128, D]))), writes=[Bbc])
            w36 = A.f32(8 * 36)
            w36_3 = r3(w36, 8)
            b36 = A.f32(36)
            S.dma("sp", ("dma_start", dict(out=w36_3[:, :, 0:4], in_=wgrp_d[L].rearrange("(k p) n -> p k n", p=128), allow_slow_non_contiguous=True)), writes=[Bbc])
            S.dma("sp", ("dma_start", dict(out=w36_3[:, :, 4:36], in_=wrt_d[L].rearrange("(k p) n -> p k n", p=128), allow_slow_non_contiguous=True)), writes=[Bbc])
            S.dma("sp", ("dma_start", dict(out=b36[:, 0:4], in_=bgrp_d[L].broadcast_to([128, 4]))), writes=[Bbc])
            S.dma("sp", ("dma_start", dict(out=b36[:, 4:36], in_=brt_d[L].broadcast_to([128, 32]))), writes=[Bbc])
            slot_i = A.i32(ntiles * 2)
            slot_i3 = r3(slot_i, ntiles)
            wts = A.f32(ntiles * 2)
            wts3 = r3(wts, ntiles)
            Bslot = Buf()
            Bwts = Buf()
            run = A.f32(32)
            Brun = Buf()
            S.op("dve", ("memset", dict(ap=run, constant=0.0)), writes=[Brun])
            ztile = A.f32(D)
            Bz = Buf()
            Byall = Buf()
            Bxall = Buf()
            S.op("dve", ("memset", dict(ap=ztile, constant=0.0)), writes=[Bz])
            S.dma("sp", ("dma_start", dict(out=yall_d[NSLOT:NSLOT + 128, :], in_=ztile)), reads=[Bz], writes=[Byall])
            mark = A.off
            xt = [A.f32(D) for _ in range(2)]
            Bxt = [Buf(), Buf()]
            ff = [A.f32(D) for _ in range(2)]
            Bff = [Buf(), Buf()]
            fb = [A.bf16(D) for _ in range(3)]
            Bfb = [Buf(), Buf(), Buf()]
            fTs = A.f32(D)
            BfTs = Buf()
            junk = A.bf16(D)
            Bjunk = Buf()
            sm = A.f32(256)
            Bsmall = Buf()
            ss = sm[:, 0:1]
            lg = sm[:, 4:40]
            gmax = sm[:, 40:41]
            ngmax = sm[:, 41:42]
            gsum = sm[:, 42:43]
            eg = sm[:, 44:48]
            gmask = sm[:, 48:52]
            pen = sm[:, 52:56]
            masked = sm[:, 56:88]
            m8 = sm[:, 88:96]
            ntop1 = sm[:, 96:97]
            e2 = sm[:, 97:98]
            wa = sm[:, 98:99]
            wb = sm[:, 99:100]
            slotf = sm[:, 100:102]
            vab = sm[:, 102:104]
            sel1 = sm[:, 104:136]
            sel = sm[:, 136:168]
            pos = sm[:, 168:200]
            valid = sm[:, 200:232]
            tmp32 = A.f32(32)
            selb = A.bf16(32)
            fTs2 = [fTs, A.f32(D)]
            BfTs2 = [BfTs, Buf()]
            ssA = [A.f32(1), A.f32(1)]
            BssA = [Buf(), Buf()]
            lgb = [A.f32(36), A.f32(36)]
            Blg = [Buf(), Buf()]

            def stageA(ti):
                col = tile_col(ti)
                x_, Bx_ = xt[ti % 2], Bxt[ti % 2]
                f_, Bf_ = ff[ti % 2], Bff[ti % 2]
                fb_, Bfb_ = fb[ti % 3], Bfb[ti % 3]
                ss_, Bss_ = ssA[ti % 2], BssA[ti % 2]
                fT_, BfT_ = fTs2[ti % 2], BfTs2[ti % 2]
                S.dma("sp", ("dma_start", dict(out=x_, in_=xr_d[ti * 128:(ti + 1) * 128, :])), reads=[Bsrc], writes=[Bx_])
                S.op("act", ("activation", dict(out=junk, in_=x_, func=AF.Square, accum_out=ss_)), reads=[Bx_], writes=[Bjunk, Bss_])
                S.op("act", ("activation", dict(out=ss_, in_=ss_, func=AF.Sqrt, scale=1.0 / D, bias=EPS)), reads=[Bss_], writes=[Bss_])
                S.op("dve", ("reciprocal", dict(out=ss_, in_=ss_)), reads=[Bss_], writes=[Bss_])
                S.op("dve", ("scalar_tensor_tensor", dict(out=f_, in0=x_, scalar=ss_, in1=Abc[col], op0=ALU.mult, op1=ALU.mult)), reads=[Bx_, Bss_, Bbc], writes=[Bf_])
                S.op("dve", ("tensor_tensor", dict(out=f_, in0=f_, in1=Sbc[col], op=ALU.add)), reads=[Bf_, Bbc], writes=[Bf_])
                pT = r3(psum_t[:, 0:1024], 8)
                for k in range(8):
                    S.op("pe", ("transpose", dict(out=pT[:, k, :], in_=f_[:, k * 128:(k + 1) * 128], identity=ident_f)), reads=[Bf_, Bc], writes=[PB[0], PB[1]])
                S.op("act", ("copy", dict(out=fT_, in_=psum_t[:, 0:1024])), reads=[PB[0], PB[1]], writes=[BfT_])
                S.op("act", ("copy", dict(out=fb_, in_=f_)), reads=[Bf_], writes=[Bfb_])

            def stageA2(ti):
                fT_, BfT_ = fTs2[ti % 2], BfTs2[ti % 2]
                pl = 2 + ti % 2
                for k in range(8):
                    S.op("pe", ("matmul", dict(out=bank(pl)[:, 0:36], lhsT=fT_[:, k * 128:(k + 1) * 128], rhs=w36_3[:, k, :], start=(k == 0), stop=(k == 7))),
                         reads=[BfT_, Bbc], writes=[PB[pl]])

            def stageB(ti):
                lg = lgb[ti % 2]
                fb_, Bfb_ = fb[ti % 3], Bfb[ti % 3]
                pl = 2 + ti % 2
                S.op("dve", ("tensor_tensor", dict(out=lgb[ti % 2], in0=bank(pl)[:, 0:36], in1=b36, op=ALU.add)), reads=[PB[pl], Bbc], writes=[Blg[ti % 2]])
                dv = lambda name, **kw: S.op("dve", (name, kw), reads=[Bsmall, Brun, Blg[ti % 2]], writes=[Bsmall])
                exv = ex2[ti % 2]
                Bex = Bex2[ti % 2]
                ngm, nt1, tp2 = exv[:, 0:1], exv[:, 1:2], exv[:, 2:3]
                S.op("dve", ("tensor_reduce", dict(out=ngm, in_=lg[:, 0:4], axis=AX.X, op=ALU.max, negate=True)), reads=[Blg[ti % 2]], writes=[Bex])
                S.op("dve", ("tensor_scalar", dict(out=gmask, in0=lg[:, 0:4], scalar1=ngm, scalar2=0.0, op0=ALU.add, op1=ALU.is_ge)), reads=[Blg[ti % 2], Bex, Bsmall], writes=[Bsmall])
                dv("tensor_scalar", out=pen, in0=gmask, scalar1=1e30, scalar2=-1e30, op0=ALU.mult, op1=ALU.add)
                dv("tensor_tensor", out=r3(masked, 4), in0=r3(lg[:, 4:36], 4), in1=pen.unsqueeze(2).broadcast_to([128, 4, 8]), op=ALU.add)
                dv("max", out=m8, in_=masked)
                S.op("dve", ("tensor_copy", dict(out=exv[:, 1:3], in_=m8[:, 0:2])), reads=[Bsmall], writes=[Bex])
                S.op("dve", ("tensor_scalar", dict(out=nt1, in0=nt1, scalar1=-1.0, scalar2=None, op0=ALU.mult)), reads=[Bex], writes=[Bex])
                S.op("act", ("activation", dict(out=eg, in_=lg[:, 0:4], func=AF.Exp, bias=ngm, scale=1.0, accum_out=gsum)), reads=[Bex, Blg[ti % 2]], writes=[Bact])
                S.op("act", ("activation", dict(out=e2, in_=tp2, func=AF.Exp, bias=nt1, scale=1.0)), reads=[Bex], writes=[Bact])
                dv("tensor_scalar", out=sel1, in0=masked, scalar1=m8[:, 0:1], scalar2=None, op0=ALU.is_ge)
                dv("tensor_scalar", out=sel, in0=masked, scalar1=m8[:, 1:2], scalar2=None, op0=ALU.is_ge)
                dv("tensor_copy", out=selb, in_=sel)
                S.op("pe", ("matmul", dict(out=bank(4)[:, 0:32], lhsT=utri_b, rhs=selb, start=True, stop=True)), reads=[Bsmall, Bc], writes=[PB[4]])
                S.op("pe", ("matmul", dict(out=bank(4)[:, 32:64], lhsT=ones_b, rhs=selb, start=True, stop=True)), reads=[Bsmall, Bc], writes=[PB[4]])

            def stageB2(ti):
                fb_, Bfb_ = fb[ti % 3], Bfb[ti % 3]
                dv = lambda name, **kw: S.op("dve", (name, kw), reads=[Bsmall, Brun, Blg[ti % 2]], writes=[Bsmall])
                dv("tensor_tensor", out=sel, in0=sel, in1=sel1, op=ALU.subtract)
                S.op("dve", ("tensor_tensor", dict(out=pos, in0=bank(4)[:, 0:32], in1=run, op=ALU.add)), reads=[PB[4], Brun, Bsmall], writes=[Bsmall])
                S.op("dve", ("tensor_tensor", dict(out=run, in0=bank(4)[:, 32:64], in1=run, op=ALU.add)), reads=[PB[4], Brun, Bsmall], writes=[Brun])
                dv("tensor_scalar", out=valid, in0=pos, scalar1=float(CAP), scalar2=None, op0=ALU.is_lt)
                dv("tensor_tensor", out=pos, in0=pos, in1=iotaC, op=ALU.add)
                dv("scalar_tensor_tensor", out=pos, in0=pos, scalar=-float(TRASH), in1=valid, op0=ALU.add, op1=ALU.mult)
                selcat = r3(sm[:, 104:168], 2)
                pvcat = r3(sm[:, 168:232], 2)
                t4 = tmp128.rearrange("p (a c e) -> p a c e", a=2, c=2)
                dv("tensor_tensor", out=t4, in0=selcat.unsqueeze(2).broadcast_to([128, 2, 2, 32]), in1=pvcat.unsqueeze(1).broadcast_to([128, 2, 2, 32]), op=ALU.mult)
                dv("tensor_reduce", out=r4, in_=r3(tmp128, 4), axis=AX.X, op=ALU.add)
                S.op("dve", ("tensor_scalar", dict(out=slot_i3[:, ti, :], in0=r4[:, 0:4:2], scalar1=float(TRASH), scalar2=None, op0=ALU.add)), reads=[Bsmall], writes=[Bslot])
                for a_ in range(2):
                    S.dma("pool", ("indirect_dma_start", dict(out=xall_d, out_offset=bass.IndirectOffsetOnAxis(ap=slot_i3[:, ti, a_:a_ + 1], axis=0),
                                                             in_=fb_, in_offset=None)), reads=[Bfb_, Bslot], writes=[])
                S.op("dve", ("tensor_scalar", dict(out=wa, in0=e2, scalar1=1.0, scalar2=gsum, op0=ALU.add, op1=ALU.mult)), reads=[Bact, Bsmall], writes=[Bsmall])
                dv("reciprocal", out=wa, in_=wa)
                S.op("dve", ("tensor_tensor", dict(out=wb, in0=wa, in1=e2, op=ALU.mult)), reads=[Bact, Bsmall], writes=[Bsmall])
                S.op("dve", ("tensor_tensor", dict(out=wts3[:, ti, :], in0=sm[:, 98:100], in1=r4[:, 1:4:2], op=ALU.mult)), reads=[Bsmall], writes=[Bwts])

            tmp128 = A.f32(128)
            r4 = sm[:, 240:244]
            Bact = Buf()
            ex2 = [A.f32(4), A.f32(4)]
            Bex2 = [Buf(), Buf()]
            sma = A.f32(8)
            eg = sma[:, 0:4]
            gsum = sma[:, 4:5]
            e2 = sma[:, 5:6]
            for step in range(ntiles + 1):
                if step < ntiles:
                    stageA(step)
                if step >= 1:
                    stageB(step - 1)
                if step < ntiles:
                    stageA2(step)
                if step >= 1:
                    stageB2(step - 1)
            S.barrier()
            A.off = mark
            if last is False and "slots" in dbg_d:
                pass
            NJ = CAP // 128
            wbuf = [(A.bf16(8 * 512), A.bf16(8 * 512), A.bf16(4 * 1024)) for _ in range(2)]
            Bwb = [Buf(), Buf()]
            xrows = [A.bf16(NJ * D) for _ in range(2)]
            Bxrows = [Buf(), Buf()]
            XT = A.bf16(8 * CAP)
            XT3 = r3(XT, 8)
            BXT = Buf()
            sl = [A.f32(512) for _ in range(2)]
            Bsl = [Buf(), Buf()]
            hs = A.bf16(4 * CAP)
            hs3 = r3(hs, 4)
            Bhs = Buf()
            ysb = [A.f32(D) for _ in range(2)]
            Bysb = [Buf(), Buf()]
            yi = 0
            stg = (A.f32(8 * 512), A.f32(8 * 512), A.f32(4 * 1024))
            Bstg = [Buf(), Buf(), Buf()]

            def load_w(e_):
                S.dma("sp", ("dma_start", dict(out=r3(stg[0], 8), in_=w1_d[L, e_].rearrange("(k p) n -> p k n", p=128))), writes=[Bstg[0]])
                S.dma("sp", ("dma_start", dict(out=r3(stg[1], 8), in_=w3_d[L, e_].rearrange("(k p) n -> p k n", p=128))), writes=[Bstg[1]])
                S.dma("sp", ("dma_start", dict(out=r3(stg[2], 4), in_=w2_d[L, e_].rearrange("(k p) n -> p k n", p=128))), writes=[Bstg[2]])

            def cast_w(e_, which):
                dst = wbuf[e_ % 2][which]
                Bw_ = Bwb[e_ % 2]
                if which == 0:
                    S.op("act", ("copy", dict(out=dst, in_=stg[0])), reads=[Bstg[0]], writes=[Bw_])
                elif which == 1:
                    S.op("dve", ("tensor_copy", dict(out=dst, in_=stg[1])), reads=[Bstg[1]], writes=[Bw_])
                else:
                    S.op("act", ("copy", dict(out=dst[:, 0:2048], in_=stg[2][:, 0:2048])), reads=[Bstg[2]], writes=[Bw_])
                    S.op("dve", ("tensor_copy", dict(out=dst[:, 2048:4096], in_=stg[2][:, 2048:4096])), reads=[Bstg[2]], writes=[Bw_])

            def load_x(e_):
                S.dma("sp", ("dma_start", dict(out=r3(xrows[e_ % 2], NJ), in_=xall_d[e_ * CAP:(e_ + 1) * CAP, :].rearrange("(j p) d -> p j d", p=128))),
                      reads=[Bxall], writes=[Bxrows[e_ % 2]])

            load_w(0)
            load_x(0)
            for w_ in range(3):
                cast_w(0, w_)
            for e_ in range(32):
                w1b, w3b, w2b = wbuf[e_ % 2]
                Bw = Bwb[e_ % 2]
                xr_, Bxr_ = xrows[e_ % 2], Bxrows[e_ % 2]
                if e_ + 1 < 32:
                    load_w(e_ + 1)
                    load_x(e_ + 1)
                for j in range(NJ):
                    pb = j % 2
                    pT = r3(bank_bf(pb), 8)
                    for k in range(8):
                        S.op("pe", ("transpose", dict(out=pT[:, k, :], in_=r3(xr_, NJ)[:, j, k * 128:(k + 1) * 128], identity=ident_b)),
                             reads=[Bxr_, Bc], writes=[PB[pb]])
                    S.op("act" if j % 2 == 0 else "dve", ("tensor_copy" if j % 2 else "copy", dict(out=XT3[:, :, j * 128:(j + 1) * 128], in_=pT)),
                         reads=[PB[pb]], writes=[BXT])
                w1v, w3v, w2v = r3(w1b, 8), r3(w3b, 8), r3(w2b, 4)
                for bi_, (c0, n) in enumerate(((0, 512), (512, CAP - 512))):
                    for m in range(4):
                        for (pb, wv) in ((2 + (m % 2) * 2, w1v), (3 + (m % 2) * 2, w3v)):
                            for k in range(8):
                                S.op("pe", ("matmul", dict(out=bank(pb)[:, 0:n], lhsT=wv[:, k, m * 128:(m + 1) * 128], rhs=XT3[:, k, c0:c0 + n], start=(k == 0), stop=(k == 7))),
                                     reads=[Bw, BXT], writes=[PB[pb]])
                        p1, p3 = 2 + (m % 2) * 2, 3 + (m % 2) * 2
                        s_, Bs_ = sl[m % 2], Bsl[m % 2]
                        S.op("act", ("activation", dict(out=s_[:, 0:n], in_=bank(p1)[:, 0:n], func=AF.Silu)), reads=[PB[p1]], writes=[Bs_])
                        S.op("dve", ("tensor_tensor", dict(out=hs3[:, m, c0:c0 + n], in0=bank(p3)[:, 0:n], in1=s_[:, 0:n], op=ALU.mult)), reads=[PB[p3], Bs_], writes=[Bhs])
                    if e_ + 1 < 32:
                        cast_w(e_ + 1, bi_)
                for j in range(NJ):
                    ob = 6
                    for hf in range(2):
                        for m in range(4):
                            S.op("pe", ("matmul", dict(out=bank(ob + hf), lhsT=hs3[:, m, j * 128:(j + 1) * 128], rhs=w2v[:, m, hf * 512:(hf + 1) * 512], start=(m == 0), stop=(m == 3))),
                                 reads=[Bhs, Bw], writes=[PB[ob + hf]])
                    y_, By_ = ysb[yi % 2], Bysb[yi % 2]
                    yi += 1
                    S.op("act", ("copy", dict(out=y_[:, 0:512], in_=bank(ob))), reads=[PB[ob]], writes=[By_])
                    S.op("dve", ("tensor_copy", dict(out=y_[:, 512:1024], in_=bank(ob + 1))), reads=[PB[ob + 1]], writes=[By_])
                    r0 = e_ * CAP + j * 128
                    S.dma("sp", ("dma_start", dict(out=yall_d[r0:r0 + 128, :], in_=y_)), reads=[By_], writes=[Byall])
                    if j == 1 and e_ + 1 < 32:
                        cast_w(e_ + 1, 2)
            S.barrier()
            A.off = mark
            ya = [A.f32(D) for _ in range(2)]
            yb = [A.f32(D) for _ in range(2)]
            x1 = [A.f32(D) for _ in range(2)]
            Bya, Byb, Bx1 = [Buf(), Buf()], [Buf(), Buf()], [Buf(), Buf()]
            junk2 = A.bf16(D)
            Bj2 = Buf()
            ss2 = [A.f32(1) for _ in range(2)]
            Bss2 = [Buf(), Buf()]
            Bdst = Buf()
            for ti in range(ntiles):
                col = tile_col(ti)
                i2 = ti % 2
                S.dma("pool", ("indirect_dma_start", dict(out=ya[i2], out_offset=None, in_=yall_d, in_offset=bass.IndirectOffsetOnAxis(ap=slot_i3[:, ti, 0:1], axis=0))),
                      reads=[Byall, Bslot], writes=[Bya[i2]])
                S.dma("pool", ("indirect_dma_start", dict(out=yb[i2], out_offset=None, in_=yall_d, in_offset=bass.IndirectOffsetOnAxis(ap=slot_i3[:, ti, 1:2], axis=0))),
                      reads=[Byall, Bslot], writes=[Byb[i2]])
                S.dma("sp", ("dma_start", dict(out=x1[i2], in_=xr_d[ti * 128:(ti + 1) * 128, :])), reads=[Bsrc], writes=[Bx1[i2]])
                S.op("act", ("activation", dict(out=ya[i2], in_=ya[i2], func=AF.Copy, scale=wts3[:, ti, 0:1])), reads=[Bya[i2], Bwts], writes=[Bya[i2]])
                S.op("dve", ("scalar_tensor_tensor", dict(out=yb[i2], in0=yb[i2], scalar=wts3[:, ti, 1:2], in1=ya[i2], op0=ALU.mult, op1=ALU.add)),
                     reads=[Byb[i2], Bya[i2], Bwts], writes=[Byb[i2]])
                S.op("dve", ("tensor_tensor", dict(out=yb[i2], in0=yb[i2], in1=Gbc[col], op=ALU.mult)), reads=[Byb[i2], Bbc], writes=[Byb[i2]])
                S.op("dve", ("tensor_tensor", dict(out=x1[i2], in0=x1[i2], in1=yb[i2], op=ALU.add)), reads=[Bx1[i2], Byb[i2]], writes=[Bx1[i2]])
                if not last:
                    S.dma("sp", ("dma_start", dict(out=xr2_d[ti * 128:(ti + 1) * 128, :], in_=x1[i2])), reads=[Bx1[i2]], writes=[Bdst])
                else:
                    S.op("act", ("activation", dict(out=junk2, in_=x1[i2], func=AF.Square, accum_out=ss2[i2])), reads=[Bx1[i2]], writes=[Bj2, Bss2[i2]])
                    S.op("act", ("activation", dict(out=ss2[i2], in_=ss2[i2], func=AF.Sqrt, scale=1.0 / D, bias=EPS)), reads=[Bss2[i2]], writes=[Bss2[i2]])
                    S.op("dve", ("reciprocal", dict(out=ss2[i2], in_=ss2[i2])), reads=[Bss2[i2]], writes=[Bss2[i2]])
                    S.op("dve", ("scalar_tensor_tensor", dict(out=x1[i2], in0=x1[i2], scalar=ss2[i2], in1=fng, op0=ALU.mult, op1=ALU.mult)),
                         reads=[Bx1[i2], Bss2[i2], Bbc], writes=[Bx1[i2]])
                    S.dma("sp", ("dma_start", dict(out=out_d[ti * 128:(ti + 1) * 128, :], in_=x1[i2])), reads=[Bx1[i2]], writes=[Bdst])
            return Bdst

        if stop_after >= 2:
            Bxr2 = moe(0, 36, Bxr)
            S.barrier()
            A.off = persist_off
            if "xr2" in dbg_d:
                S.dma("sp", ("dma_start", dict(out=dbg_d["xr2"], in_=xr2_d)), reads=[Bxr2])
        def s5_mixer(Bsrc):
            L = 1
            TWO_PI = 6.283185307179586
            MAGIC = 12582912.0
            yacc = [A.f32(4 * S_LAT) for _ in range(NB)]
            yacc3 = [r3(y, 4) for y in yacc]
            Byacc = [Buf(), Buf()]
            uT = [A.bf16(4 * SEQ) for _ in range(NB)]
            uT3 = [r3(u, 4) for u in uT]
            BuT = [Buf(), Buf()]
            sd = A.f32(8)
            Bsd = Buf()
            S.dma("sp", ("dma_start", dict(out=sd[:, 0:4], in_=s5d_d)), writes=[Bsd])
            S.dma("sp", ("dma_start", dict(out=sd[:, 4:8], in_=s5bglu_d)), writes=[Bsd])
            mark = A.off
            if S5_DEBUG_STAGE < 1:
                return Byacc[0]
            w5 = A.bf16(8 * 512)
            w5_3 = r3(w5, 8)
            Bw5 = Buf()
            S.dma("pool", ("dma_start", dict(out=w5_3, in_=s5win_d.rearrange("(k p) n -> p k n", p=128))), writes=[Bw5])
            hT1 = A.bf16(8 * 512)
            h3 = r3(hT1, 8)
            Bh = Buf()
            NT = NormT()
            for b in range(NB):
                blocks = [([32 + 2 * b, 33 + 2 * b], 0, 256)]
                for i in range(4):
                    blocks.append(([16 * b + 4 * i + t for t in range(4)], 256 + 512 * i, 512))
                for (tiles, c0, N) in blocks:
                    for t, ti in enumerate(tiles):
                        NT.run(L, ti, h3, Bh, t * 128, t % 2)
                    for r in range(4 if S5_DEBUG_STAGE >= 1.5 else 0):
                        pb = 2 + r % 2
                        for k in range(8):
                            S.op("pe", ("matmul", dict(out=bank(pb)[:, 0:N], lhsT=w5_3[:, k, r * 128:(r + 1) * 128], rhs=h3[:, k, 0:N], start=(k == 0), stop=(k == 7))),
                                 reads=[Bw5, Bh], writes=[PB[pb]])
                        S.op("act", ("copy", dict(out=uT3[b][:, r, c0:c0 + N], in_=bank(pb)[:, 0:N])), reads=[PB[pb]], writes=[BuT[b]])
                        if c0 >= 256:
                            S.op("dve", ("tensor_scalar", dict(out=yacc3[b][:, r, c0 - 256:c0 - 256 + N], in0=uT3[b][:, r, c0:c0 + N], scalar1=sd[:, r:r + 1], scalar2=None, op0=ALU.mult)),
                                 reads=[BuT[b], Bsd], writes=[Byacc[b]])
            S.barrier()
            A.off = mark
            if S5_DEBUG_STAGE < 2:
                return Byacc[0]
            par = A.f32(96)
            par3 = par.rearrange("p (c t) -> p c t", t=3)
            Bpar = Buf()
            S.dma("sp", ("dma_start", dict(out=par, in_=s5par_d)), writes=[Bpar])
            iot = A.f32(SEQ)
            S.dma("sp", ("dma_start", dict(out=iot, in_=s5iota_d)), writes=[Bpar])
            NCB = 32
            pr_ = A.f32(NCB * 16)
            P3 = r3(pr_, 16)
            dtv, rho, tht, frv, sn, cs, nr, ni, inv, cfr, cfi, ncfr, ncfi, tmpa, tmpb, tmpc = [P3[:, i, :] for i in range(16)]
            are, aim, ldt = par3[:, :, 0], par3[:, :, 1], par3[:, :, 2]
            pv = lambda name, **kw: S.op("dve", (name, kw), reads=[Bpar], writes=[Bpar])
            pa = lambda **kw: S.op("act", ("activation", kw), reads=[Bpar], writes=[Bpar])
            pa(out=dtv, in_=ldt, func=AF.Exp)
            pv("tensor_tensor", out=tmpa, in0=are, in1=dtv, op=ALU.mult)
            pa(out=rho, in_=tmpa, func=AF.Exp)
            pv("tensor_tensor", out=tht, in0=aim, in1=dtv, op=ALU.mult)
            pv("tensor_scalar", out=tht, in0=tht, scalar1=1.0 / TWO_PI, scalar2=None, op0=ALU.mult)
            pv("tensor_scalar", out=tmpa, in0=tht, scalar1=MAGIC, scalar2=None, op0=ALU.add)
            pv("tensor_scalar", out=tmpa, in0=tmpa, scalar1=MAGIC, scalar2=None, op0=ALU.subtract)
            pv("tensor_tensor", out=frv, in0=tht, in1=tmpa, op=ALU.subtract)
            SC = TWO_PI * (1.0 - 1e-6)
            pa(out=sn, in_=frv, func=AF.Sin, scale=SC)
            pa(out=tmpb, in_=frv, func=AF.Sin, scale=SC / 2)
            pv("tensor_tensor", out=tmpb, in0=tmpb, in1=tmpb, op=ALU.mult)
            pv("tensor_scalar", out=cs, in0=tmpb, scalar1=-2.0, scalar2=1.0, op0=ALU.mult, op1=ALU.add)
            pv("tensor_tensor", out=nr, in0=rho, in1=cs, op=ALU.mult)
            pv("tensor_scalar", out=nr, in0=nr, scalar1=-1.0, scalar2=None, op0=ALU.add)
            pv("tensor_tensor", out=ni, in0=rho, in1=sn, op=ALU.mult)
            pv("tensor_tensor", out=tmpa, in0=are, in1=are, op=ALU.mult)
            pv("tensor_tensor", out=tmpb, in0=aim, in1=aim, op=ALU.mult)
            pv("tensor_tensor", out=inv, in0=tmpa, in1=tmpb, op=ALU.add)
            pv("reciprocal", out=inv, in_=inv)
            pv("tensor_tensor", out=tmpa, in0=nr, in1=are, op=ALU.mult)
            pv("tensor_tensor", out=tmpb, in0=ni, in1=aim, op=ALU.mult)
            pv("tensor_tensor", out=tmpa, in0=tmpa, in1=tmpb, op=ALU.add)
            pv("tensor_tensor", out=cfr, in0=tmpa, in1=inv, op=ALU.mult)
            pv("tensor_tensor", out=tmpa, in0=ni, in1=are, op=ALU.mult)
            pv("tensor_tensor", out=tmpb, in0=nr, in1=aim, op=ALU.mult)
            pv("tensor_tensor", out=tmpa, in0=tmpa, in1=tmpb, op=ALU.subtract)
            pv("tensor_tensor", out=cfi, in0=tmpa, in1=inv, op=ALU.mult)
            pv("tensor_scalar", out=ncfr, in0=cfr, scalar1=-1.0, scalar2=None, op0=ALU.mult)
            pv("tensor_scalar", out=ncfi, in0=cfi, scalar1=-1.0, scalar2=None, op0=ALU.mult)

            if S5_DEBUG_STAGE < 3:
                return Bpar
            cosT = A.f32(SEQ)
            sinT = A.f32(SEQ)
            tA = A.f32(SEQ)
            tB = A.f32(SEQ)
            Btab, BtA, BtB = Buf(), Buf(), Buf()
            dr = A.f32(SEQ)
            di = A.f32(SEQ)
            qr = A.bf16(SEQ)
            qi = A.bf16(SEQ)
            cosb = A.bf16(SEQ)
            sinb = A.bf16(SEQ)
            Btabb = Buf()
            m1b = [A.bf16(512) for _ in range(2)]
            m2b = [A.bf16(512) for _ in range(2)]
            Bdr, Bdi, Bqr, Bqi = Buf(), Buf(), Buf(), Buf()
            m1 = [A.f32(512) for _ in range(2)]
            m2 = [A.f32(512) for _ in range(2)]
            Bm1, Bm2 = [Buf(), Buf()], [Buf(), Buf()]
            bt = [(A.bf16(128), A.bf16(128)) for _ in range(2)]
            Bbt = [Buf(), Buf()]
            cst_ = [(A.f32(128), A.f32(128)) for _ in range(2)]
            Bcst = [Buf(), Buf()]
            cw = [(A.bf16(128), A.bf16(128)) for _ in range(2)]
            Bcw = [Buf(), Buf()]
            ctmp = A.f32(128)
            Bctmp = Buf()
            ncwr = [A.bf16(128), A.bf16(128)]
            mi = 0
            for d_ in range(2):
                for pr in range(16):
                    ci_ = d_ * 16 + pr
                    r = pr // 4
                    k2 = ci_ % 2
                    btr, bti = bt[k2]
                    S.dma("pool", ("dma_start", dict(out=btr, in_=s5bT_d[d_, 0, pr])), writes=[Bbt[k2]])
                    S.dma("pool", ("dma_start", dict(out=bti, in_=s5bT_d[d_, 1, pr])), writes=[Bbt[k2]])
                    c_r, c_i = cst_[k2]
                    S.dma("sp", ("dma_start", dict(out=c_r, in_=s5c_d[d_, 0, pr])), writes=[Bcst[k2]])
                    S.dma("sp", ("dma_start", dict(out=c_i, in_=s5c_d[d_, 1, pr])), writes=[Bcst[k2]])
                    cwr, cwi = cw[k2]
                    col1 = lambda v, ci_=ci_: v[:, ci_:ci_ + 1]
                    S.op("dve", ("tensor_scalar", dict(out=ctmp, in0=c_r, scalar1=col1(cfr), scalar2=None, op0=ALU.mult)), reads=[Bcst[k2], Bpar], writes=[Bctmp])
                    S.op("dve", ("scalar_tensor_tensor", dict(out=cwr, in0=c_i, scalar=col1(ncfi), in1=ctmp, op0=ALU.mult, op1=ALU.add)), reads=[Bcst[k2], Bpar, Bctmp], writes=[Bcw[k2]])
                    S.op("dve", ("tensor_scalar", dict(out=ncwr[k2], in0=cwr, scalar1=-1.0, scalar2=None, op0=ALU.mult)), reads=[Bcw[k2]], writes=[Bcw[k2]])
                    S.op("dve", ("tensor_scalar", dict(out=ctmp, in0=c_r, scalar1=col1(ncfi), scalar2=None, op0=ALU.mult)), reads=[Bcst[k2], Bpar, Bcw[k2]], writes=[Bctmp])
                    S.op("dve", ("scalar_tensor_tensor", dict(out=cwi, in0=c_i, scalar=col1(ncfr), in1=ctmp, op0=ALU.mult, op1=ALU.add)), reads=[Bcst[k2], Bpar, Bctmp], writes=[Bcw[k2]])
                    S.op("act", ("activation", dict(out=tA, in_=iot, func=AF.Copy, scale=col1(tht))), reads=[Bpar], writes=[BtA])
                    S.op("act", ("activation", dict(out=tB, in_=tA, func=AF.Identity, bias=MAGIC, scale=1.0)), reads=[BtA], writes=[BtB])
                    S.op("act", ("activation", dict(out=tB, in_=tB, func=AF.Identity, bias=-MAGIC, scale=1.0)), reads=[BtB], writes=[BtB])
                    S.op("dve", ("tensor_tensor", dict(out=tA, in0=tA, in1=tB, op=ALU.subtract)), reads=[BtA, BtB], writes=[BtA])
                    S.op("act", ("activation", dict(out=sinT, in_=tA, func=AF.Sin, scale=SC)), reads=[BtA], writes=[Btab])
                    S.op("act", ("activation", dict(out=tB, in_=tA, func=AF.Sin, scale=SC / 2)), reads=[BtA], writes=[BtB])
                    S.op("act", ("activation", dict(out=tB, in_=tB, func=AF.Square, scale=1.4142135623730951)), reads=[BtB], writes=[BtB])
                    S.op("act", ("activation", dict(out=cosT, in_=tB, func=AF.Identity, scale=-1.0, bias=1.0)), reads=[BtB], writes=[Btab])
                    S.op("act", ("copy", dict(out=cosb, in_=cosT)), reads=[Btab], writes=[Btabb])
                    S.op("act", ("copy", dict(out=sinb, in_=sinT)), reads=[Btab], writes=[Btabb])
                    rho_c = col1(rho)
                    for b in range(NB):
                        blocks = [(0, 256)] + [(256 + 512 * i, 512) for i in range(4)]
                        for bi, (s0, N) in enumerate(blocks):
                            if d_ == 0:
                                ucols = uT3[b][:, r, s0:s0 + N]
                            else:
                                if bi == 0:
                                    ucols = uT3[b][:, r, 255::-1]
                                else:
                                    hi_c = SEQ - 1 - (bi - 1) * 512
                                    ucols = uT3[b][:, r, hi_c:hi_c - 512:-1]
                            pbr, pbi = (bi % 2) * 2, (bi % 2) * 2 + 1
                            S.op("pe", ("matmul", dict(out=bank(pbr)[:, 0:N], lhsT=btr, rhs=ucols, start=True, stop=True)), reads=[Bbt[k2], BuT[b]], writes=[PB[pbr]])
                            S.op("pe", ("matmul", dict(out=bank(pbi)[:, 0:N], lhsT=bti, rhs=ucols, start=True, stop=True)), reads=[Bbt[k2], BuT[b]], writes=[PB[pbi]])
                            a1, a2 = m1[mi % 2], m2[mi % 2]
                            Ba1, Ba2 = Bm1[mi % 2], Bm2[mi % 2]
                            mi += 1
                            cS, sS = cosT[:, s0:s0 + N], sinT[:, s0:s0 + N]
                            S.op("dve", ("tensor_tensor", dict(out=a1[:, 0:N], in0=bank(pbr)[:, 0:N], in1=cS, op=ALU.mult)), reads=[PB[pbr], Btab], writes=[Ba1])
                            S.op("dve", ("tensor_tensor", dict(out=a2[:, 0:N], in0=bank(pbi)[:, 0:N], in1=sS, op=ALU.mult)), reads=[PB[pbi], Btab], writes=[Ba2])
                            S.op("dve", ("tensor_tensor", dict(out=dr[:, s0:s0 + N], in0=a1[:, 0:N], in1=a2[:, 0:N], op=ALU.add)), reads=[Ba1, Ba2], writes=[Bdr])
                            a1, a2 = m1[mi % 2], m2[mi % 2]
                            Ba1, Ba2 = Bm1[mi % 2], Bm2[mi % 2]
                            mi += 1
                            S.op("dve", ("tensor_tensor", dict(out=a1[:, 0:N], in0=bank(pbi)[:, 0:N], in1=cS, op=ALU.mult)), reads=[PB[pbi], Btab], writes=[Ba1])
                            S.op("dve", ("tensor_tensor", dict(out=a2[:, 0:N], in0=bank(pbr)[:, 0:N], in1=sS, op=ALU.mult)), reads=[PB[pbr], Btab], writes=[Ba2])
                            S.op("dve", ("tensor_tensor", dict(out=di[:, s0:s0 + N], in0=a1[:, 0:N], in1=a2[:, 0:N], op=ALU.subtract)), reads=[Ba1, Ba2], writes=[Bdi])
                        rb = rho_c.broadcast_to([128, SEQ])
                        S.op("dve", ("tensor_tensor_scan", dict(out=qr, data0=rb, data1=dr, initial=0.0, op0=ALU.mult, op1=ALU.add)), reads=[Bdr, Bpar], writes=[Bqr])
                        S.op("dve", ("tensor_tensor_scan", dict(out=qi, data0=rb, data1=di, initial=0.0, op0=ALU.mult, op1=ALU.add)), reads=[Bdi, Bpar], writes=[Bqi])
                        for bi in range(1, 5):
                            s0, N = blocks[bi]
                            cS, sS = cosb[:, s0:s0 + N], sinb[:, s0:s0 + N]
                            a1, a2 = m1b[mi % 2], m2b[mi % 2]
                            Ba1, Ba2 = Bm1[mi % 2], Bm2[mi % 2]
                            mi += 1
                            S.op("dve", ("tensor_tensor", dict(out=a1, in0=qr[:, s0:s0 + N], in1=cS, op=ALU.mult)), reads=[Bqr, Btabb], writes=[Ba1])
                            S.op("dve", ("tensor_tensor", dict(out=a2, in0=qi[:, s0:s0 + N], in1=sS, op=ALU.mult)), reads=[Bqi, Btabb], writes=[Ba2])
                            pby = 4 + bi % 2
                            S.op("pe", ("matmul", dict(out=bank(pby), lhsT=cwr, rhs=a1, start=True, stop=False)), reads=[Bcw[k2], Ba1], writes=[PB[pby]])
                            S.op("pe", ("matmul", dict(out=bank(pby), lhsT=ncwr[k2], rhs=a2, start=False, stop=False)), reads=[Bcw[k2], Ba2], writes=[PB[pby]])
                            a1, a2 = m1b[mi % 2], m2b[mi % 2]
                            Ba1, Ba2 = Bm1[mi % 2], Bm2[mi % 2]
                            mi += 1
                            S.op("dve", ("tensor_tensor", dict(out=a1, in0=qr[:, s0:s0 + N], in1=sS, op=ALU.mult)), reads=[Bqr, Btabb], writes=[Ba1])
                            S.op("dve", ("tensor_tensor", dict(out=a2, in0=qi[:, s0:s0 + N], in1=cS, op=ALU.mult)), reads=[Bqi, Btabb], writes=[Ba2])
                            S.op("pe", ("matmul", dict(out=bank(pby), lhsT=cwi, rhs=a1, start=False, stop=False)), reads=[Bcw[k2], Ba1], writes=[PB[pby]])
                            S.op("pe", ("matmul", dict(out=bank(pby), lhsT=cwi, rhs=a2, start=False, stop=True)), reads=[Bcw[k2], Ba2], writes=[PB[pby]])
                            if d_ == 0:
                                j0 = s0 - 256
                                ycols = yacc3[b][:, r, j0:j0 + 512]
                            else:
                                hj = S_LAT - 1 - (bi - 1) * 512
                                stop = hj - 512
                                ycols = yacc3[b][:, r, hj::-1] if stop < 0 else yacc3[b][:, r, hj:stop:-1]
                            S.op("dve", ("tensor_tensor", dict(out=ycols, in0=bank(pby), in1=ycols, op=ALU.add)), reads=[PB[pby], Byacc[b]], writes=[Byacc[b]])
            S.barrier()
            A.off = mark
            if "yacc" in dbg_d:
                for b in range(NB):
                    S.dma("sp", ("dma_start", dict(out=dbg_d["yacc"][b * 128:(b + 1) * 128, :], in_=yacc[b])), reads=[Byacc[b]])
            wg = A.bf16(4 * 512)
            wg3 = r3(wg, 4)
            wo = A.bf16(4 * 1024)
            wo3 = r3(wo, 4)
            BwC = Buf()
            S.dma("pool", ("dma_start", dict(out=wg3, in_=s5glu_d.rearrange("(k p) n -> p k n", p=128))), writes=[BwC])
            S.dma("pool", ("dma_start", dict(out=wo3, in_=s5wout_d.rearrange("(k p) n -> p k n", p=128))), writes=[BwC])
            gate_bc = [A.f32(D) for _ in range(2)]
            for c in range(2):
                S.dma("sp", ("dma_start", dict(out=gate_bc[c], in_=modrows_d[L, 2, c:c + 1, :].broadcast_to([128, D]))), reads=[Bmodrows], writes=[BwC])
            gT = A.bf16(4 * 512)
            gT3 = r3(gT, 4)
            vT = A.bf16(4 * 512)
            vT3 = r3(vT, 4)
            BgT, BvT = Buf(), Buf()
            sg = [A.f32(512) for _ in range(2)]
            Bsg = [Buf(), Buf()]
            yt = [A.f32(D) for _ in range(2)]
            xt2 = [A.f32(D) for _ in range(2)]
            Byt, Bxt2 = [Buf(), Buf()], [Buf(), Buf()]
            Bxr = Buf()
            oi = 0
            for b in range(NB):
                for i in range(4):
                    j0 = i * 512
                    S.op("act", ("activation", dict(out=gT3, in_=yacc3[b][:, :, j0:j0 + 512], func=AF.Gelu_apprx_tanh)), reads=[Byacc[b]], writes=[BgT])
                    for m in range(4):
                        pb = 2 + m % 2
                        for k in range(4):
                            S.op("pe", ("matmul", dict(out=bank(pb), lhsT=wg3[:, k, m * 128:(m + 1) * 128], rhs=gT3[:, k, :], start=(k == 0), stop=(k == 3))),
                                 reads=[BwC, BgT], writes=[PB[pb]])
                        S.op("act", ("activation", dict(out=sg[m % 2], in_=bank(pb), func=AF.Sigmoid, bias=sd[:, 4 + m:5 + m], scale=1.0)), reads=[PB[pb], Bsd], writes=[Bsg[m % 2]])
                        S.op("dve", ("tensor_tensor", dict(out=vT3[:, m, :], in0=gT3[:, m, :], in1=sg[m % 2], op=ALU.mult)), reads=[BgT, Bsg[m % 2]], writes=[BvT])
                    for t in range(4):
                        ti = 16 * b + 4 * i + t
                        for hf in range(2):
                            for k in range(4):
                                S.op("pe", ("matmul", dict(out=bank(6 + hf), lhsT=vT3[:, k, t * 128:(t + 1) * 128], rhs=wo3[:, k, hf * 512:(hf + 1) * 512], start=(k == 0), stop=(k == 3))),
                                     reads=[BvT, BwC], writes=[PB[6 + hf]])
                        o2 = oi % 2
                        oi += 1
                        S.dma("sp", ("dma_start", dict(out=xt2[o2], in_=xr2_d[ti * 128:(ti + 1) * 128, :])), reads=[Bsrc], writes=[Bxt2[o2]])
                        S.op("dve", ("tensor_tensor", dict(out=yt[o2], in0=psum_t[:, 6 * 512:8 * 512], in1=gate_bc[b], op=ALU.mult)), reads=[PB[6], PB[7], BwC], writes=[Byt[o2]])
                        S.op("dve", ("tensor_tensor", dict(out=yt[o2], in0=yt[o2], in1=xt2[o2], op=ALU.add)), reads=[Byt[o2], Bxt2[o2]], writes=[Byt[o2]])
                        S.dma("sp", ("dma_start", dict(out=xr_d[ti * 128:(ti + 1) * 128, :], in_=yt[o2])), reads=[Byt[o2]], writes=[Bxr])
            return Bxr

        if stop_after >= 3:
            Bxr_b = s5_mixer(Bxr2)
            S.barrier()
            A.off = persist_off
            if "xr3" in dbg_d:
                S.dma("sp", ("dma_start", dict(out=dbg_d["xr3"], in_=xr_d[0:T_LAT, :])), reads=[Bxr_b])
        if stop_after >= 4:
            Bout = moe(1, 32, Bxr_b)

        S.barrier()
        with nc.Block() as block:
            S.emit(block)
    return nc


def _rope_tables():
    inv = np.power(10000.0, -np.arange(0, 32, 2, dtype=np.float32) / 32).astype(np.float32)
    t = np.arange(S_LAT)
    row = (t // 64).astype(np.float32)
    colp = (t % 64).astype(np.float32)
    ang_r = row[:, None] * inv[None, :]
    ang_c = colp[:, None] * inv[None, :]
    cos64 = np.ones((64, SEQ), np.float32)
    sin64 = np.zeros((64, SEQ), np.float32)
    for d in range(64):
        ang = ang_r if d < 32 else ang_c
        i = d % 16
        sgn = -1.0 if (d % 32) < 16 else 1.0
        cos64[d, C_CTX:] = np.cos(ang[:, i])
        sin64[d, C_CTX:] = sgn * np.sin(ang[:, i])
    return np.concatenate([np.concatenate([cos64, cos64], 0), np.concatenate([sin64, sin64], 0)], 1).astype(np.float32)


_PERM64 = np.array([(d // 32) * 32 + ((d % 32) + 16) % 32 for d in range(64)])


def _consts():
    c = np.zeros((128, NCONST), np.float32)
    c[:, 0:128] = np.eye(128)
    c[:, 128:256] = np.triu(np.ones((128, 128)), 1)
    c[:, 256:384] = 1.0
    c[0:64, 384:448] = 1.0 / 64
    c[64:128, 448:512] = 1.0 / 64
    c[:, 512:544] = (np.arange(32) * CAP)[None, :]
    return c


def make_in_maps(inp, cores):
    f = lambda a: np.ascontiguousarray(np.asarray(a, dtype=np.float32))
    shared = {
        "consts": _consts(), "rope": _rope_tables(),
        "w_mod": f(inp["w_mod"]), "b_mod": f(inp["b_mod"]).reshape(2, 1, 6 * D),
        "norm_mix_g": f(inp["norm_mix_g"]).reshape(2, 1, D), "norm_ffn_g": f(inp["norm_ffn_g"]).reshape(2, 1, D),
        "mix_w_in": f(inp["mix_w_in"][0]), "mix_w_out": f(inp["mix_w_out"][0]),
        "gmlp_norm_g": f(inp["gmlp_norm_g"]).reshape(1, 512), "gmlp_w_spatial": f(inp["gmlp_w_spatial"][0]),
        "gmlp_b_spatial": f(inp["gmlp_b_spatial"][0]).reshape(1, 1024),
        "moe_w_group": f(inp["moe_w_group"]), "moe_b_group": f(inp["moe_b_group"]).reshape(2, 1, 4),
        "moe_w_router": f(inp["moe_w_router"]), "moe_b_router": f(inp["moe_b_router"]).reshape(2, 1, 32),
        "moe_w1": f(inp["moe_w1"]), "moe_w3": f(inp["moe_w3"]), "moe_w2": f(inp["moe_w2"]),
        "final_norm_g": f(inp["final_norm_g"]).reshape(1, D),
        "s5_w_in": f(inp["s5_w_in"][0]), "s5_w_glu": f(inp["s5_w_glu"][0]), "s5_w_out": f(inp["s5_w_out"][0]),
    }
    qg = f(inp["q_norm_g"][0]); kg = f(inp["k_norm_g"][0])
    idx = np.arange(128) % 64
    shared["qkg"] = np.stack([qg[idx], qg[_PERM64[idx]], kg[idx], kg[_PERM64[idx]]], 1).astype(np.float32)
    a_re = f(inp["s5_a_re"][0]); a_im = f(inp["s5_a_im"][0]); ldt = f(inp["s5_log_dt"][0])
    par = np.zeros((128, 2, 16, 3), np.float32)
    for d in range(2):
        for pr in range(16):
            for gl in range(2):
                g = 2 * pr + gl
                par[gl * 64:(gl + 1) * 64, d, pr, 0] = a_re[d, g]
                par[gl * 64:(gl + 1) * 64, d, pr, 1] = a_im[d, g]
                par[gl * 64:(gl + 1) * 64, d, pr, 2] = ldt[d, g]
    shared["s5_par"] = par.reshape(128, 96)
    b_re = f(inp["s5_b_re"][0]); b_im = f(inp["s5_b_im"][0]); c_re = f(inp["s5_c_re"][0]); c_im = f(inp["s5_c_im"][0])
    bT = np.zeros((2, 2, 16, 128, 128), np.float32)
    cc = np.zeros((2, 2, 16, 128, 128), np.float32)
    for d in range(2):
        for pr in range(16):
            for gl in range(2):
                g = 2 * pr + gl
                gic = g % 8
                for ri, (bsrc, csrc) in enumerate(((b_re, c_re), (b_im, c_im))):
                    bT[d, ri, pr, gic * 16:(gic + 1) * 16, gl * 64:(gl + 1) * 64] = bsrc[d, g].T
                    cc[d, ri, pr, gl * 64:(gl + 1) * 64, gic * 16:(gic + 1) * 16] = csrc[d, g].T
    shared["s5_bT"] = bT
    shared["s5_iota"] = np.tile(np.arange(SEQ, dtype=np.float32)[None, :], (128, 1))
    shared["s5_c"] = cc
    shared["s5_d"] = f(inp["s5_d"][0]).reshape(4, 128).T.copy()
    shared["s5_b_glu"] = f(inp["s5_b_glu"][0]).reshape(4, 128).T.copy()
    maps = []
    x = np.asarray(inp["x"]); ctx = np.asarray(inp["ctx"]); c = np.asarray(inp["c"]); cc_ = np.asarray(inp["c_ctx"])
    for core in cores:
        m = dict(shared)
        m["x"] = f(x[2 * core:2 * core + 2]).reshape(T_LAT, D)
        m["ctx"] = f(ctx[2 * core:2 * core + 2]).reshape(T_CTX, D)
        cvec = np.stack([c[2 * core], c[2 * core + 1], cc_], 0).astype(np.float32)
        m["cT"] = np.ascontiguousarray(cvec.reshape(3, 8, 128).transpose(2, 1, 0).reshape(128, 24))
        maps.append(m)
    return maps


_NC_CACHE = {}


def kernel(**inputs):
    if "nc" not in _NC_CACHE:
        _NC_CACHE["nc"] = build_program()
    nc = _NC_CACHE["nc"]
    cores = list(range(8))
    maps = make_in_maps(inputs, cores)
    res = run_bass_kernel_spmd(nc, maps, core_ids=cores)
    outs = [np.asarray(r["out"]).reshape(NB, S_LAT, D) for r in res.results]
    return np.concatenate(outs, 0).astype(np.float32)
```

```python
import numpy as np
from contextlib import ExitStack
import concourse.bass as bass
import concourse.mybir as mybir
from concourse.alu_op_type import AluOpType as ALU
from concourse.bass_utils import run_bass_kernel_spmd

F32 = mybir.dt.float32
BF16 = mybir.dt.bfloat16
I32 = mybir.dt.int32
AF = mybir.ActivationFunctionType
AX = mybir.AxisListType

D = 1024
S_LAT = 2048
C_CTX = 256
NB = 2
T_LAT = NB * S_LAT
T_CTX = NB * C_CTX
SEQ = C_CTX + S_LAT
EPS = 1e-6
CAP = 640
NSLOT = 32 * CAP
TRASH = NSLOT
NCONST = 128 * 4 + 32
S5_DEBUG_STAGE = 9


class Buf:
    __slots__ = ("w", "r")

    def __init__(self):
        self.w = None
        self.r = []


class Sched:
    COMPUTE = ("pe", "act", "dve", "pool")
    NDMA = 8

    def __init__(self, nc, es):
        self.nc = nc
        self.streams = {e: [] for e in ("pe", "act", "dve", "pool", "sp")}
        self.sem = {}
        self.cnt = {}
        for e in self.COMPUTE:
            self.sem[e] = es.enter_context(nc.semaphore("s_" + e))
            self.cnt[e] = 0
        self.drr = {}
        for q in ("sp", "act", "pool"):
            for k in range(self.NDMA):
                key = "d_%s%d" % (q, k)
                self.sem[key] = es.enter_context(nc.semaphore(key))
                self.cnt[key] = 0
            self.drr[q] = 0
        self.waited = {e: {} for e in self.streams}
        self.nops = 0

    def _deps(self, reads, writes):
        deps = []
        for b in reads:
            if b.w is not None:
                deps.append(b.w)
        for b in writes:
            if b.w is not None:
                deps.append(b.w)
            deps.extend(b.r)
        return deps

    def _emit_waits(self, eng, deps, skip_self=None):
        best = {}
        for (k, v) in deps:
            if k == skip_self:
                continue
            if v > best.get(k, 0):
                best[k] = v
        w = self.waited[eng]
        for k, v in best.items():
            if w.get(k, 0) < v:
                w[k] = v
                self.streams[eng].append(("wait", k, v))

    def _mark(self, tok, reads, writes):
        for b in writes:
            b.w = tok
            b.r = []
        for b in reads:
            if b.w is tok:
                continue
            b.r.append(tok)
            if len(b.r) > 16:
                best = {}
                for (k, v) in b.r:
                    if v > best.get(k, 0):
                        best[k] = v
                b.r = list(best.items())

    def op(self, eng, fn, reads=(), writes=()):
        deps = self._deps(reads, writes)
        self._emit_waits(eng, deps, skip_self=("pe" if eng == "pe" else None))
        self.cnt[eng] += 1
        tok = (eng, self.cnt[eng])
        self.streams[eng].append(("op", fn, eng, 1))
        self._mark(tok, reads, writes)
        self.nops += 1
        return tok

    def dma(self, q, fn, reads=(), writes=()):
        k = self.drr[q]
        self.drr[q] = (k + 1) % self.NDMA
        key = "d_%s%d" % (q, k)
        deps = self._deps(reads, writes)
        if self.cnt[key] > 0:
            deps.append((key, self.cnt[key]))
        self._emit_waits(q, deps)
        self.cnt[key] += 16
        tok = (key, self.cnt[key])
        self.streams[q].append(("op", fn, key, 16))
        self._mark(tok, reads, writes)
        self.nops += 1
        return tok

    def barrier(self):
        toks = [(k, v) for k, v in self.cnt.items() if v > 0]
        for e in self.streams:
            self._emit_waits(e, toks)

    def emit(self, block):
        sem = self.sem
        streams = self.streams

        def run(e, lst):
            for it in lst:
                if it[0] == "wait":
                    e.wait_ge(sem[it[1]], it[2])
                else:
                    getattr(e, it[1][0])(**it[1][1]).then_inc(sem[it[2]], it[3])

        @block.sync
        def _(e):
            run(e, streams["sp"])

        @block.scalar
        def _(e):
            run(e, streams["act"])

        @block.vector
        def _(e):
            run(e, streams["dve"])

        @block.gpsimd
        def _(e):
            run(e, streams["pool"])

        @block.tensor
        def _(e):
            run(e, streams["pe"])


class Arena:
    def __init__(self, t, size):
        self.t = t
        self.size = size
        self.off = 0

    def f32(self, n):
        a = self.t[:, self.off:self.off + n]
        self.off += n
        assert self.off <= self.size, ("arena overflow", self.off, self.size)
        return a

    def bf16(self, n):
        return self.f32((n + 1) // 2).bitcast(BF16)[:, 0:n]

    def i32(self, n):
        return self.f32(n).bitcast(I32)


def r3(ap, a):
    return ap.rearrange("p (a b) -> p a b", a=a)


def build_program(stop_after=99, dbg=()):
    nc = bass.Bass("TRN2", target_bir_lowering=False)
    dt = nc.dram_tensor

    def din(name, shape, dtype=F32):
        return dt(name, list(shape), dtype, kind="ExternalInput").ap()

    x_d = din("x", [T_LAT, D])
    ctx_d = din("ctx", [T_CTX, D])
    cT_d = din("cT", [128, 24])
    consts_d = din("consts", [128, NCONST])
    rope_d = din("rope", [128, 2 * SEQ])
    qkg_d = din("qkg", [128, 4])
    w_mod_d = din("w_mod", [2, D, 6 * D])
    b_mod_d = din("b_mod", [2, 1, 6 * D])
    gmix_d = din("norm_mix_g", [2, 1, D])
    gffn_d = din("norm_ffn_g", [2, 1, D])
    w_in_d = din("mix_w_in", [D, 1792])
    w_out_d = din("mix_w_out", [D, D])
    lng_d = din("gmlp_norm_g", [1, 512])
    wsp_d = din("gmlp_w_spatial", [8, 128, 128])
    bsp_d = din("gmlp_b_spatial", [1, 1024])
    wgrp_d = din("moe_w_group", [2, D, 4])
    bgrp_d = din("moe_b_group", [2, 1, 4])
    wrt_d = din("moe_w_router", [2, D, 32])
    brt_d = din("moe_b_router", [2, 1, 32])
    w1_d = din("moe_w1", [2, 32, D, 512])
    w3_d = din("moe_w3", [2, 32, D, 512])
    w2_d = din("moe_w2", [2, 32, 512, D])
    fng_d = din("final_norm_g", [1, D])
    s5win_d = din("s5_w_in", [D, 512])
    s5par_d = din("s5_par", [128, 2 * 16 * 3])
    s5iota_d = din("s5_iota", [128, SEQ])
    s5bT_d = din("s5_bT", [2, 2, 16, 128, 128])
    s5c_d = din("s5_c", [2, 2, 16, 128, 128])
    s5d_d = din("s5_d", [128, 4])
    s5glu_d = din("s5_w_glu", [512, 512])
    s5bglu_d = din("s5_b_glu", [128, 4])
    s5wout_d = din("s5_w_out", [512, D])

    out_d = dt("out", [T_LAT, D], F32, kind="ExternalOutput").ap()
    modrows_d = dt("modrows", [2, 6, 3, D], F32, kind="Internal").ap()
    xr_d = dt("xr", [T_LAT + T_CTX, D], F32, kind="Internal").ap()
    xr2_d = dt("xr2", [T_LAT + T_CTX, D], F32, kind="Internal").ap()
    xall_d = dt("xall", [NSLOT + 128, D], BF16, kind="Internal").ap()
    yall_d = dt("yall", [NSLOT + 128, D], F32, kind="Internal").ap()
    dbg_d = {}
    for name, shape in dbg:
        dbg_d[name] = dt("dbg_" + name, list(shape), F32, kind="ExternalOutput").ap()

    with ExitStack() as es:
        S = Sched(nc, es)
        ARENA_WORDS = 53100
        arena_t = es.enter_context(nc.sbuf_tensor("arena", [128, ARENA_WORDS], F32))
        psum_t = es.enter_context(nc.psum_tensor("psum", [128, 4096], F32))
        A = Arena(arena_t, ARENA_WORDS)

        def bank(i, n=512, off=0):
            return psum_t[:, i * 512 + off:i * 512 + off + n]

        def bank_bf(i):
            return psum_t[:, i * 512:(i + 1) * 512].bitcast(BF16)

        PB = [Buf() for _ in range(8)]

        cst = A.f32(NCONST)
        Bc = Buf()
        S.dma("sp", ("dma_start", dict(out=cst, in_=consts_d)), writes=[Bc])
        ident_f = cst[:, 0:128]
        utri_f = cst[:, 128:256]
        ones_f = cst[:, 256:384]
        blk_f = cst[:, 384:512]
        iotaC = cst[:, 512:544]
        cbf = A.bf16(512)
        S.op("dve", ("tensor_copy", dict(out=cbf, in_=cst[:, 0:512])), reads=[Bc], writes=[Bc])
        ident_b = cbf[:, 0:128]
        utri_b = cbf[:, 128:256]
        ones_b = cbf[:, 256:384]
        blk_b = cbf[:, 384:512]
        modT = A.f32(2 * 2 * 8 * 3)
        BmodT = Buf()
        ztile_b = A.bf16(512)
        Bzt = Buf()
        S.op("dve", ("memset", dict(ap=ztile_b, constant=0.0)), writes=[Bzt])
        persist_off = A.off

        def fill_xall():
            xv = xall_d.rearrange("r (c d) -> (r c) d", d=512)
            NT_ = (NSLOT + 128) * 2 // 128
            step = 46
            for j0 in range(0, NT_, step):
                nj = min(step, NT_ - j0)
                S.dma("act", ("dma_start", dict(
                    out=xv[j0 * 128:(j0 + nj) * 128, :].rearrange("(j p) d -> p j d", p=128),
                    in_=ztile_b.unsqueeze(1).broadcast_to([128, nj, 512]))), reads=[Bzt])

        cT = A.f32(24)
        sc = A.f32(24)
        Bsc = Buf()
        S.dma("sp", ("dma_start", dict(out=cT, in_=cT_d)), writes=[Bsc])
        S.op("act", ("activation", dict(out=sc, in_=cT, func=AF.Silu)), reads=[Bsc], writes=[Bsc])
        sc3 = r3(sc, 8)
        wblk = [A.f32(8 * 512) for _ in range(2)]
        Bwblk = [Buf(), Buf()]
        mrow = A.f32(6 * D)
        gb = A.f32(2 * D)
        bb = A.f32(6 * D)
        Bmrow, Bgb, Bbb = Buf(), Buf(), Buf()
        Bmodrows = Buf()
        for l in range(2):
            S.dma("sp", ("dma_start", dict(out=bb[0:3, :], in_=b_mod_d[l].broadcast_to([3, 6 * D]))), writes=[Bbb])
            S.dma("sp", ("dma_start", dict(out=gb[0:3, 0:D], in_=gmix_d[l].broadcast_to([3, D]))), writes=[Bgb])
            S.dma("sp", ("dma_start", dict(out=gb[0:3, D:2 * D], in_=gffn_d[l].broadcast_to([3, D]))), writes=[Bgb])
            for nb in range(12):
                wb = wblk[nb % 2]
                Bw = Bwblk[nb % 2]
                S.dma("sp", ("dma_start", dict(
                    out=r3(wb, 8), in_=w_mod_d[l][:, nb * 512:(nb + 1) * 512].rearrange("(k p) n -> p k n", p=128))), writes=[Bw])
                pb = nb % 2
                for k in range(8):
                    S.op("pe", ("matmul", dict(out=bank(pb)[0:3, :], lhsT=sc3[:, k, :], rhs=r3(wb, 8)[:, k, :],
                                                                       start=(k == 0), stop=(k == 7))), reads=[Bsc, Bw], writes=[PB[pb]])
                S.op("dve", ("tensor_tensor", dict(out=mrow[0:3, nb * 512:(nb + 1) * 512], in0=bank(pb)[0:3, :],
                                                                      in1=bb[0:3, nb * 512:(nb + 1) * 512], op=ALU.add)),
                     reads=[PB[pb], Bbb], writes=[Bmrow])
            for (kind, goff) in ((1, 0), (4, D)):
                S.op("dve", ("scalar_tensor_tensor", dict(
                    out=mrow[0:3, kind * D:(kind + 1) * D], in0=mrow[0:3, kind * D:(kind + 1) * D], scalar=1.0,
                    in1=gb[0:3, goff:goff + D], op0=ALU.add, op1=ALU.mult)), reads=[Bmrow, Bgb], writes=[Bmrow])
            S.dma("sp", ("dma_start", dict(out=modrows_d[l].rearrange("k c d -> c k d"), in_=r3(mrow[0:3, :], 6))),
                  reads=[Bmrow], writes=[Bmodrows])
        modT5 = modT.rearrange("p (l m k c) -> p l m k c", l=2, m=2, k=8)
        for l in range(2):
            for m in range(2):
                for c in range(3):
                    S.dma("sp", ("dma_start", dict(
                        out=modT5[:, l, m, :, c], in_=modrows_d[l, m, c].rearrange("(k p) -> p k", p=128),
                        allow_slow_non_contiguous=True)), reads=[Bmodrows], writes=[BmodT])
        S.barrier()
        A.off = persist_off
        if "modrows" in dbg_d:
            S.dma("sp", ("dma_start", dict(out=dbg_d["modrows"], in_=modrows_d.rearrange("l k c d -> (l k c) d"))), reads=[Bmodrows])

        def tile_src(layer, ti):
            if layer == 0:
                if ti < 32:
                    return x_d[ti * 128:(ti + 1) * 128, :]
                return ctx_d[(ti - 32) * 128:(ti - 31) * 128, :]
            return xr2_d[ti * 128:(ti + 1) * 128, :]

        def tile_col(ti):
            if ti < 32:
                return ti // 16
            return 2

        class NormT:
            def __init__(self):
                self.xt = [A.f32(D) for _ in range(2)]
                self.Bxt = [Buf(), Buf()]
                self.junk = A.bf16(D)
                self.Bjunk = Buf()
                self.xn = [A.bf16(D) for _ in range(2)]
                self.Bxn = [Buf(), Buf()]
                self.ss = [A.f32(1) for _ in range(2)]
                self.Bss = [Buf(), Buf()]
                self.i = 0

            def run(self, layer, ti, hT3, BhT, c0, psb):
                i = self.i
                self.i += 1
                xt, Bxt = self.xt[i % 2], self.Bxt[i % 2]
                xn, Bxn = self.xn[i % 2], self.Bxn[i % 2]
                ss, Bss = self.ss[i % 2], self.Bss[i % 2]
                junk, Bjunk = self.junk, self.Bjunk
                src = tile_src(layer, ti)
                col = tile_col(ti)
                S.dma("sp", ("dma_start", dict(out=xt, in_=src)), writes=[Bxt])
                S.op("act", ("activation", dict(out=junk, in_=xt, func=AF.Square, accum_out=ss)), reads=[Bxt], writes=[Bjunk, Bss])
                S.op("act", ("activation", dict(out=ss, in_=ss, func=AF.Sqrt, scale=1.0 / D, bias=EPS)), reads=[Bss], writes=[Bss])
                S.op("dve", ("reciprocal", dict(out=ss, in_=ss)), reads=[Bss], writes=[Bss])
                S.op("dve", ("tensor_scalar", dict(out=xn, in0=xt, scalar1=ss, scalar2=None, op0=ALU.mult)), reads=[Bxt, Bss], writes=[Bxn])
                pT = r3(bank_bf(psb), 8)
                for k in range(8):
                    S.op("pe", ("transpose", dict(out=pT[:, k, :], in_=xn[:, k * 128:(k + 1) * 128], identity=ident_b)),
                         reads=[Bxn, Bc], writes=[PB[psb]])
                for k in range(8):
                    eng = "act" if k % 2 == 0 else "dve"
                    if eng == "act":
                        S.op("act", ("activation", dict(out=hT3[:, k, c0:c0 + 128], in_=pT[:, k, :], func=AF.Identity,
                                                                  scale=modT5[:, layer, 1, k, col:col + 1], bias=modT5[:, layer, 0, k, col:col + 1])),
                             reads=[PB[psb], BmodT], writes=[BhT])
                    else:
                        S.op("dve", ("tensor_scalar", dict(out=hT3[:, k, c0:c0 + 128], in0=pT[:, k, :],
                                                                     scalar1=modT5[:, layer, 1, k, col:col + 1], scalar2=modT5[:, layer, 0, k, col:col + 1],
                                                                     op0=ALU.mult, op1=ALU.add)),
                             reads=[PB[psb], BmodT], writes=[BhT])

        def phase1():
            L = 0
            GELU = AF.Gelu_apprx_tanh
            WC = 1280
            w_in = A.bf16(8 * WC)
            w_in3 = r3(w_in, 8)
            Bwin = Buf()
            for k in range(8):
                S.dma("pool", ("dma_start", dict(out=w_in3[:, k, :], in_=w_in_d[k * 128:(k + 1) * 128, 512:1792])), writes=[Bwin])
            wst = A.bf16(8 * 512)
            wst5 = wst.rearrange("p (k j two d) -> p k j two d", k=8, j=4, two=2)
            for k in range(8):
                for two in range(2):
                    S.dma("pool", ("dma_start", dict(out=wst5[:, k, :, two, :],
                                                     in_=w_in_d[k * 128:(k + 1) * 128, two * 256:(two + 1) * 256].rearrange("p (j d) -> p j d", j=4))), writes=[Bwin])
            wst3 = r3(wst, 8)
            wpst = A.bf16(8 * 512)
            wpst3 = r3(wpst, 8)
            wkp = A.bf16(8 * 128)
            wkp3 = r3(wkp, 8)
            Bwperm = Buf()
            sv = wst.rearrange("p (k x b i) -> p k x b i", k=8, b=2, i=16)
            dv = wpst.rearrange("p (k x b i) -> p k x b i", k=8, b=2, i=16)
            svk = w_in3[:, :, 0:128].rearrange("p k (x b i) -> p k x b i", b=2, i=16)
            dvk = wkp3.rearrange("p k (x b i) -> p k x b i", b=2, i=16)
            for b_ in range(2):
                S.op("dve", ("tensor_copy", dict(out=dv[:, :, :, b_, :], in_=sv[:, :, :, 1 - b_, :])), reads=[Bwin], writes=[Bwperm])
                S.op("dve", ("tensor_copy", dict(out=dvk[:, :, :, b_, :], in_=svk[:, :, :, 1 - b_, :])), reads=[Bwin], writes=[Bwperm])
            wout = A.bf16(12 * 1024)
            wout3 = r3(wout, 12)
            Bwout = Buf()
            S.dma("pool", ("dma_start", dict(out=wout3[0:64, 0:4, :], in_=w_out_d[0:256, :].rearrange("(c r) n -> r c n", r=64))), writes=[Bwout])
            S.dma("pool", ("dma_start", dict(out=wout3[64:128, 0:4, :], in_=w_out_d[256:512, :].rearrange("(c r) n -> r c n", r=64))), writes=[Bwout])
            S.dma("pool", ("dma_start", dict(out=wout3[0:64, 4:8, :], in_=w_out_d[512:768, :].rearrange("(c r) n -> r c n", r=64))), writes=[Bwout])
            S.dma("pool", ("dma_start", dict(out=wout3[0:64, 8:12, :], in_=w_out_d[768:1024, :].rearrange("(c r) n -> r c n", r=64))), writes=[Bwout])
            yt = A.f32(D)
            wspn = yt
            Bwspn = Buf()
            S.dma("sp", ("dma_start", dict(out=r3(wspn, 8), in_=wsp_d.rearrange("g p q -> p g q"))), writes=[Bwspn])
            wspT = A.bf16(1024)
            wspT3 = r3(wspT, 8)
            BwspT = Buf()
            pw = r3(psum_t[:, 0:1024], 8)
            for g in range(8):
                S.op("pe", ("transpose", dict(out=pw[:, g, :], in_=r3(wspn, 8)[:, g, :], identity=ident_f)),
                     reads=[Bwspn, Bc], writes=[PB[0], PB[1]])
            S.op("act", ("copy", dict(out=wspT, in_=psum_t[:, 0:1024])), reads=[PB[0], PB[1]], writes=[BwspT])
            lng_bc = A.f32(512)
            bsp_bc = A.f32(1024)
            qkg = A.f32(4)
            cosT = A.f32(SEQ)
            sinT = A.f32(SEQ)
            gate_bc = [A.f32(D) for _ in range(3)]
            Bsm = Buf()
            S.dma("sp", ("dma_start", dict(out=lng_bc, in_=lng_d.broadcast_to([128, 512]))), writes=[Bsm])
            S.dma("sp", ("dma_start", dict(out=bsp_bc[0:64, :], in_=bsp_d.broadcast_to([64, 1024]))), writes=[Bsm])
            S.dma("sp", ("dma_start", dict(out=qkg, in_=qkg_d)), writes=[Bsm])
            S.dma("sp", ("dma_start", dict(out=cosT, in_=rope_d[:, 0:SEQ])), writes=[Bsm])
            S.dma("sp", ("dma_start", dict(out=sinT, in_=rope_d[:, SEQ:2 * SEQ])), writes=[Bsm])
            for c in range(3):
                S.dma("sp", ("dma_start", dict(out=gate_bc[c], in_=modrows_d[L, 2, c:c + 1, :].broadcast_to([128, D]))),
                      reads=[Bmodrows], writes=[Bsm])
            bsp3 = r3(bsp_bc[0:64, :], 8)

            BK0 = Buf()
            QT = A.bf16(4 * SEQ)
            QT3 = r3(QT, 4)
            KTz = [A.bf16(SEQ), A.bf16(SEQ)]
            S.op("dve", ("memset", dict(ap=KTz[0][64:128, :], constant=0.0)), writes=[BK0])
            S.op("dve", ("memset", dict(ap=KTz[1][0:64, :], constant=0.0)), writes=[BK0])
            Vt = A.bf16(18 * 128)
            V3 = r3(Vt, 18)
            BQ, BK, BV = Buf(), Buf(), Buf()
            hT = [A.bf16(8 * 512), A.bf16(8 * 512)]
            BhT = [Buf(), Buf()]
            NT = NormT()
            sqb = A.bf16(512)
            rs = A.f32(512)
            t1 = A.f32(512)
            t2 = A.f32(512)
            Bsq, Brs, Bt1, Bt2 = Buf(), Buf(), Buf(), Buf()
            mx = A.f32(1024)
            Bmx = Buf()
            rec = A.f32(512)
            Brec = Buf()
            sqb2 = [sqb, A.bf16(512)]
            rs2 = [rs, rec]
            t12 = [t1, mx[:, 0:512]]
            t22 = [t2, mx[:, 512:1024]]
            Bsq2, Brs2, Bt12, Bt22 = [Bsq, Buf()], [Brs, Brec], [Bt1, Bmx], [Bt2, Bmx]
            UG = A.bf16(8 * 512)
            UG3 = r3(UG, 8)
            GT3 = UG3
            OT = A.bf16(4 * 512)
            OT3 = r3(OT, 4)
            BUG = Buf()
            BGT = BUG
            BOT = Buf()
            gv = t1
            vnf = t2
            vnb = A.bf16(512)
            st6 = A.f32(6)
            mv = A.f32(2)
            Bgv, Bvnf = Bt1, Bt2
            Bvnb, Bst, Bmv = Buf(), Buf(), Buf()
            PT = [A.bf16(512) for _ in range(4)]
            BPT = [Buf(), Buf(), Buf(), Buf()]
            rec = A.f32(512)
            Brec = Buf()
            xt2 = A.f32(D)
            Byt, Bxt2 = Buf(), Buf()
            Bxr = Buf()
            print('phase1 arena words', A.off)
            hcount = [0]

            def make_hT(blk):
                tiles, c0seq, N = blk
                i = hcount[0]
                hcount[0] += 1
                h3 = r3(hT[i % 2], 8)
                for t, ti in enumerate(tiles):
                    NT.run(L, ti, h3, BhT[i % 2], t * 128, t % 2)
                return h3, BhT[i % 2]

            for b in range(NB):
                blocks = [([32 + 2 * b, 33 + 2 * b], 0, 256)]
                for i in range(4):
                    blocks.append(([16 * b + 4 * i + t for t in range(4)], 256 + 512 * i, 512))
                nxt = make_hT(blocks[0])
                for bi1, blk in enumerate(blocks):
                    tiles, c0, N = blk
                    h3, Bh = nxt
                    if bi1 + 1 < len(blocks):
                        nxt = make_hT(blocks[bi1 + 1])
                    for j in range(5):
                        bq, bqp, bms = (2, 3, 4) if j % 2 == 0 else (5, 6, 7)
                        sqb_, rs_, t1_, t2_ = sqb2[j % 2], rs2[j % 2], t12[j % 2], t22[j % 2]
                        Bsq_, Brs_, Bt1_, Bt2_ = Bsq2[j % 2], Brs2[j % 2], Bt12[j % 2], Bt22[j % 2]
                        for (pb, wq, wk) in ((bq, wst3, w_in3), (bqp, wpst3, wkp3)):
                            for k in range(8):
                                lw = wq[:, k, j * 128:(j + 1) * 128] if j < 4 else wk[:, k, 0:128]
                                S.op("pe", ("matmul", dict(out=bank(pb)[:, 0:N], lhsT=lw, rhs=h3[:, k, 0:N], start=(k == 0), stop=(k == 7))),
                                     reads=[Bwin, Bwperm, Bh], writes=[PB[pb]])
                        gi = 0 if j < 4 else 2
                        S.op("act", ("activation", dict(out=sqb_[:, 0:N], in_=bank(bq)[:, 0:N], func=AF.Square)), reads=[PB[bq]], writes=[Bsq_])
                        S.op("pe", ("matmul", dict(out=bank(bms)[:, 0:N], lhsT=blk_b, rhs=sqb_[:, 0:N], start=True, stop=True)), reads=[Bsq_, Bc], writes=[PB[bms]])
                        S.op("act", ("activation", dict(out=rs_[:, 0:N], in_=bank(bms)[:, 0:N], func=AF.Sqrt, bias=EPS, scale=1.0)), reads=[PB[bms]], writes=[Brs_])
                        S.op("dve", ("reciprocal", dict(out=rs_[:, 0:N], in_=rs_[:, 0:N])), reads=[Brs_], writes=[Brs_])
                        S.op("dve", ("scalar_tensor_tensor", dict(out=t1_[:, 0:N], in0=bank(bq)[:, 0:N], scalar=qkg[:, gi:gi + 1], in1=cosT[:, c0:c0 + N],
                                                                             op0=ALU.mult, op1=ALU.mult)), reads=[PB[bq], Bsm], writes=[Bt1_])
                        S.op("dve", ("scalar_tensor_tensor", dict(out=t2_[:, 0:N], in0=bank(bqp)[:, 0:N], scalar=qkg[:, gi + 1:gi + 2], in1=sinT[:, c0:c0 + N],
                                                                             op0=ALU.mult, op1=ALU.mult)), reads=[PB[bqp], Bsm], writes=[Bt2_])
                        S.op("dve", ("tensor_tensor", dict(out=t1_[:, 0:N], in0=t1_[:, 0:N], in1=t2_[:, 0:N], op=ALU.add)), reads=[Bt1_, Bt2_], writes=[Bt1_])
                        if j < 4:
                            S.op("dve", ("tensor_tensor", dict(out=QT3[:, j, c0:c0 + N], in0=t1_[:, 0:N], in1=rs_[:, 0:N], op=ALU.mult)), reads=[Bt1_, Brs_], writes=[BQ])
                        else:
                            for hf_ in range(2):
                                ps_ = slice(hf_ * 64, hf_ * 64 + 64)
                                S.op("dve", ("tensor_tensor", dict(out=KTz[hf_][ps_, c0:c0 + N], in0=t1_[ps_, 0:N], in1=rs_[ps_, 0:N], op=ALU.mult)),
                                     reads=[Bt1_, Brs_, BK0], writes=[BK])
                    for t in range(len(tiles)):
                        kt = c0 // 128 + t
                        for k in range(8):
                            S.op("pe", ("matmul", dict(out=bank(5)[:, 0:128], lhsT=h3[:, k, t * 128:(t + 1) * 128], rhs=w_in3[:, k, 128:256],
                                                                      start=(k == 0), stop=(k == 7))), reads=[Bwin, Bh], writes=[PB[5]])
                        S.op("act", ("copy", dict(out=V3[:, kt, :], in_=bank(5)[:, 0:128])), reads=[PB[5]], writes=[BV])
                nxt = make_hT(blocks[0])
                for bi, blk in enumerate(blocks):
                    tiles, c0, N = blk
                    h3, Bh = nxt
                    if bi + 1 < len(blocks):
                        nxt = make_hT(blocks[bi + 1])
                    for g in range(8):
                        pb = 2 + g % 2
                        for k in range(8):
                            S.op("pe", ("matmul", dict(out=bank(pb)[0:64, 0:N], lhsT=w_in3[:, k, 256 + g * 64:320 + g * 64], rhs=h3[:, k, 0:N],
                                                                             start=(k == 0), stop=(k == 7))), reads=[Bwin, Bh], writes=[PB[pb]])
                        S.op("act", ("activation", dict(out=UG3[0:64, g, 0:N], in_=bank(pb)[0:64, 0:N], func=GELU)), reads=[PB[pb]], writes=[BUG])
                    for t in range(len(tiles)):
                        tc0 = t * 128
                        for k in range(8):
                            S.op("pe", ("matmul", dict(out=bank(4), lhsT=h3[:, k, tc0:tc0 + 128], rhs=w_in3[:, k, 768:1280],
                                                                          start=(k == 0), stop=(k == 7))), reads=[Bwin, Bh], writes=[PB[4]])
                        S.op("act", ("activation", dict(out=gv, in_=bank(4), func=GELU)), reads=[PB[4]], writes=[Bgv])
                        S.op("dve", ("bn_stats", dict(out=st6, in_=gv)), reads=[Bgv], writes=[Bst])
                        S.op("dve", ("bn_aggr", dict(out=mv, in_=st6)), reads=[Bst], writes=[Bmv])
                        S.op("act", ("activation", dict(out=mv[:, 1:2], in_=mv[:, 1:2], func=AF.Sqrt, bias=EPS, scale=1.0)), reads=[Bmv], writes=[Bmv])
                        S.op("dve", ("reciprocal", dict(out=mv[:, 1:2], in_=mv[:, 1:2])), reads=[Bmv], writes=[Bmv])
                        S.op("dve", ("tensor_scalar", dict(out=vnf, in0=gv, scalar1=mv[:, 0:1], scalar2=mv[:, 1:2], op0=ALU.subtract, op1=ALU.mult)),
                             reads=[Bgv, Bmv], writes=[Bvnf])
                        S.op("dve", ("tensor_tensor", dict(out=vnb, in0=vnf, in1=lng_bc, op=ALU.mult)), reads=[Bvnf, Bsm], writes=[Bvnb])
                        pm = r3(psum_t[0:64, 6 * 512:8 * 512], 8)
                        for g in range(8):
                            S.op("pe", ("matmul", dict(out=pm[:, g, :], lhsT=vnb[:, g * 64:(g + 1) * 64], rhs=wspT3[:, g, :], start=True, stop=True)),
                                 reads=[Bvnb, BwspT], writes=[PB[6], PB[7]])
                        S.op("dve", ("tensor_tensor", dict(out=r3(mx[0:64, :], 8), in0=pm, in1=bsp3, op=ALU.add)), reads=[PB[6], PB[7], Bsm], writes=[Bmx])
                        S.op("dve", ("tensor_tensor", dict(out=GT3[0:64, :, tc0:tc0 + 128], in0=r3(mx[0:64, :], 8), in1=UG3[0:64, :, tc0:tc0 + 128], op=ALU.mult)),
                             reads=[Bmx, BUG], writes=[BGT])
                    kts = list(range(2)) if bi == 0 else list(range(18))
                    steps = [(h, ki, kt) for h in range(8) for ki, kt in enumerate(kts)]
                    nk = len(kts)

                    def issue_S(idx):
                        h, ki, kt = steps[idx]
                        half, j = h // 4, h % 4
                        sb_ = (0, 1, 6)[idx % 3]
                        S.op("pe", ("matmul", dict(out=bank(sb_)[:, 0:N], lhsT=KTz[half][:, kt * 128:(kt + 1) * 128],
                                                   rhs=QT3[:, j, c0:c0 + N], start=True, stop=True)), reads=[BK, BQ], writes=[PB[sb_]])
                        pt, Bpt = PT[idx % 4], BPT[idx % 4]
                        S.op("act", ("activation", dict(out=pt[:, 0:N], in_=bank(sb_)[:, 0:N], func=AF.Exp, scale=0.125)), reads=[PB[sb_]], writes=[Bpt])

                    def issue_PV(idx):
                        h, ki, kt = steps[idx]
                        half, j = h // 4, h % 4
                        p0 = half * 64
                        bo, bd = 2 + (h % 2) * 2, 3 + (h % 2) * 2
                        pt, Bpt = PT[idx % 4], BPT[idx % 4]
                        S.op("pe", ("matmul", dict(out=bank(bo)[:, 0:N], lhsT=V3[:, kt, :], rhs=pt[:, 0:N],
                                                   start=(ki == 0), stop=(ki == nk - 1))), reads=[BV, Bpt], writes=[PB[bo]])
                        S.op("pe", ("matmul", dict(out=bank(bd)[:, 0:N], lhsT=ones_b, rhs=pt[:, 0:N],
                                                   start=(ki == 0), stop=(ki == nk - 1))), reads=[Bc, Bpt], writes=[PB[bd]])
                        if ki == nk - 1:
                            S.op("dve", ("reciprocal", dict(out=rec[p0:p0 + 64, 0:N], in_=bank(bd)[p0:p0 + 64, 0:N])), reads=[PB[bd]], writes=[Brec])
                            S.op("dve", ("tensor_tensor", dict(out=OT3[p0:p0 + 64, j, 0:N], in0=bank(bo)[p0:p0 + 64, 0:N], in1=rec[p0:p0 + 64, 0:N], op=ALU.mult)),
                                 reads=[PB[bo], Brec], writes=[BOT])

                    for idx in range(len(steps) + 2):
                        if idx < len(steps):
                            issue_S(idx)
                        if idx >= 2:
                            issue_PV(idx - 2)
                    for t, ti in enumerate(tiles):
                        tc0 = t * 128
                        col = tile_col(ti)
                        for hf in range(2):
                            for c in range(12):
                                if c < 4:
                                    lw, rw = OT3[:, c, tc0:tc0 + 128], wout3[:, c, hf * 512:(hf + 1) * 512]
                                else:
                                    lw, rw = GT3[0:64, c - 4, tc0:tc0 + 128], wout3[0:64, c, hf * 512:(hf + 1) * 512]
                                S.op("pe", ("matmul", dict(out=bank(6 + hf), lhsT=lw, rhs=rw,
                                                                                 start=(c == 0), stop=(c == 11))), reads=[BOT, BGT, Bwout], writes=[PB[6 + hf]])
                        S.dma("sp", ("dma_start", dict(out=xt2, in_=tile_src(L, ti))), writes=[Bxt2])
                        S.op("dve", ("tensor_tensor", dict(out=yt, in0=psum_t[:, 6 * 512:8 * 512], in1=gate_bc[col], op=ALU.mult)),
                             reads=[PB[6], PB[7], Bsm], writes=[Byt])
                        S.op("dve", ("tensor_tensor", dict(out=yt, in0=yt, in1=xt2, op=ALU.add)), reads=[Byt, Bxt2], writes=[Byt])
                        S.dma("sp", ("dma_start", dict(out=xr_d[ti * 128:(ti + 1) * 128, :], in_=yt)), reads=[Byt], writes=[Bxr])
            return Bxr

        if stop_after >= 1:
            fill_xall()
            Bxr = phase1()
            S.barrier()
            A.off = persist_off
            if "xr" in dbg_d:
                S.dma("sp", ("dma_start", dict(out=dbg_d["xr"], in_=xr_d)), reads=[Bxr])
        def moe(L, ntiles, Bsrc):
            last = (L == 1)
            ncol = 2 if last else 3
            Abc = [A.f32(D) for _ in range(ncol)]
            Sbc = [A.f32(D) for _ in range(ncol)]
            Gbc = [A.f32(D) for _ in range(ncol)]
            Bbc = Buf()
            for c in range(ncol):
                for (kind, dst) in ((4, Abc), (3, Sbc), (5, Gbc)):
                    S.dma("sp", ("dma_start", dict(out=dst[c], in_=modrows_d[L, kind, c:c + 1, :].broadcast_to([128, D]))), reads=[Bmodrows], writes=[Bbc])
            fng = A.f32(D)
            if last:
                S.dma("sp", ("dma_start", dict(out=fng, in_=fng_d.broadcast_to([128, D]))), writes=[Bbc])
            w36 = A.f32(8 * 36)
            w36_3 = r3(w36, 8)
            b36 = A.f32(36)
            S.dma("sp", ("dma_start", dict(out=w36_3[:, :, 0:4], in_=wgrp_d[L].rearrange("(k p) n -> p k n", p=128), allow_slow_non_contiguous=True)), writes=[Bbc])
            S.dma("sp", ("dma_start", dict(out=w36_3[:, :, 4:36], in_=wrt_d[L].rearrange("(k p) n -> p k n", p=128), allow_slow_non_contiguous=True)), writes=[Bbc])
            S.dma("sp", ("dma_start", dict(out=b36[:, 0:4], in_=bgrp_d[L].broadcast_to([128, 4]))), writes=[Bbc])
            S.dma("sp", ("dma_start", dict(out=b36[:, 4:36], in_=brt_d[L].broadcast_to([128, 32]))), writes=[Bbc])
            slot_i = A.i32(ntiles * 2)
            slot_i3 = r3(slot_i, ntiles)
            wts = A.f32(ntiles * 2)
            wts3 = r3(wts, ntiles)
            Bslot = Buf()
            Bwts = Buf()
            run = A.f32(32)
            Brun = Buf()
            S.op("dve", ("memset", dict(ap=run, constant=0.0)), writes=[Brun])
            ztile = A.f32(D)
            Bz = Buf()
            Byall = Buf()
            Bxall = Buf()
            S.op("dve", ("memset", dict(ap=ztile, constant=0.0)), writes=[Bz])
            S.dma("sp", ("dma_start", dict(out=yall_d[NSLOT:NSLOT + 128, :], in_=ztile)), reads=[Bz], writes=[Byall])
            mark = A.off
            xt = [A.f32(D) for _ in range(2)]
            Bxt = [Buf(), Buf()]
            ff = [A.f32(D) for _ in range(2)]
            Bff = [Buf(), Buf()]
            fb = [A.bf16(D) for _ in range(3)]
            Bfb = [Buf(), Buf(), Buf()]
            fTs = A.f32(D)
            BfTs = Buf()
            junk = A.bf16(D)
            Bjunk = Buf()
            sm = A.f32(256)
            Bsmall = Buf()
            ss = sm[:, 0:1]
            lg = sm[:, 4:40]
            gmax = sm[:, 40:41]
            ngmax = sm[:, 41:42]
            gsum = sm[:, 42:43]
            eg = sm[:, 44:48]
            gmask = sm[:, 48:52]
            pen = sm[:, 52:56]
            masked = sm[:, 56:88]
            m8 = sm[:, 88:96]
            ntop1 = sm[:, 96:97]
            e2 = sm[:, 97:98]
            wa = sm[:, 98:99]
            wb = sm[:, 99:100]
            slotf = sm[:, 100:102]
            vab = sm[:, 102:104]
            sel1 = sm[:, 104:136]
            sel = sm[:, 136:168]
            pos = sm[:, 168:200]
            valid = sm[:, 200:232]
            tmp32 = A.f32(32)
            selb = A.bf16(32)
            fTs2 = [fTs, A.f32(D)]
            BfTs2 = [BfTs, Buf()]
            ssA = [A.f32(1), A.f32(1)]
            BssA = [Buf(), Buf()]
            lgb = [A.f32(36), A.f32(36)]
            Blg = [Buf(), Buf()]

            def stageA(ti):
                col = tile_col(ti)
                x_, Bx_ = xt[ti % 2], Bxt[ti % 2]
                f_, Bf_ = ff[ti % 2], Bff[ti % 2]
                fb_, Bfb_ = fb[ti % 3], Bfb[ti % 3]
                ss_, Bss_ = ssA[ti % 2], BssA[ti % 2]
                fT_, BfT_ = fTs2[ti % 2], BfTs2[ti % 2]
                S.dma("sp", ("dma_start", dict(out=x_, in_=xr_d[ti * 128:(ti + 1) * 128, :])), reads=[Bsrc], writes=[Bx_])
                S.op("act", ("activation", dict(out=junk, in_=x_, func=AF.Square, accum_out=ss_)), reads=[Bx_], writes=[Bjunk, Bss_])
                S.op("act", ("activation", dict(out=ss_, in_=ss_, func=AF.Sqrt, scale=1.0 / D, bias=EPS)), reads=[Bss_], writes=[Bss_])
                S.op("dve", ("reciprocal", dict(out=ss_, in_=ss_)), reads=[Bss_], writes=[Bss_])
                S.op("dve", ("scalar_tensor_tensor", dict(out=f_, in0=x_, scalar=ss_, in1=Abc[col], op0=ALU.mult, op1=ALU.mult)), reads=[Bx_, Bss_, Bbc], writes=[Bf_])
                S.op("dve", ("tensor_tensor", dict(out=f_, in0=f_, in1=Sbc[col], op=ALU.add)), reads=[Bf_, Bbc], writes=[Bf_])
                pT = r3(psum_t[:, 0:1024], 8)
                for k in range(8):
                    S.op("pe", ("transpose", dict(out=pT[:, k, :], in_=f_[:, k * 128:(k + 1) * 128], identity=ident_f)), reads=[Bf_, Bc], writes=[PB[0], PB[1]])
                S.op("act", ("copy", dict(out=fT_, in_=psum_t[:, 0:1024])), reads=[PB[0], PB[1]], writes=[BfT_])
                S.op("act", ("copy", dict(out=fb_, in_=f_)), reads=[Bf_], writes=[Bfb_])

            def stageA2(ti):
                fT_, BfT_ = fTs2[ti % 2], BfTs2[ti % 2]
                pl = 2 + ti % 2
                for k in range(8):
                    S.op("pe", ("matmul", dict(out=bank(pl)[:, 0:36], lhsT=fT_[:, k * 128:(k + 1) * 128], rhs=w36_3[:, k, :], start=(k == 0), stop=(k == 7))),
                         reads=[BfT_, Bbc], writes=[PB[pl]])

            def stageB(ti):
                lg = lgb[ti % 2]
                fb_, Bfb_ = fb[ti % 3], Bfb[ti % 3]
                pl = 2 + ti % 2
                S.op("dve", ("tensor_tensor", dict(out=lgb[ti % 2], in0=bank(pl)[:, 0:36], in1=b36, op=ALU.add)), reads=[PB[pl], Bbc], writes=[Blg[ti % 2]])
                dv = lambda name, **kw: S.op("dve", (name, kw), reads=[Bsmall, Brun, Blg[ti % 2]], writes=[Bsmall])
                exv = ex2[ti % 2]
                Bex = Bex2[ti % 2]
                ngm, nt1, tp2 = exv[:, 0:1], exv[:, 1:2], exv[:, 2:3]
                S.op("dve", ("tensor_reduce", dict(out=ngm, in_=lg[:, 0:4], axis=AX.X, op=ALU.max, negate=True)), reads=[Blg[ti % 2]], writes=[Bex])
                S.op("dve", ("tensor_scalar", dict(out=gmask, in0=lg[:, 0:4], scalar1=ngm, scalar2=0.0, op0=ALU.add, op1=ALU.is_ge)), reads=[Blg[ti % 2], Bex, Bsmall], writes=[Bsmall])
                dv("tensor_scalar", out=pen, in0=gmask, scalar1=1e30, scalar2=-1e30, op0=ALU.mult, op1=ALU.add)
                dv("tensor_tensor", out=r3(masked, 4), in0=r3(lg[:, 4:36], 4), in1=pen.unsqueeze(2).broadcast_to([128, 4, 8]), op=ALU.add)
                dv("max", out=m8, in_=masked)
                S.op("dve", ("tensor_copy", dict(out=exv[:, 1:3], in_=m8[:, 0:2])), reads=[Bsmall], writes=[Bex])
                S.op("dve", ("tensor_scalar", dict(out=nt1, in0=nt1, scalar1=-1.0, scalar2=None, op0=ALU.mult)), reads=[Bex], writes=[Bex])
                S.op("act", ("activation", dict(out=eg, in_=lg[:, 0:4], func=AF.Exp, bias=ngm, scale=1.0, accum_out=gsum)), reads=[Bex, Blg[ti % 2]], writes=[Bact])
                S.op("act", ("activation", dict(out=e2, in_=tp2, func=AF.Exp, bias=nt1, scale=1.0)), reads=[Bex], writes=[Bact])
                dv("tensor_scalar", out=sel1, in0=masked, scalar1=m8[:, 0:1], scalar2=None, op0=ALU.is_ge)
                dv("tensor_scalar", out=sel, in0=masked, scalar1=m8[:, 1:2], scalar2=None, op0=ALU.is_ge)
                dv("tensor_copy", out=selb, in_=sel)
                S.op("pe", ("matmul", dict(out=bank(4)[:, 0:32], lhsT=utri_b, rhs=selb, start=True, stop=True)), reads=[Bsmall, Bc], writes=[PB[4]])
                S.op("pe", ("matmul", dict(out=bank(4)[:, 32:64], lhsT=ones_b, rhs=selb, start=True, stop=True)), reads=[Bsmall, Bc], writes=[PB[4]])

            def stageB2(ti):
                fb_, Bfb_ = fb[ti % 3], Bfb[ti % 3]
                dv = lambda name, **kw: S.op("dve", (name, kw), reads=[Bsmall, Brun, Blg[ti % 2]], writes=[Bsmall])
                dv("tensor_tensor", out=sel, in0=sel, in1=sel1, op=ALU.subtract)
                S.op("dve", ("tensor_tensor", dict(out=pos, in0=bank(4)[:, 0:32], in1=run, op=ALU.add)), reads=[PB[4], Brun, Bsmall], writes=[Bsmall])
                S.op("dve", ("tensor_tensor", dict(out=run, in0=bank(4)[:, 32:64], in1=run, op=ALU.add)), reads=[PB[4], Brun, Bsmall], writes=[Brun])
                dv("tensor_scalar", out=valid, in0=pos, scalar1=float(CAP), scalar2=None, op0=ALU.is_lt)
                dv("tensor_tensor", out=pos, in0=pos, in1=iotaC, op=ALU.add)
                dv("scalar_tensor_tensor", out=pos, in0=pos, scalar=-float(TRASH), in1=valid, op0=ALU.add, op1=ALU.mult)
                selcat = r3(sm[:, 104:168], 2)
                pvcat = r3(sm[:, 168:232], 2)
                t4 = tmp128.rearrange("p (a c e) -> p a c e", a=2, c=2)
                dv("tensor_tensor", out=t4, in0=selcat.unsqueeze(2).broadcast_to([128, 2, 2, 32]), in1=pvcat.unsqueeze(1).broadcast_to([128, 2, 2, 32]), op=ALU.mult)
                dv("tensor_reduce", out=r4, in_=r3(tmp128, 4), axis=AX.X, op=ALU.add)
                S.op("dve", ("tensor_scalar", dict(out=slot_i3[:, ti, :], in0=r4[:, 0:4:2], scalar1=float(TRASH), scalar2=None, op0=ALU.add)), reads=[Bsmall], writes=[Bslot])
                for a_ in range(2):
                    S.dma("pool", ("indirect_dma_start", dict(out=xall_d, out_offset=bass.IndirectOffsetOnAxis(ap=slot_i3[:, ti, a_:a_ + 1], axis=0),
                                                             in_=fb_, in_offset=None)), reads=[Bfb_, Bslot], writes=[])
                S.op("dve", ("tensor_scalar", dict(out=wa, in0=e2, scalar1=1.0, scalar2=gsum, op0=ALU.add, op1=ALU.mult)), reads=[Bact, Bsmall], writes=[Bsmall])
                dv("reciprocal", out=wa, in_=wa)
                S.op("dve", ("tensor_tensor", dict(out=wb, in0=wa, in1=e2, op=ALU.mult)), reads=[Bact, Bsmall], writes=[Bsmall])
                S.op("dve", ("tensor_tensor", dict(out=wts3[:, ti, :], in0=sm[:, 98:100], in1=r4[:, 1:4:2], op=ALU.mult)), reads=[Bsmall], writes=[Bwts])

            tmp128 = A.f32(128)
            r4 = sm[:, 240:244]
            Bact = Buf()
            ex2 = [A.f32(4), A.f32(4)]
            Bex2 = [Buf(), Buf()]
            sma = A.f32(8)
            eg = sma[:, 0:4]
            gsum = sma[:, 4:5]
            e2 = sma[:, 5:6]
            for step in range(ntiles + 1):
                if step < ntiles:
                    stageA(step)
                if step >= 1:
                    stageB(step - 1)
                if step < ntiles:
                    stageA2(step)
                if step >= 1:
                    stageB2(step - 1)
            S.barrier()
            A.off = mark
            if last is False and "slots" in dbg_d:
                pass
            NJ = CAP // 128
            wbuf = [(A.bf16(8 * 512), A.bf16(8 * 512), A.bf16(4 * 1024)) for _ in range(2)]
            Bwb = [Buf(), Buf()]
            xrows = [A.bf16(NJ * D) for _ in range(2)]
            Bxrows = [Buf(), Buf()]
            XT = A.bf16(8 * CAP)
            XT3 = r3(XT, 8)
            BXT = Buf()
            sl = [A.f32(512) for _ in range(2)]
            Bsl = [Buf(), Buf()]
            hs = A.bf16(4 * CAP)
            hs3 = r3(hs, 4)
            Bhs = Buf()
            ysb = [A.f32(D) for _ in range(2)]
            Bysb = [Buf(), Buf()]
            yi = 0
            stg = (A.f32(8 * 512), A.f32(8 * 512), A.f32(4 * 1024))
            Bstg = [Buf(), Buf(), Buf()]

            def load_w(e_):
                S.dma("sp", ("dma_start", dict(out=r3(stg[0], 8), in_=w1_d[L, e_].rearrange("(k p) n -> p k n", p=128))), writes=[Bstg[0]])
                S.dma("sp", ("dma_start", dict(out=r3(stg[1], 8), in_=w3_d[L, e_].rearrange("(k p) n -> p k n", p=128))), writes=[Bstg[1]])
                S.dma("sp", ("dma_start", dict(out=r3(stg[2], 4), in_=w2_d[L, e_].rearrange("(k p) n -> p k n", p=128))), writes=[Bstg[2]])

            def cast_w(e_, which):
                dst = wbuf[e_ % 2][which]
                Bw_ = Bwb[e_ % 2]
                if which == 0:
                    S.op("act", ("copy", dict(out=dst, in_=stg[0])), reads=[Bstg[0]], writes=[Bw_])
                elif which == 1:
                    S.op("dve", ("tensor_copy", dict(out=dst, in_=stg[1])), reads=[Bstg[1]], writes=[Bw_])
                else:
                    S.op("act", ("copy", dict(out=dst[:, 0:2048], in_=stg[2][:, 0:2048])), reads=[Bstg[2]], writes=[Bw_])
                    S.op("dve", ("tensor_copy", dict(out=dst[:, 2048:4096], in_=stg[2][:, 2048:4096])), reads=[Bstg[2]], writes=[Bw_])

            def load_x(e_):
                S.dma("sp", ("dma_start", dict(out=r3(xrows[e_ % 2], NJ), in_=xall_d[e_ * CAP:(e_ + 1) * CAP, :].rearrange("(j p) d -> p j d", p=128))),
                      reads=[Bxall], writes=[Bxrows[e_ % 2]])

            load_w(0)
            load_x(0)
            for w_ in range(3):
                cast_w(0, w_)
            for e_ in range(32):
                w1b, w3b, w2b = wbuf[e_ % 2]
                Bw = Bwb[e_ % 2]
                xr_, Bxr_ = xrows[e_ % 2], Bxrows[e_ % 2]
                if e_ + 1 < 32:
                    load_w(e_ + 1)
                    load_x(e_ + 1)
                for j in range(NJ):
                    pb = j % 2
                    pT = r3(bank_bf(pb), 8)
                    for k in range(8):
                        S.op("pe", ("transpose", dict(out=pT[:, k, :], in_=r3(xr_, NJ)[:, j, k * 128:(k + 1) * 128], identity=ident_b)),
                             reads=[Bxr_, Bc], writes=[PB[pb]])
                    S.op("act" if j % 2 == 0 else "dve", ("tensor_copy" if j % 2 else "copy", dict(out=XT3[:, :, j * 128:(j + 1) * 128], in_=pT)),
                         reads=[PB[pb]], writes=[BXT])
                w1v, w3v, w2v = r3(w1b, 8), r3(w3b, 8), r3(w2b, 4)
                for bi_, (c0, n) in enumerate(((0, 512), (512, CAP - 512))):
                    for m in range(4):
                        for (pb, wv) in ((2 + (m % 2) * 2, w1v), (3 + (m % 2) * 2, w3v)):
                            for k in range(8):
                                S.op("pe", ("matmul", dict(out=bank(pb)[:, 0:n], lhsT=wv[:, k, m * 128:(m + 1) * 128], rhs=XT3[:, k, c0:c0 + n], start=(k == 0), stop=(k == 7))),
                                     reads=[Bw, BXT], writes=[PB[pb]])
                        p1, p3 = 2 + (m % 2) * 2, 3 + (m % 2) * 2
                        s_, Bs_ = sl[m % 2], Bsl[m % 2]
                        S.op("act", ("activation", dict(out=s_[:, 0:n], in_=bank(p1)[:, 0:n], func=AF.Silu)), reads=[PB[p1]], writes=[Bs_])
                        S.op("dve", ("tensor_tensor", dict(out=hs3[:, m, c0:c0 + n], in0=bank(p3)[:, 0:n], in1=s_[:, 0:n], op=ALU.mult)), reads=[PB[p3], Bs_], writes=[Bhs])
                    if e_ + 1 < 32:
                        cast_w(e_ + 1, bi_)
                for j in range(NJ):
                    ob = 6
                    for hf in range(2):
                        for m in range(4):
                            S.op("pe", ("matmul", dict(out=bank(ob + hf), lhsT=hs3[:, m, j * 128:(j + 1) * 128], rhs=w2v[:, m, hf * 512:(hf + 1) * 512], start=(m == 0), stop=(m == 3))),
                                 reads=[Bhs, Bw], writes=[PB[ob + hf]])
                    y_, By_ = ysb[yi % 2], Bysb[yi % 2]
                    yi += 1
                    S.op("act", ("copy", dict(out=y_[:, 0:512], in_=bank(ob))), reads=[PB[ob]], writes=[By_])
                    S.op("dve", ("tensor_copy", dict(out=y_[:, 512:1024], in_=bank(ob + 1))), reads=[PB[ob + 1]], writes=[By_])
                    r0 = e_ * CAP + j * 128
                    S.dma("sp", ("dma_start", dict(out=yall_d[r0:r0 + 128, :], in_=y_)), reads=[By_], writes=[Byall])
                    if j == 1 and e_ + 1 < 32:
                        cast_w(e_ + 1, 2)
            S.barrier()
            A.off = mark
            ya = [A.f32(D) for _ in range(2)]
            yb = [A.f32(D) for _ in range(2)]
            x1 = [A.f32(D) for _ in range(2)]
            Bya, Byb, Bx1 = [Buf(), Buf()], [Buf(), Buf()], [Buf(), Buf()]
            junk2 = A.bf16(D)
            Bj2 = Buf()
            ss2 = [A.f32(1) for _ in range(2)]
            Bss2 = [Buf(), Buf()]
            Bdst = Buf()
            for ti in range(ntiles):
                col = tile_col(ti)
                i2 = ti % 2
                S.dma("pool", ("indirect_dma_start", dict(out=ya[i2], out_offset=None, in_=yall_d, in_offset=bass.IndirectOffsetOnAxis(ap=slot_i3[:, ti, 0:1], axis=0))),
                      reads=[Byall, Bslot], writes=[Bya[i2]])
                S.dma("pool", ("indirect_dma_start", dict(out=yb[i2], out_offset=None, in_=yall_d, in_offset=bass.IndirectOffsetOnAxis(ap=slot_i3[:, ti, 1:2], axis=0))),
                      reads=[Byall, Bslot], writes=[Byb[i2]])
                S.dma("sp", ("dma_start", dict(out=x1[i2], in_=xr_d[ti * 128:(ti + 1) * 128, :])), reads=[Bsrc], writes=[Bx1[i2]])
                S.op("act", ("activation", dict(out=ya[i2], in_=ya[i2], func=AF.Copy, scale=wts3[:, ti, 0:1])), reads=[Bya[i2], Bwts], writes=[Bya[i2]])
                S.op("dve", ("scalar_tensor_tensor", dict(out=yb[i2], in0=yb[i2], scalar=wts3[:, ti, 1:2], in1=ya[i2], op0=ALU.mult, op1=ALU.add)),
                     reads=[Byb[i2], Bya[i2], Bwts], writes=[Byb[i2]])
                S.op("dve", ("tensor_tensor", dict(out=yb[i2], in0=yb[i2], in1=Gbc[col], op=ALU.mult)), reads=[Byb[i2], Bbc], writes=[Byb[i2]])
                S.op("dve", ("tensor_tensor", dict(out=x1[i2], in0=x1[i2], in1=yb[i2], op=ALU.add)), reads=[Bx1[i2], Byb[i2]], writes=[Bx1[i2]])
                if not last:
                    S.dma("sp", ("dma_start", dict(out=xr2_d[ti * 128:(ti + 1) * 128, :], in_=x1[i2])), reads=[Bx1[i2]], writes=[Bdst])
                else:
                    S.op("act", ("activation", dict(out=junk2, in_=x1[i2], func=AF.Square, accum_out=ss2[i2])), reads=[Bx1[i2]], writes=[Bj2, Bss2[i2]])
                    S.op("act", ("activation", dict(out=ss2[i2], in_=ss2[i2], func=AF.Sqrt, scale=1.0 / D, bias=EPS)), reads=[Bss2[i2]], writes=[Bss2[i2]])
                    S.op("dve", ("reciprocal", dict(out=ss2[i2], in_=ss2[i2])), reads=[Bss2[i2]], writes=[Bss2[i2]])
                    S.op("dve", ("scalar_tensor_tensor", dict(out=x1[i2], in0=x1[i2], scalar=ss2[i2], in1=fng, op0=ALU.mult, op1=ALU.mult)),
                         reads=[Bx1[i2], Bss2[i2], Bbc], writes=[Bx1[i2]])
                    S.dma("sp", ("dma_start", dict(out=out_d[ti * 128:(ti + 1) * 128, :], in_=x1[i2])), reads=[Bx1[i2]], writes=[Bdst])
            return Bdst

        if stop_after >= 2:
            Bxr2 = moe(0, 36, Bxr)
            S.barrier()
            A.off = persist_off
            if "xr2" in dbg_d:
                S.dma("sp", ("dma_start", dict(out=dbg_d["xr2"], in_=xr2_d)), reads=[Bxr2])
        def s5_mixer(Bsrc):
            L = 1
            TWO_PI = 6.283185307179586
            MAGIC = 12582912.0
            yacc = [A.f32(4 * S_LAT) for _ in range(NB)]
            yacc3 = [r3(y, 4) for y in yacc]
            Byacc = [Buf(), Buf()]
            uT = [A.bf16(4 * SEQ) for _ in range(NB)]
            uT3 = [r3(u, 4) for u in uT]
            BuT = [Buf(), Buf()]
            sd = A.f32(8)
            Bsd = Buf()
            S.dma("sp", ("dma_start", dict(out=sd[:, 0:4], in_=s5d_d)), writes=[Bsd])
            S.dma("sp", ("dma_start", dict(out=sd[:, 4:8], in_=s5bglu_d)), writes=[Bsd])
            mark = A.off
            if S5_DEBUG_STAGE < 1:
                return Byacc[0]
            w5 = A.bf16(8 * 512)
            w5_3 = r3(w5, 8)
            Bw5 = Buf()
            S.dma("pool", ("dma_start", dict(out=w5_3, in_=s5win_d.rearrange("(k p) n -> p k n", p=128))), writes=[Bw5])
            hT1 = A.bf16(8 * 512)
            h3 = r3(hT1, 8)
            Bh = Buf()
            NT = NormT()
            for b in range(NB):
                blocks = [([32 + 2 * b, 33 + 2 * b], 0, 256)]
                for i in range(4):
                    blocks.append(([16 * b + 4 * i + t for t in range(4)], 256 + 512 * i, 512))
                for (tiles, c0, N) in blocks:
                    for t, ti in enumerate(tiles):
                        NT.run(L, ti, h3, Bh, t * 128, t % 2)
                    for r in range(4 if S5_DEBUG_STAGE >= 1.5 else 0):
                        pb = 2 + r % 2
                        for k in range(8):
                            S.op("pe", ("matmul", dict(out=bank(pb)[:, 0:N], lhsT=w5_3[:, k, r * 128:(r + 1) * 128], rhs=h3[:, k, 0:N], start=(k == 0), stop=(k == 7))),
                                 reads=[Bw5, Bh], writes=[PB[pb]])
                        S.op("act", ("copy", dict(out=uT3[b][:, r, c0:c0 + N], in_=bank(pb)[:, 0:N])), reads=[PB[pb]], writes=[BuT[b]])
                        if c0 >= 256:
                            S.op("dve", ("tensor_scalar", dict(out=yacc3[b][:, r, c0 - 256:c0 - 256 + N], in0=uT3[b][:, r, c0:c0 + N], scalar1=sd[:, r:r + 1], scalar2=None, op0=ALU.mult)),
                                 reads=[BuT[b], Bsd], writes=[Byacc[b]])
            S.barrier()
            A.off = mark
            if S5_DEBUG_STAGE < 2:
                return Byacc[0]
            par = A.f32(96)
            par3 = par.rearrange("p (c t) -> p c t", t=3)
            Bpar = Buf()
            S.dma("sp", ("dma_start", dict(out=par, in_=s5par_d)), writes=[Bpar])
            iot = A.f32(SEQ)
            S.dma("sp", ("dma_start", dict(out=iot, in_=s5iota_d)), writes=[Bpar])
            NCB = 32
            pr_ = A.f32(NCB * 16)
            P3 = r3(pr_, 16)
            dtv, rho, tht, frv, sn, cs, nr, ni, inv, cfr, cfi, ncfr, ncfi, tmpa, tmpb, tmpc = [P3[:, i, :] for i in range(16)]
            are, aim, ldt = par3[:, :, 0], par3[:, :, 1], par3[:, :, 2]
            pv = lambda name, **kw: S.op("dve", (name, kw), reads=[Bpar], writes=[Bpar])
            pa = lambda **kw: S.op("act", ("activation", kw), reads=[Bpar], writes=[Bpar])
            pa(out=dtv, in_=ldt, func=AF.Exp)
            pv("tensor_tensor", out=tmpa, in0=are, in1=dtv, op=ALU.mult)
            pa(out=rho, in_=tmpa, func=AF.Exp)
            pv("tensor_tensor", out=tht, in0=aim, in1=dtv, op=ALU.mult)
            pv("tensor_scalar", out=tht, in0=tht, scalar1=1.0 / TWO_PI, scalar2=None, op0=ALU.mult)
            pv("tensor_scalar", out=tmpa, in0=tht, scalar1=MAGIC, scalar2=None, op0=ALU.add)
            pv("tensor_scalar", out=tmpa, in0=tmpa, scalar1=MAGIC, scalar2=None, op0=ALU.subtract)
            pv("tensor_tensor", out=frv, in0=tht, in1=tmpa, op=ALU.subtract)
            SC = TWO_PI * (1.0 - 1e-6)
            pa(out=sn, in_=frv, func=AF.Sin, scale=SC)
            pa(out=tmpb, in_=frv, func=AF.Sin, scale=SC / 2)
            pv("tensor_tensor", out=tmpb, in0=tmpb, in1=tmpb, op=ALU.mult)
            pv("tensor_scalar", out=cs, in0=tmpb, scalar1=-2.0, scalar2=1.0, op0=ALU.mult, op1=ALU.add)
            pv("tensor_tensor", out=nr, in0=rho, in1=cs, op=ALU.mult)
            pv("tensor_scalar", out=nr, in0=nr, scalar1=-1.0, scalar2=None, op0=ALU.add)
            pv("tensor_tensor", out=ni, in0=rho, in1=sn, op=ALU.mult)
            pv("tensor_tensor", out=tmpa, in0=are, in1=are, op=ALU.mult)
            pv("tensor_tensor", out=tmpb, in0=aim, in1=aim, op=ALU.mult)
            pv("tensor_tensor", out=inv, in0=tmpa, in1=tmpb, op=ALU.add)
            pv("reciprocal", out=inv, in_=inv)
            pv("tensor_tensor", out=tmpa, in0=nr, in1=are, op=ALU.mult)
            pv("tensor_tensor", out=tmpb, in0=ni, in1=aim, op=ALU.mult)
            pv("tensor_tensor", out=tmpa, in0=tmpa, in1=tmpb, op=ALU.add)
            pv("tensor_tensor", out=cfr, in0=tmpa, in1=inv, op=ALU.mult)
            pv("tensor_tensor", out=tmpa, in0=ni, in1=are, op=ALU.mult)
            pv("tensor_tensor", out=tmpb, in0=nr, in1=aim, op=ALU.mult)
            pv("tensor_tensor", out=tmpa, in0=tmpa, in1=tmpb, op=ALU.subtract)
            pv("tensor_tensor", out=cfi, in0=tmpa, in1=inv, op=ALU.mult)
            pv("tensor_scalar", out=ncfr, in0=cfr, scalar1=-1.0, scalar2=None, op0=ALU.mult)
            pv("tensor_scalar", out=ncfi, in0=cfi, scalar1=-1.0, scalar2=None, op0=ALU.mult)

            if S5_DEBUG_STAGE < 3:
                return Bpar
            cosT = A.f32(SEQ)
            sinT = A.f32(SEQ)
            tA = A.f32(SEQ)
            tB = A.f32(SEQ)
            Btab, BtA, BtB = Buf(), Buf(), Buf()
            dr = A.f32(SEQ)
            di = A.f32(SEQ)
            qr = A.bf16(SEQ)
            qi = A.bf16(SEQ)
            cosb = A.bf16(SEQ)
            sinb = A.bf16(SEQ)
            Btabb = Buf()
            m1b = [A.bf16(512) for _ in range(2)]
            m2b = [A.bf16(512) for _ in range(2)]
            Bdr, Bdi, Bqr, Bqi = Buf(), Buf(), Buf(), Buf()
            m1 = [A.f32(512) for _ in range(2)]
            m2 = [A.f32(512) for _ in range(2)]
            Bm1, Bm2 = [Buf(), Buf()], [Buf(), Buf()]
            bt = [(A.bf16(128), A.bf16(128)) for _ in range(2)]
            Bbt = [Buf(), Buf()]
            cst_ = [(A.f32(128), A.f32(128)) for _ in range(2)]
            Bcst = [Buf(), Buf()]
            cw = [(A.bf16(128), A.bf16(128)) for _ in range(2)]
            Bcw = [Buf(), Buf()]
            ctmp = A.f32(128)
            Bctmp = Buf()
            ncwr = [A.bf16(128), A.bf16(128)]
            mi = 0
            for d_ in range(2):
                for pr in range(16):
                    ci_ = d_ * 16 + pr
                    r = pr // 4
                    k2 = ci_ % 2
                    btr, bti = bt[k2]
                    S.dma("pool", ("dma_start", dict(out=btr, in_=s5bT_d[d_, 0, pr])), writes=[Bbt[k2]])
                    S.dma("pool", ("dma_start", dict(out=bti, in_=s5bT_d[d_, 1, pr])), writes=[Bbt[k2]])
                    c_r, c_i = cst_[k2]
                    S.dma("sp", ("dma_start", dict(out=c_r, in_=s5c_d[d_, 0, pr])), writes=[Bcst[k2]])
                    S.dma("sp", ("dma_start", dict(out=c_i, in_=s5c_d[d_, 1, pr])), writes=[Bcst[k2]])
                    cwr, cwi = cw[k2]
                    col1 = lambda v, ci_=ci_: v[:, ci_:ci_ + 1]
                    S.op("dve", ("tensor_scalar", dict(out=ctmp, in0=c_r, scalar1=col1(cfr), scalar2=None, op0=ALU.mult)), reads=[Bcst[k2], Bpar], writes=[Bctmp])
                    S.op("dve", ("scalar_tensor_tensor", dict(out=cwr, in0=c_i, scalar=col1(ncfi), in1=ctmp, op0=ALU.mult, op1=ALU.add)), reads=[Bcst[k2], Bpar, Bctmp], writes=[Bcw[k2]])
                    S.op("dve", ("tensor_scalar", dict(out=ncwr[k2], in0=cwr, scalar1=-1.0, scalar2=None, op0=ALU.mult)), reads=[Bcw[k2]], writes=[Bcw[k2]])
                    S.op("dve", ("tensor_scalar", dict(out=ctmp, in0=c_r, scalar1=col1(ncfi), scalar2=None, op0=ALU.mult)), reads=[Bcst[k2], Bpar, Bcw[k2]], writes=[Bctmp])
                    S.op("dve", ("scalar_tensor_tensor", dict(out=cwi, in0=c_i, scalar=col1(ncfr), in1=ctmp, op0=ALU.mult, op1=ALU.add)), reads=[Bcst[k2], Bpar, Bctmp], writes=[Bcw[k2]])
                    S.op("act", ("activation", dict(out=tA, in_=iot, func=AF.Copy, scale=col1(tht))), reads=[Bpar], writes=[BtA])
                    S.op("act", ("activation", dict(out=tB, in_=tA, func=AF.Identity, bias=MAGIC, scale=1.0)), reads=[BtA], writes=[BtB])
                    S.op("act", ("activation", dict(out=tB, in_=tB, func=AF.Identity, bias=-MAGIC, scale=1.0)), reads=[BtB], writes=[BtB])
                    S.op("dve", ("tensor_tensor", dict(out=tA, in0=tA, in1=tB, op=ALU.subtract)), reads=[BtA, BtB], writes=[BtA])
                    S.op("act", ("activation", dict(out=sinT, in_=tA, func=AF.Sin, scale=SC)), reads=[BtA], writes=[Btab])
                    S.op("act", ("activation", dict(out=tB, in_=tA, func=AF.Sin, scale=SC / 2)), reads=[BtA], writes=[BtB])
                    S.op("act", ("activation", dict(out=tB, in_=tB, func=AF.Square, scale=1.4142135623730951)), reads=[BtB], writes=[BtB])
                    S.op("act", ("activation", dict(out=cosT, in_=tB, func=AF.Identity, scale=-1.0, bias=1.0)), reads=[BtB], writes=[Btab])
                    S.op("act", ("copy", dict(out=cosb, in_=cosT)), reads=[Btab], writes=[Btabb])
                    S.op("act", ("copy", dict(out=sinb, in_=sinT)), reads=[Btab], writes=[Btabb])
                    rho_c = col1(rho)
                    for b in range(NB):
                        blocks = [(0, 256)] + [(256 + 512 * i, 512) for i in range(4)]
                        for bi, (s0, N) in enumerate(blocks):
                            if d_ == 0:
                                ucols = uT3[b][:, r, s0:s0 + N]
                            else:
                                if bi == 0:
                                    ucols = uT3[b][:, r, 255::-1]
                                else:
                                    hi_c = SEQ - 1 - (bi - 1) * 512
                                    ucols = uT3[b][:, r, hi_c:hi_c - 512:-1]
                            pbr, pbi = (bi % 2) * 2, (bi % 2) * 2 + 1
                            S.op("pe", ("matmul", dict(out=bank(pbr)[:, 0:N], lhsT=btr, rhs=ucols, start=True, stop=True)), reads=[Bbt[k2], BuT[b]], writes=[PB[pbr]])
                            S.op("pe", ("matmul", dict(out=bank(pbi)[:, 0:N], lhsT=bti, rhs=ucols, start=True, stop=True)), reads=[Bbt[k2], BuT[b]], writes=[PB[pbi]])
                            a1, a2 = m1[mi % 2], m2[mi % 2]
                            Ba1, Ba2 = Bm1[mi % 2], Bm2[mi % 2]
                            mi += 1
                            cS, sS = cosT[:, s0:s0 + N], sinT[:, s0:s0 + N]
                            S.op("dve", ("tensor_tensor", dict(out=a1[:, 0:N], in0=bank(pbr)[:, 0:N], in1=cS, op=ALU.mult)), reads=[PB[pbr], Btab], writes=[Ba1])
                            S.op("dve", ("tensor_tensor", dict(out=a2[:, 0:N], in0=bank(pbi)[:, 0:N], in1=sS, op=ALU.mult)), reads=[PB[pbi], Btab], writes=[Ba2])
                            S.op("dve", ("tensor_tensor", dict(out=dr[:, s0:s0 + N], in0=a1[:, 0:N], in1=a2[:, 0:N], op=ALU.add)), reads=[Ba1, Ba2], writes=[Bdr])
                            a1, a2 = m1[mi % 2], m2[mi % 2]
                            Ba1, Ba2 = Bm1[mi % 2], Bm2[mi % 2]
                            mi += 1
                            S.op("dve", ("tensor_tensor", dict(out=a1[:, 0:N], in0=bank(pbi)[:, 0:N], in1=cS, op=ALU.mult)), reads=[PB[pbi], Btab], writes=[Ba1])
                            S.op("dve", ("tensor_tensor", dict(out=a2[:, 0:N], in0=bank(pbr)[:, 0:N], in1=sS, op=ALU.mult)), reads=[PB[pbr], Btab], writes=[Ba2])
                            S.op("dve", ("tensor_tensor", dict(out=di[:, s0:s0 + N], in0=a1[:, 0:N], in1=a2[:, 0:N], op=ALU.subtract)), reads=[Ba1, Ba2], writes=[Bdi])
                        rb = rho_c.broadcast_to([128, SEQ])
                        S.op("dve", ("tensor_tensor_scan", dict(out=qr, data0=rb, data1=dr, initial=0.0, op0=ALU.mult, op1=ALU.add)), reads=[Bdr, Bpar], writes=[Bqr])
                        S.op("dve", ("tensor_tensor_scan", dict(out=qi, data0=rb, data1=di, initial=0.0, op0=ALU.mult, op1=ALU.add)), reads=[Bdi, Bpar], writes=[Bqi])
                        for bi in range(1, 5):
                            s0, N = blocks[bi]
                            cS, sS = cosb[:, s0:s0 + N], sinb[:, s0:s0 + N]
                            a1, a2 = m1b[mi % 2], m2b[mi % 2]
                            Ba1, Ba2 = Bm1[mi % 2], Bm2[mi % 2]
                            mi += 1
                            S.op("dve", ("tensor_tensor", dict(out=a1, in0=qr[:, s0:s0 + N], in1=cS, op=ALU.mult)), reads=[Bqr, Btabb], writes=[Ba1])
                            S.op("dve", ("tensor_tensor", dict(out=a2, in0=qi[:, s0:s0 + N], in1=sS, op=ALU.mult)), reads=[Bqi, Btabb], writes=[Ba2])
                            pby = 4 + bi % 2
                            S.op("pe", ("matmul", dict(out=bank(pby), lhsT=cwr, rhs=a1, start=True, stop=False)), reads=[Bcw[k2], Ba1], writes=[PB[pby]])
                            S.op("pe", ("matmul", dict(out=bank(pby), lhsT=ncwr[k2], rhs=a2, start=False, stop=False)), reads=[Bcw[k2], Ba2], writes=[PB[pby]])
                            a1, a2 = m1b[mi % 2], m2b[mi % 2]
                            Ba1, Ba2 = Bm1[mi % 2], Bm2[mi % 2]
                            mi += 1
                            S.op("dve", ("tensor_tensor", dict(out=a1, in0=qr[:, s0:s0 + N], in1=sS, op=ALU.mult)), reads=[Bqr, Btabb], writes=[Ba1])
                            S.op("dve", ("tensor_tensor", dict(out=a2, in0=qi[:, s0:s0 + N], in1=cS, op=ALU.mult)), reads=[Bqi, Btabb], writes=[Ba2])
                            S.op("pe", ("matmul", dict(out=bank(pby), lhsT=cwi, rhs=a1, start=False, stop=False)), reads=[Bcw[k2], Ba1], writes=[PB[pby]])
                            S.op("pe", ("matmul", dict(out=bank(pby), lhsT=cwi, rhs=a2, start=False, stop=True)), reads=[Bcw[k2], Ba2], writes=[PB[pby]])
                            if d_ == 0:
                                j0 = s0 - 256
                                ycols = yacc3[b][:, r, j0:j0 + 512]
                            else:
                                hj = S_LAT - 1 - (bi - 1) * 512
                                stop = hj - 512
                                ycols = yacc3[b][:, r, hj::-1] if stop < 0 else yacc3[b][:, r, hj:stop:-1]
                            S.op("dve", ("tensor_tensor", dict(out=ycols, in0=bank(pby), in1=ycols, op=ALU.add)), reads=[PB[pby], Byacc[b]], writes=[Byacc[b]])
            S.barrier()
            A.off = mark
            if "yacc" in dbg_d:
                for b in range(NB):
                    S.dma("sp", ("dma_start", dict(out=dbg_d["yacc"][b * 128:(b + 1) * 128, :], in_=yacc[b])), reads=[Byacc[b]])
            wg = A.bf16(4 * 512)
            wg3 = r3(wg, 4)
            wo = A.bf16(4 * 1024)
            wo3 = r3(wo, 4)
            BwC = Buf()
            S.dma("pool", ("dma_start", dict(out=wg3, in_=s5glu_d.rearrange("(k p) n -> p k n", p=128))), writes=[BwC])
            S.dma("pool", ("dma_start", dict(out=wo3, in_=s5wout_d.rearrange("(k p) n -> p k n", p=128))), writes=[BwC])
            gate_bc = [A.f32(D) for _ in range(2)]
            for c in range(2):
                S.dma("sp", ("dma_start", dict(out=gate_bc[c], in_=modrows_d[L, 2, c:c + 1, :].broadcast_to([128, D]))), reads=[Bmodrows], writes=[BwC])
            gT = A.bf16(4 * 512)
            gT3 = r3(gT, 4)
            vT = A.bf16(4 * 512)
            vT3 = r3(vT, 4)
            BgT, BvT = Buf(), Buf()
            sg = [A.f32(512) for _ in range(2)]
            Bsg = [Buf(), Buf()]
            yt = [A.f32(D) for _ in range(2)]
            xt2 = [A.f32(D) for _ in range(2)]
            Byt, Bxt2 = [Buf(), Buf()], [Buf(), Buf()]
            Bxr = Buf()
            oi = 0
            for b in range(NB):
                for i in range(4):
                    j0 = i * 512
                    S.op("act", ("activation", dict(out=gT3, in_=yacc3[b][:, :, j0:j0 + 512], func=AF.Gelu_apprx_tanh)), reads=[Byacc[b]], writes=[BgT])
                    for m in range(4):
                        pb = 2 + m % 2
                        for k in range(4):
                            S.op("pe", ("matmul", dict(out=bank(pb), lhsT=wg3[:, k, m * 128:(m + 1) * 128], rhs=gT3[:, k, :], start=(k == 0), stop=(k == 3))),
                                 reads=[BwC, BgT], writes=[PB[pb]])
                        S.op("act", ("activation", dict(out=sg[m % 2], in_=bank(pb), func=AF.Sigmoid, bias=sd[:, 4 + m:5 + m], scale=1.0)), reads=[PB[pb], Bsd], writes=[Bsg[m % 2]])
                        S.op("dve", ("tensor_tensor", dict(out=vT3[:, m, :], in0=gT3[:, m, :], in1=sg[m % 2], op=ALU.mult)), reads=[BgT, Bsg[m % 2]], writes=[BvT])
                    for t in range(4):
                        ti = 16 * b + 4 * i + t
                        for hf in range(2):
                            for k in range(4):
                                S.op("pe", ("matmul", dict(out=bank(6 + hf), lhsT=vT3[:, k, t * 128:(t + 1) * 128], rhs=wo3[:, k, hf * 512:(hf + 1) * 512], start=(k == 0), stop=(k == 3))),
                                     reads=[BvT, BwC], writes=[PB[6 + hf]])
                        o2 = oi % 2
                        oi += 1
                        S.dma("sp", ("dma_start", dict(out=xt2[o2], in_=xr2_d[ti * 128:(ti + 1) * 128, :])), reads=[Bsrc], writes=[Bxt2[o2]])
                        S.op("dve", ("tensor_tensor", dict(out=yt[o2], in0=psum_t[:, 6 * 512:8 * 512], in1=gate_bc[b], op=ALU.mult)), reads=[PB[6], PB[7], BwC], writes=[Byt[o2]])
                        S.op("dve", ("tensor_tensor", dict(out=yt[o2], in0=yt[o2], in1=xt2[o2], op=ALU.add)), reads=[Byt[o2], Bxt2[o2]], writes=[Byt[o2]])
                        S.dma("sp", ("dma_start", dict(out=xr_d[ti * 128:(ti + 1) * 128, :], in_=yt[o2])), reads=[Byt[o2]], writes=[Bxr])
            return Bxr

        if stop_after >= 3:
            Bxr_b = s5_mixer(Bxr2)
            S.barrier()
            A.off = persist_off
            if "xr3" in dbg_d:
                S.dma("sp", ("dma_start", dict(out=dbg_d["xr3"], in_=xr_d[0:T_LAT, :])), reads=[Bxr_b])
        if stop_after >= 4:
            Bout = moe(1, 32, Bxr_b)

        S.barrier()
        with nc.Block() as block:
            S.emit(block)
    return nc


def _rope_tables():
    inv = np.power(10000.0, -np.arange(0, 32, 2, dtype=np.float32) / 32).astype(np.float32)
    t = np.arange(S_LAT)
    row = (t // 64).astype(np.float32)
    colp = (t % 64).astype(np.float32)
    ang_r = row[:, None] * inv[None, :]
    ang_c = colp[:, None] * inv[None, :]
    cos64 = np.ones((64, SEQ), np.float32)
    sin64 = np.zeros((64, SEQ), np.float32)
    for d in range(64):
        ang = ang_r if d < 32 else ang_c
        i = d % 16
        sgn = -1.0 if (d % 32) < 16 else 1.0
        cos64[d, C_CTX:] = np.cos(ang[:, i])
        sin64[d, C_CTX:] = sgn * np.sin(ang[:, i])
    return np.concatenate([np.concatenate([cos64, cos64], 0), np.concatenate([sin64, sin64], 0)], 1).astype(np.float32)


_PERM64 = np.array([(d // 32) * 32 + ((d % 32) + 16) % 32 for d in range(64)])


def _consts():
    c = np.zeros((128, NCONST), np.float32)
    c[:, 0:128] = np.eye(128)
    c[:, 128:256] = np.triu(np.ones((128, 128)), 1)
    c[:, 256:384] = 1.0
    c[0:64, 384:448] = 1.0 / 64
    c[64:128, 448:512] = 1.0 / 64
    c[:, 512:544] = (np.arange(32) * CAP)[None, :]
    return c


def make_in_maps(inp, cores):
    f = lambda a: np.ascontiguousarray(np.asarray(a, dtype=np.float32))
    shared = {
        "consts": _consts(), "rope": _rope_tables(),
        "w_mod": f(inp["w_mod"]), "b_mod": f(inp["b_mod"]).reshape(2, 1, 6 * D),
        "norm_mix_g": f(inp["norm_mix_g"]).reshape(2, 1, D), "norm_ffn_g": f(inp["norm_ffn_g"]).reshape(2, 1, D),
        "mix_w_in": f(inp["mix_w_in"][0]), "mix_w_out": f(inp["mix_w_out"][0]),
        "gmlp_norm_g": f(inp["gmlp_norm_g"]).reshape(1, 512), "gmlp_w_spatial": f(inp["gmlp_w_spatial"][0]),
        "gmlp_b_spatial": f(inp["gmlp_b_spatial"][0]).reshape(1, 1024),
        "moe_w_group": f(inp["moe_w_group"]), "moe_b_group": f(inp["moe_b_group"]).reshape(2, 1, 4),
        "moe_w_router": f(inp["moe_w_router"]), "moe_b_router": f(inp["moe_b_router"]).reshape(2, 1, 32),
        "moe_w1": f(inp["moe_w1"]), "moe_w3": f(inp["moe_w3"]), "moe_w2": f(inp["moe_w2"]),
        "final_norm_g": f(inp["final_norm_g"]).reshape(1, D),
        "s5_w_in": f(inp["s5_w_in"][0]), "s5_w_glu": f(inp["s5_w_glu"][0]), "s5_w_out": f(inp["s5_w_out"][0]),
    }
    qg = f(inp["q_norm_g"][0]); kg = f(inp["k_norm_g"][0])
    idx = np.arange(128) % 64
    shared["qkg"] = np.stack([qg[idx], qg[_PERM64[idx]], kg[idx], kg[_PERM64[idx]]], 1).astype(np.float32)
    a_re = f(inp["s5_a_re"][0]); a_im = f(inp["s5_a_im"][0]); ldt = f(inp["s5_log_dt"][0])
    par = np.zeros((128, 2, 16, 3), np.float32)
    for d in range(2):
        for pr in range(16):
            for gl in range(2):
                g = 2 * pr + gl
                par[gl * 64:(gl + 1) * 64, d, pr, 0] = a_re[d, g]
                par[gl * 64:(gl + 1) * 64, d, pr, 1] = a_im[d, g]
                par[gl * 64:(gl + 1) * 64, d, pr, 2] = ldt[d, g]
    shared["s5_par"] = par.reshape(128, 96)
    b_re = f(inp["s5_b_re"][0]); b_im = f(inp["s5_b_im"][0]); c_re = f(inp["s5_c_re"][0]); c_im = f(inp["s5_c_im"][0])
    bT = np.zeros((2, 2, 16, 128, 128), np.float32)
    cc = np.zeros((2, 2, 16, 128, 128), np.float32)
    for d in range(2):
        for pr in range(16):
            for gl in range(2):
                g = 2 * pr + gl
                gic = g % 8
                for ri, (bsrc, csrc) in enumerate(((b_re, c_re), (b_im, c_im))):
                    bT[d, ri, pr, gic * 16:(gic + 1) * 16, gl * 64:(gl + 1) * 64] = bsrc[d, g].T
                    cc[d, ri, pr, gl * 64:(gl + 1) * 64, gic * 16:(gic + 1) * 16] = csrc[d, g].T
    shared["s5_bT"] = bT
    shared["s5_iota"] = np.tile(np.arange(SEQ, dtype=np.float32)[None, :], (128, 1))
    shared["s5_c"] = cc
    shared["s5_d"] = f(inp["s5_d"][0]).reshape(4, 128).T.copy()
    shared["s5_b_glu"] = f(inp["s5_b_glu"][0]).reshape(4, 128).T.copy()
    maps = []
    x = np.asarray(inp["x"]); ctx = np.asarray(inp["ctx"]); c = np.asarray(inp["c"]); cc_ = np.asarray(inp["c_ctx"])
    for core in cores:
        m = dict(shared)
        m["x"] = f(x[2 * core:2 * core + 2]).reshape(T_LAT, D)
        m["ctx"] = f(ctx[2 * core:2 * core + 2]).reshape(T_CTX, D)
        cvec = np.stack([c[2 * core], c[2 * core + 1], cc_], 0).astype(np.float32)
        m["cT"] = np.ascontiguousarray(cvec.reshape(3, 8, 128).transpose(2, 1, 0).reshape(128, 24))
        maps.append(m)
    return maps


_NC_CACHE = {}


def kernel(**inputs):
    if "nc" not in _NC_CACHE:
        _NC_CACHE["nc"] = build_program()
    nc = _NC_CACHE["nc"]
    cores = list(range(8))
    maps = make_in_maps(inputs, cores)
    res = run_bass_kernel_spmd(nc, maps, core_ids=cores)
    outs = [np.asarray(r["out"]).reshape(NB, S_LAT, D) for r in res.results]
    return np.concatenate(outs, 0).astype(np.float32)
```
